# Optimizing a Trainium2 kernel written in Bass

```python
import math
import functools
import jax
import jax.numpy as jnp
from jax import lax
import numpy as np

D_MODEL = 2048
BATCH = 4
SEQ = 2048
DEPTH = 2

GRID_W = 64
CTX_LEN = 256
EPS = 1e-6
CHUNK = 64
HEAD_DIM = 128

S5_WIDTH = D_MODEL // 4
S5_GROUP = 16
S5_GROUPS = S5_WIDTH // S5_GROUP
S5_STATE = 64

GDN_WIDTH = 3 * D_MODEL // 8
GDN_HEADS = GDN_WIDTH // HEAD_DIM
GDN_DK = HEAD_DIM
GDN_DV = HEAD_DIM
GDN_CONV = 3

MLSTM_WIDTH = D_MODEL - S5_WIDTH - GDN_WIDTH
MLSTM_HEADS = MLSTM_WIDTH // HEAD_DIM
MLSTM_DK = HEAD_DIM // 2
MLSTM_DV = HEAD_DIM

FFN_HIDDEN = -(-8 * D_MODEL // (3 * 256)) * 256

IN_SIZES = (S5_WIDTH,
            GDN_HEADS * GDN_DK, GDN_HEADS * GDN_DK, GDN_HEADS * GDN_DV, GDN_HEADS * GDN_DV,
            2 * GDN_HEADS, 2 * GDN_HEADS,
            MLSTM_HEADS * MLSTM_DK, MLSTM_HEADS * MLSTM_DK, MLSTM_HEADS * MLSTM_DV, MLSTM_HEADS * MLSTM_DV,
            2 * MLSTM_HEADS, 2 * MLSTM_HEADS)
IN_COLS = sum(IN_SIZES)

kernel_name = 'hybrid_s5_gdn_mlstm_prefix_backbone'

F32 = jnp.float32


def rms_norm(x, g):
    xf = x.astype(F32)
    y = xf * lax.rsqrt(jnp.mean(xf * xf, axis=-1, keepdims=True) + EPS)
    return (y * g.astype(F32)).astype(x.dtype)


def l2norm(x):
    return x * lax.rsqrt(jnp.sum(x * x, axis=-1, keepdims=True) + EPS)


def modulate(x, g, shift, scale):
    return rms_norm(x, g) * (1 + scale) + shift


def swiglu(h, w_gate, w_up, w_down):
    return (jax.nn.silu(h @ w_gate) * (h @ w_up)) @ w_down


def split_cols(p):
    idx, off = [], 0
    for s in IN_SIZES[:-1]:
        off += s
        idx.append(off)
    return jnp.split(p, idx, axis=-1)


def short_conv(x, w):
    x = x.astype(F32)
    w = w.astype(F32)
    k = w.shape[0]
    pad = k // 2
    t = x.shape[1]
    xp = jnp.pad(x, ((0, 0), (pad, pad), (0, 0)))
    y = xp[:, 0:t] * w[0]
    for j in range(1, k):
        y = y + xp[:, j:j + t] * w[j]
    return jax.nn.silu(y)


def to_col_major(a, rows):
    b = a.shape[0]
    a = a.reshape(b, rows, GRID_W, *a.shape[2:])
    a = jnp.swapaxes(a, 1, 2)
    return a.reshape(b, rows * GRID_W, *a.shape[3:])


def from_col_major(a, rows):
    b = a.shape[0]
    a = a.reshape(b, GRID_W, rows, *a.shape[2:])
    a = jnp.swapaxes(a, 1, 2)
    return a.reshape(b, rows * GRID_W, *a.shape[3:])


def to_chunks(a):
    b, t, h = a.shape[:3]
    a = a.reshape(b, t // CHUNK, CHUNK, h, *a.shape[3:])
    return jnp.moveaxis(a, 3, 1)


def from_chunks(a):
    b, h, n, c = a.shape[:4]
    return jnp.moveaxis(a, 1, 3).reshape(b, n * c, h, *a.shape[4:])


def flip_time(tup):
    return tuple(jnp.flip(a, axis=1) for a in tup)


def bidirectional(run_f, run_b, ctx_f, lat_f, ctx_b, lat_b, state0):
    y_ctx_f, st_f = run_f(ctx_f, state0)
    y_lat_f, _ = run_f(lat_f, st_f)
    y_ctx_b, st_b = run_b(flip_time(ctx_b), state0)
    y_lat_b, _ = run_b(flip_time(lat_b), st_b)
    return y_ctx_f + jnp.flip(y_ctx_b, axis=1), y_lat_f + jnp.flip(y_lat_b, axis=1)


def s5_discretise(lam_re, lam_im, log_dt, b_re, b_im):
    dt = jnp.exp(log_dt)[:, None]
    mag = jnp.exp(lam_re * dt)
    ab_re = mag * jnp.cos(lam_im * dt)
    ab_im = mag * jnp.sin(lam_im * dt)
    den = lam_re * lam_re + lam_im * lam_im
    f_re = ((ab_re - 1.0) * lam_re + ab_im * lam_im) / den
    f_im = (ab_im * lam_re - (ab_re - 1.0) * lam_im) / den
    bb_re = f_re[..., None] * b_re - f_im[..., None] * b_im
    bb_im = f_re[..., None] * b_im + f_im[..., None] * b_re
    return ab_re, ab_im, bb_re, bb_im


def complex_affine_combine(e1, e2):
    a1r, a1i, b1r, b1i = e1
    a2r, a2i, b2r, b2i = e2
    return (a2r * a1r - a2i * a1i, a2r * a1i + a2i * a1r,
            a2r * b1r - a2i * b1i + b2r, a2r * b1i + a2i * b1r + b2i)


def s5_run(inputs, state0, par):
    (u,) = inputs
    ab_re, ab_im, bb_re, bb_im, c_re, c_im = par
    bu_re = jnp.einsum('gph,btgh->btgp', bb_re, u)
    bu_im = jnp.einsum('gph,btgh->btgp', bb_im, u)
    a_re = jnp.broadcast_to(ab_re, bu_re.shape)
    a_im = jnp.broadcast_to(ab_im, bu_im.shape)
    ar, ai, sr, si = lax.associative_scan(complex_affine_combine, (a_re, a_im, bu_re, bu_im), axis=1)
    s0r, s0i = state0
    s_re = sr + ar * s0r[:, None] - ai * s0i[:, None]
    s_im = si + ar * s0i[:, None] + ai * s0r[:, None]
    y = jnp.einsum('ghp,btgp->btgh', c_re, s_re) - jnp.einsum('ghp,btgp->btgh', c_im, s_im)
    return y, (s_re[:, -1], s_im[:, -1])


def s5_mixer(u_ctx, u_lat, lam_re, lam_im, log_dt, b_re, b_im, c_re, c_im, d, glu_w, glu_b):
    def par(i):
        ab_re, ab_im, bb_re, bb_im = s5_discretise(lam_re[i].astype(F32), lam_im[i].astype(F32),
                                                   log_dt[i].astype(F32), b_re[i].astype(F32),
                                                   b_im[i].astype(F32))
        return (ab_re, ab_im, bb_re, bb_im, c_re[i].astype(F32), c_im[i].astype(F32))

    def grp(u):
        return u.astype(F32).reshape(u.shape[0], u.shape[1], S5_GROUPS, S5_GROUP)

    uc, ul = grp(u_ctx), grp(u_lat)
    zeros = jnp.zeros((u_lat.shape[0], S5_GROUPS, S5_STATE), F32)
    run_f = functools.partial(s5_run, par=par(0))
    run_b = functools.partial(s5_run, par=par(1))
    y_ctx, y_lat = bidirectional(run_f, run_b, (uc,), (ul,), (uc,), (ul,), (zeros, zeros))

    def out(y, u):
        y = jax.nn.gelu(y + d.astype(F32) * u).reshape(u.shape[0], u.shape[1], S5_WIDTH)
        return y * jax.nn.sigmoid(y @ glu_w.astype(F32) + glu_b.astype(F32))

    return out(y_ctx, uc), out(y_lat, ul)


def gdn_run(inputs, state0):
    q, k, v, g, beta = (to_chunks(a) for a in inputs)
    tri = jnp.tril(jnp.ones((CHUNK, CHUNK), dtype=bool))
    strict = jnp.tril(jnp.ones((CHUNK, CHUNK), dtype=bool), -1)
    eye = jnp.eye(CHUNK, dtype=F32)
    gc = jnp.cumsum(g, axis=-1)
    diff = gc[..., :, None] - gc[..., None, :]
    decay = jnp.where(tri, jnp.exp(jnp.where(tri, diff, 0.0)), 0.0)
    kb = k * beta[..., None]
    vb = v * beta[..., None]
    m = jnp.where(strict, jnp.einsum('bhnid,bhnjd->bhnij', kb, k) * decay, 0.0)
    t_inv = lax.linalg.triangular_solve(eye + m, jnp.broadcast_to(eye, m.shape),
                                        left_side=True, lower=True, unit_diagonal=True)
    u = t_inv @ vb
    w = t_inv @ (kb * jnp.exp(gc)[..., None])
    qk = jnp.where(tri, jnp.einsum('bhnid,bhnjd->bhnij', q, k) * decay, 0.0)
    q_dec = q * jnp.exp(gc)[..., None]
    k_dec = k * jnp.exp(gc[..., -1:] - gc)[..., None]
    g_tot = jnp.exp(gc[..., -1])

    def step(state, xs):
        w_c, u_c, qk_c, qd_c, kd_c, gt_c = xs
        v_new = u_c - w_c @ state
        o = qd_c @ state + qk_c @ v_new
        state = state * gt_c[..., None, None] + jnp.einsum('bhcd,bhce->bhde', kd_c, v_new)
        return state, o

    xs = tuple(jnp.moveaxis(a, 2, 0) for a in (w, u, qk, q_dec, k_dec, g_tot))
    s_fin, o = lax.scan(step, state0, xs)
    return from_chunks(jnp.moveaxis(o, 0, 2)), s_fin


def gdn_prepare(q, k, v, a, b, conv_w, a_log, dt_bias):
    bsz, t = q.shape[:2]
    qkv = short_conv(jnp.concatenate([q, k, v], axis=-1), conv_w)
    q, k, v = jnp.split(qkv, [GDN_HEADS * GDN_DK, 2 * GDN_HEADS * GDN_DK], axis=-1)
    q = l2norm(q.reshape(bsz, t, GDN_HEADS, GDN_DK)) * GDN_DK ** -0.5
    k = l2norm(k.reshape(bsz, t, GDN_HEADS, GDN_DK))
    v = v.reshape(bsz, t, GDN_HEADS, GDN_DV)
    a = a.astype(F32).reshape(bsz, t, 2, GDN_HEADS)
    b = b.astype(F32).reshape(bsz, t, 2, GDN_HEADS)
    g = -jnp.exp(a_log.astype(F32)) * jax.nn.softplus(a + dt_bias.astype(F32))
    beta = jax.nn.sigmoid(b)
    return (q, k, v, g[:, :, 0], beta[:, :, 0]), (q, k, v, g[:, :, 1], beta[:, :, 1])


def gdn_mixer(ctx_parts, lat_parts, conv_w, a_log, dt_bias, norm):
    cq, ck, cv, cz, ca, cb = ctx_parts
    lq, lk, lv, lz, la, lb = lat_parts
    c_f, c_b = gdn_prepare(cq, ck, cv, ca, cb, conv_w, a_log, dt_bias)
    l_f, l_b = gdn_prepare(lq, lk, lv, la, lb, conv_w, a_log, dt_bias)
    s0 = jnp.zeros((lq.shape[0], GDN_HEADS, GDN_DK, GDN_DV), F32)
    o_ctx, o_lat = bidirectional(gdn_run, gdn_run, c_f, l_f, c_b, l_b, s0)

    def gate_out(o, z):
        z = z.astype(F32).reshape(o.shape)
        return (rms_norm(o, norm) * jax.nn.silu(z)).reshape(o.shape[0], o.shape[1], GDN_WIDTH)

    return gate_out(o_ctx, cz), gate_out(o_lat, lz)


def mlstm_run(inputs, state0):
    q, k, v, ig, lf = (to_chunks(a) for a in inputs)
    tri = jnp.tril(jnp.ones((CHUNK, CHUNK), dtype=bool))
    b = jnp.cumsum(lf, axis=-1)
    log_d = jnp.where(tri, b[..., :, None] - b[..., None, :] + ig[..., None, :], -jnp.inf)
    m_intra = jnp.max(log_d, axis=-1)
    s = jnp.einsum('bhnid,bhnjd->bhnij', q, k) * jnp.exp(log_d - m_intra[..., None])
    num_intra = s @ v
    den_intra = jnp.sum(s, axis=-1)
    log_w = b[..., -1:] - b + ig
    m_chunk = jnp.max(log_w, axis=-1)
    wk = k * jnp.exp(log_w - m_chunk[..., None])[..., None]
    kv_chunk = jnp.einsum('bhncd,bhnce->bhnde', wk, v)
    k_chunk = jnp.sum(wk, axis=-2)
    b_tot = b[..., -1]

    def step(carry, xs):
        c_st, n_st, m_st = carry
        q_c, b_c, mi_c, ni_c, di_c, kv_c, kc_c, bt_c, mc_c = xs
        m_t = jnp.maximum(b_c + m_st[..., None], mi_c)
        inter = jnp.exp(b_c + m_st[..., None] - m_t)
        intra = jnp.exp(mi_c - m_t)
        num = inter[..., None] * (q_c @ c_st) + intra[..., None] * ni_c
        den = inter * jnp.einsum('bhcd,bhd->bhc', q_c, n_st) + intra * di_c
        h = num / jnp.maximum(jnp.abs(den), jnp.exp(-m_t))[..., None]
        m_new = jnp.maximum(bt_c + m_st, mc_c)
        a_old = jnp.exp(bt_c + m_st - m_new)
        a_new = jnp.exp(mc_c - m_new)
        c_st = a_old[..., None, None] * c_st + a_new[..., None, None] * kv_c
        n_st = a_old[..., None] * n_st + a_new[..., None] * kc_c
        return (c_st, n_st, m_new), h

    xs = tuple(jnp.moveaxis(a, 2, 0) for a in
               (q, b, m_intra, num_intra, den_intra, kv_chunk, k_chunk, b_tot, m_chunk))
    state, h = lax.scan(step, state0, xs)
    return from_chunks(jnp.moveaxis(h, 0, 2)), state


def mlstm_prepare(q, k, v, i, f, i_bias, f_bias):
    bsz, t = q.shape[:2]
    q = q.astype(F32).reshape(bsz, t, MLSTM_HEADS, MLSTM_DK) * MLSTM_DK ** -0.5
    k = k.astype(F32).reshape(bsz, t, MLSTM_HEADS, MLSTM_DK)
    v = v.astype(F32).reshape(bsz, t, MLSTM_HEADS, MLSTM_DV)
    ig = i.astype(F32).reshape(bsz, t, 2, MLSTM_HEADS) + i_bias.astype(F32)
    lf = jax.nn.log_sigmoid(f.astype(F32).reshape(bsz, t, 2, MLSTM_HEADS) + f_bias.astype(F32))
    return (q, k, v, ig[:, :, 0], lf[:, :, 0]), (q, k, v, ig[:, :, 1], lf[:, :, 1])


def mlstm_mixer(ctx_parts, lat_parts, rows, i_bias, f_bias, norm):
    cq, ck, cv, co, ci, cf = ctx_parts
    lq, lk, lv, lo, li, lf = lat_parts
    c_f, c_b = mlstm_prepare(cq, ck, cv, ci, cf, i_bias, f_bias)
    l_f, l_b = mlstm_prepare(lq, lk, lv, li, lf, i_bias, f_bias)
    l_f = tuple(to_col_major(a, rows) for a in l_f)
    l_b = tuple(to_col_major(a, rows) for a in l_b)
    bsz = lq.shape[0]
    s0 = (jnp.zeros((bsz, MLSTM_HEADS, MLSTM_DK, MLSTM_DV), F32),
          jnp.zeros((bsz, MLSTM_HEADS, MLSTM_DK), F32),
          jnp.zeros((bsz, MLSTM_HEADS), F32))
    h_ctx, h_lat = bidirectional(mlstm_run, mlstm_run, c_f, l_f, c_b, l_b, s0)
    h_lat = from_col_major(h_lat, rows)

    def gate_out(h, o):
        o = o.astype(F32).reshape(h.shape)
        return (rms_norm(h, norm) * jax.nn.sigmoid(o)).reshape(h.shape[0], h.shape[1], MLSTM_WIDTH)

    return gate_out(h_ctx, co), gate_out(h_lat, lo)


def token_mixer(h_ctx, h_lat, rows, w_in, s5_lam_re, s5_lam_im, s5_log_dt, s5_b_re, s5_b_im,
                s5_c_re, s5_c_im, s5_d, s5_glu_w, s5_glu_b, gdn_conv_w, gdn_a_log, gdn_dt_bias,
                gdn_norm, mlstm_i_bias, mlstm_f_bias, mlstm_norm):
    pc = split_cols(h_ctx @ w_in)
    pl = split_cols(h_lat @ w_in)
    s5_c, s5_l = s5_mixer(pc[0], pl[0], s5_lam_re, s5_lam_im, s5_log_dt, s5_b_re, s5_b_im,
                          s5_c_re, s5_c_im, s5_d, s5_glu_w, s5_glu_b)
    gdn_c, gdn_l = gdn_mixer(pc[1:7], pl[1:7], gdn_conv_w, gdn_a_log, gdn_dt_bias, gdn_norm)
    ml_c, ml_l = mlstm_mixer(pc[7:13], pl[7:13], rows, mlstm_i_bias, mlstm_f_bias, mlstm_norm)
    mix_ctx = jnp.concatenate([s5_c, gdn_c, ml_c], axis=-1).astype(h_ctx.dtype)
    mix_lat = jnp.concatenate([s5_l, gdn_l, ml_l], axis=-1).astype(h_lat.dtype)
    return mix_ctx, mix_lat


def setup_inputs(seed: int = 0) -> dict:
    key = jax.random.key(seed)
    ks = jax.random.split(key, 32)
    L, D, G, P, Hg = DEPTH, D_MODEL, S5_GROUPS, S5_STATE, S5_GROUP

    def nrm(i, shape, std):
        return std * jax.random.normal(ks[i], shape, F32)

    def gain(i, shape):
        return 1.0 + 0.02 * jax.random.normal(ks[i], shape, F32)

    dt_gdn = jnp.exp(jax.random.uniform(ks[23], (L, 2, GDN_HEADS), F32, math.log(1e-3), math.log(1e-1)))
    return {
        'x': nrm(0, (BATCH, SEQ, D), 1.0),
        'c': nrm(1, (BATCH, D), 1.0),
        'ctx': nrm(2, (BATCH, CTX_LEN, D), 1.0),
        'c_ctx': nrm(3, (D,), 1.0),
        'ada_w': nrm(4, (L, D, 6 * D), 0.5 * D ** -0.5),
        'ada_b': nrm(5, (L, 6 * D), 0.01),
        'norm_mix_pre': gain(6, (L, D)),
        'norm_mix_post': gain(7, (L, D)),
        'norm_ffn_pre': gain(8, (L, D)),
        'norm_ffn_post': gain(9, (L, D)),
        'w_in': nrm(10, (L, D, IN_COLS), D ** -0.5),
        'w_out': nrm(11, (L, D, D), D ** -0.5),
        's5_lam_re': -0.5 + nrm(12, (L, 2, G, P), 0.01),
        's5_lam_im': math.pi * jnp.arange(P, dtype=F32) + nrm(13, (L, 2, G, P), 0.01),
        's5_log_dt': jax.random.uniform(ks[14], (L, 2, G), F32, math.log(1e-3), math.log(1e-1)),
        's5_b_re': nrm(15, (L, 2, G, P, Hg), (2 * Hg) ** -0.5),
        's5_b_im': nrm(16, (L, 2, G, P, Hg), (2 * Hg) ** -0.5),
        's5_c_re': nrm(17, (L, 2, G, Hg, P), P ** -0.5),
        's5_c_im': nrm(18, (L, 2, G, Hg, P), P ** -0.5),
        's5_d': nrm(19, (L, G, Hg), 1.0),
        's5_glu_w': nrm(20, (L, S5_WIDTH, S5_WIDTH), S5_WIDTH ** -0.5),
        's5_glu_b': nrm(21, (L, S5_WIDTH), 0.01),
        'gdn_conv_w': nrm(22, (L, GDN_CONV, 2 * GDN_HEADS * GDN_DK + GDN_HEADS * GDN_DV), GDN_CONV ** -0.5),
        'gdn_a_log': jnp.log(jax.random.uniform(ks[24], (L, 2, GDN_HEADS), F32, 1.0, 16.0)),
        'gdn_dt_bias': dt_gdn + jnp.log(-jnp.expm1(-dt_gdn)),
        'gdn_norm': gain(25, (L, GDN_DV)),
        'mlstm_i_bias': nrm(26, (L, 2, MLSTM_HEADS), 0.1),
        'mlstm_f_bias': jnp.linspace(3.0, 6.0, MLSTM_HEADS, dtype=F32) + nrm(27, (L, 2, MLSTM_HEADS), 0.01),
        'mlstm_norm': gain(28, (L, MLSTM_DV)),
        'ffn_w_gate': nrm(29, (L, D, FFN_HIDDEN), D ** -0.5),
        'ffn_w_up': nrm(30, (L, D, FFN_HIDDEN), D ** -0.5),
        'ffn_w_down': nrm(31, (L, FFN_HIDDEN, D), FFN_HIDDEN ** -0.5),
    }


def reference(x, c, ctx, c_ctx, ada_w, ada_b, norm_mix_pre, norm_mix_post, norm_ffn_pre, norm_ffn_post,
              w_in, w_out, s5_lam_re, s5_lam_im, s5_log_dt, s5_b_re, s5_b_im, s5_c_re, s5_c_im, s5_d,
              s5_glu_w, s5_glu_b, gdn_conv_w, gdn_a_log, gdn_dt_bias, gdn_norm, mlstm_i_bias,
              mlstm_f_bias, mlstm_norm, ffn_w_gate, ffn_w_up, ffn_w_down):
    rows = x.shape[1] // GRID_W
    x_lat, x_ctx = x, ctx
    for l in range(DEPTH):
        mods = jnp.split(jax.nn.silu(c) @ ada_w[l] + ada_b[l], 6, axis=-1)
        sh_m, sc_m, gt_m, sh_f, sc_f, gt_f = (m[:, None, :] for m in mods)
        c_sh_m, c_sc_m, c_gt_m, c_sh_f, c_sc_f, c_gt_f = jnp.split(
            jax.nn.silu(c_ctx) @ ada_w[l] + ada_b[l], 6, axis=-1)

        h_lat = modulate(x_lat, norm_mix_pre[l], sh_m, sc_m)
        h_ctx = modulate(x_ctx, norm_mix_pre[l], c_sh_m, c_sc_m)
        mix_ctx, mix_lat = token_mixer(
            h_ctx, h_lat, rows, w_in[l], s5_lam_re[l], s5_lam_im[l], s5_log_dt[l], s5_b_re[l],
            s5_b_im[l], s5_c_re[l], s5_c_im[l], s5_d[l], s5_glu_w[l], s5_glu_b[l], gdn_conv_w[l],
            gdn_a_log[l], gdn_dt_bias[l], gdn_norm[l], mlstm_i_bias[l], mlstm_f_bias[l], mlstm_norm[l])

        x_lat = x_lat + gt_m * rms_norm(mix_lat @ w_out[l], norm_mix_post[l])
        f_lat = swiglu(modulate(x_lat, norm_ffn_pre[l], sh_f, sc_f), ffn_w_gate[l], ffn_w_up[l], ffn_w_down[l])
        x_lat = x_lat + gt_f * rms_norm(f_lat, norm_ffn_post[l])

        if l < DEPTH - 1:
            x_ctx = x_ctx + c_gt_m * rms_norm(mix_ctx @ w_out[l], norm_mix_post[l])
            f_ctx = swiglu(modulate(x_ctx, norm_ffn_pre[l], c_sh_f, c_sc_f),
                           ffn_w_gate[l], ffn_w_up[l], ffn_w_down[l])
            x_ctx = x_ctx + c_gt_f * rms_norm(f_ctx, norm_ffn_post[l])
    return x_lat
```

```python
import numpy as np
from contextlib import ExitStack
import concourse.bass as bass
import concourse.mybir as mybir
from concourse.bass_utils import run_bass_kernel_spmd

F32 = mybir.dt.float32
BF16 = mybir.dt.bfloat16
ALU = mybir.AluOpType
AF = mybir.ActivationFunctionType
AX = mybir.AxisListType

D = 2048
T = 2304
NCTX = 256
NLAT = 2048
L = 2
KC = 16
IN_COLS = 5936
FFN = 5632
EPS = 1e-6
NEG = -30000.0
SEM_ROT = 30000
NSLOT = 6
LC = 128


class Ev:
    __slots__ = ("sem", "val")

    def __init__(self, sem, val):
        self.sem = sem
        self.val = val


class Buf:
    __slots__ = ("w", "r", "name")

    def __init__(self, name=""):
        self.w = None
        self.r = {}
        self.name = name


class Eng:
    def __init__(self, kb, name, h):
        self.kb = kb
        self.name = name
        self.h = h
        self.sem = kb.newsem("e_" + name)
        self.count = 0
        self.seen = {}
        self.n = 0


class Slot:
    def __init__(self, sem):
        self.sem = sem
        self.uses = 0


class KB:
    def __init__(self, nc, es):
        self.nc = nc
        self.es = es
        self.nsem = 0
        self.E = {}
        for name, h in (("pe", nc.tensor), ("act", nc.scalar), ("dve", nc.vector), ("pool", nc.gpsimd), ("sp", nc.sync)):
            self.E[name] = Eng(self, name, h)
        self.slots = {}
        self.rr = {}
        for q in ("sp", "pool", "act"):
            self.slots[q] = [Slot(self.newsem("d_%s%d" % (q, i))) for i in range(NSLOT)]
            self.rr[q] = 0

    def newsem(self, name):
        self.nsem += 1
        return self.es.enter_context(self.nc.semaphore("%s_%d" % (name, self.nsem)))

    def _wait(self, eng, ev):
        k = id(ev.sem)
        if eng.seen.get(k, 0) < ev.val:
            eng.h.wait_ge(ev.sem, ev.val)
            eng.seen[k] = ev.val

    def _deps(self, eng, reads, writes):
        need = {}

        def add(ev):
            k = id(ev.sem)
            if k not in need or need[k].val < ev.val:
                need[k] = ev

        for b in reads:
            if b.w is not None:
                add(b.w)
        for b in writes:
            if b.w is not None:
                add(b.w)
            for ev in b.r.values():
                add(ev)
        for ev in need.values():
            if eng.name == "pe" and ev.sem is eng.sem:
                continue
            self._wait(eng, ev)

    def _post(self, ev, reads, writes):
        k = id(ev.sem)
        for b in reads:
            b.r[k] = ev
        for b in writes:
            b.w = ev
            b.r = {}

    def op(self, e, fn, reads=(), writes=()):
        eng = self.E[e]
        self._deps(eng, reads, writes)
        inst = fn(eng.h)
        if eng.count >= SEM_ROT:
            eng.sem = self.newsem("e_" + eng.name)
            eng.count = 0
        eng.count += 1
        eng.n += 1
        inst.then_inc(eng.sem, 1)
        ev = Ev(eng.sem, eng.count)
        self._post(ev, reads, writes)
        return ev

    def dma(self, q, out, in_, reads=(), writes=(), **kw):
        eng = self.E[q]
        self._deps(eng, reads, writes)
        sl = self.slots[q][self.rr[q] % NSLOT]
        self.rr[q] += 1
        if sl.uses * 16 >= SEM_ROT:
            self._wait(eng, Ev(sl.sem, 16 * sl.uses))
            sl.sem = self.newsem("d_" + q)
            sl.uses = 0
        if sl.uses > 0:
            self._wait(eng, Ev(sl.sem, 16 * sl.uses))
        inst = eng.h.dma_start(out=out, in_=in_, **kw)
        sl.uses += 1
        inst.then_inc(sl.sem, 16)
        ev = Ev(sl.sem, 16 * sl.uses)
        self._post(ev, reads, writes)
        return ev

    def barrier(self, engines=("pe", "act", "dve", "pool", "sp")):
        evs = []
        for e in self.E.values():
            if e.count > 0:
                evs.append(Ev(e.sem, e.count))
        for q in self.slots:
            for sl in self.slots[q]:
                if sl.uses > 0:
                    evs.append(Ev(sl.sem, 16 * sl.uses))
        for en in engines:
            eng = self.E[en]
            for ev in evs:
                self._wait(eng, ev)


class Tl:
    def __init__(self, t, name=""):
        self.t = t
        self.b = Buf(name)

    def __getitem__(self, idx):
        return self.t[idx]


def host_prep_batch(inp, b):
    f = np.float32
    m = {}
    m["xin"] = np.ascontiguousarray(np.concatenate([inp["ctx"][b], inp["x"][b]], axis=0).astype(f))
    cv = np.stack([inp["c"][b].reshape(KC, 128).T, inp["c_ctx"].reshape(KC, 128).T], axis=-1)
    m["cv"] = np.ascontiguousarray(cv.astype(f))
    return m


def host_prep(inp, b):
    f = np.float32
    m = host_prep_batch(inp, b)
    m["ada_w"] = inp["ada_w"]
    m["ada_bT"] = np.ascontiguousarray(inp["ada_b"].reshape(L, 96, 128).transpose(2, 0, 1).reshape(128, L * 96).astype(f))
    g = np.stack([inp["norm_mix_pre"], inp["norm_mix_post"], inp["norm_ffn_pre"], inp["norm_ffn_post"]], 0)
    m["gT"] = np.ascontiguousarray(g.reshape(4, L, KC, 128).transpose(3, 0, 1, 2).reshape(128, 4 * L * KC).astype(f))
    m["w_in"] = inp["w_in"]
    for k_ in ("w_out", "ffn_w_gate", "ffn_w_up", "ffn_w_down"):
        m[k_] = inp[k_]
    m["ident"] = np.eye(128, dtype=f)
    def st_major(a):
        return np.ascontiguousarray(a.reshape(L, 2, 16, 128).transpose(3, 0, 1, 2).reshape(128, L * 2 * 16).astype(f))
    m["s5_lre"] = st_major(inp["s5_lam_re"].reshape(L, 2, 2048))
    m["s5_lim"] = st_major(inp["s5_lam_im"].reshape(L, 2, 2048))
    m["s5_ldt"] = st_major(np.repeat(inp["s5_log_dt"], 64, axis=-1))
    Bb = np.zeros((128, L, 2, 2, 16, 128), f)
    Cb = np.zeros((128, L, 2, 2, 16, 128), f)
    for ci, (bn, cn) in enumerate((("s5_b_re", "s5_c_re"), ("s5_b_im", "s5_c_im"))):
        bsrc = inp[bn]
        csrc = inp[cn]
        for g in range(32):
            st = g // 2
            r0 = (g % 8) * 16
            c0 = (g % 2) * 64
            Bb[r0:r0 + 16, :, :, ci, st, c0:c0 + 64] = bsrc[:, :, g].transpose(3, 0, 1, 2)
            Cb[c0:c0 + 64, :, :, ci, st, r0:r0 + 16] = csrc[:, :, g].transpose(3, 0, 1, 2)
    m["s5_Bb"] = np.ascontiguousarray(Bb.reshape(128, L * 2 * 2 * 16, 128))
    m["s5_Cb"] = np.ascontiguousarray(Cb.reshape(128, L * 2 * 2 * 16, 128))
    m["s5_dT"] = np.ascontiguousarray(inp["s5_d"].reshape(L, 4, 128).transpose(2, 0, 1).reshape(128, L * 4).astype(f))
    m["s5_gbT"] = np.ascontiguousarray(inp["s5_glu_b"].reshape(L, 4, 128).transpose(2, 0, 1).reshape(128, L * 4).astype(f))
    m["s5_glu_w"] = inp["s5_glu_w"]
    tt = np.arange(T)
    rm = np.ones((64, T), f)
    rm[0:32, tt % 64 == 0] = 0.0
    rm[32:64, tt % 64 == 63] = 0.0
    m["rm"] = rm
    sel = np.zeros((64, 12, 128), f)
    for r_ in range(12):
        sel[(r_ // 6) * 32 + r_ % 6, r_, :] = 1.0
    m["sel"] = sel
    a_ = np.arange(64)[:, None]; b_ = np.arange(64)[None, :]
    mb = np.stack([np.where(b_ < a_, 0.0, NEG), np.where(b_ > a_, 0.0, NEG), np.where(b_ <= a_, 0.0, NEG), np.where(b_ >= a_, 0.0, NEG)], 1).astype(f)
    m["mb"] = np.ascontiguousarray(mb)
    m["gdn_normr"] = np.ascontiguousarray(np.tile(inp["gdn_norm"].reshape(1, L * 128), (64, 1)).astype(f))
    gp = np.zeros((64, L, 2), f)
    for dr_ in range(2):
        gp[dr_ * 32:dr_ * 32 + 6, :, 0] = inp["gdn_a_log"][:, dr_, :].T
        gp[dr_ * 32:dr_ * 32 + 6, :, 1] = inp["gdn_dt_bias"][:, dr_, :].T
    m["gdn_gp"] = np.ascontiguousarray(gp.reshape(64, L * 2))
    cw = inp["gdn_conv_w"].reshape(L, 3, 18, 128).transpose(3, 0, 2, 1)
    m["gdn_cw"] = np.ascontiguousarray(cw.reshape(128, L * 54).astype(f))
    rma = np.zeros((64, T), f)
    rma[0:32, tt % 64 == 0] = -1e30
    rma[32:64, tt % 64 == 63] = -1e30
    m["rma"] = rma
    m["ml_normr"] = np.ascontiguousarray(np.tile(inp["mlstm_norm"].reshape(1, L * 128), (64, 1)).astype(f))
    mp = np.zeros((64, L, 2), f)
    for dr_ in range(2):
        mp[dr_ * 32:dr_ * 32 + 6, :, 0] = inp["mlstm_i_bias"][:, dr_, :].T
        mp[dr_ * 32:dr_ * 32 + 6, :, 1] = inp["mlstm_f_bias"][:, dr_, :].T
    m["ml_mp"] = np.ascontiguousarray(mp.reshape(64, L * 2))
    m["tau1"] = np.ascontiguousarray(np.tile(np.arange(1, LC + 1, dtype=f)[None, :], (128, 1)))
    return m


def build(stage=99):
    nc = bass.Bass("TRN2", target_bir_lowering=False)
    es = ExitStack()
    with es:
        def din(name, shape, dt=F32):
            return nc.dram_tensor(name, list(shape), dt, kind="ExternalInput").ap()

        def dout(name, shape, dt=F32):
            return nc.dram_tensor(name, list(shape), dt, kind="ExternalOutput").ap()

        def dscr(name, shape, dt=F32):
            return nc.dram_tensor(name, list(shape), dt, kind="Internal").ap()

        xin = din("xin", [T, D])
        cv_d = din("cv", [128, KC, 2])
        ada_w = din("ada_w", [L, D, 6 * D])
        ada_bT = din("ada_bT", [128, L * 96])
        gT_d = din("gT", [128, 4 * L * KC])
        w_in = din("w_in", [L, D, IN_COLS])
        ident_d = din("ident", [128, 128])
        s5_lre_d = din("s5_lre", [128, L * 32])
        s5_lim_d = din("s5_lim", [128, L * 32])
        s5_ldt_d = din("s5_ldt", [128, L * 32])
        s5_Bb_d = din("s5_Bb", [128, L * 64, 128])
        s5_Cb_d = din("s5_Cb", [128, L * 64, 128])
        s5_dT_d = din("s5_dT", [128, L * 4])
        s5_gbT_d = din("s5_gbT", [128, L * 4])
        s5_gluw_d = din("s5_glu_w", [L, 512, 512])
        tau1_d = din("tau1", [128, LC])
        rm_d = din("rm", [64, T])
        sel_d = din("sel", [64, 12, 128])
        mb_d = din("mb", [64, 4, 64])
        gdn_norm_d = din("gdn_normr", [64, L * 128])
        gdn_gp_d = din("gdn_gp", [64, L * 2])
        gdn_cw_d = din("gdn_cw", [128, L * 54])
        rma_d = din("rma", [64, T])
        ml_norm_d = din("ml_normr", [64, L * 128])
        ml_mp_d = din("ml_mp", [64, L * 2])
        w_out = din("w_out", [L, D, D])
        w_gate = din("ffn_w_gate", [L, D, FFN])
        w_up = din("ffn_w_up", [L, D, FFN])
        w_down = din("ffn_w_down", [L, FFN, D])
        out_d = dout("out", [NLAT, D])
        xres = dscr("xres", [T, D])
        if stage <= 1:
            projT = dout("projT", [47 * 128, T])
            mods_o = dout("mods_o", [128, 96 * 2])
        else:
            projT = dscr("projT", [47 * 128, T])
        if stage == 3:
            dbg_cv = dout("dbg_cv", [128, 3, T])
            dbg_rows = dout("dbg_rows", [64, 6, T])
            dbg_oacc = dout("dbg_oacc", [64, 36, 128])
            dbg_oaccf = dout("dbg_oaccf", [64, 36, 128])
        if stage == 5:
            mixT = din("mixT", [D, T], BF16)
            xres_o = dout("xres_o", [T, D])
        elif 2 <= stage <= 4:
            mixT = dout("mixT", [D, T], BF16)
        else:
            mixT = dscr("mixT", [D, T], BF16)

        kb = KB(nc, es)

        cnt = [0]

        def sb(st, name, shape, dt=F32):
            cnt[0] += 1
            nm = "s%d_%s" % (cnt[0], name)
            return Tl(st.enter_context(nc.sbuf_tensor(nm, list(shape), dt)), nm)

        def ps(st, name, shape, dt=F32):
            cnt[0] += 1
            nm = "p%d_%s" % (cnt[0], name)
            return Tl(st.enter_context(nc.psum_tensor(nm, list(shape), dt)), nm)

        ident = sb(es, "ident", [128, 128])
        gT = sb(es, "gT", [128, 4 * L * KC])
        abT = sb(es, "abT", [128, L * 96])
        cvs = sb(es, "cvs", [128, KC, 2])
        modsT = sb(es, "modsT", [128, 96, 2])
        gsm = sb(es, "gsm", [128, KC, 2])
        gsf = sb(es, "gsf", [128, KC, 2])
        kb.dma("sp", ident[:], ident_d, writes=[ident.b])
        kb.dma("sp", gT[:], gT_d, writes=[gT.b])
        kb.dma("sp", abT[:], ada_bT, writes=[abT.b])
        kb.dma("sp", cvs[:], cv_d, writes=[cvs.b])
        kb.op("act", lambda e: e.activation(out=cvs[:], in_=cvs[:], func=AF.Silu), reads=[cvs.b], writes=[cvs.b])

        banks = [ps(es, "bank%d" % i, [128, 512]) for i in range(8)]
        xres_b = Buf("xres")
        kb.dma("sp", xres, xin, writes=[xres_b])
        kb.barrier()

        for l in range(L):
            with ExitStack() as ph:
                wA = [sb(ph, "wA%d" % i, [128, KC, 512]) for i in range(2)]
                pm = banks[0]
                adv = ada_w[l].rearrange("(k p) c -> p k c", p=128)
                for cb in range(24):
                    w = wA[cb % 2]
                    kb.dma("sp", w[:], adv[:, :, cb * 512:(cb + 1) * 512], writes=[w.b])
                    for jj in range(4):
                        jo = cb * 4 + jj
                        for k in range(KC):
                            kb.op("pe", lambda e, w=w, k=k, jj=jj, jo=jo: e.matmul(
                                out=pm[:, jo * 2:jo * 2 + 2], lhsT=w[:, k, jj * 128:(jj + 1) * 128], rhs=cvs[:, k, :],
                                start=(k == 0), stop=(k == KC - 1)), reads=[w.b, cvs.b], writes=[pm.b])
                for wi in range(2):
                    kb.op("dve", lambda e, wi=wi: e.tensor_tensor(
                        out=modsT[:, :, wi], in0=pm[:, wi:192:2], in1=abT[:, l * 96:(l + 1) * 96], op=ALU.add),
                        reads=[pm.b, abT.b], writes=[modsT.b])
                for (gs, mi, gi) in ((gsm, 1, 0), (gsf, 4, 2)):
                    for wi in range(2):
                        kb.op("dve", lambda e, gs=gs, mi=mi, gi=gi, wi=wi: e.scalar_tensor_tensor(
                            out=gs[:, :, wi], in0=modsT[:, mi * KC:(mi + 1) * KC, wi], scalar=1.0,
                            in1=gT[:, (gi * L + l) * KC:(gi * L + l + 1) * KC], op0=ALU.add, op1=ALU.mult),
                            reads=[modsT.b, gT.b], writes=[gs.b])
                kb.barrier()
            if stage <= 1 and l == 0:
                kb.dma("sp", mods_o, modsT[:].rearrange("p a b -> p (a b)"), reads=[modsT.b])

            with ExitStack() as lay:
                hT = sb(lay, "hT", [128, KC, T], BF16)
                with ExitStack() as ph:
                    xt = [sb(ph, "xt%d" % i, [128, D]) for i in range(2)]
                    junk = sb(ph, "junk", [128, D], BF16)
                    st = [sb(ph, "st%d" % i, [128, 4]) for i in range(2)]
                    for i in range(T // 128):
                        x_ = xt[i % 2]
                        s_ = st[i % 2]
                        wsel = 1 if i < 2 else 0
                        kb.dma("sp", x_[:], xres[i * 128:(i + 1) * 128, :], writes=[x_.b])
                        kb.op("act", lambda e, x_=x_, s_=s_: e.activation(out=junk[:], in_=x_[:], func=AF.Square,
                                                                           accum_out=s_[:, 0:1]),
                              reads=[x_.b], writes=[junk.b, s_.b])
                        kb.op("act", lambda e, s_=s_: e.activation(out=s_[:, 1:2], in_=s_[:, 0:1], func=AF.Sqrt,
                                                                    bias=EPS, scale=1.0 / D), reads=[s_.b], writes=[s_.b])
                        kb.op("dve", lambda e, s_=s_: e.reciprocal(out=s_[:, 2:3], in_=s_[:, 1:2]), reads=[s_.b], writes=[s_.b])
                        kb.op("dve", lambda e, x_=x_, s_=s_: e.tensor_scalar(out=x_[:], in0=x_[:], scalar1=s_[:, 2:3], scalar2=None,
                                                                          op0=ALU.mult), reads=[x_.b, s_.b], writes=[x_.b])
                        for jb in range(4):
                            pt = banks[1 + (i * 4 + jb) % 4]
                            for jj in range(4):
                                j = jb * 4 + jj
                                kb.op("pe", lambda e, pt=pt, x_=x_, jj=jj, j=j: e.transpose(
                                    out=pt[:, jj * 128:(jj + 1) * 128], in_=x_[:, j * 128:(j + 1) * 128], identity=ident[:]),
                                    reads=[x_.b, ident.b], writes=[pt.b])
                            for jj in range(4):
                                j = jb * 4 + jj
                                kb.op("act", lambda e, pt=pt, jj=jj, j=j, i=i, wsel=wsel: e.activation(
                                    out=hT[:, j, i * 128:(i + 1) * 128], in_=pt[:, jj * 128:(jj + 1) * 128], func=AF.Identity,
                                    bias=modsT[:, 0 * KC + j, wsel:wsel + 1], scale=gsm[:, j, wsel:wsel + 1]),
                                    reads=[pt.b, modsT.b, gsm.b], writes=[hT.b])
                    kb.barrier()
                with ExitStack() as ph:
                    wC = [sb(ph, "wC%d" % i, [128, KC, 128], BF16) for i in range(2)]
                    sg = [sb(ph, "sg%d" % i, [128, 512]) for i in range(3)]
                    wv = w_in[l].rearrange("(k p) c -> p k c", p=128)
                    nev = 0
                    for cc in range(47):
                        c0 = cc * 128
                        n = min(128, IN_COLS - c0)
                        w = wC[cc % 2]
                        kb.dma("pool", w[:, :, :n], wv[:, :, c0:c0 + n], writes=[w.b])
                        for tb in range(5):
                            t0 = tb * 512
                            nt = min(512, T - t0)
                            pj = banks[5 + nev % 3]
                            for k in range(KC):
                                kb.op("pe", lambda e, pj=pj, w=w, k=k, n=n, t0=t0, nt=nt: e.matmul(
                                    out=pj[:n, :nt], lhsT=w[:, k, :n], rhs=hT[:, k, t0:t0 + nt],
                                    start=(k == 0), stop=(k == KC - 1)), reads=[w.b, hT.b], writes=[pj.b])
                            s_ = sg[nev % 3]
                            eng = "act" if nev % 2 == 0 else "dve"
                            if eng == "act":
                                kb.op("act", lambda e, s_=s_, pj=pj, n=n, nt=nt: e.activation(out=s_[:n, :nt], in_=pj[:n, :nt],
                                                                                            func=AF.Identity),
                                      reads=[pj.b], writes=[s_.b])
                            else:
                                kb.op("dve", lambda e, s_=s_, pj=pj, n=n, nt=nt: e.tensor_copy(out=s_[:n, :nt], in_=pj[:n, :nt]),
                                      reads=[pj.b], writes=[s_.b])
                            kb.dma("sp", projT[c0:c0 + n, t0:t0 + nt], s_[:n, :nt], reads=[s_.b])
                            nev += 1
                    kb.barrier()
            if stage <= 1:
                break
            with ExitStack() as ph:
                TWO_PI = 6.283185307179586
                C1 = 6.28125
                C2 = TWO_PI - C1
                PI = 3.141592653589793
                uT = sb(ph, "uT", [128, 4, T])
                yT = sb(ph, "yT", [128, 4, T])
                Bb = sb(ph, "Bb", [128, 32, 128])
                Cb = sb(ph, "Cb", [128, 32, 128])
                lre = sb(ph, "lre", [128, 32]); lim = sb(ph, "lim", [128, 32]); ldt = sb(ph, "ldt", [128, 32])
                tau1 = sb(ph, "tau1", [128, LC])
                dTt = sb(ph, "dTt", [128, L * 4]); gbT = sb(ph, "gbT", [128, L * 4])
                gluw = sb(ph, "gluw", [128, 4, 512])
                kb.dma("sp", uT[:], projT[0:512, :].rearrange("(c p) t -> p c t", p=128), writes=[uT.b])
                kb.dma("sp", lre[:], s5_lre_d[:, l * 32:(l + 1) * 32], writes=[lre.b])
                kb.dma("sp", lim[:], s5_lim_d[:, l * 32:(l + 1) * 32], writes=[lim.b])
                kb.dma("sp", ldt[:], s5_ldt_d[:, l * 32:(l + 1) * 32], writes=[ldt.b])
                kb.dma("sp", tau1[:], tau1_d, writes=[tau1.b])
                kb.dma("sp", dTt[:], s5_dT_d, writes=[dTt.b])
                kb.dma("sp", gbT[:], s5_gbT_d, writes=[gbT.b])
                kb.dma("sp", gluw[:], s5_gluw_d[l].rearrange("(c p) n -> p c n", p=128), writes=[gluw.b])

                def sincos(n, ang, o_sin, o_cos, tf, ti, tm, tb_):
                    B = [tb_]
                    kb.op("dve", lambda e: e.tensor_scalar(out=tf, in0=ang, scalar1=1.0 / TWO_PI, scalar2=None, op0=ALU.mult), reads=B, writes=B)
                    kb.op("dve", lambda e: e.tensor_copy(out=ti, in_=tf), reads=B, writes=B)
                    kb.op("dve", lambda e: e.tensor_copy(out=tf, in_=ti), reads=B, writes=B)
                    kb.op("dve", lambda e: e.scalar_tensor_tensor(out=tm, in0=tf, scalar=-C1, in1=ang, op0=ALU.mult, op1=ALU.add), reads=B, writes=B)
                    kb.op("dve", lambda e: e.scalar_tensor_tensor(out=tm, in0=tf, scalar=-C2, in1=tm, op0=ALU.mult, op1=ALU.add), reads=B, writes=B)

                    def wrap(y):
                        kb.op("dve", lambda e: e.tensor_scalar(out=tf, in0=y, scalar1=PI, scalar2=TWO_PI, op0=ALU.is_gt, op1=ALU.mult), reads=B, writes=B)
                        kb.op("dve", lambda e: e.tensor_tensor(out=y, in0=y, in1=tf, op=ALU.subtract), reads=B, writes=B)
                        kb.op("dve", lambda e: e.tensor_scalar(out=tf, in0=y, scalar1=-PI, scalar2=TWO_PI, op0=ALU.is_lt, op1=ALU.mult), reads=B, writes=B)
                        kb.op("dve", lambda e: e.tensor_tensor(out=y, in0=y, in1=tf, op=ALU.add), reads=B, writes=B)
                    wrap(tm)
                    kb.op("act", lambda e: e.activation(out=o_sin, in_=tm, func=AF.Sin), reads=B, writes=B)
                    kb.op("dve", lambda e: e.tensor_scalar(out=tm, in0=tm, scalar1=PI / 2, scalar2=None, op0=ALU.add), reads=B, writes=B)
                    wrap(tm)
                    kb.op("act", lambda e: e.activation(out=o_cos, in_=tm, func=AF.Sin), reads=B, writes=B)

                with ExitStack() as ph2:
                    tabs = sb(ph2, "tabs", [128, 4, 16 * LC])
                    tw = sb(ph2, "tw", [128, 3, 16 * LC])
                    twi = sb(ph2, "twi", [128, 16 * LC], mybir.dt.int32)
                    sp_ = sb(ph2, "s5par", [128, 16, 16])
                    spi = sb(ph2, "s5pari", [128, 16], mybir.dt.int32)
                    car = sb(ph2, "car", [128, 2, 16])
                    tmp = [sb(ph2, "s5t%d" % i, [128, 10, LC]) for i in range(2)]
                    S5B = [tabs.b, tw.b, twi.b, sp_.b, spi.b]

                    def P_(k):
                        return sp_[:, k, :]
                    for i in range(2):
                        o = (l * 2 + i) * 16
                        B = [sp_.b]
                        kb.dma("sp", Bb[:], s5_Bb_d[:, (l * 2 + i) * 32:(l * 2 + i + 1) * 32, :], writes=[Bb.b])
                        kb.dma("sp", Cb[:], s5_Cb_d[:, (l * 2 + i) * 32:(l * 2 + i + 1) * 32, :], writes=[Cb.b])
                        kb.op("act", lambda e: e.activation(out=Cb[:, 16:32, :], in_=Cb[:, 16:32, :], func=AF.Identity, scale=-1.0),
                              reads=[Cb.b], writes=[Cb.b])
                        kb.op("act", lambda e: e.activation(out=P_(0), in_=ldt[:, (l * 2 + i) * 16 - l * 32 + 0:(l * 2 + i) * 16 - l * 32 + 16], func=AF.Exp), reads=[ldt.b], writes=B)
                        kb.op("dve", lambda e: e.tensor_tensor(out=P_(1), in0=lre[:, i * 16:(i + 1) * 16], in1=P_(0), op=ALU.mult), reads=[lre.b] + B, writes=B)
                        kb.op("dve", lambda e: e.tensor_tensor(out=P_(2), in0=lim[:, i * 16:(i + 1) * 16], in1=P_(0), op=ALU.mult), reads=[lim.b] + B, writes=B)
                        kb.op("act", lambda e: e.activation(out=P_(3), in_=P_(1), func=AF.Exp), reads=B, writes=B)
                        sincos(16, P_(2), P_(4), P_(5), P_(6), spi[:], P_(7), sp_.b)
                        kb.op("dve", lambda e: e.tensor_tensor(out=P_(6), in0=P_(3), in1=P_(5), op=ALU.mult), reads=B, writes=B)
                        kb.op("dve", lambda e: e.tensor_scalar(out=P_(6), in0=P_(6), scalar1=-1.0, scalar2=None, op0=ALU.add), reads=B, writes=B)
                        kb.op("dve", lambda e: e.tensor_tensor(out=P_(7), in0=P_(3), in1=P_(4), op=ALU.mult), reads=B, writes=B)
                        kb.op("dve", lambda e: e.tensor_tensor(out=P_(8), in0=lre[:, i * 16:(i + 1) * 16], in1=lre[:, i * 16:(i + 1) * 16], op=ALU.mult), reads=[lre.b] + B, writes=B)
                        kb.op("dve", lambda e: e.tensor_tensor(out=P_(9), in0=lim[:, i * 16:(i + 1) * 16], in1=lim[:, i * 16:(i + 1) * 16], op=ALU.mult), reads=[lim.b] + B, writes=B)
                        kb.op("dve", lambda e: e.tensor_tensor(out=P_(8), in0=P_(8), in1=P_(9), op=ALU.add), reads=B, writes=B)
                        kb.op("dve", lambda e: e.reciprocal(out=P_(8), in_=P_(8)), reads=B, writes=B)
                        kb.op("dve", lambda e: e.tensor_tensor(out=P_(10), in0=P_(6), in1=lre[:, i * 16:(i + 1) * 16], op=ALU.mult), reads=[lre.b] + B, writes=B)
                        kb.op("dve", lambda e: e.tensor_tensor(out=P_(9), in0=P_(7), in1=lim[:, i * 16:(i + 1) * 16], op=ALU.mult), reads=[lim.b] + B, writes=B)
                        kb.op("dve", lambda e: e.tensor_tensor(out=P_(10), in0=P_(10), in1=P_(9), op=ALU.add), reads=B, writes=B)
                        kb.op("dve", lambda e: e.tensor_tensor(out=P_(10), in0=P_(10), in1=P_(8), op=ALU.mult), reads=B, writes=B)
                        kb.op("dve", lambda e: e.tensor_tensor(out=P_(11), in0=P_(7), in1=lre[:, i * 16:(i + 1) * 16], op=ALU.mult), reads=[lre.b] + B, writes=B)
                        kb.op("dve", lambda e: e.tensor_tensor(out=P_(9), in0=P_(6), in1=lim[:, i * 16:(i + 1) * 16], op=ALU.mult), reads=[lim.b] + B, writes=B)
                        kb.op("dve", lambda e: e.tensor_tensor(out=P_(11), in0=P_(11), in1=P_(9), op=ALU.subtract), reads=B, writes=B)
                        kb.op("dve", lambda e: e.tensor_tensor(out=P_(11), in0=P_(11), in1=P_(8), op=ALU.mult), reads=B, writes=B)
                        for st in range(16):
                            kb.op("dve", lambda e, st=st: e.tensor_scalar(out=tw[:, 0, st * LC:(st + 1) * LC], in0=tau1[:], scalar1=sp_[:, 2, st:st + 1],
                                                                         scalar2=None, op0=ALU.mult), reads=[tau1.b] + B, writes=[tw.b])
                        sincos(16 * LC, tw[:, 0, :], tabs[:, 3, :], tabs[:, 2, :], tw[:, 1, :], twi[:], tw[:, 2, :], tw.b)
                        kb.op("dve", lambda e: e.tensor_copy(out=tw[:, 0, 0:1], in_=tw[:, 0, 0:1]), reads=[tw.b, tabs.b], writes=[tw.b, tabs.b])
                        for st in range(16):
                            sl = slice(st * LC, (st + 1) * LC)
                            kb.op("dve", lambda e, st=st, sl=sl: e.tensor_scalar(out=tw[:, 1, sl], in0=tabs[:, 3, sl], scalar1=sp_[:, 11, st:st + 1], scalar2=None, op0=ALU.mult), reads=[tabs.b] + B, writes=[tw.b])
                            kb.op("dve", lambda e, st=st, sl=sl: e.scalar_tensor_tensor(out=tabs[:, 0, sl], in0=tabs[:, 2, sl], scalar=sp_[:, 10, st:st + 1], in1=tw[:, 1, sl], op0=ALU.mult, op1=ALU.add), reads=[tw.b] + B, writes=[tabs.b])
                            kb.op("dve", lambda e, st=st, sl=sl: e.tensor_scalar(out=tw[:, 1, sl], in0=tabs[:, 3, sl], scalar1=sp_[:, 10, st:st + 1], scalar2=None, op0=ALU.mult), reads=[tabs.b] + B, writes=[tw.b])
                            kb.op("dve", lambda e, st=st, sl=sl: e.scalar_tensor_tensor(out=tabs[:, 1, sl], in0=tabs[:, 2, sl], scalar=sp_[:, 11, st:st + 1], in1=tw[:, 1, sl], op0=ALU.mult, op1=ALU.subtract), reads=[tw.b] + B, writes=[tabs.b])
                        kb.op("dve", lambda e: e.memset(car[:], 0.0), writes=[car.b])
                        NCH = T // LC
                        NCC = NCTX // LC
                        order = list(range(NCH)) if i == 0 else list(range(NCC - 1, -1, -1)) + list(range(NCH - 1, NCC - 1, -1))
                        nit = 0
                        for n in order:
                            t0 = n * LC
                            for st in range(16):
                                fc = st // 4
                                sl = slice(st * LC, (st + 1) * LC)
                                pb = banks[nit % 2]
                                tm_ = tmp[nit % 2]
                                nit += 1

                                def rv(ap):
                                    return ap[:, ::-1] if i == 1 else ap
                                for ci in range(2):
                                    kb.op("pe", lambda e, ci=ci, pb=pb: e.matmul(out=pb[:, ci * LC:(ci + 1) * LC], lhsT=Bb[:, ci * 16 + st, :],
                                                                                 rhs=uT[:, fc, t0:t0 + LC], start=True, stop=True),
                                          reads=[Bb.b, uT.b], writes=[pb.b])
                                bre = rv(pb[:, 0:LC]); bim = rv(pb[:, LC:2 * LC])
                                Tm = lambda k, tm_=tm_: tm_[:, k, :]
                                TB = [tm_.b]
                                kb.op("dve", lambda e: e.tensor_tensor(out=Tm(0), in0=bre, in1=tabs[:, 0, sl], op=ALU.mult), reads=[pb.b, tabs.b], writes=TB)
                                kb.op("dve", lambda e: e.tensor_tensor(out=Tm(1), in0=bim, in1=tabs[:, 1, sl], op=ALU.mult), reads=[pb.b, tabs.b], writes=TB)
                                kb.op("dve", lambda e: e.tensor_tensor(out=Tm(2), in0=bre, in1=tabs[:, 1, sl], op=ALU.mult), reads=[pb.b, tabs.b], writes=TB)
                                kb.op("dve", lambda e: e.tensor_tensor(out=Tm(3), in0=bim, in1=tabs[:, 0, sl], op=ALU.mult), reads=[pb.b, tabs.b], writes=TB)
                                kb.op("pool", lambda e: e.tensor_tensor(out=Tm(0), in0=Tm(0), in1=Tm(1), op=ALU.subtract), reads=TB, writes=TB)
                                kb.op("pool", lambda e: e.tensor_tensor(out=Tm(2), in0=Tm(2), in1=Tm(3), op=ALU.add), reads=TB, writes=TB)
                                rb = sp_[:, 3, st:st + 1].to_broadcast([128, LC])
                                kb.op("dve", lambda e: e.tensor_tensor_scan(out=Tm(4), data0=rb, data1=Tm(0), initial=car[:, 0, st:st + 1], op0=ALU.mult, op1=ALU.add),
                                      reads=TB + [sp_.b, car.b], writes=TB)
                                kb.op("dve", lambda e: e.tensor_tensor_scan(out=Tm(5), data0=rb, data1=Tm(2), initial=car[:, 1, st:st + 1], op0=ALU.mult, op1=ALU.add),
                                      reads=TB + [sp_.b, car.b], writes=TB)
                                kb.op("pool", lambda e: e.tensor_tensor(out=Tm(6), in0=Tm(4), in1=tabs[:, 2, sl], op=ALU.mult), reads=TB + [tabs.b], writes=TB)
                                kb.op("pool", lambda e: e.tensor_tensor(out=Tm(7), in0=Tm(5), in1=tabs[:, 3, sl], op=ALU.mult), reads=TB + [tabs.b], writes=TB)
                                kb.op("pool", lambda e: e.tensor_tensor(out=rv(Tm(8)), in0=Tm(6), in1=Tm(7), op=ALU.subtract), reads=TB, writes=TB)
                                kb.op("pool", lambda e: e.tensor_tensor(out=Tm(6), in0=Tm(4), in1=tabs[:, 3, sl], op=ALU.mult), reads=TB + [tabs.b], writes=TB)
                                kb.op("pool", lambda e: e.tensor_tensor(out=Tm(7), in0=Tm(5), in1=tabs[:, 2, sl], op=ALU.mult), reads=TB + [tabs.b], writes=TB)
                                kb.op("pool", lambda e: e.tensor_tensor(out=rv(Tm(9)), in0=Tm(6), in1=Tm(7), op=ALU.add), reads=TB, writes=TB)
                                lastc = LC - 1 if i == 0 else 0
                                kb.op("act", lambda e: e.activation(out=car[:, 0, st:st + 1], in_=tm_[:, 8, lastc:lastc + 1], func=AF.Identity), reads=TB, writes=[car.b])
                                kb.op("act", lambda e: e.activation(out=car[:, 1, st:st + 1], in_=tm_[:, 9, lastc:lastc + 1], func=AF.Identity), reads=TB, writes=[car.b])
                                py = banks[2 + (nit // 4) % 2] if False else banks[2 + ((nit - 1) // 4) % 2]
                                kb.op("pe", lambda e, py=py: e.matmul(out=py[:, 0:LC], lhsT=Cb[:, st, :], rhs=Tm(8), start=(st % 4 == 0), stop=False),
                                      reads=[Cb.b] + TB, writes=[py.b])
                                kb.op("pe", lambda e, py=py: e.matmul(out=py[:, 0:LC], lhsT=Cb[:, 16 + st, :], rhs=Tm(9), start=False, stop=(st % 4 == 3)),
                                      reads=[Cb.b] + TB, writes=[py.b])
                                if st % 4 == 3:
                                    if i == 0:
                                        kb.op("act", lambda e, py=py: e.activation(out=yT[:, fc, t0:t0 + LC], in_=py[:, 0:LC], func=AF.Identity), reads=[py.b], writes=[yT.b])
                                    else:
                                        kb.op("dve", lambda e, py=py: e.tensor_tensor(out=yT[:, fc, t0:t0 + LC], in0=py[:, 0:LC], in1=yT[:, fc, t0:t0 + LC], op=ALU.add),
                                              reads=[py.b, yT.b], writes=[yT.b])
                    kb.barrier()
                with ExitStack() as ph2:
                    g1 = sb(ph2, "g1", [128, T]); g2 = sb(ph2, "g2", [128, T])
                    og = [sb(ph2, "og%d" % i, [128, 512], BF16) for i in range(2)]
                    sgl = [sb(ph2, "sgl%d" % i, [128, 512]) for i in range(2)]
                    for fc in range(4):
                        kb.op("dve", lambda e: e.scalar_tensor_tensor(out=yT[:, fc, :], in0=uT[:, fc, :], scalar=dTt[:, l * 4 + fc:l * 4 + fc + 1], in1=yT[:, fc, :],
                                                                      op0=ALU.mult, op1=ALU.add), reads=[uT.b, dTt.b, yT.b], writes=[yT.b])
                        kb.op("act", lambda e: e.activation(out=g1[:], in_=yT[:, fc, :], func=AF.Square), reads=[yT.b], writes=[g1.b])
                        kb.op("dve", lambda e: e.tensor_scalar(out=g1[:], in0=g1[:], scalar1=0.044715, scalar2=1.0, op0=ALU.mult, op1=ALU.add), reads=[g1.b], writes=[g1.b])
                        kb.op("dve", lambda e: e.tensor_tensor(out=g1[:], in0=g1[:], in1=yT[:, fc, :], op=ALU.mult), reads=[g1.b, yT.b], writes=[g1.b])
                        kb.op("act", lambda e: e.activation(out=g2[:], in_=g1[:], func=AF.Sigmoid, scale=1.5957691216057308), reads=[g1.b], writes=[g2.b])
                        kb.op("dve", lambda e: e.tensor_tensor(out=yT[:, fc, :], in0=yT[:, fc, :], in1=g2[:], op=ALU.mult), reads=[g2.b, yT.b], writes=[yT.b])
                    ne = 0
                    for fo in range(4):
                        for tb in range(5):
                            t0 = tb * 512
                            nt = min(512, T - t0)
                            pg = banks[4 + ne % 2]
                            for fi in range(4):
                                kb.op("pe", lambda e, fi=fi, pg=pg: e.matmul(out=pg[:, :nt], lhsT=gluw[:, fi, fo * 128:(fo + 1) * 128], rhs=yT[:, fi, t0:t0 + nt],
                                                                             start=(fi == 0), stop=(fi == 3)), reads=[gluw.b, yT.b], writes=[pg.b])
                            s_ = sgl[ne % 2]; o_ = og[ne % 2]
                            kb.op("act", lambda e, pg=pg, s_=s_: e.activation(out=s_[:, :nt], in_=pg[:, :nt], func=AF.Sigmoid, bias=gbT[:, l * 4 + fo:l * 4 + fo + 1]),
                                  reads=[pg.b, gbT.b], writes=[s_.b])
                            kb.op("dve", lambda e, s_=s_, o_=o_: e.tensor_tensor(out=o_[:, :nt], in0=s_[:, :nt], in1=yT[:, fo, t0:t0 + nt], op=ALU.mult),
                                  reads=[s_.b, yT.b], writes=[o_.b])
                            kb.dma("sp", mixT[fo * 128:(fo + 1) * 128, t0:t0 + nt], o_[:, :nt], reads=[o_.b])
                            ne += 1
                kb.barrier()
            if stage <= 2:
                break
            with ExitStack() as ph:
                QOFF, KOFF, VOFF, ZOFF, AOFF, BOFF = 512, 1280, 2048, 2816, 3584, 3596
                NCK = T // 64
                RM = sb(ph, "RM", [64, T])
                SEL = sb(ph, "SEL", [64, 12, 128]); NSEL = sb(ph, "NSEL", [64, 12, 128])
                MB = sb(ph, "MB", [64, 4, 64])
                ONES = sb(ph, "ONES", [128, 128])
                NORM = sb(ph, "NORM", [64, 128])
                GP = sb(ph, "GP", [64, 2])
                CW = sb(ph, "CW", [128, 18 * 3])
                kb.dma("sp", RM[:], rm_d, writes=[RM.b])
                kb.dma("sp", SEL[:], sel_d, writes=[SEL.b])
                kb.dma("sp", MB[:], mb_d, writes=[MB.b])
                kb.dma("sp", NORM[:], gdn_norm_d[:, l * 128:(l + 1) * 128], writes=[NORM.b])
                kb.dma("sp", GP[:], gdn_gp_d[:, l * 2:(l + 1) * 2], writes=[GP.b])
                kb.dma("sp", CW[:], gdn_cw_d[:, l * 54:(l + 1) * 54], writes=[CW.b])
                kb.op("act", lambda e: e.activation(out=NSEL[:], in_=SEL[:], func=AF.Identity, scale=-1.0), reads=[SEL.b], writes=[NSEL.b])
                kb.op("dve", lambda e: e.memset(ONES[:], 1.0), writes=[ONES.b])
                RA = sb(ph, "RA", [64, T]); RB = sb(ph, "RB", [64, T]); GC = sb(ph, "GC", [64, T])
                R1 = sb(ph, "R1", [64, T]); EG = sb(ph, "EG", [64, T]); BG = sb(ph, "BG", [64, T])
                EGL = sb(ph, "EGL", [64, NCK])
                COL = sb(ph, "COL", [64, NCK, 3, 12])
                EGLB = sb(ph, "EGLB", [128, 12, NCK])
                kb.op("dve", lambda e: e.memset(RA[:], 0.0), writes=[RA.b])
                kb.op("dve", lambda e: e.memset(RB[:], 0.0), writes=[RB.b])
                for dr in range(2):
                    kb.dma("sp", RA[dr * 32:dr * 32 + 6, :], projT[AOFF + dr * 6:AOFF + dr * 6 + 6, :], writes=[RA.b])
                    kb.dma("sp", RB[dr * 32:dr * 32 + 6, :], projT[BOFF + dr * 6:BOFF + dr * 6 + 6, :], writes=[RB.b])
                kb.op("act", lambda e: e.activation(out=RA[:], in_=RA[:], func=AF.Exp, bias=GP[:, 1:2]), reads=[RA.b, GP.b], writes=[RA.b])
                kb.op("act", lambda e: e.activation(out=RA[:], in_=RA[:], func=AF.Ln, bias=1.0), reads=[RA.b], writes=[RA.b])
                kb.op("act", lambda e: e.activation(out=GP[:, 0:1], in_=GP[:, 0:1], func=AF.Exp), reads=[GP.b], writes=[GP.b])
                kb.op("dve", lambda e: e.tensor_scalar(out=RA[:], in0=RA[:], scalar1=GP[:, 0:1], scalar2=-1.0, op0=ALU.mult, op1=ALU.mult), reads=[RA.b, GP.b], writes=[RA.b])
                kb.op("act", lambda e: e.activation(out=RB[:], in_=RB[:], func=AF.Sigmoid), reads=[RB.b], writes=[RB.b])
                kb.op("act", lambda e: e.activation(out=R1[:], in_=RB[:], func=AF.Ln), reads=[RB.b], writes=[R1.b])
                kb.op("dve", lambda e: e.tensor_tensor_scan(out=GC[0:32, :], data0=RM[0:32, :], data1=RA[0:32, :], initial=0.0, op0=ALU.mult, op1=ALU.add),
                      reads=[RM.b, RA.b], writes=[GC.b])
                kb.op("dve", lambda e: e.tensor_tensor_scan(out=GC[32:64, ::-1], data0=RM[32:64, ::-1], data1=RA[32:64, ::-1], initial=0.0, op0=ALU.mult, op1=ALU.add),
                      reads=[RM.b, RA.b], writes=[GC.b])
                kb.op("dve", lambda e: e.tensor_tensor(out=R1[:], in0=R1[:], in1=GC[:], op=ALU.add), reads=[R1.b, GC.b], writes=[R1.b])
                kb.op("act", lambda e: e.activation(out=EG[:], in_=GC[:], func=AF.Exp), reads=[GC.b], writes=[EG.b])
                kb.op("dve", lambda e: e.tensor_tensor(out=BG[:], in0=EG[:], in1=RB[:], op=ALU.mult), reads=[EG.b, RB.b], writes=[BG.b])
                GCL = sb(ph, "GCL", [64, NCK])
                for dr in range(2):
                    pr = slice(dr * 32, dr * 32 + 32)
                    lastpos = 63 if dr == 0 else 0
                    gv = GC[pr, :].rearrange("p (c s) -> p c s", s=64)
                    kb.op("dve", lambda e, pr=pr, gv=gv, lastpos=lastpos: e.tensor_copy(out=GCL[pr, :], in_=gv[:, :, lastpos]), reads=[GC.b], writes=[GCL.b])
                kb.op("act", lambda e: e.activation(out=EGL[:], in_=GCL[:], func=AF.Exp), reads=[GCL.b], writes=[EGL.b])
                for c in range(NCK):
                    kb.op("dve", lambda e, c=c: e.tensor_scalar(out=RA[:, c * 64:(c + 1) * 64], in0=GC[:, c * 64:(c + 1) * 64], scalar1=-1.0, scalar2=GCL[:, c:c + 1],
                                                                 op0=ALU.mult, op1=ALU.add), reads=[GC.b, GCL.b], writes=[RA.b])
                kb.op("act", lambda e: e.activation(out=RA[:], in_=RA[:], func=AF.Exp), reads=[RA.b], writes=[RA.b])
                for c in range(NCK):
                    pcol = banks[c % 4]
                    for qi, row in enumerate((BG, RA, RB)):
                        kb.op("pe", lambda e, row=row, qi=qi, pcol=pcol, c=c: e.matmul(out=pcol[0:64, qi * 12:(qi + 1) * 12], lhsT=row[:, c * 64:(c + 1) * 64],
                                                                                   rhs=SEL[:, :, 0], start=True, stop=True), reads=[row.b, SEL.b], writes=[pcol.b])
                    kb.op("act", lambda e, pcol=pcol, c=c: e.activation(out=COL[:, c, :, :].rearrange("p a b -> p (a b)"), in_=pcol[0:64, 0:36], func=AF.Identity),
                          reads=[pcol.b], writes=[COL.b])
                for r in range(12):
                    pcol = banks[4 + r % 2]
                    kb.op("pe", lambda e, r=r, pcol=pcol: e.matmul(out=pcol[:, 0:NCK], lhsT=SEL[:, r, :], rhs=EGL[:], start=True, stop=True), reads=[SEL.b, EGL.b], writes=[pcol.b])
                    kb.op("act", lambda e, r=r, pcol=pcol: e.activation(out=EGLB[:, r, :], in_=pcol[:, 0:NCK], func=AF.Identity), reads=[pcol.b], writes=[EGLB.b])
                kb.barrier()
                import os as _os
                DBG = int(_os.environ.get("DBG_GDN", "9"))
                if stage == 3:
                    for qi_, row_ in enumerate((GC, R1, EG, RA, RB, BG)):
                        kb.dma("sp", dbg_rows[:, qi_, :], row_[:], reads=[row_.b])
                psn = [0]

                def pget():
                    bk = banks[psn[0] % 6]
                    psn[0] += 1
                    return bk
                RAW = sb(ph, "RAW", [128, 3, T]); CV = sb(ph, "CV", [128, 3, T])
                OACC = sb(ph, "OACC", [64, NCK, 128]); OUT = sb(ph, "OUT", [128, T], BF16)
                S = sb(ph, "Sst", [128, 128])
                G = 4
                U = []
                for u in range(G):
                    d_ = {}
                    for nm, shp in (("AB0", [64, 2, 64]), ("AB1", [64, 2, 64]), ("X0", [64, 64]), ("X1", [64, 64]),
                                    ("E", [64, 3, 64]), ("kbg", [64, 128]), ("kdec", [64, 128]), ("vb", [64, 128]),
                                    ("nWT", [128, 64]), ("VN", [64, 128])):
                        d_[nm] = sb(ph, "u%d%s" % (u, nm), shp)
                    U.append(d_)
                fin = [sb(ph, "fin%d" % i, [64, 128]) for i in range(2)]
                fst = [sb(ph, "fst%d" % i, [64, 4]) for i in range(2)]
                I64 = ident[0:64, 0:64]
                for hd in range(6 if DBG >= 1 else 0):
                    for ci, off in enumerate((QOFF, KOFF, VOFF)):
                        kb.dma("sp", RAW[:, ci, :], projT[off + hd * 128:off + (hd + 1) * 128, :], writes=[RAW.b])
                    for ci in range(3):
                        cch = ci * 6 + hd
                        for (s0, s1) in ((0, NCTX), (NCTX, T)):
                            kb.op("dve", lambda e, ci=ci, cch=cch, s0=s0, s1=s1: e.tensor_scalar(out=CV[:, ci, s0:s1], in0=RAW[:, ci, s0:s1], scalar1=CW[:, cch * 3 + 1:cch * 3 + 2],
                                                                                              scalar2=None, op0=ALU.mult), reads=[RAW.b, CW.b], writes=[CV.b])
                            kb.op("dve", lambda e, ci=ci, cch=cch, s0=s0, s1=s1: e.scalar_tensor_tensor(out=CV[:, ci, s0 + 1:s1], in0=RAW[:, ci, s0:s1 - 1], scalar=CW[:, cch * 3:cch * 3 + 1],
                                                                                                     in1=CV[:, ci, s0 + 1:s1], op0=ALU.mult, op1=ALU.add), reads=[RAW.b, CW.b, CV.b], writes=[CV.b])
                            kb.op("dve", lambda e, ci=ci, cch=cch, s0=s0, s1=s1: e.scalar_tensor_tensor(out=CV[:, ci, s0:s1 - 1], in0=RAW[:, ci, s0 + 1:s1], scalar=CW[:, cch * 3 + 2:cch * 3 + 3],
                                                                                                     in1=CV[:, ci, s0:s1 - 1], op0=ALU.mult, op1=ALU.add), reads=[RAW.b, CW.b, CV.b], writes=[CV.b])
                    kb.op("act", lambda e: e.activation(out=CV[:], in_=CV[:], func=AF.Silu), reads=[CV.b], writes=[CV.b])
                    for ci in range(2):
                        kb.op("act", lambda e, ci=ci: e.activation(out=RAW[:, 0, :], in_=CV[:, ci, :], func=AF.Square), reads=[CV.b], writes=[RAW.b])
                        for tb in range(5):
                            t0 = tb * 512
                            nt = min(512, T - t0)
                            pb = banks[6 + tb % 2]
                            kb.op("pe", lambda e, pb=pb, t0=t0, nt=nt: e.matmul(out=pb[:, :nt], lhsT=ONES[:], rhs=RAW[:, 0, t0:t0 + nt], start=True, stop=True),
                                  reads=[ONES.b, RAW.b], writes=[pb.b])
                            kb.op("act", lambda e, pb=pb, t0=t0, nt=nt: e.activation(out=RAW[:, 1, t0:t0 + nt], in_=pb[:, :nt], func=AF.Sqrt, bias=EPS), reads=[RAW.b], writes=[RAW.b, pb.b])
                        kb.op("dve", lambda e: e.reciprocal(out=RAW[:, 1, :], in_=RAW[:, 1, :]), reads=[RAW.b], writes=[RAW.b])
                        sc_ = (128.0 ** -0.5) if ci == 0 else 1.0
                        kb.op("dve", lambda e, ci=ci, sc_=sc_: e.scalar_tensor_tensor(out=CV[:, ci, :], in0=RAW[:, 1, :], scalar=sc_, in1=CV[:, ci, :], op0=ALU.mult, op1=ALU.mult),
                              reads=[RAW.b, CV.b], writes=[CV.b])
                    QT = lambda a, b_: CV[:, 0, a:b_]
                    KT = lambda a, b_: CV[:, 1, a:b_]
                    VT = lambda a, b_: CV[:, 2, a:b_]
                    for dr in range(2 if DBG >= 2 else 0):
                        r = dr * 6 + hd
                        for tb in range(5):
                            t0 = tb * 512
                            nt = min(512, T - t0)
                            pb = banks[6 + tb % 2]
                            kb.op("pe", lambda e, pb=pb, t0=t0, nt=nt: e.matmul(out=pb[:, :nt], lhsT=SEL[:, r, :], rhs=EG[:, t0:t0 + nt], start=True, stop=True),
                                  reads=[SEL.b, EG.b], writes=[pb.b])
                            kb.op("dve", lambda e, pb=pb, t0=t0, nt=nt: e.tensor_tensor(out=RAW[:, dr, t0:t0 + nt], in0=pb[:, :nt], in1=CV[:, 0, t0:t0 + nt], op=ALU.mult),
                                  reads=[CV.b], writes=[RAW.b, pb.b])
                        kb.op("dve", lambda e: e.memset(S[:], 0.0), writes=[S.b])
                        if stage == 3 and hd == 0 and dr == 1:
                            kb.dma("sp", dbg_oaccf, OACC[:], reads=[OACC.b])
                        if dr == 0:
                            order = list(range(NCK))
                        else:
                            order = list(range(3, -1, -1)) + list(range(NCK - 1, 3, -1))
                        m_s, m_st, m_it = (0, 1, 3) if dr == 0 else (1, 0, 2)
                        for g0 in range(0, NCK if DBG >= 3 else 0, G):
                            grp = order[g0:g0 + G]
                            for u, c in enumerate(grp):
                                t0 = c * 64
                                d_ = U[u]
                                bk = pget()
                                kb.op("pe", lambda e, bk=bk, t0=t0: e.transpose(out=bk[0:64, 0:128], in_=KT(t0, t0 + 64), identity=ident[:]), reads=[CV.b, ident.b], writes=[bk.b])
                                kb.op("pe", lambda e, bk=bk, t0=t0: e.transpose(out=bk[0:64, 128:256], in_=VT(t0, t0 + 64), identity=ident[:]), reads=[CV.b, ident.b], writes=[bk.b])
                                kb.op("act", lambda e, bk=bk, d_=d_, c=c: e.activation(out=d_["kbg"][:], in_=bk[0:64, 0:128], func=AF.Identity, scale=COL[:, c, 0, r:r + 1]), reads=[COL.b], writes=[d_["kbg"].b, bk.b])
                                kb.op("act", lambda e, bk=bk, d_=d_, c=c: e.activation(out=d_["kdec"][:], in_=bk[0:64, 0:128], func=AF.Identity, scale=COL[:, c, 1, r:r + 1]), reads=[COL.b], writes=[d_["kdec"].b, bk.b])
                                kb.op("act", lambda e, bk=bk, d_=d_, c=c: e.activation(out=d_["vb"][:], in_=bk[0:64, 128:256], func=AF.Identity, scale=COL[:, c, 2, r:r + 1]), reads=[COL.b], writes=[d_["vb"].b, bk.b])
                            PB = {}
                            for u, c in enumerate(grp):
                                t0 = c * 64
                                ch = slice(t0, t0 + 64)
                                bp = pget()
                                kb.op("pe", lambda e, bp=bp, t0=t0: e.matmul(out=bp[0:64, 0:64], lhsT=KT(t0, t0 + 64), rhs=KT(t0, t0 + 64), start=True, stop=True), reads=[CV.b], writes=[bp.b])
                                kb.op("pe", lambda e, bp=bp, t0=t0: e.matmul(out=bp[0:64, 64:128], lhsT=KT(t0, t0 + 64), rhs=QT(t0, t0 + 64), start=True, stop=True), reads=[CV.b], writes=[bp.b])
                                be = pget()
                                kb.op("pe", lambda e, be=be, ch=ch: e.matmul(out=be[0:64, 0:64], lhsT=R1[:, ch], rhs=SEL[:, r, 0:64], start=True, stop=False), reads=[R1.b, SEL.b], writes=[be.b])
                                kb.op("pe", lambda e, be=be, ch=ch: e.matmul(out=be[0:64, 0:64], lhsT=NSEL[:, r, 0:64], rhs=GC[:, ch], start=False, stop=False), reads=[GC.b, NSEL.b], writes=[be.b])
                                kb.op("pe", lambda e, be=be: e.matmul(out=be[0:64, 0:64], lhsT=I64, rhs=MB[:, m_s, :], start=False, stop=True), reads=[MB.b, ident.b], writes=[be.b])
                                kb.op("pe", lambda e, be=be, ch=ch: e.matmul(out=be[0:64, 64:128], lhsT=GC[:, ch], rhs=NSEL[:, r, 0:64], start=True, stop=False), reads=[GC.b, NSEL.b], writes=[be.b])
                                kb.op("pe", lambda e, be=be, ch=ch: e.matmul(out=be[0:64, 64:128], lhsT=SEL[:, r, 0:64], rhs=R1[:, ch], start=False, stop=False), reads=[R1.b, SEL.b], writes=[be.b])
                                kb.op("pe", lambda e, be=be: e.matmul(out=be[0:64, 64:128], lhsT=I64, rhs=MB[:, m_st, :], start=False, stop=True), reads=[MB.b, ident.b], writes=[be.b])
                                kb.op("pe", lambda e, be=be, ch=ch: e.matmul(out=be[0:64, 128:192], lhsT=GC[:, ch], rhs=NSEL[:, r, 0:64], start=True, stop=False), reads=[GC.b, NSEL.b], writes=[be.b])
                                kb.op("pe", lambda e, be=be, ch=ch: e.matmul(out=be[0:64, 128:192], lhsT=SEL[:, r, 0:64], rhs=GC[:, ch], start=False, stop=False), reads=[GC.b, SEL.b], writes=[be.b])
                                kb.op("pe", lambda e, be=be: e.matmul(out=be[0:64, 128:192], lhsT=I64, rhs=MB[:, m_it, :], start=False, stop=True), reads=[MB.b, ident.b], writes=[be.b])
                                PB[u] = (bp, be)
                                d_ = U[u]
                                kb.op("act", lambda e, be=be, d_=d_: e.activation(out=d_["E"][:].rearrange("p a b -> p (a b)"), in_=be[0:64, 0:192], func=AF.Exp), writes=[d_["E"].b, be.b])
                                kb.op("dve", lambda e, bp=bp, d_=d_: e.tensor_tensor(out=d_["AB0"][:, 1, :], in0=bp[0:64, 0:64], in1=d_["E"][:, 0, :], op=ALU.mult), reads=[d_["E"].b], writes=[d_["AB0"].b, bp.b])
                                kb.op("dve", lambda e, bp=bp, d_=d_: e.tensor_tensor(out=d_["AB0"][:, 0, :], in0=bp[0:64, 0:64], in1=d_["E"][:, 1, :], op=ALU.mult), reads=[d_["E"].b], writes=[d_["AB0"].b, bp.b])
                                kb.op("dve", lambda e, bp=bp, d_=d_: e.tensor_tensor(out=d_["E"][:, 2, :], in0=bp[0:64, 64:128], in1=d_["E"][:, 2, :], op=ALU.mult), reads=[d_["E"].b], writes=[d_["E"].b, bp.b])
                                kb.op("dve", lambda e, d_=d_: e.tensor_tensor(out=d_["X0"][:], in0=I64, in1=d_["AB0"][:, 0, :], op=ALU.subtract), reads=[ident.b, d_["AB0"].b], writes=[d_["X0"].b])
                            for k in range(1, 6):
                                pa, pn = (k - 1) % 2, k % 2
                                for u, c in enumerate(grp):
                                    d_ = U[u]
                                    ABp, ABn = d_["AB%d" % pa], d_["AB%d" % pn]
                                    bk = pget()
                                    if k < 5:
                                        kb.op("pe", lambda e, bk=bk, ABp=ABp: e.matmul(out=bk[0:64, 0:64], lhsT=ABp[:, 1, :], rhs=ABp[:, 0, :], start=True, stop=True), reads=[ABp.b], writes=[bk.b])
                                    kb.op("pe", lambda e, bk=bk, ABp=ABp: e.matmul(out=bk[0:64, 64:128], lhsT=ABp[:, 0, :], rhs=ABp[:, 1, :], start=True, stop=True), reads=[ABp.b], writes=[bk.b])
                                    lo = 0 if k < 5 else 64
                                    eng = "act" if (u + k) % 2 == 0 else "dve"
                                    if eng == "act":
                                        kb.op("act", lambda e, bk=bk, ABn=ABn, lo=lo: e.activation(out=ABn[:].rearrange("p a b -> p (a b)")[:, lo:128], in_=bk[0:64, lo:128], func=AF.Identity), writes=[ABn.b, bk.b])
                                    else:
                                        kb.op("dve", lambda e, bk=bk, ABn=ABn, lo=lo: e.tensor_copy(out=ABn[:].rearrange("p a b -> p (a b)")[:, lo:128], in_=bk[0:64, lo:128]), writes=[ABn.b, bk.b])
                                for u, c in enumerate(grp):
                                    d_ = U[u]
                                    ABn, Xp, Xn = d_["AB%d" % pn], d_["X%d" % pa], d_["X%d" % pn]
                                    bk = pget()
                                    kb.op("pe", lambda e, bk=bk, Xp=Xp: e.matmul(out=bk[0:64, 0:64], lhsT=I64, rhs=Xp[:], start=True, stop=False), reads=[Xp.b, ident.b], writes=[bk.b])
                                    kb.op("pe", lambda e, bk=bk, Xp=Xp, ABn=ABn: e.matmul(out=bk[0:64, 0:64], lhsT=ABn[:, 1, :], rhs=Xp[:], start=False, stop=True), reads=[Xp.b, ABn.b], writes=[bk.b])
                                    eng = "dve" if (u + k) % 2 == 0 else "act"
                                    if eng == "act":
                                        kb.op("act", lambda e, bk=bk, Xn=Xn: e.activation(out=Xn[:], in_=bk[0:64, 0:64], func=AF.Identity), writes=[Xn.b, bk.b])
                                    else:
                                        kb.op("dve", lambda e, bk=bk, Xn=Xn: e.tensor_copy(out=Xn[:], in_=bk[0:64, 0:64]), writes=[Xn.b, bk.b])
                            for u, c in enumerate(grp if DBG >= 4 else []):
                                t0 = c * 64
                                d_ = U[u]
                                TT = d_["X1"]
                                bk = pget()
                                kb.op("pe", lambda e, bk=bk, d_=d_, TT=TT: e.matmul(out=bk[:, 0:64], lhsT=d_["kbg"][:], rhs=TT[:], start=True, stop=True), reads=[d_["kbg"].b, TT.b], writes=[bk.b])
                                kb.op("act", lambda e, bk=bk, d_=d_: e.activation(out=d_["nWT"][:], in_=bk[:, 0:64], func=AF.Identity, scale=-1.0), writes=[d_["nWT"].b, bk.b])
                                bk = pget()
                                kb.op("pe", lambda e, bk=bk, d_=d_, TT=TT: e.matmul(out=bk[0:64, 0:128], lhsT=TT[:], rhs=d_["vb"][:], start=True, stop=False), reads=[d_["vb"].b, TT.b], writes=[bk.b])
                                kb.op("pe", lambda e, bk=bk, d_=d_: e.matmul(out=bk[0:64, 0:128], lhsT=d_["nWT"][:], rhs=S[:], start=False, stop=True), reads=[d_["nWT"].b, S.b], writes=[bk.b])
                                kb.op("act", lambda e, bk=bk, d_=d_: e.activation(out=d_["VN"][:], in_=bk[0:64, 0:128], func=AF.Identity), writes=[d_["VN"].b, bk.b])
                                bk = pget()
                                kb.op("pe", lambda e, bk=bk, t0=t0: e.matmul(out=bk[0:64, 0:128], lhsT=RAW[:, dr, t0:t0 + 64], rhs=S[:], start=True, stop=False), reads=[RAW.b, S.b], writes=[bk.b])
                                kb.op("pe", lambda e, bk=bk, d_=d_: e.matmul(out=bk[0:64, 0:128], lhsT=d_["E"][:, 2, :], rhs=d_["VN"][:], start=False, stop=True), reads=[d_["E"].b, d_["VN"].b], writes=[bk.b])
                                if dr == 0:
                                    kb.op("act", lambda e, bk=bk, c=c: e.activation(out=OACC[:, c, :], in_=bk[0:64, 0:128], func=AF.Identity), writes=[OACC.b, bk.b])
                                else:
                                    kb.op("dve", lambda e, bk=bk, c=c: e.tensor_tensor(out=OACC[:, c, :], in0=bk[0:64, 0:128], in1=OACC[:, c, :], op=ALU.add), writes=[OACC.b, bk.b])
                                bk = pget()
                                kb.op("pe", lambda e, bk=bk, d_=d_: e.matmul(out=bk[:, 0:128], lhsT=d_["kdec"][:], rhs=d_["VN"][:], start=True, stop=True), reads=[d_["kdec"].b, d_["VN"].b], writes=[bk.b])
                                kb.op("dve", lambda e, bk=bk, c=c: e.scalar_tensor_tensor(out=S[:], in0=S[:], scalar=EGLB[:, r, c:c + 1], in1=bk[:, 0:128], op0=ALU.mult, op1=ALU.add),
                                      reads=[EGLB.b], writes=[S.b, bk.b])
                    if stage == 3 and hd == 0:
                        kb.dma("sp", dbg_cv, CV[:], reads=[CV.b])
                        kb.dma("sp", dbg_oacc, OACC[:], reads=[OACC.b])
                    kb.dma("sp", RAW[:, 2, :], projT[ZOFF + hd * 128:ZOFF + (hd + 1) * 128, :], reads=[CV.b], writes=[RAW.b])
                    kb.op("act", lambda e: e.activation(out=RAW[:, 2, :], in_=RAW[:, 2, :], func=AF.Silu), reads=[RAW.b], writes=[RAW.b])
                    for c in range(NCK):
                        f_ = fin[c % 2]; s_ = fst[c % 2]
                        kb.op("act", lambda e, f_=f_, s_=s_, c=c: e.activation(out=f_[:], in_=OACC[:, c, :], func=AF.Square, accum_out=s_[:, 0:1]), reads=[OACC.b], writes=[f_.b, s_.b])
                        kb.op("act", lambda e, s_=s_: e.activation(out=s_[:, 1:2], in_=s_[:, 0:1], func=AF.Sqrt, bias=EPS, scale=1.0 / 128), reads=[s_.b], writes=[s_.b])
                        kb.op("dve", lambda e, s_=s_: e.reciprocal(out=s_[:, 2:3], in_=s_[:, 1:2]), reads=[s_.b], writes=[s_.b])
                        kb.op("dve", lambda e, f_=f_, s_=s_, c=c: e.scalar_tensor_tensor(out=f_[:], in0=OACC[:, c, :], scalar=s_[:, 2:3], in1=NORM[:], op0=ALU.mult, op1=ALU.mult),
                              reads=[OACC.b, s_.b, NORM.b], writes=[f_.b])
                        bk = pget()
                        kb.op("pe", lambda e, bk=bk, f_=f_: e.transpose(out=bk[:, 0:64], in_=f_[:], identity=I64), reads=[f_.b, ident.b], writes=[bk.b])
                        kb.op("dve", lambda e, bk=bk, c=c: e.tensor_tensor(out=OUT[:, c * 64:(c + 1) * 64], in0=bk[:, 0:64], in1=RAW[:, 2, c * 64:(c + 1) * 64], op=ALU.mult),
                              reads=[RAW.b], writes=[OUT.b, bk.b])
                    kb.dma("sp", mixT[512 + hd * 128:512 + (hd + 1) * 128, :], OUT[:], reads=[OUT.b])
                kb.barrier()
            if stage <= 3:
                break
            with ExitStack() as ph:
                MQ, MK, MV, MO, MI, MF = 3608, 3992, 4376, 5144, 5912, 5924
                NCK = T // 64
                SEL = sb(ph, "mSEL", [64, 12, 128]); NSEL = sb(ph, "mNSEL", [64, 12, 128])
                MB = sb(ph, "mMB", [64, 4, 64])
                NORM = sb(ph, "mNORM", [64, 128])
                MP = sb(ph, "mMP", [64, 2])
                kb.dma("sp", SEL[:], sel_d, writes=[SEL.b])
                kb.dma("sp", MB[:], mb_d, writes=[MB.b])
                kb.dma("sp", NORM[:], ml_norm_d[:, l * 128:(l + 1) * 128], writes=[NORM.b])
                kb.dma("sp", MP[:], ml_mp_d[:, l * 2:(l + 1) * 2], writes=[MP.b])
                kb.op("act", lambda e: e.activation(out=NSEL[:], in_=SEL[:], func=AF.Identity, scale=-1.0), reads=[SEL.b], writes=[NSEL.b])
                RI = sb(ph, "mRI", [64, T]); CM = sb(ph, "mCM", [64, T])
                COL = sb(ph, "mCOL", [64, NCK, 4, 12])
                AOB = sb(ph, "mAOB", [128, 2, 12, NCK])
                I64 = ident[0:64, 0:64]

                def seqperm(eng, dst, dstb, src, srcb, npart, p0=0):
                    kb.op(eng, lambda e: (e.tensor_copy(out=dst[p0:p0 + npart, 0:NCTX], in_=src[p0:p0 + npart, 0:NCTX]) if eng != "act" else
                                          e.activation(out=dst[p0:p0 + npart, 0:NCTX], in_=src[p0:p0 + npart, 0:NCTX], func=AF.Identity)), reads=[srcb], writes=[dstb])
                    ov = dst[p0:p0 + npart, NCTX:T].rearrange("p (c r) -> p c r", r=32)
                    iv = src[p0:p0 + npart, NCTX:T].rearrange("p (r c) -> p c r", c=64)
                    kb.op(eng, lambda e: (e.tensor_copy(out=ov, in_=iv) if eng != "act" else e.activation(out=ov, in_=iv, func=AF.Identity)), reads=[srcb], writes=[dstb])

                with ExitStack() as ph2:
                    RM = sb(ph2, "mRM", [64, T]); RMA = sb(ph2, "mRMA", [64, T])
                    RF = sb(ph2, "mRF", [64, T]); BC = sb(ph2, "mBC", [64, T]); X1 = sb(ph2, "mX1", [64, T]); X2 = sb(ph2, "mX2", [64, T])
                    CH = sb(ph2, "mCH", [64, 8, NCK])
                    kb.dma("sp", RM[:], rm_d, writes=[RM.b])
                    kb.dma("sp", RMA[:], rma_d, writes=[RMA.b])
                    kb.op("dve", lambda e: e.memset(X1[:], 0.0), writes=[X1.b])
                    kb.op("dve", lambda e: e.memset(X2[:], 0.0), writes=[X2.b])
                    for dr in range(2):
                        kb.dma("sp", X1[dr * 32:dr * 32 + 6, :], projT[MI + dr * 6:MI + dr * 6 + 6, :], writes=[X1.b])
                        kb.dma("sp", X2[dr * 32:dr * 32 + 6, :], projT[MF + dr * 6:MF + dr * 6 + 6, :], writes=[X2.b])
                    seqperm("dve", RI, RI.b, X1, X1.b, 64)
                    seqperm("dve", RF, RF.b, X2, X2.b, 64)
                    kb.op("act", lambda e: e.activation(out=RI[:], in_=RI[:], func=AF.Identity, bias=MP[:, 0:1]), reads=[RI.b, MP.b], writes=[RI.b])
                    kb.op("dve", lambda e: e.tensor_scalar(out=MP[:, 1:2], in0=MP[:, 1:2], scalar1=-1.0, scalar2=None, op0=ALU.mult), reads=[MP.b], writes=[MP.b])
                    kb.op("act", lambda e: e.activation(out=RF[:], in_=RF[:], func=AF.Exp, bias=MP[:, 1:2], scale=-1.0), reads=[RF.b, MP.b], writes=[RF.b])
                    kb.op("act", lambda e: e.activation(out=RF[:], in_=RF[:], func=AF.Ln, bias=1.0), reads=[RF.b], writes=[RF.b])
                    kb.op("dve", lambda e: e.tensor_scalar(out=RF[:], in0=RF[:], scalar1=-1.0, scalar2=None, op0=ALU.mult), reads=[RF.b], writes=[RF.b])
                    kb.op("dve", lambda e: e.tensor_tensor_scan(out=BC[0:32, :], data0=RM[0:32, :], data1=RF[0:32, :], initial=0.0, op0=ALU.mult, op1=ALU.add), reads=[RM.b, RF.b], writes=[BC.b])
                    kb.op("dve", lambda e: e.tensor_tensor_scan(out=BC[32:64, ::-1], data0=RM[32:64, ::-1], data1=RF[32:64, ::-1], initial=0.0, op0=ALU.mult, op1=ALU.add), reads=[RM.b, RF.b], writes=[BC.b])
                    kb.op("dve", lambda e: e.tensor_tensor(out=RI[:], in0=RI[:], in1=BC[:], op=ALU.subtract), reads=[RI.b, BC.b], writes=[RI.b])
                    kb.op("dve", lambda e: e.tensor_tensor_scan(out=CM[0:32, :], data0=RMA[0:32, :], data1=RI[0:32, :], initial=-1e30, op0=ALU.add, op1=ALU.max), reads=[RMA.b, RI.b], writes=[CM.b])
                    kb.op("dve", lambda e: e.tensor_tensor_scan(out=CM[32:64, ::-1], data0=RMA[32:64, ::-1], data1=RI[32:64, ::-1], initial=-1e30, op0=ALU.add, op1=ALU.max), reads=[RMA.b, RI.b], writes=[CM.b])
                    for dr in range(2):
                        pr = slice(dr * 32, dr * 32 + 32)
                        lastpos = 63 if dr == 0 else 0
                        kb.op("dve", lambda e, pr=pr, lastpos=lastpos: e.tensor_copy(out=CH[pr, 0, :], in_=BC[pr, :].rearrange("p (c s) -> p c s", s=64)[:, :, lastpos]), reads=[BC.b], writes=[CH.b])
                        kb.op("dve", lambda e, pr=pr, lastpos=lastpos: e.tensor_copy(out=CH[pr, 1, :], in_=CM[pr, :].rearrange("p (c s) -> p c s", s=64)[:, :, lastpos]), reads=[CM.b], writes=[CH.b])
                    B_ = [CH.b]
                    kb.op("dve", lambda e: e.tensor_tensor(out=CH[:, 2, :], in0=CH[:, 0, :], in1=CH[:, 1, :], op=ALU.add), reads=B_, writes=B_)
                    kb.op("dve", lambda e: e.tensor_tensor_scan(out=CH[0:32, 3, :], data0=CH[0:32, 0, :], data1=CH[0:32, 2, :], initial=0.0, op0=ALU.add, op1=ALU.max), reads=B_, writes=B_)
                    kb.op("dve", lambda e: e.tensor_tensor_scan(out=CH[32:64, 3, 0:4][:, ::-1], data0=CH[32:64, 0, 0:4][:, ::-1], data1=CH[32:64, 2, 0:4][:, ::-1], initial=0.0, op0=ALU.add, op1=ALU.max), reads=B_, writes=B_)
                    kb.op("dve", lambda e: e.tensor_tensor_scan(out=CH[32:64, 3, 4:NCK][:, ::-1], data0=CH[32:64, 0, 4:NCK][:, ::-1], data1=CH[32:64, 2, 4:NCK][:, ::-1], initial=CH[32:64, 3, 0:1], op0=ALU.add, op1=ALU.max), reads=B_, writes=B_)
                    kb.op("dve", lambda e: e.memset(CH[:, 4, :], 0.0), reads=B_, writes=B_)
                    kb.op("dve", lambda e: e.tensor_copy(out=CH[0:32, 4, 1:NCK], in_=CH[0:32, 3, 0:NCK - 1]), reads=B_, writes=B_)
                    kb.op("dve", lambda e: e.tensor_copy(out=CH[32:64, 4, 0:3], in_=CH[32:64, 3, 1:4]), reads=B_, writes=B_)
                    kb.op("dve", lambda e: e.tensor_copy(out=CH[32:64, 4, 4:NCK - 1], in_=CH[32:64, 3, 5:NCK]), reads=B_, writes=B_)
                    kb.op("dve", lambda e: e.tensor_copy(out=CH[32:64, 4, NCK - 1:NCK], in_=CH[32:64, 3, 0:1]), reads=B_, writes=B_)
                    kb.op("dve", lambda e: e.tensor_tensor(out=CH[:, 5, :], in0=CH[:, 0, :], in1=CH[:, 4, :], op=ALU.add), reads=B_, writes=B_)
                    kb.op("dve", lambda e: e.tensor_tensor(out=CH[:, 5, :], in0=CH[:, 5, :], in1=CH[:, 3, :], op=ALU.subtract), reads=B_, writes=B_)
                    kb.op("dve", lambda e: e.tensor_tensor(out=CH[:, 6, :], in0=CH[:, 2, :], in1=CH[:, 3, :], op=ALU.subtract), reads=B_, writes=B_)
                    kb.op("act", lambda e: e.activation(out=CH[:, 5:7, :], in_=CH[:, 5:7, :], func=AF.Exp), reads=B_, writes=B_)
                    for c in range(NCK):
                        kb.op("dve", lambda e, c=c: e.tensor_scalar(out=RF[:, c * 64:(c + 1) * 64], in0=CM[:, c * 64:(c + 1) * 64], scalar1=-1.0, scalar2=CH[:, 4, c:c + 1],
                                                                     op0=ALU.mult, op1=ALU.add), reads=[CM.b, CH.b], writes=[RF.b])
                        kb.op("dve", lambda e, c=c: e.tensor_scalar(out=X2[:, c * 64:(c + 1) * 64], in0=RI[:, c * 64:(c + 1) * 64], scalar1=CH[:, 1, c:c + 1], scalar2=None,
                                                                     op0=ALU.subtract), reads=[RI.b, CH.b], writes=[X2.b])
                    kb.op("act", lambda e: e.activation(out=X2[:], in_=X2[:], func=AF.Exp), reads=[X2.b], writes=[X2.b])
                    kb.op("dve", lambda e: e.tensor_tensor(out=BC[:], in0=BC[:], in1=CM[:], op=ALU.add), reads=[BC.b, CM.b], writes=[BC.b])
                    kb.op("dve", lambda e: e.scalar_tensor_tensor(out=BC[:], in0=RF[:], scalar=0.0, in1=BC[:], op0=ALU.max, op1=ALU.add), reads=[RF.b, BC.b], writes=[BC.b])
                    kb.op("act", lambda e: e.activation(out=BC[:], in_=BC[:], func=AF.Exp, scale=-1.0), reads=[BC.b], writes=[BC.b])
                    kb.op("dve", lambda e: e.tensor_scalar(out=X1[:], in0=RF[:], scalar1=0.0, scalar2=None, op0=ALU.min), reads=[RF.b], writes=[X1.b])
                    kb.op("act", lambda e: e.activation(out=X1[:], in_=X1[:], func=AF.Exp), reads=[X1.b], writes=[X1.b])
                    kb.op("dve", lambda e: e.tensor_scalar(out=RF[:], in0=RF[:], scalar1=-1.0, scalar2=0.0, op0=ALU.mult, op1=ALU.min), reads=[RF.b], writes=[RF.b])
                    kb.op("act", lambda e: e.activation(out=RF[:], in_=RF[:], func=AF.Exp), reads=[RF.b], writes=[RF.b])
                    for c in range(NCK):
                        pcol = banks[c % 4]
                        for qi, row in enumerate((X1, RF, BC, X2)):
                            kb.op("pe", lambda e, row=row, qi=qi, pcol=pcol, c=c: e.matmul(out=pcol[0:64, qi * 12:(qi + 1) * 12], lhsT=row[:, c * 64:(c + 1) * 64],
                                                                                       rhs=SEL[:, :, 0], start=True, stop=True), reads=[row.b, SEL.b], writes=[pcol.b])
                        kb.op("act", lambda e, pcol=pcol, c=c: e.activation(out=COL[:, c, :, :].rearrange("p a b -> p (a b)"), in_=pcol[0:64, 0:48], func=AF.Identity),
                              writes=[COL.b, pcol.b])
                    for qi in range(2):
                        for r in range(12):
                            pcol = banks[4 + (qi * 12 + r) % 2]
                            kb.op("pe", lambda e, r=r, pcol=pcol, qi=qi: e.matmul(out=pcol[:, 0:NCK], lhsT=SEL[:, r, :], rhs=CH[:, 5 + qi, :], start=True, stop=True), reads=[SEL.b, CH.b], writes=[pcol.b])
                            kb.op("act", lambda e, r=r, pcol=pcol, qi=qi: e.activation(out=AOB[:, qi, r, :], in_=pcol[:, 0:NCK], func=AF.Identity), writes=[AOB.b, pcol.b])
                    kb.barrier()
                psn = [0]

                def pget():
                    bk = banks[psn[0] % 8]
                    psn[0] += 1
                    return bk
                RAW = sb(ph, "mRAW", [128, T])
                QT = sb(ph, "mQT", [64, T]); KT = sb(ph, "mKT", [64, T]); VT = sb(ph, "mVT", [128, T])
                VA = sb(ph, "mVA", [64, NCK, 132])
                HACC = sb(ph, "mHACC", [64, NCK, 128]); OUT = sb(ph, "mOUT", [128, T], BF16)
                CA = sb(ph, "mCA", [64, 132])
                G = 4
                U = []
                for u in range(G):
                    d_ = {}
                    for nm, shp in (("E", [64, 64]), ("wk", [64, 64]), ("tmp", [64, 132]), ("comb", [64, 132]), ("sc", [64, 4])):
                        d_[nm] = sb(ph, "mu%d%s" % (u, nm), shp)
                    U.append(d_)
                fin = [sb(ph, "mfin%d" % i, [64, 128]) for i in range(2)]
                fst = [sb(ph, "mfst%d" % i, [64, 4]) for i in range(2)]
                kb.op("dve", lambda e: e.memset(VA[:], 1.0), writes=[VA.b])
                for hd in range(6):
                    kb.dma("sp", RAW[0:64, :], projT[MQ + hd * 64:MQ + (hd + 1) * 64, :], writes=[RAW.b])
                    seqperm("act", QT, QT.b, RAW, RAW.b, 64)
                    kb.op("act", lambda e: e.activation(out=QT[:], in_=QT[:], func=AF.Identity, scale=0.125), reads=[QT.b], writes=[QT.b])
                    kb.dma("sp", RAW[0:64, :], projT[MK + hd * 64:MK + (hd + 1) * 64, :], reads=[QT.b], writes=[RAW.b])
                    seqperm("dve", KT, KT.b, RAW, RAW.b, 64)
                    kb.dma("sp", RAW[:], projT[MV + hd * 128:MV + (hd + 1) * 128, :], reads=[KT.b], writes=[RAW.b])
                    seqperm("act", VT, VT.b, RAW, RAW.b, 128)
                    for c in range(NCK):
                        bk = pget()
                        kb.op("pe", lambda e, bk=bk, c=c: e.transpose(out=bk[0:64, 0:128], in_=VT[:, c * 64:(c + 1) * 64], identity=ident[:]), reads=[VT.b, ident.b], writes=[bk.b])
                        if c % 2 == 0:
                            kb.op("act", lambda e, bk=bk, c=c: e.activation(out=VA[:, c, 0:128], in_=bk[0:64, 0:128], func=AF.Identity), writes=[VA.b, bk.b])
                        else:
                            kb.op("dve", lambda e, bk=bk, c=c: e.tensor_copy(out=VA[:, c, 0:128], in_=bk[0:64, 0:128]), writes=[VA.b, bk.b])
                    for dr in range(2):
                        r = dr * 6 + hd
                        kb.op("dve", lambda e: e.memset(CA[:], 0.0), writes=[CA.b])
                        order = list(range(NCK)) if dr == 0 else list(range(3, -1, -1)) + list(range(NCK - 1, 3, -1))
                        m_it = 3 if dr == 0 else 2
                        for g0 in range(0, NCK, G):
                            grp = order[g0:g0 + G]
                            for u, c in enumerate(grp):
                                d_ = U[u]
                                ch = slice(c * 64, (c + 1) * 64)
                                be = pget()
                                kb.op("pe", lambda e, be=be, ch=ch: e.matmul(out=be[0:64, 0:64], lhsT=RI[:, ch], rhs=SEL[:, r, 0:64], start=True, stop=False), reads=[RI.b, SEL.b], writes=[be.b])
                                kb.op("pe", lambda e, be=be, ch=ch: e.matmul(out=be[0:64, 0:64], lhsT=NSEL[:, r, 0:64], rhs=CM[:, ch], start=False, stop=False), reads=[CM.b, NSEL.b], writes=[be.b])
                                kb.op("pe", lambda e, be=be: e.matmul(out=be[0:64, 0:64], lhsT=I64, rhs=MB[:, m_it, :], start=False, stop=True), reads=[MB.b, ident.b], writes=[be.b])
                                kb.op("act", lambda e, be=be, d_=d_: e.activation(out=d_["E"][:], in_=be[0:64, 0:64], func=AF.Exp), writes=[d_["E"].b, be.b])
                                bp = pget()
                                kb.op("pe", lambda e, bp=bp, ch=ch: e.matmul(out=bp[0:64, 0:64], lhsT=KT[:, ch], rhs=QT[:, ch], start=True, stop=True), reads=[KT.b, QT.b], writes=[bp.b])
                                kb.op("pe", lambda e, bp=bp, ch=ch: e.transpose(out=bp[0:64, 64:128], in_=KT[:, ch], identity=I64), reads=[KT.b, ident.b], writes=[bp.b])
                                kb.op("dve", lambda e, bp=bp, d_=d_: e.tensor_tensor(out=d_["E"][:], in0=bp[0:64, 0:64], in1=d_["E"][:], op=ALU.mult), reads=[d_["E"].b], writes=[d_["E"].b, bp.b])
                                kb.op("dve", lambda e, bp=bp, d_=d_, c=c: e.tensor_scalar(out=d_["wk"][:], in0=bp[0:64, 64:128], scalar1=COL[:, c, 3, r:r + 1], scalar2=None, op0=ALU.mult),
                                      reads=[COL.b], writes=[d_["wk"].b, bp.b])
                            for u, c in enumerate(grp):
                                d_ = U[u]
                                ch = slice(c * 64, (c + 1) * 64)
                                b1 = pget()
                                kb.op("pe", lambda e, b1=b1, ch=ch: e.matmul(out=b1[0:64, 0:129], lhsT=QT[:, ch], rhs=CA[:, 0:129], start=True, stop=True), reads=[QT.b, CA.b], writes=[b1.b])
                                kb.op("act", lambda e, b1=b1, d_=d_, c=c: e.activation(out=d_["tmp"][:, 0:129], in_=b1[0:64, 0:129], func=AF.Identity, scale=COL[:, c, 0, r:r + 1]),
                                      reads=[COL.b], writes=[d_["tmp"].b, b1.b])
                                b2 = pget()
                                kb.op("pe", lambda e, b2=b2, d_=d_, c=c: e.matmul(out=b2[0:64, 0:129], lhsT=d_["E"][:], rhs=VA[:, c, 0:129], start=True, stop=True), reads=[d_["E"].b, VA.b], writes=[b2.b])
                                kb.op("dve", lambda e, b2=b2, d_=d_, c=c: e.scalar_tensor_tensor(out=d_["comb"][:, 0:129], in0=b2[0:64, 0:129], scalar=COL[:, c, 1, r:r + 1], in1=d_["tmp"][:, 0:129],
                                                                                              op0=ALU.mult, op1=ALU.add), reads=[COL.b, d_["tmp"].b], writes=[d_["comb"].b, b2.b])
                                kb.op("act", lambda e, d_=d_: e.activation(out=d_["sc"][:, 2:3], in_=d_["comb"][:, 128:129], func=AF.Abs), reads=[d_["comb"].b], writes=[d_["sc"].b])
                                kb.op("dve", lambda e, d_=d_, c=c: e.tensor_scalar(out=d_["sc"][:, 0:1], in0=d_["sc"][:, 2:3], scalar1=COL[:, c, 2, r:r + 1], scalar2=None, op0=ALU.max),
                                      reads=[COL.b, d_["sc"].b], writes=[d_["sc"].b])
                                kb.op("dve", lambda e, d_=d_: e.reciprocal(out=d_["sc"][:, 1:2], in_=d_["sc"][:, 0:1]), reads=[d_["sc"].b], writes=[d_["sc"].b])
                                if dr == 0:
                                    kb.op("dve", lambda e, d_=d_, c=c: e.tensor_scalar(out=HACC[:, c, :], in0=d_["comb"][:, 0:128], scalar1=d_["sc"][:, 1:2], scalar2=None, op0=ALU.mult),
                                          reads=[d_["comb"].b, d_["sc"].b], writes=[HACC.b])
                                else:
                                    kb.op("dve", lambda e, d_=d_, c=c: e.scalar_tensor_tensor(out=HACC[:, c, :], in0=d_["comb"][:, 0:128], scalar=d_["sc"][:, 1:2], in1=HACC[:, c, :], op0=ALU.mult, op1=ALU.add),
                                          reads=[d_["comb"].b, d_["sc"].b, HACC.b], writes=[HACC.b])
                                b3 = pget()
                                kb.op("pe", lambda e, b3=b3, d_=d_, c=c: e.matmul(out=b3[0:64, 0:129], lhsT=d_["wk"][:], rhs=VA[:, c, 0:129], start=True, stop=True), reads=[d_["wk"].b, VA.b], writes=[b3.b])
                                kb.op("act", lambda e, c=c: e.activation(out=CA[:, 0:129], in_=CA[:, 0:129], func=AF.Identity, scale=AOB[0:64, 0, r, c:c + 1]), reads=[AOB.b, CA.b], writes=[CA.b])
                                kb.op("dve", lambda e, b3=b3, c=c: e.scalar_tensor_tensor(out=CA[:, 0:129], in0=b3[0:64, 0:129], scalar=AOB[0:64, 1, r, c:c + 1], in1=CA[:, 0:129], op0=ALU.mult, op1=ALU.add),
                                      reads=[AOB.b, CA.b], writes=[CA.b, b3.b])
                    kb.dma("sp", RAW[:], projT[MO + hd * 128:MO + (hd + 1) * 128, :], reads=[VT.b], writes=[RAW.b])
                    seqperm("act", VT, VT.b, RAW, RAW.b, 128)
                    kb.op("act", lambda e: e.activation(out=VT[:], in_=VT[:], func=AF.Sigmoid), reads=[VT.b], writes=[VT.b])
                    for c in range(NCK):
                        f_ = fin[c % 2]; s_ = fst[c % 2]
                        kb.op("act", lambda e, f_=f_, s_=s_, c=c: e.activation(out=f_[:], in_=HACC[:, c, :], func=AF.Square, accum_out=s_[:, 0:1]), reads=[HACC.b], writes=[f_.b, s_.b])
                        kb.op("act", lambda e, s_=s_: e.activation(out=s_[:, 1:2], in_=s_[:, 0:1], func=AF.Sqrt, bias=EPS, scale=1.0 / 128), reads=[s_.b], writes=[s_.b])
                        kb.op("dve", lambda e, s_=s_: e.reciprocal(out=s_[:, 2:3], in_=s_[:, 1:2]), reads=[s_.b], writes=[s_.b])
                        kb.op("dve", lambda e, f_=f_, s_=s_, c=c: e.scalar_tensor_tensor(out=f_[:], in0=HACC[:, c, :], scalar=s_[:, 2:3], in1=NORM[:], op0=ALU.mult, op1=ALU.mult),
                              reads=[HACC.b, s_.b, NORM.b], writes=[f_.b])
                        bk = pget()
                        kb.op("pe", lambda e, bk=bk, f_=f_: e.transpose(out=bk[:, 0:64], in_=f_[:], identity=I64), reads=[f_.b, ident.b], writes=[bk.b])
                        kb.op("dve", lambda e, bk=bk, c=c: e.tensor_tensor(out=RAW[:, c * 64:(c + 1) * 64], in0=bk[:, 0:64], in1=VT[:, c * 64:(c + 1) * 64], op=ALU.mult),
                              reads=[VT.b], writes=[RAW.b, bk.b])
                    kb.op("act", lambda e: e.activation(out=OUT[:, 0:NCTX], in_=RAW[:, 0:NCTX], func=AF.Identity), reads=[RAW.b], writes=[OUT.b])
                    kb.op("act", lambda e: e.activation(out=OUT[:, NCTX:T].rearrange("p (r c) -> p c r", c=64), in_=RAW[:, NCTX:T].rearrange("p (c r) -> p c r", r=32), func=AF.Identity),
                          reads=[RAW.b], writes=[OUT.b])
                    kb.dma("sp", mixT[1280 + hd * 128:1280 + (hd + 1) * 128, :], OUT[:], reads=[OUT.b])
                kb.barrier()
            if stage <= 4:
                break
            with ExitStack() as ph:
                last = (l == L - 1)
                mixb = sb(ph, "mixb", [128, KC, 512], BF16)
                yTb = sb(ph, "yTb", [128, KC, 512])
                h2T = sb(ph, "h2T", [128, KC, 512], BF16)
                hidT = sb(ph, "hidT", [128, 44, 512], BF16)
                xt = [sb(ph, "xt%d" % i, [128, D]) for i in range(2)]
                GG = [[sb(ph, "GG%d%d" % (a, w_), [128, D]) for w_ in range(2)] for a in range(2)]
                wk = [sb(ph, "wk%d" % i, [128, KC, 128], BF16) for i in range(4)]
                wd = [sb(ph, "wd%d" % i, [128, 44, 128], BF16) for i in range(2)]
                sgt = [sb(ph, "sgt%d" % i, [128, 512]) for i in range(2)]
                junk = sb(ph, "junk2", [128, 512], BF16)
                stt_ = [sb(ph, "st2%d" % i, [128, 8]) for i in range(2)]
                bc = sb(ph, "bc", [128, 128])
                for a, (mi, gi) in enumerate(((2, 1), (5, 3))):
                    for w_ in range(2):
                        for j in range(KC):
                            kb.op("dve", lambda e, mi=mi, gi=gi, w_=w_, j=j: e.tensor_tensor(
                                out=bc[:, 0:1], in0=modsT[:, mi * KC + j, w_:w_ + 1], in1=gT[:, (gi * L + l) * KC + j:(gi * L + l) * KC + j + 1],
                                op=ALU.mult), reads=[modsT.b, gT.b], writes=[bc.b])
                            kb.op("dve", lambda e: e.tensor_copy(out=bc[:, 1:128], in_=bc[:, 0:1].to_broadcast([128, 127])), reads=[bc.b], writes=[bc.b])
                            pb = banks[j % 4]
                            kb.op("pe", lambda e, pb=pb: e.transpose(out=pb[:, 0:128], in_=bc[:], identity=ident[:]), reads=[bc.b, ident.b], writes=[pb.b])
                            kb.op("act", lambda e, pb=pb, a=a, w_=w_, j=j: e.activation(out=GG[a][w_][:, j * 128:(j + 1) * 128], in_=pb[:, 0:128], func=AF.Identity),
                                  reads=[pb.b], writes=[GG[a][w_].b])
                wov = w_out[l].rearrange("(k p) c -> p k c", p=128)
                wgv = w_gate[l].rearrange("(k p) c -> p k c", p=128)
                wuv = w_up[l].rearrange("(k p) c -> p k c", p=128)
                wdv = w_down[l].rearrange("(h p) c -> p h c", p=128)
                mixv = mixT.rearrange("(k p) t -> p k t", p=128)
                tstart = NCTX if last else 0
                nwk = [0]
                nx = [0]

                def post_res(tglob, tloc, a, dst):
                    w_ = 1 if tglob < 2 else 0
                    x_ = xt[nx[0] % 2]
                    s_ = stt_[nx[0] % 2]
                    nx[0] += 1
                    kb.dma("sp", x_[:], xres[tglob * 128:(tglob + 1) * 128, :], writes=[x_.b])
                    for jb in range(4):
                        pb = banks[jb]
                        for jj in range(4):
                            j = jb * 4 + jj
                            kb.op("pe", lambda e, pb=pb, jj=jj, j=j: e.transpose(out=pb[:, jj * 128:(jj + 1) * 128], in_=yTb[:, j, tloc * 128:(tloc + 1) * 128],
                                                                              identity=ident[:]), reads=[yTb.b, ident.b], writes=[pb.b])
                        kb.op("act", lambda e, pb=pb, jb=jb, s_=s_: e.activation(out=junk[:], in_=pb[:], func=AF.Square, accum_out=s_[:, jb:jb + 1]),
                              reads=[pb.b], writes=[junk.b, s_.b])
                    kb.op("dve", lambda e, s_=s_: e.tensor_reduce(out=s_[:, 4:5], in_=s_[:, 0:4], axis=AX.X, op=ALU.add), reads=[s_.b], writes=[s_.b])
                    kb.op("act", lambda e, s_=s_: e.activation(out=s_[:, 5:6], in_=s_[:, 4:5], func=AF.Sqrt, bias=EPS, scale=1.0 / D), reads=[s_.b], writes=[s_.b])
                    kb.op("dve", lambda e, s_=s_: e.reciprocal(out=s_[:, 6:7], in_=s_[:, 5:6]), reads=[s_.b], writes=[s_.b])
                    for jb in range(4):
                        pb = banks[jb]
                        sg_ = sgt[jb % 2]
                        kb.op("dve", lambda e, pb=pb, sg_=sg_, jb=jb, s_=s_: e.scalar_tensor_tensor(
                            out=sg_[:], in0=pb[:], scalar=s_[:, 6:7], in1=GG[a][w_][:, jb * 512:(jb + 1) * 512], op0=ALU.mult, op1=ALU.mult),
                            reads=[pb.b, s_.b, GG[a][w_].b], writes=[sg_.b])
                        kb.op("dve", lambda e, sg_=sg_, jb=jb, x_=x_: e.tensor_tensor(out=x_[:, jb * 512:(jb + 1) * 512], in0=x_[:, jb * 512:(jb + 1) * 512], in1=sg_[:],
                                                                                op=ALU.add), reads=[sg_.b, x_.b], writes=[x_.b])
                    kb.dma("sp", dst, x_[:], reads=[x_.b])
                    return x_, s_, w_

                def dense(src, nk, wview, j0, nj, wbufs, t_nt, consume):
                    for j in range(j0, j0 + nj):
                        w = wbufs[nwk[0] % len(wbufs)]
                        nwk[0] += 1
                        kb.dma("pool", w[:, :nk, :], wview[:, :, j * 128:(j + 1) * 128], writes=[w.b])
                        pj = banks[4 + nwk[0] % 2]
                        for k in range(nk):
                            kb.op("pe", lambda e, pj=pj, w=w, k=k: e.matmul(out=pj[:, :t_nt], lhsT=w[:, k, :], rhs=src[:, k, :t_nt], start=(k == 0), stop=(k == nk - 1)),
                                  reads=[w.b, src.b], writes=[pj.b])
                        consume(j, pj)

                t0 = tstart
                while t0 < T:
                    nt = min(512, T - t0)
                    ntl = nt // 128
                    kb.dma("sp", mixb[:, :, :nt], mixv[:, :, t0:t0 + nt], writes=[mixb.b])

                    def cons_y(j, pj):
                        if j % 2 == 0:
                            kb.op("act", lambda e: e.activation(out=yTb[:, j, :nt], in_=pj[:, :nt], func=AF.Identity), reads=[pj.b], writes=[yTb.b])
                        else:
                            kb.op("dve", lambda e: e.tensor_copy(out=yTb[:, j, :nt], in_=pj[:, :nt]), reads=[pj.b], writes=[yTb.b])
                    dense(mixb, KC, wov, 0, KC, wk, nt, cons_y)
                    for tl in range(ntl):
                        tg = t0 // 128 + tl
                        x_, s_, w_ = post_res(tg, tl, 0, xres[tg * 128:(tg + 1) * 128, :])
                        kb.op("act", lambda e, x_=x_, s_=s_: e.activation(out=junk[:], in_=x_[:, 0:512], func=AF.Square, accum_out=s_[:, 0:1]), reads=[x_.b], writes=[junk.b, s_.b])
                        for q in range(1, 4):
                            kb.op("act", lambda e, x_=x_, s_=s_, q=q: e.activation(out=junk[:], in_=x_[:, q * 512:(q + 1) * 512], func=AF.Square, accum_out=s_[:, q:q + 1]),
                                  reads=[x_.b], writes=[junk.b, s_.b])
                        kb.op("dve", lambda e, s_=s_: e.tensor_reduce(out=s_[:, 4:5], in_=s_[:, 0:4], axis=AX.X, op=ALU.add), reads=[s_.b], writes=[s_.b])
                        kb.op("act", lambda e, s_=s_: e.activation(out=s_[:, 5:6], in_=s_[:, 4:5], func=AF.Sqrt, bias=EPS, scale=1.0 / D), reads=[s_.b], writes=[s_.b])
                        kb.op("dve", lambda e, s_=s_: e.reciprocal(out=s_[:, 6:7], in_=s_[:, 5:6]), reads=[s_.b], writes=[s_.b])
                        kb.op("dve", lambda e, x_=x_, s_=s_: e.tensor_scalar(out=x_[:], in0=x_[:], scalar1=s_[:, 6:7], scalar2=None, op0=ALU.mult), reads=[x_.b, s_.b], writes=[x_.b])
                        for jb in range(4):
                            pt = banks[jb]
                            for jj in range(4):
                                j = jb * 4 + jj
                                kb.op("pe", lambda e, pt=pt, x_=x_, jj=jj, j=j: e.transpose(out=pt[:, jj * 128:(jj + 1) * 128], in_=x_[:, j * 128:(j + 1) * 128], identity=ident[:]),
                                      reads=[x_.b, ident.b], writes=[pt.b])
                            for jj in range(4):
                                j = jb * 4 + jj
                                kb.op("act", lambda e, pt=pt, jj=jj, j=j, tl=tl, w_=w_: e.activation(
                                    out=h2T[:, j, tl * 128:(tl + 1) * 128], in_=pt[:, jj * 128:(jj + 1) * 128], func=AF.Identity,
                                    bias=modsT[:, 3 * KC + j, w_:w_ + 1], scale=gsf[:, j, w_:w_ + 1]), reads=[pt.b, modsT.b, gsf.b], writes=[h2T.b])
                    for hc in range(44):
                        got = {}

                        def cons_g(j, pj):
                            got["g"] = pj
                        dense(h2T, KC, wgv, hc, 1, wk, nt, cons_g)
                        pg = got["g"]
                        sg_ = sgt[hc % 2]
                        kb.op("act", lambda e, pg=pg, sg_=sg_: e.activation(out=sg_[:, :nt], in_=pg[:, :nt], func=AF.Silu), reads=[pg.b], writes=[sg_.b])

                        def cons_u(j, pj):
                            kb.op("dve", lambda e: e.tensor_tensor(out=hidT[:, hc, :nt], in0=pj[:, :nt], in1=sg_[:, :nt], op=ALU.mult), reads=[pj.b, sg_.b], writes=[hidT.b])
                        dense(h2T, KC, wuv, hc, 1, wk, nt, cons_u)
                    dense(hidT, 44, wdv, 0, KC, wd, nt, cons_y)
                    for tl in range(ntl):
                        tg = t0 // 128 + tl
                        dst = out_d[(tg - 2) * 128:(tg - 1) * 128, :] if last else xres[tg * 128:(tg + 1) * 128, :]
                        post_res(tg, tl, 1, dst)
                    t0 += nt
                kb.barrier()
            if stage == 5:
                kb.dma("sp", xres_o, xres)
                break
        kb.barrier()
    return nc


def kernel(**inp):
    inp = {k: np.asarray(v) for k, v in inp.items()}
    nc = build(99)
    base = host_prep(inp, 0)
    in_maps = []
    for core in range(8):
        b = core % 4
        m = dict(base)
        if b != 0:
            pb = host_prep_batch(inp, b)
            m.update(pb)
        in_maps.append(m)
    res = run_bass_kernel_spmd(nc, in_maps, core_ids=list(range(8)))
    out = np.stack([np.asarray(res.results[b]["out"]) for b in range(4)], 0).astype(np.float32)
    return out
```

```python
import numpy as np
from contextlib import ExitStack
import concourse.bass as bass
import concourse.mybir as mybir
from concourse.bass_utils import run_bass_kernel_spmd

F32 = mybir.dt.float32
BF16 = mybir.dt.bfloat16
ALU = mybir.AluOpType
AF = mybir.ActivationFunctionType
AX = mybir.AxisListType

D = 2048
T = 2304
NCTX = 256
NLAT = 2048
L = 2
KC = 16
IN_COLS = 5936
FFN = 5632
EPS = 1e-6
NEG = -30000.0
SEM_ROT = 30000
NSLOT = 6
LC = 128


class Ev:
    __slots__ = ("sem", "val")

    def __init__(self, sem, val):
        self.sem = sem
        self.val = val


class Buf:
    __slots__ = ("w", "r", "name")

    def __init__(self, name=""):
        self.w = None
        self.r = {}
        self.name = name


class Eng:
    def __init__(self, kb, name, h):
        self.kb = kb
        self.name = name
        self.h = h
        self.sem = kb.newsem("e_" + name)
        self.count = 0
        self.seen = {}
        self.n = 0


class Slot:
    def __init__(self, sem):
        self.sem = sem
        self.uses = 0


class KB:
    def __init__(self, nc, es):
        self.nc = nc
        self.es = es
        self.nsem = 0
        self.E = {}
        for name, h in (("pe", nc.tensor), ("act", nc.scalar), ("dve", nc.vector), ("pool", nc.gpsimd), ("sp", nc.sync)):
            self.E[name] = Eng(self, name, h)
        self.slots = {}
        self.rr = {}
        for q in ("sp", "pool", "act"):
            self.slots[q] = [Slot(self.newsem("d_%s%d" % (q, i))) for i in range(NSLOT)]
            self.rr[q] = 0

    def newsem(self, name):
        self.nsem += 1
        return self.es.enter_context(self.nc.semaphore("%s_%d" % (name, self.nsem)))

    def _wait(self, eng, ev):
        k = id(ev.sem)
        if eng.seen.get(k, 0) < ev.val:
            eng.h.wait_ge(ev.sem, ev.val)
            eng.seen[k] = ev.val

    def _deps(self, eng, reads, writes):
        need = {}

        def add(ev):
            k = id(ev.sem)
            if k not in need or need[k].val < ev.val:
                need[k] = ev

        for b in reads:
            if b.w is not None:
                add(b.w)
        for b in writes:
            if b.w is not None:
                add(b.w)
            for ev in b.r.values():
                add(ev)
        for ev in need.values():
            if eng.name == "pe" and ev.sem is eng.sem:
                continue
            self._wait(eng, ev)

    def _post(self, ev, reads, writes):
        k = id(ev.sem)
        for b in reads:
            b.r[k] = ev
        for b in writes:
            b.w = ev
            b.r = {}

    def op(self, e, fn, reads=(), writes=()):
        eng = self.E[e]
        self._deps(eng, reads, writes)
        inst = fn(eng.h)
        if eng.count >= SEM_ROT:
            eng.sem = self.newsem("e_" + eng.name)
            eng.count = 0
        eng.count += 1
        eng.n += 1
        inst.then_inc(eng.sem, 1)
        ev = Ev(eng.sem, eng.count)
        self._post(ev, reads, writes)
        return ev

    def dma(self, q, out, in_, reads=(), writes=(), **kw):
        eng = self.E[q]
        self._deps(eng, reads, writes)
        sl = self.slots[q][self.rr[q] % NSLOT]
        self.rr[q] += 1
        if sl.uses * 16 >= SEM_ROT:
            self._wait(eng, Ev(sl.sem, 16 * sl.uses))
            sl.sem = self.newsem("d_" + q)
            sl.uses = 0
        if sl.uses > 0:
            self._wait(eng, Ev(sl.sem, 16 * sl.uses))
        inst = eng.h.dma_start(out=out, in_=in_, **kw)
        sl.uses += 1
        inst.then_inc(sl.sem, 16)
        ev = Ev(sl.sem, 16 * sl.uses)
        self._post(ev, reads, writes)
        return ev

    def barrier(self, engines=("pe", "act", "dve", "pool", "sp")):
        evs = []
        for e in self.E.values():
            if e.count > 0:
                evs.append(Ev(e.sem, e.count))
        for q in self.slots:
            for sl in self.slots[q]:
                if sl.uses > 0:
                    evs.append(Ev(sl.sem, 16 * sl.uses))
        for en in engines:
            eng = self.E[en]
            for ev in evs:
                self._wait(eng, ev)


class Tl:
    def __init__(self, t, name=""):
        self.t = t
        self.b = Buf(name)

    def __getitem__(self, idx):
        return self.t[idx]


def host_prep_batch(inp, b):
    f = np.float32
    m = {}
    m["xin"] = np.ascontiguousarray(np.concatenate([inp["ctx"][b], inp["x"][b]], axis=0).astype(f))
    cv = np.stack([inp["c"][b].reshape(KC, 128).T, inp["c_ctx"].reshape(KC, 128).T], axis=-1)
    m["cv"] = np.ascontiguousarray(cv.astype(f))
    return m


def host_prep(inp, b):
    f = np.float32
    m = host_prep_batch(inp, b)
    m["ada_w"] = inp["ada_w"]
    m["ada_bT"] = np.ascontiguousarray(inp["ada_b"].reshape(L, 96, 128).transpose(2, 0, 1).reshape(128, L * 96).astype(f))
    g = np.stack([inp["norm_mix_pre"], inp["norm_mix_post"], inp["norm_ffn_pre"], inp["norm_ffn_post"]], 0)
    m["gT"] = np.ascontiguousarray(g.reshape(4, L, KC, 128).transpose(3, 0, 1, 2).reshape(128, 4 * L * KC).astype(f))
    m["w_in"] = inp["w_in"]
    for k_ in ("w_out", "ffn_w_gate", "ffn_w_up", "ffn_w_down"):
        m[k_] = inp[k_]
    m["ident"] = np.eye(128, dtype=f)
    def st_major(a):
        return np.ascontiguousarray(a.reshape(L, 2, 16, 128).transpose(3, 0, 1, 2).reshape(128, L * 2 * 16).astype(f))
    m["s5_lre"] = st_major(inp["s5_lam_re"].reshape(L, 2, 2048))
    m["s5_lim"] = st_major(inp["s5_lam_im"].reshape(L, 2, 2048))
    m["s5_ldt"] = st_major(np.repeat(inp["s5_log_dt"], 64, axis=-1))
    Bb = np.zeros((128, L, 2, 2, 16, 128), f)
    Cb = np.zeros((128, L, 2, 2, 16, 128), f)
    for ci, (bn, cn) in enumerate((("s5_b_re", "s5_c_re"), ("s5_b_im", "s5_c_im"))):
        bsrc = inp[bn]
        csrc = inp[cn]
        for g in range(32):
            st = g // 2
            r0 = (g % 8) * 16
            c0 = (g % 2) * 64
            Bb[r0:r0 + 16, :, :, ci, st, c0:c0 + 64] = bsrc[:, :, g].transpose(3, 0, 1, 2)
            Cb[c0:c0 + 64, :, :, ci, st, r0:r0 + 16] = csrc[:, :, g].transpose(3, 0, 1, 2)
    m["s5_Bb"] = np.ascontiguousarray(Bb.reshape(128, L * 2 * 2 * 16, 128))
    m["s5_Cb"] = np.ascontiguousarray(Cb.reshape(128, L * 2 * 2 * 16, 128))
    m["s5_dT"] = np.ascontiguousarray(inp["s5_d"].reshape(L, 4, 128).transpose(2, 0, 1).reshape(128, L * 4).astype(f))
    m["s5_gbT"] = np.ascontiguousarray(inp["s5_glu_b"].reshape(L, 4, 128).transpose(2, 0, 1).reshape(128, L * 4).astype(f))
    m["s5_glu_w"] = inp["s5_glu_w"]
    tt = np.arange(T)
    rm = np.ones((64, T), f)
    rm[0:32, tt % 64 == 0] = 0.0
    rm[32:64, tt % 64 == 63] = 0.0
    m["rm"] = rm
    sel = np.zeros((64, 12, 128), f)
    for r_ in range(12):
        sel[(r_ // 6) * 32 + r_ % 6, r_, :] = 1.0
    m["sel"] = sel
    a_ = np.arange(64)[:, None]; b_ = np.arange(64)[None, :]
    mb = np.stack([np.where(b_ < a_, 0.0, NEG), np.where(b_ > a_, 0.0, NEG), np.where(b_ <= a_, 0.0, NEG), np.where(b_ >= a_, 0.0, NEG)], 1).astype(f)
    m["mb"] = np.ascontiguousarray(mb)
    m["gdn_normr"] = np.ascontiguousarray(np.tile(inp["gdn_norm"].reshape(1, L * 128), (64, 1)).astype(f))
    gp = np.zeros((64, L, 2), f)
    for dr_ in range(2):
        gp[dr_ * 32:dr_ * 32 + 6, :, 0] = inp["gdn_a_log"][:, dr_, :].T
        gp[dr_ * 32:dr_ * 32 + 6, :, 1] = inp["gdn_dt_bias"][:, dr_, :].T
    m["gdn_gp"] = np.ascontiguousarray(gp.reshape(64, L * 2))
    cw = inp["gdn_conv_w"].reshape(L, 3, 18, 128).transpose(3, 0, 2, 1)
    m["gdn_cw"] = np.ascontiguousarray(cw.reshape(128, L * 54).astype(f))
    rma = np.zeros((64, T), f)
    rma[0:32, tt % 64 == 0] = -1e30
    rma[32:64, tt % 64 == 63] = -1e30
    m["rma"] = rma
    m["ml_normr"] = np.ascontiguousarray(np.tile(inp["mlstm_norm"].reshape(1, L * 128), (64, 1)).astype(f))
    mp = np.zeros((64, L, 2), f)
    for dr_ in range(2):
        mp[dr_ * 32:dr_ * 32 + 6, :, 0] = inp["mlstm_i_bias"][:, dr_, :].T
        mp[dr_ * 32:dr_ * 32 + 6, :, 1] = inp["mlstm_f_bias"][:, dr_, :].T
    m["ml_mp"] = np.ascontiguousarray(mp.reshape(64, L * 2))
    m["tau1"] = np.ascontiguousarray(np.tile(np.arange(1, LC + 1, dtype=f)[None, :], (128, 1)))
    return m


def build(stage=99):
    nc = bass.Bass("TRN2", target_bir_lowering=False)
    es = ExitStack()
    with es:
        def din(name, shape, dt=F32):
            return nc.dram_tensor(name, list(shape), dt, kind="ExternalInput").ap()

        def dout(name, shape, dt=F32):
            return nc.dram_tensor(name, list(shape), dt, kind="ExternalOutput").ap()

        def dscr(name, shape, dt=F32):
            return nc.dram_tensor(name, list(shape), dt, kind="Internal").ap()

        xin = din("xin", [T, D])
        cv_d = din("cv", [128, KC, 2])
        ada_w = din("ada_w", [L, D, 6 * D])
        ada_bT = din("ada_bT", [128, L * 96])
        gT_d = din("gT", [128, 4 * L * KC])
        w_in = din("w_in", [L, D, IN_COLS])
        ident_d = din("ident", [128, 128])
        s5_lre_d = din("s5_lre", [128, L * 32])
        s5_lim_d = din("s5_lim", [128, L * 32])
        s5_ldt_d = din("s5_ldt", [128, L * 32])
        s5_Bb_d = din("s5_Bb", [128, L * 64, 128])
        s5_Cb_d = din("s5_Cb", [128, L * 64, 128])
        s5_dT_d = din("s5_dT", [128, L * 4])
        s5_gbT_d = din("s5_gbT", [128, L * 4])
        s5_gluw_d = din("s5_glu_w", [L, 512, 512])
        tau1_d = din("tau1", [128, LC])
        rm_d = din("rm", [64, T])
        sel_d = din("sel", [64, 12, 128])
        mb_d = din("mb", [64, 4, 64])
        gdn_norm_d = din("gdn_normr", [64, L * 128])
        gdn_gp_d = din("gdn_gp", [64, L * 2])
        gdn_cw_d = din("gdn_cw", [128, L * 54])
        rma_d = din("rma", [64, T])
        ml_norm_d = din("ml_normr", [64, L * 128])
        ml_mp_d = din("ml_mp", [64, L * 2])
        w_out = din("w_out", [L, D, D])
        w_gate = din("ffn_w_gate", [L, D, FFN])
        w_up = din("ffn_w_up", [L, D, FFN])
        w_down = din("ffn_w_down", [L, FFN, D])
        out_d = dout("out", [NLAT, D])
        xres = dscr("xres", [T, D])
        if stage <= 1:
            projT = dout("projT", [47 * 128, T])
            mods_o = dout("mods_o", [128, 96 * 2])
        else:
            projT = dscr("projT", [47 * 128, T])
        if stage == 3:
            dbg_cv = dout("dbg_cv", [128, 3, T])
            dbg_rows = dout("dbg_rows", [64, 6, T])
            dbg_oacc = dout("dbg_oacc", [64, 36, 128])
            dbg_oaccf = dout("dbg_oaccf", [64, 36, 128])
        if stage == 5:
            mixT = din("mixT", [D, T], BF16)
            xres_o = dout("xres_o", [T, D])
        elif 2 <= stage <= 4:
            mixT = dout("mixT", [D, T], BF16)
        else:
            mixT = dscr("mixT", [D, T], BF16)

        kb = KB(nc, es)

        cnt = [0]

        def sb(st, name, shape, dt=F32):
            cnt[0] += 1
            nm = "s%d_%s" % (cnt[0], name)
            return Tl(st.enter_context(nc.sbuf_tensor(nm, list(shape), dt)), nm)

        def ps(st, name, shape, dt=F32):
            cnt[0] += 1
            nm = "p%d_%s" % (cnt[0], name)
            return Tl(st.enter_context(nc.psum_tensor(nm, list(shape), dt)), nm)

        ident = sb(es, "ident", [128, 128])
        gT = sb(es, "gT", [128, 4 * L * KC])
        abT = sb(es, "abT", [128, L * 96])
        cvs = sb(es, "cvs", [128, KC, 2])
        modsT = sb(es, "modsT", [128, 96, 2])
        gsm = sb(es, "gsm", [128, KC, 2])
        gsf = sb(es, "gsf", [128, KC, 2])
        kb.dma("sp", ident[:], ident_d, writes=[ident.b])
        kb.dma("sp", gT[:], gT_d, writes=[gT.b])
        kb.dma("sp", abT[:], ada_bT, writes=[abT.b])
        kb.dma("sp", cvs[:], cv_d, writes=[cvs.b])
        kb.op("act", lambda e: e.activation(out=cvs[:], in_=cvs[:], func=AF.Silu), reads=[cvs.b], writes=[cvs.b])

        banks = [ps(es, "bank%d" % i, [128, 512]) for i in range(8)]
        xres_b = Buf("xres")
        kb.dma("sp", xres, xin, writes=[xres_b])
        kb.barrier()

        for l in range(L):
            with ExitStack() as ph:
                wA = [sb(ph, "wA%d" % i, [128, KC, 512]) for i in range(2)]
                pm = banks[0]
                adv = ada_w[l].rearrange("(k p) c -> p k c", p=128)
                for cb in range(24):
                    w = wA[cb % 2]
                    kb.dma("sp", w[:], adv[:, :, cb * 512:(cb + 1) * 512], writes=[w.b])
                    for jj in range(4):
                        jo = cb * 4 + jj
                        for k in range(KC):
                            kb.op("pe", lambda e, w=w, k=k, jj=jj, jo=jo: e.matmul(
                                out=pm[:, jo * 2:jo * 2 + 2], lhsT=w[:, k, jj * 128:(jj + 1) * 128], rhs=cvs[:, k, :],
                                start=(k == 0), stop=(k == KC - 1)), reads=[w.b, cvs.b], writes=[pm.b])
                for wi in range(2):
                    kb.op("dve", lambda e, wi=wi: e.tensor_tensor(
                        out=modsT[:, :, wi], in0=pm[:, wi:192:2], in1=abT[:, l * 96:(l + 1) * 96], op=ALU.add),
                        reads=[pm.b, abT.b], writes=[modsT.b])
                for (gs, mi, gi) in ((gsm, 1, 0), (gsf, 4, 2)):
                    for wi in range(2):
                        kb.op("dve", lambda e, gs=gs, mi=mi, gi=gi, wi=wi: e.scalar_tensor_tensor(
                            out=gs[:, :, wi], in0=modsT[:, mi * KC:(mi + 1) * KC, wi], scalar=1.0,
                            in1=gT[:, (gi * L + l) * KC:(gi * L + l + 1) * KC], op0=ALU.add, op1=ALU.mult),
                            reads=[modsT.b, gT.b], writes=[gs.b])
                kb.barrier()
            if stage <= 1 and l == 0:
                kb.dma("sp", mods_o, modsT[:].rearrange("p a b -> p (a b)"), reads=[modsT.b])

            with ExitStack() as lay:
                hT = sb(lay, "hT", [128, KC, T], BF16)
                with ExitStack() as ph:
                    xt = [sb(ph, "xt%d" % i, [128, D]) for i in range(2)]
                    junk = sb(ph, "junk", [128, D], BF16)
                    st = [sb(ph, "st%d" % i, [128, 4]) for i in range(2)]
                    for i in range(T // 128):
                        x_ = xt[i % 2]
                        s_ = st[i % 2]
                        wsel = 1 if i < 2 else 0
                        kb.dma("sp", x_[:], xres[i * 128:(i + 1) * 128, :], writes=[x_.b])
                        kb.op("act", lambda e, x_=x_, s_=s_: e.activation(out=junk[:], in_=x_[:], func=AF.Square,
                                                                           accum_out=s_[:, 0:1]),
                              reads=[x_.b], writes=[junk.b, s_.b])
                        kb.op("act", lambda e, s_=s_: e.activation(out=s_[:, 1:2], in_=s_[:, 0:1], func=AF.Sqrt,
                                                                    bias=EPS, scale=1.0 / D), reads=[s_.b], writes=[s_.b])
                        kb.op("dve", lambda e, s_=s_: e.reciprocal(out=s_[:, 2:3], in_=s_[:, 1:2]), reads=[s_.b], writes=[s_.b])
                        kb.op("dve", lambda e, x_=x_, s_=s_: e.tensor_scalar(out=x_[:], in0=x_[:], scalar1=s_[:, 2:3], scalar2=None,
                                                                          op0=ALU.mult), reads=[x_.b, s_.b], writes=[x_.b])
                        for jb in range(4):
                            pt = banks[1 + (i * 4 + jb) % 4]
                            for jj in range(4):
                                j = jb * 4 + jj
                                kb.op("pe", lambda e, pt=pt, x_=x_, jj=jj, j=j: e.transpose(
                                    out=pt[:, jj * 128:(jj + 1) * 128], in_=x_[:, j * 128:(j + 1) * 128], identity=ident[:]),
                                    reads=[x_.b, ident.b], writes=[pt.b])
                            for jj in range(4):
                                j = jb * 4 + jj
                                kb.op("act", lambda e, pt=pt, jj=jj, j=j, i=i, wsel=wsel: e.activation(
                                    out=hT[:, j, i * 128:(i + 1) * 128], in_=pt[:, jj * 128:(jj + 1) * 128], func=AF.Identity,
                                    bias=modsT[:, 0 * KC + j, wsel:wsel + 1], scale=gsm[:, j, wsel:wsel + 1]),
                                    reads=[pt.b, modsT.b, gsm.b], writes=[hT.b])
                    kb.barrier()
                with ExitStack() as ph:
                    wC = [sb(ph, "wC%d" % i, [128, KC, 128], BF16) for i in range(2)]
                    sg = [sb(ph, "sg%d" % i, [128, 512]) for i in range(3)]
                    wv = w_in[l].rearrange("(k p) c -> p k c", p=128)
                    nev = 0
                    for cc in range(47):
                        c0 = cc * 128
                        n = min(128, IN_COLS - c0)
                        w = wC[cc % 2]
                        kb.dma("pool", w[:, :, :n], wv[:, :, c0:c0 + n], writes=[w.b])
                        for tb in range(5):
                            t0 = tb * 512
                            nt = min(512, T - t0)
                            pj = banks[5 + nev % 3]
                            for k in range(KC):
                                kb.op("pe", lambda e, pj=pj, w=w, k=k, n=n, t0=t0, nt=nt: e.matmul(
                                    out=pj[:n, :nt], lhsT=w[:, k, :n], rhs=hT[:, k, t0:t0 + nt],
                                    start=(k == 0), stop=(k == KC - 1)), reads=[w.b, hT.b], writes=[pj.b])
                            s_ = sg[nev % 3]
                            eng = "act" if nev % 2 == 0 else "dve"
                            if eng == "act":
                                kb.op("act", lambda e, s_=s_, pj=pj, n=n, nt=nt: e.activation(out=s_[:n, :nt], in_=pj[:n, :nt],
                                                                                            func=AF.Identity),
                                      reads=[pj.b], writes=[s_.b])
                            else:
                                kb.op("dve", lambda e, s_=s_, pj=pj, n=n, nt=nt: e.tensor_copy(out=s_[:n, :nt], in_=pj[:n, :nt]),
                                      reads=[pj.b], writes=[s_.b])
                            kb.dma("sp", projT[c0:c0 + n, t0:t0 + nt], s_[:n, :nt], reads=[s_.b])
                            nev += 1
                    kb.barrier()
            if stage <= 1:
                break
            with ExitStack() as ph:
                TWO_PI = 6.283185307179586
                C1 = 6.28125
                C2 = TWO_PI - C1
                PI = 3.141592653589793
                uT = sb(ph, "uT", [128, 4, T])
                yT = sb(ph, "yT", [128, 4, T])
                Bb = sb(ph, "Bb", [128, 32, 128])
                Cb = sb(ph, "Cb", [128, 32, 128])
                lre = sb(ph, "lre", [128, 32]); lim = sb(ph, "lim", [128, 32]); ldt = sb(ph, "ldt", [128, 32])
                tau1 = sb(ph, "tau1", [128, LC])
                dTt = sb(ph, "dTt", [128, L * 4]); gbT = sb(ph, "gbT", [128, L * 4])
                gluw = sb(ph, "gluw", [128, 4, 512])
                kb.dma("sp", uT[:], projT[0:512, :].rearrange("(c p) t -> p c t", p=128), writes=[uT.b])
                kb.dma("sp", lre[:], s5_lre_d[:, l * 32:(l + 1) * 32], writes=[lre.b])
                kb.dma("sp", lim[:], s5_lim_d[:, l * 32:(l + 1) * 32], writes=[lim.b])
                kb.dma("sp", ldt[:], s5_ldt_d[:, l * 32:(l + 1) * 32], writes=[ldt.b])
                kb.dma("sp", tau1[:], tau1_d, writes=[tau1.b])
                kb.dma("sp", dTt[:], s5_dT_d, writes=[dTt.b])
                kb.dma("sp", gbT[:], s5_gbT_d, writes=[gbT.b])
                kb.dma("sp", gluw[:], s5_gluw_d[l].rearrange("(c p) n -> p c n", p=128), writes=[gluw.b])

                def sincos(n, ang, o_sin, o_cos, tf, ti, tm, tb_):
                    B = [tb_]
                    kb.op("dve", lambda e: e.tensor_scalar(out=tf, in0=ang, scalar1=1.0 / TWO_PI, scalar2=None, op0=ALU.mult), reads=B, writes=B)
                    kb.op("dve", lambda e: e.tensor_copy(out=ti, in_=tf), reads=B, writes=B)
                    kb.op("dve", lambda e: e.tensor_copy(out=tf, in_=ti), reads=B, writes=B)
                    kb.op("dve", lambda e: e.scalar_tensor_tensor(out=tm, in0=tf, scalar=-C1, in1=ang, op0=ALU.mult, op1=ALU.add), reads=B, writes=B)
                    kb.op("dve", lambda e: e.scalar_tensor_tensor(out=tm, in0=tf, scalar=-C2, in1=tm, op0=ALU.mult, op1=ALU.add), reads=B, writes=B)

                    def wrap(y):
                        kb.op("dve", lambda e: e.tensor_scalar(out=tf, in0=y, scalar1=PI, scalar2=TWO_PI, op0=ALU.is_gt, op1=ALU.mult), reads=B, writes=B)
                        kb.op("dve", lambda e: e.tensor_tensor(out=y, in0=y, in1=tf, op=ALU.subtract), reads=B, writes=B)
                        kb.op("dve", lambda e: e.tensor_scalar(out=tf, in0=y, scalar1=-PI, scalar2=TWO_PI, op0=ALU.is_lt, op1=ALU.mult), reads=B, writes=B)
                        kb.op("dve", lambda e: e.tensor_tensor(out=y, in0=y, in1=tf, op=ALU.add), reads=B, writes=B)
                    wrap(tm)
                    kb.op("act", lambda e: e.activation(out=o_sin, in_=tm, func=AF.Sin), reads=B, writes=B)
                    kb.op("dve", lambda e: e.tensor_scalar(out=tm, in0=tm, scalar1=PI / 2, scalar2=None, op0=ALU.add), reads=B, writes=B)
                    wrap(tm)
                    kb.op("act", lambda e: e.activation(out=o_cos, in_=tm, func=AF.Sin), reads=B, writes=B)

                with ExitStack() as ph2:
                    tabs = sb(ph2, "tabs", [128, 4, 16 * LC])
                    tw = sb(ph2, "tw", [128, 3, 16 * LC])
                    twi = sb(ph2, "twi", [128, 16 * LC], mybir.dt.int32)
                    sp_ = sb(ph2, "s5par", [128, 16, 16])
                    spi = sb(ph2, "s5pari", [128, 16], mybir.dt.int32)
                    car = sb(ph2, "car", [128, 2, 16])
                    S5U = [sb(ph2, "s5u%d" % i, [128, 6, LC]) for i in range(8)]
                    S5B = [tabs.b, tw.b, twi.b, sp_.b, spi.b]

                    def P_(k):
                        return sp_[:, k, :]
                    for i in range(2):
                        o = (l * 2 + i) * 16
                        B = [sp_.b]
                        kb.dma("sp", Bb[:], s5_Bb_d[:, (l * 2 + i) * 32:(l * 2 + i + 1) * 32, :], writes=[Bb.b])
                        kb.dma("sp", Cb[:], s5_Cb_d[:, (l * 2 + i) * 32:(l * 2 + i + 1) * 32, :], writes=[Cb.b])
                        kb.op("act", lambda e: e.activation(out=Cb[:, 16:32, :], in_=Cb[:, 16:32, :], func=AF.Identity, scale=-1.0),
                              reads=[Cb.b], writes=[Cb.b])
                        kb.op("act", lambda e: e.activation(out=P_(0), in_=ldt[:, (l * 2 + i) * 16 - l * 32 + 0:(l * 2 + i) * 16 - l * 32 + 16], func=AF.Exp), reads=[ldt.b], writes=B)
                        kb.op("dve", lambda e: e.tensor_tensor(out=P_(1), in0=lre[:, i * 16:(i + 1) * 16], in1=P_(0), op=ALU.mult), reads=[lre.b] + B, writes=B)
                        kb.op("dve", lambda e: e.tensor_tensor(out=P_(2), in0=lim[:, i * 16:(i + 1) * 16], in1=P_(0), op=ALU.mult), reads=[lim.b] + B, writes=B)
                        kb.op("act", lambda e: e.activation(out=P_(3), in_=P_(1), func=AF.Exp), reads=B, writes=B)
                        sincos(16, P_(2), P_(4), P_(5), P_(6), spi[:], P_(7), sp_.b)
                        kb.op("dve", lambda e: e.tensor_tensor(out=P_(6), in0=P_(3), in1=P_(5), op=ALU.mult), reads=B, writes=B)
                        kb.op("dve", lambda e: e.tensor_scalar(out=P_(6), in0=P_(6), scalar1=-1.0, scalar2=None, op0=ALU.add), reads=B, writes=B)
                        kb.op("dve", lambda e: e.tensor_tensor(out=P_(7), in0=P_(3), in1=P_(4), op=ALU.mult), reads=B, writes=B)
                        kb.op("dve", lambda e: e.tensor_tensor(out=P_(8), in0=lre[:, i * 16:(i + 1) * 16], in1=lre[:, i * 16:(i + 1) * 16], op=ALU.mult), reads=[lre.b] + B, writes=B)
                        kb.op("dve", lambda e: e.tensor_tensor(out=P_(9), in0=lim[:, i * 16:(i + 1) * 16], in1=lim[:, i * 16:(i + 1) * 16], op=ALU.mult), reads=[lim.b] + B, writes=B)
                        kb.op("dve", lambda e: e.tensor_tensor(out=P_(8), in0=P_(8), in1=P_(9), op=ALU.add), reads=B, writes=B)
                        kb.op("dve", lambda e: e.reciprocal(out=P_(8), in_=P_(8)), reads=B, writes=B)
                        kb.op("dve", lambda e: e.tensor_tensor(out=P_(10), in0=P_(6), in1=lre[:, i * 16:(i + 1) * 16], op=ALU.mult), reads=[lre.b] + B, writes=B)
                        kb.op("dve", lambda e: e.tensor_tensor(out=P_(9), in0=P_(7), in1=lim[:, i * 16:(i + 1) * 16], op=ALU.mult), reads=[lim.b] + B, writes=B)
                        kb.op("dve", lambda e: e.tensor_tensor(out=P_(10), in0=P_(10), in1=P_(9), op=ALU.add), reads=B, writes=B)
                        kb.op("dve", lambda e: e.tensor_tensor(out=P_(10), in0=P_(10), in1=P_(8), op=ALU.mult), reads=B, writes=B)
                        kb.op("dve", lambda e: e.tensor_tensor(out=P_(11), in0=P_(7), in1=lre[:, i * 16:(i + 1) * 16], op=ALU.mult), reads=[lre.b] + B, writes=B)
                        kb.op("dve", lambda e: e.tensor_tensor(out=P_(9), in0=P_(6), in1=lim[:, i * 16:(i + 1) * 16], op=ALU.mult), reads=[lim.b] + B, writes=B)
                        kb.op("dve", lambda e: e.tensor_tensor(out=P_(11), in0=P_(11), in1=P_(9), op=ALU.subtract), reads=B, writes=B)
                        kb.op("dve", lambda e: e.tensor_tensor(out=P_(11), in0=P_(11), in1=P_(8), op=ALU.mult), reads=B, writes=B)
                        for st in range(16):
                            kb.op("dve", lambda e, st=st: e.tensor_scalar(out=tw[:, 0, st * LC:(st + 1) * LC], in0=tau1[:], scalar1=sp_[:, 2, st:st + 1],
                                                                         scalar2=None, op0=ALU.mult), reads=[tau1.b] + B, writes=[tw.b])
                        sincos(16 * LC, tw[:, 0, :], tabs[:, 3, :], tabs[:, 2, :], tw[:, 1, :], twi[:], tw[:, 2, :], tw.b)
                        kb.op("dve", lambda e: e.tensor_copy(out=tw[:, 0, 0:1], in_=tw[:, 0, 0:1]), reads=[tw.b, tabs.b], writes=[tw.b, tabs.b])
                        for st in range(16):
                            sl = slice(st * LC, (st + 1) * LC)
                            kb.op("dve", lambda e, st=st, sl=sl: e.tensor_scalar(out=tw[:, 1, sl], in0=tabs[:, 3, sl], scalar1=sp_[:, 11, st:st + 1], scalar2=None, op0=ALU.mult), reads=[tabs.b] + B, writes=[tw.b])
                            kb.op("dve", lambda e, st=st, sl=sl: e.scalar_tensor_tensor(out=tabs[:, 0, sl], in0=tabs[:, 2, sl], scalar=sp_[:, 10, st:st + 1], in1=tw[:, 1, sl], op0=ALU.mult, op1=ALU.add), reads=[tw.b] + B, writes=[tabs.b])
                            kb.op("dve", lambda e, st=st, sl=sl: e.tensor_scalar(out=tw[:, 1, sl], in0=tabs[:, 3, sl], scalar1=sp_[:, 10, st:st + 1], scalar2=None, op0=ALU.mult), reads=[tabs.b] + B, writes=[tw.b])
                            kb.op("dve", lambda e, st=st, sl=sl: e.scalar_tensor_tensor(out=tabs[:, 1, sl], in0=tabs[:, 2, sl], scalar=sp_[:, 11, st:st + 1], in1=tw[:, 1, sl], op0=ALU.mult, op1=ALU.subtract), reads=[tw.b] + B, writes=[tabs.b])
                        kb.op("dve", lambda e: e.memset(car[:], 0.0), writes=[car.b])
                        NCH = T // LC
                        NCC = NCTX // LC
                        order = list(range(NCH)) if i == 0 else list(range(NCC - 1, -1, -1)) + list(range(NCH - 1, NCC - 1, -1))

                        def rv(ap):
                            return ap[:, ::-1] if i == 1 else ap
                        lastc = LC - 1 if i == 0 else 0
                        for n in order:
                            t0 = n * LC
                            for hf in range(2):
                                for q in range(8):
                                    st = hf * 8 + q
                                    pb = banks[q // 2]
                                    for ci in range(2):
                                        c0 = (q % 2) * 2 * LC + ci * LC
                                        kb.op("pe", lambda e, ci=ci, pb=pb, st=st, c0=c0: e.matmul(out=pb[:, c0:c0 + LC], lhsT=Bb[:, ci * 16 + st, :], rhs=uT[:, st // 4, t0:t0 + LC],
                                                                                              start=True, stop=True), reads=[Bb.b, uT.b], writes=[pb.b])
                                for q in range(8):
                                    st = hf * 8 + q
                                    pb = banks[q // 2]
                                    sl = slice(st * LC, (st + 1) * LC)
                                    u_ = S5U[q]
                                    bre = rv(pb[:, (q % 2) * 2 * LC:(q % 2) * 2 * LC + LC]); bim = rv(pb[:, (q % 2) * 2 * LC + LC:(q % 2) * 2 * LC + 2 * LC])
                                    kb.op("dve", lambda e, u_=u_, bre=bre, sl=sl: e.tensor_tensor(out=u_[:, 0, :], in0=bre, in1=tabs[:, 0, sl], op=ALU.mult), reads=[pb.b, tabs.b], writes=[u_.b])
                                    kb.op("dve", lambda e, u_=u_, bim=bim, sl=sl: e.tensor_tensor(out=u_[:, 1, :], in0=bim, in1=tabs[:, 1, sl], op=ALU.mult), reads=[pb.b, tabs.b], writes=[u_.b])
                                    kb.op("dve", lambda e, u_=u_, bre=bre, sl=sl: e.tensor_tensor(out=u_[:, 2, :], in0=bre, in1=tabs[:, 1, sl], op=ALU.mult), reads=[pb.b, tabs.b], writes=[u_.b])
                                    kb.op("dve", lambda e, u_=u_, bim=bim, sl=sl: e.tensor_tensor(out=u_[:, 3, :], in0=bim, in1=tabs[:, 0, sl], op=ALU.mult), reads=[pb.b, tabs.b], writes=[u_.b])
                                for q in range(8):
                                    u_ = S5U[q]
                                    kb.op("pool", lambda e, u_=u_: e.tensor_tensor(out=u_[:, 0, :], in0=u_[:, 0, :], in1=u_[:, 1, :], op=ALU.subtract), reads=[u_.b], writes=[u_.b])
                                    kb.op("pool", lambda e, u_=u_: e.tensor_tensor(out=u_[:, 2, :], in0=u_[:, 2, :], in1=u_[:, 3, :], op=ALU.add), reads=[u_.b], writes=[u_.b])
                                for q in range(8):
                                    st = hf * 8 + q
                                    u_ = S5U[q]
                                    rb = sp_[:, 3, st:st + 1].to_broadcast([128, LC])
                                    kb.op("dve", lambda e, u_=u_, rb=rb, st=st: e.tensor_tensor_scan(out=u_[:, 1, :], data0=rb, data1=u_[:, 0, :], initial=car[:, 0, st:st + 1], op0=ALU.mult, op1=ALU.add),
                                          reads=[u_.b, sp_.b, car.b], writes=[u_.b])
                                    kb.op("dve", lambda e, u_=u_, rb=rb, st=st: e.tensor_tensor_scan(out=u_[:, 3, :], data0=rb, data1=u_[:, 2, :], initial=car[:, 1, st:st + 1], op0=ALU.mult, op1=ALU.add),
                                          reads=[u_.b, sp_.b, car.b], writes=[u_.b])
                                for q in range(8):
                                    st = hf * 8 + q
                                    sl = slice(st * LC, (st + 1) * LC)
                                    u_ = S5U[q]
                                    kb.op("pool", lambda e, u_=u_, sl=sl: e.tensor_tensor(out=u_[:, 0, :], in0=u_[:, 1, :], in1=tabs[:, 2, sl], op=ALU.mult), reads=[u_.b, tabs.b], writes=[u_.b])
                                    kb.op("pool", lambda e, u_=u_, sl=sl: e.tensor_tensor(out=u_[:, 2, :], in0=u_[:, 3, :], in1=tabs[:, 3, sl], op=ALU.mult), reads=[u_.b, tabs.b], writes=[u_.b])
                                    kb.op("pool", lambda e, u_=u_: e.tensor_tensor(out=rv(u_[:, 4, :]), in0=u_[:, 0, :], in1=u_[:, 2, :], op=ALU.subtract), reads=[u_.b], writes=[u_.b])
                                    kb.op("pool", lambda e, u_=u_, sl=sl: e.tensor_tensor(out=u_[:, 0, :], in0=u_[:, 1, :], in1=tabs[:, 3, sl], op=ALU.mult), reads=[u_.b, tabs.b], writes=[u_.b])
                                    kb.op("pool", lambda e, u_=u_, sl=sl: e.tensor_tensor(out=u_[:, 2, :], in0=u_[:, 3, :], in1=tabs[:, 2, sl], op=ALU.mult), reads=[u_.b, tabs.b], writes=[u_.b])
                                    kb.op("pool", lambda e, u_=u_: e.tensor_tensor(out=rv(u_[:, 5, :]), in0=u_[:, 0, :], in1=u_[:, 2, :], op=ALU.add), reads=[u_.b], writes=[u_.b])
                                for q in range(8):
                                    st = hf * 8 + q
                                    u_ = S5U[q]
                                    kb.op("act", lambda e, u_=u_, st=st: e.activation(out=car[:, 0, st:st + 1], in_=u_[:, 4, lastc:lastc + 1], func=AF.Identity), reads=[u_.b], writes=[car.b])
                                    kb.op("act", lambda e, u_=u_, st=st: e.activation(out=car[:, 1, st:st + 1], in_=u_[:, 5, lastc:lastc + 1], func=AF.Identity), reads=[u_.b], writes=[car.b])
                                for f2 in range(2):
                                    fc = hf * 2 + f2
                                    py = banks[4 + fc]
                                    for q4 in range(4):
                                        q = f2 * 4 + q4
                                        st = hf * 8 + q
                                        u_ = S5U[q]
                                        kb.op("pe", lambda e, py=py, st=st, u_=u_, q4=q4: e.matmul(out=py[:, 0:LC], lhsT=Cb[:, st, :], rhs=u_[:, 4, :], start=(q4 == 0), stop=False),
                                              reads=[Cb.b, u_.b], writes=[py.b])
                                        kb.op("pe", lambda e, py=py, st=st, u_=u_, q4=q4: e.matmul(out=py[:, 0:LC], lhsT=Cb[:, 16 + st, :], rhs=u_[:, 5, :], start=False, stop=(q4 == 3)),
                                              reads=[Cb.b, u_.b], writes=[py.b])
                                    if i == 0:
                                        kb.op("act", lambda e, py=py, fc=fc: e.activation(out=yT[:, fc, t0:t0 + LC], in_=py[:, 0:LC], func=AF.Identity), writes=[yT.b, py.b])
                                    else:
                                        kb.op("dve", lambda e, py=py, fc=fc: e.tensor_tensor(out=yT[:, fc, t0:t0 + LC], in0=py[:, 0:LC], in1=yT[:, fc, t0:t0 + LC], op=ALU.add),
                                              writes=[yT.b, py.b])
                    kb.barrier()
                with ExitStack() as ph2:
                    g1 = sb(ph2, "g1", [128, T]); g2 = sb(ph2, "g2", [128, T])
                    og = [sb(ph2, "og%d" % i, [128, 512], BF16) for i in range(2)]
                    sgl = [sb(ph2, "sgl%d" % i, [128, 512]) for i in range(2)]
                    for fc in range(4):
                        kb.op("dve", lambda e: e.scalar_tensor_tensor(out=yT[:, fc, :], in0=uT[:, fc, :], scalar=dTt[:, l * 4 + fc:l * 4 + fc + 1], in1=yT[:, fc, :],
                                                                      op0=ALU.mult, op1=ALU.add), reads=[uT.b, dTt.b, yT.b], writes=[yT.b])
                        kb.op("act", lambda e: e.activation(out=g1[:], in_=yT[:, fc, :], func=AF.Square), reads=[yT.b], writes=[g1.b])
                        kb.op("dve", lambda e: e.tensor_scalar(out=g1[:], in0=g1[:], scalar1=0.044715, scalar2=1.0, op0=ALU.mult, op1=ALU.add), reads=[g1.b], writes=[g1.b])
                        kb.op("dve", lambda e: e.tensor_tensor(out=g1[:], in0=g1[:], in1=yT[:, fc, :], op=ALU.mult), reads=[g1.b, yT.b], writes=[g1.b])
                        kb.op("act", lambda e: e.activation(out=g2[:], in_=g1[:], func=AF.Sigmoid, scale=1.5957691216057308), reads=[g1.b], writes=[g2.b])
                        kb.op("dve", lambda e: e.tensor_tensor(out=yT[:, fc, :], in0=yT[:, fc, :], in1=g2[:], op=ALU.mult), reads=[g2.b, yT.b], writes=[yT.b])
                    ne = 0
                    for fo in range(4):
                        for tb in range(5):
                            t0 = tb * 512
                            nt = min(512, T - t0)
                            pg = banks[4 + ne % 2]
                            for fi in range(4):
                                kb.op("pe", lambda e, fi=fi, pg=pg: e.matmul(out=pg[:, :nt], lhsT=gluw[:, fi, fo * 128:(fo + 1) * 128], rhs=yT[:, fi, t0:t0 + nt],
                                                                             start=(fi == 0), stop=(fi == 3)), reads=[gluw.b, yT.b], writes=[pg.b])
                            s_ = sgl[ne % 2]; o_ = og[ne % 2]
                            kb.op("act", lambda e, pg=pg, s_=s_: e.activation(out=s_[:, :nt], in_=pg[:, :nt], func=AF.Sigmoid, bias=gbT[:, l * 4 + fo:l * 4 + fo + 1]),
                                  reads=[pg.b, gbT.b], writes=[s_.b])
                            kb.op("dve", lambda e, s_=s_, o_=o_: e.tensor_tensor(out=o_[:, :nt], in0=s_[:, :nt], in1=yT[:, fo, t0:t0 + nt], op=ALU.mult),
                                  reads=[s_.b, yT.b], writes=[o_.b])
                            kb.dma("sp", mixT[fo * 128:(fo + 1) * 128, t0:t0 + nt], o_[:, :nt], reads=[o_.b])
                            ne += 1
                kb.barrier()
            if stage <= 2:
                break
            with ExitStack() as ph:
                QOFF, KOFF, VOFF, ZOFF, AOFF, BOFF = 512, 1280, 2048, 2816, 3584, 3596
                NCK = T // 64
                RM = sb(ph, "RM", [64, T])
                SEL = sb(ph, "SEL", [64, 12, 128]); NSEL = sb(ph, "NSEL", [64, 12, 128])
                MB = sb(ph, "MB", [64, 4, 64])
                ONES = sb(ph, "ONES", [128, 128])
                NORM = sb(ph, "NORM", [64, 128])
                GP = sb(ph, "GP", [64, 2])
                CW = sb(ph, "CW", [128, 18 * 3])
                kb.dma("sp", RM[:], rm_d, writes=[RM.b])
                kb.dma("sp", SEL[:], sel_d, writes=[SEL.b])
                kb.dma("sp", MB[:], mb_d, writes=[MB.b])
                kb.dma("sp", NORM[:], gdn_norm_d[:, l * 128:(l + 1) * 128], writes=[NORM.b])
                kb.dma("sp", GP[:], gdn_gp_d[:, l * 2:(l + 1) * 2], writes=[GP.b])
                kb.dma("sp", CW[:], gdn_cw_d[:, l * 54:(l + 1) * 54], writes=[CW.b])
                kb.op("act", lambda e: e.activation(out=NSEL[:], in_=SEL[:], func=AF.Identity, scale=-1.0), reads=[SEL.b], writes=[NSEL.b])
                kb.op("dve", lambda e: e.memset(ONES[:], 1.0), writes=[ONES.b])
                RA = sb(ph, "RA", [64, T]); RB = sb(ph, "RB", [64, T]); GC = sb(ph, "GC", [64, T])
                R1 = sb(ph, "R1", [64, T]); EG = sb(ph, "EG", [64, T]); BG = sb(ph, "BG", [64, T])
                EGL = sb(ph, "EGL", [64, NCK])
                COL = sb(ph, "COL", [64, NCK, 3, 12])
                EGLB = sb(ph, "EGLB", [128, 12, NCK])
                kb.op("dve", lambda e: e.memset(RA[:], 0.0), writes=[RA.b])
                kb.op("dve", lambda e: e.memset(RB[:], 0.0), writes=[RB.b])
                for dr in range(2):
                    kb.dma("sp", RA[dr * 32:dr * 32 + 6, :], projT[AOFF + dr * 6:AOFF + dr * 6 + 6, :], writes=[RA.b])
                    kb.dma("sp", RB[dr * 32:dr * 32 + 6, :], projT[BOFF + dr * 6:BOFF + dr * 6 + 6, :], writes=[RB.b])
                kb.op("act", lambda e: e.activation(out=RA[:], in_=RA[:], func=AF.Exp, bias=GP[:, 1:2]), reads=[RA.b, GP.b], writes=[RA.b])
                kb.op("act", lambda e: e.activation(out=RA[:], in_=RA[:], func=AF.Ln, bias=1.0), reads=[RA.b], writes=[RA.b])
                kb.op("act", lambda e: e.activation(out=GP[:, 0:1], in_=GP[:, 0:1], func=AF.Exp), reads=[GP.b], writes=[GP.b])
                kb.op("dve", lambda e: e.tensor_scalar(out=RA[:], in0=RA[:], scalar1=GP[:, 0:1], scalar2=-1.0, op0=ALU.mult, op1=ALU.mult), reads=[RA.b, GP.b], writes=[RA.b])
                kb.op("act", lambda e: e.activation(out=RB[:], in_=RB[:], func=AF.Sigmoid), reads=[RB.b], writes=[RB.b])
                kb.op("act", lambda e: e.activation(out=R1[:], in_=RB[:], func=AF.Ln), reads=[RB.b], writes=[R1.b])
                kb.op("dve", lambda e: e.tensor_tensor_scan(out=GC[0:32, :], data0=RM[0:32, :], data1=RA[0:32, :], initial=0.0, op0=ALU.mult, op1=ALU.add),
                      reads=[RM.b, RA.b], writes=[GC.b])
                kb.op("dve", lambda e: e.tensor_tensor_scan(out=GC[32:64, ::-1], data0=RM[32:64, ::-1], data1=RA[32:64, ::-1], initial=0.0, op0=ALU.mult, op1=ALU.add),
                      reads=[RM.b, RA.b], writes=[GC.b])
                kb.op("dve", lambda e: e.tensor_tensor(out=R1[:], in0=R1[:], in1=GC[:], op=ALU.add), reads=[R1.b, GC.b], writes=[R1.b])
                kb.op("act", lambda e: e.activation(out=EG[:], in_=GC[:], func=AF.Exp), reads=[GC.b], writes=[EG.b])
                kb.op("dve", lambda e: e.tensor_tensor(out=BG[:], in0=EG[:], in1=RB[:], op=ALU.mult), reads=[EG.b, RB.b], writes=[BG.b])
                GCL = sb(ph, "GCL", [64, NCK])
                for dr in range(2):
                    pr = slice(dr * 32, dr * 32 + 32)
                    lastpos = 63 if dr == 0 else 0
                    gv = GC[pr, :].rearrange("p (c s) -> p c s", s=64)
                    kb.op("dve", lambda e, pr=pr, gv=gv, lastpos=lastpos: e.tensor_copy(out=GCL[pr, :], in_=gv[:, :, lastpos]), reads=[GC.b], writes=[GCL.b])
                kb.op("act", lambda e: e.activation(out=EGL[:], in_=GCL[:], func=AF.Exp), reads=[GCL.b], writes=[EGL.b])
                for c in range(NCK):
                    kb.op("dve", lambda e, c=c: e.tensor_scalar(out=RA[:, c * 64:(c + 1) * 64], in0=GC[:, c * 64:(c + 1) * 64], scalar1=-1.0, scalar2=GCL[:, c:c + 1],
                                                                 op0=ALU.mult, op1=ALU.add), reads=[GC.b, GCL.b], writes=[RA.b])
                kb.op("act", lambda e: e.activation(out=RA[:], in_=RA[:], func=AF.Exp), reads=[RA.b], writes=[RA.b])
                for c in range(NCK):
                    pcol = banks[c % 4]
                    for qi, row in enumerate((BG, RA, RB)):
                        kb.op("pe", lambda e, row=row, qi=qi, pcol=pcol, c=c: e.matmul(out=pcol[0:64, qi * 12:(qi + 1) * 12], lhsT=row[:, c * 64:(c + 1) * 64],
                                                                                   rhs=SEL[:, :, 0], start=True, stop=True), reads=[row.b, SEL.b], writes=[pcol.b])
                    kb.op("act", lambda e, pcol=pcol, c=c: e.activation(out=COL[:, c, :, :].rearrange("p a b -> p (a b)"), in_=pcol[0:64, 0:36], func=AF.Identity),
                          reads=[pcol.b], writes=[COL.b])
                for r in range(12):
                    pcol = banks[4 + r % 2]
                    kb.op("pe", lambda e, r=r, pcol=pcol: e.matmul(out=pcol[:, 0:NCK], lhsT=SEL[:, r, :], rhs=EGL[:], start=True, stop=True), reads=[SEL.b, EGL.b], writes=[pcol.b])
                    kb.op("act", lambda e, r=r, pcol=pcol: e.activation(out=EGLB[:, r, :], in_=pcol[:, 0:NCK], func=AF.Identity), reads=[pcol.b], writes=[EGLB.b])
                kb.barrier()
                import os as _os
                DBG = int(_os.environ.get("DBG_GDN", "9"))
                if stage == 3:
                    for qi_, row_ in enumerate((GC, R1, EG, RA, RB, BG)):
                        kb.dma("sp", dbg_rows[:, qi_, :], row_[:], reads=[row_.b])
                psn = [0]

                def pget():
                    bk = banks[psn[0] % 8]
                    psn[0] += 1
                    return bk
                RAW = sb(ph, "RAW", [128, 3, T]); CV = sb(ph, "CV", [128, 3, T])
                OACC = sb(ph, "OACC", [64, NCK, 128]); OUT = sb(ph, "OUT", [128, T], BF16)
                Sd = [sb(ph, "Sst%d" % i, [128, 128]) for i in range(2)]
                G = 4
                U = []
                for u in range(2 * G):
                    d_ = {}
                    for nm, shp in (("AB0", [64, 2, 64]), ("AB1", [64, 2, 64]), ("X0", [64, 64]), ("X1", [64, 64]),
                                    ("E", [64, 3, 64]), ("kbg", [64, 128]), ("kdec", [64, 128]), ("vb", [64, 128]),
                                    ("nWT", [128, 64]), ("VN", [64, 128])):
                        d_[nm] = sb(ph, "u%d%s" % (u, nm), shp)
                    U.append(d_)
                fin = [sb(ph, "fin%d" % i, [64, 128]) for i in range(2)]
                fst = [sb(ph, "fst%d" % i, [64, 4]) for i in range(2)]
                I64 = ident[0:64, 0:64]
                for hd in range(6 if DBG >= 1 else 0):
                    for ci, off in enumerate((QOFF, KOFF, VOFF)):
                        kb.dma("sp", RAW[:, ci, :], projT[off + hd * 128:off + (hd + 1) * 128, :], writes=[RAW.b])
                    for ci in range(3):
                        cch = ci * 6 + hd
                        for (s0, s1) in ((0, NCTX), (NCTX, T)):
                            kb.op("dve", lambda e, ci=ci, cch=cch, s0=s0, s1=s1: e.tensor_scalar(out=CV[:, ci, s0:s1], in0=RAW[:, ci, s0:s1], scalar1=CW[:, cch * 3 + 1:cch * 3 + 2],
                                                                                              scalar2=None, op0=ALU.mult), reads=[RAW.b, CW.b], writes=[CV.b])
                            kb.op("dve", lambda e, ci=ci, cch=cch, s0=s0, s1=s1: e.scalar_tensor_tensor(out=CV[:, ci, s0 + 1:s1], in0=RAW[:, ci, s0:s1 - 1], scalar=CW[:, cch * 3:cch * 3 + 1],
                                                                                                     in1=CV[:, ci, s0 + 1:s1], op0=ALU.mult, op1=ALU.add), reads=[RAW.b, CW.b, CV.b], writes=[CV.b])
                            kb.op("dve", lambda e, ci=ci, cch=cch, s0=s0, s1=s1: e.scalar_tensor_tensor(out=CV[:, ci, s0:s1 - 1], in0=RAW[:, ci, s0 + 1:s1], scalar=CW[:, cch * 3 + 2:cch * 3 + 3],
                                                                                                     in1=CV[:, ci, s0:s1 - 1], op0=ALU.mult, op1=ALU.add), reads=[RAW.b, CW.b, CV.b], writes=[CV.b])
                    kb.op("act", lambda e: e.activation(out=CV[:], in_=CV[:], func=AF.Silu), reads=[CV.b], writes=[CV.b])
                    for ci in range(2):
                        kb.op("act", lambda e, ci=ci: e.activation(out=RAW[:, 0, :], in_=CV[:, ci, :], func=AF.Square), reads=[CV.b], writes=[RAW.b])
                        for tb in range(5):
                            t0 = tb * 512
                            nt = min(512, T - t0)
                            pb = banks[6 + tb % 2]
                            kb.op("pe", lambda e, pb=pb, t0=t0, nt=nt: e.matmul(out=pb[:, :nt], lhsT=ONES[:], rhs=RAW[:, 0, t0:t0 + nt], start=True, stop=True),
                                  reads=[ONES.b, RAW.b], writes=[pb.b])
                            kb.op("act", lambda e, pb=pb, t0=t0, nt=nt: e.activation(out=RAW[:, 1, t0:t0 + nt], in_=pb[:, :nt], func=AF.Sqrt, bias=EPS), reads=[RAW.b], writes=[RAW.b, pb.b])
                        kb.op("dve", lambda e: e.reciprocal(out=RAW[:, 1, :], in_=RAW[:, 1, :]), reads=[RAW.b], writes=[RAW.b])
                        sc_ = (128.0 ** -0.5) if ci == 0 else 1.0
                        kb.op("dve", lambda e, ci=ci, sc_=sc_: e.scalar_tensor_tensor(out=CV[:, ci, :], in0=RAW[:, 1, :], scalar=sc_, in1=CV[:, ci, :], op0=ALU.mult, op1=ALU.mult),
                              reads=[RAW.b, CV.b], writes=[CV.b])
                    QT = lambda a, b_: CV[:, 0, a:b_]
                    KT = lambda a, b_: CV[:, 1, a:b_]
                    VT = lambda a, b_: CV[:, 2, a:b_]
                    kb.op("dve", lambda e: e.memset(OACC[:], 0.0), writes=[OACC.b])
                    for dr in range(2):
                        r = dr * 6 + hd
                        for tb in range(5):
                            t0 = tb * 512
                            nt = min(512, T - t0)
                            pb = banks[6 + tb % 2]
                            kb.op("pe", lambda e, pb=pb, t0=t0, nt=nt, r=r: e.matmul(out=pb[:, :nt], lhsT=SEL[:, r, :], rhs=EG[:, t0:t0 + nt], start=True, stop=True),
                                  reads=[SEL.b, EG.b], writes=[pb.b])
                            kb.op("dve", lambda e, pb=pb, t0=t0, nt=nt, dr=dr: e.tensor_tensor(out=RAW[:, dr, t0:t0 + nt], in0=pb[:, :nt], in1=CV[:, 0, t0:t0 + nt], op=ALU.mult),
                                  reads=[CV.b], writes=[RAW.b, pb.b])
                        kb.op("dve", lambda e, dr=dr: e.memset(Sd[dr][:], 0.0), writes=[Sd[dr].b])
                    orders = [list(range(NCK)), list(range(3, -1, -1)) + list(range(NCK - 1, 3, -1))]
                    masks = [(0, 1, 3), (1, 0, 2)]
                    for g0 in range(0, NCK, G):
                        units = []
                        for dr in range(2):
                            for u, c in enumerate(orders[dr][g0:g0 + G]):
                                units.append((dr, c, U[dr * G + u]))
                        for (dr, c, d_) in units:
                            r = dr * 6 + hd
                            t0 = c * 64
                            bk = pget()
                            kb.op("pe", lambda e, bk=bk, t0=t0: e.transpose(out=bk[0:64, 0:128], in_=KT(t0, t0 + 64), identity=ident[:]), reads=[CV.b, ident.b], writes=[bk.b])
                            kb.op("pe", lambda e, bk=bk, t0=t0: e.transpose(out=bk[0:64, 128:256], in_=VT(t0, t0 + 64), identity=ident[:]), reads=[CV.b, ident.b], writes=[bk.b])
                            kb.op("act", lambda e, bk=bk, d_=d_, c=c, r=r: e.activation(out=d_["kbg"][:], in_=bk[0:64, 0:128], func=AF.Identity, scale=COL[:, c, 0, r:r + 1]), reads=[COL.b], writes=[d_["kbg"].b, bk.b])
                            kb.op("act", lambda e, bk=bk, d_=d_, c=c, r=r: e.activation(out=d_["kdec"][:], in_=bk[0:64, 0:128], func=AF.Identity, scale=COL[:, c, 1, r:r + 1]), reads=[COL.b], writes=[d_["kdec"].b, bk.b])
                            kb.op("act", lambda e, bk=bk, d_=d_, c=c, r=r: e.activation(out=d_["vb"][:], in_=bk[0:64, 128:256], func=AF.Identity, scale=COL[:, c, 2, r:r + 1]), reads=[COL.b], writes=[d_["vb"].b, bk.b])
                        for (dr, c, d_) in units:
                            r = dr * 6 + hd
                            m_s, m_st, m_it = masks[dr]
                            t0 = c * 64
                            ch = slice(t0, t0 + 64)
                            bp = pget()
                            kb.op("pe", lambda e, bp=bp, t0=t0: e.matmul(out=bp[0:64, 0:64], lhsT=KT(t0, t0 + 64), rhs=KT(t0, t0 + 64), start=True, stop=True), reads=[CV.b], writes=[bp.b])
                            kb.op("pe", lambda e, bp=bp, t0=t0: e.matmul(out=bp[0:64, 64:128], lhsT=KT(t0, t0 + 64), rhs=QT(t0, t0 + 64), start=True, stop=True), reads=[CV.b], writes=[bp.b])
                            be = pget()
                            kb.op("pe", lambda e, be=be, ch=ch, r=r: e.matmul(out=be[0:64, 0:64], lhsT=R1[:, ch], rhs=SEL[:, r, 0:64], start=True, stop=False), reads=[R1.b, SEL.b], writes=[be.b])
                            kb.op("pe", lambda e, be=be, ch=ch, r=r: e.matmul(out=be[0:64, 0:64], lhsT=NSEL[:, r, 0:64], rhs=GC[:, ch], start=False, stop=False), reads=[GC.b, NSEL.b], writes=[be.b])
                            kb.op("pe", lambda e, be=be, m_s=m_s: e.matmul(out=be[0:64, 0:64], lhsT=I64, rhs=MB[:, m_s, :], start=False, stop=True), reads=[MB.b, ident.b], writes=[be.b])
                            kb.op("pe", lambda e, be=be, ch=ch, r=r: e.matmul(out=be[0:64, 64:128], lhsT=GC[:, ch], rhs=NSEL[:, r, 0:64], start=True, stop=False), reads=[GC.b, NSEL.b], writes=[be.b])
                            kb.op("pe", lambda e, be=be, ch=ch, r=r: e.matmul(out=be[0:64, 64:128], lhsT=SEL[:, r, 0:64], rhs=R1[:, ch], start=False, stop=False), reads=[R1.b, SEL.b], writes=[be.b])
                            kb.op("pe", lambda e, be=be, m_st=m_st: e.matmul(out=be[0:64, 64:128], lhsT=I64, rhs=MB[:, m_st, :], start=False, stop=True), reads=[MB.b, ident.b], writes=[be.b])
                            kb.op("pe", lambda e, be=be, ch=ch, r=r: e.matmul(out=be[0:64, 128:192], lhsT=GC[:, ch], rhs=NSEL[:, r, 0:64], start=True, stop=False), reads=[GC.b, NSEL.b], writes=[be.b])
                            kb.op("pe", lambda e, be=be, ch=ch, r=r: e.matmul(out=be[0:64, 128:192], lhsT=SEL[:, r, 0:64], rhs=GC[:, ch], start=False, stop=False), reads=[GC.b, SEL.b], writes=[be.b])
                            kb.op("pe", lambda e, be=be, m_it=m_it: e.matmul(out=be[0:64, 128:192], lhsT=I64, rhs=MB[:, m_it, :], start=False, stop=True), reads=[MB.b, ident.b], writes=[be.b])
                            kb.op("act", lambda e, be=be, d_=d_: e.activation(out=d_["E"][:].rearrange("p a b -> p (a b)"), in_=be[0:64, 0:192], func=AF.Exp), writes=[d_["E"].b, be.b])
                            kb.op("dve", lambda e, bp=bp, d_=d_: e.tensor_tensor(out=d_["AB0"][:, 1, :], in0=bp[0:64, 0:64], in1=d_["E"][:, 0, :], op=ALU.mult), reads=[d_["E"].b], writes=[d_["AB0"].b, bp.b])
                            kb.op("dve", lambda e, bp=bp, d_=d_: e.tensor_tensor(out=d_["AB0"][:, 0, :], in0=bp[0:64, 0:64], in1=d_["E"][:, 1, :], op=ALU.mult), reads=[d_["E"].b], writes=[d_["AB0"].b, bp.b])
                            kb.op("dve", lambda e, bp=bp, d_=d_: e.tensor_tensor(out=d_["E"][:, 2, :], in0=bp[0:64, 64:128], in1=d_["E"][:, 2, :], op=ALU.mult), reads=[d_["E"].b], writes=[d_["E"].b, bp.b])
                            kb.op("dve", lambda e, d_=d_: e.tensor_tensor(out=d_["X0"][:], in0=I64, in1=d_["AB0"][:, 0, :], op=ALU.subtract), reads=[ident.b, d_["AB0"].b], writes=[d_["X0"].b])
                        for k in range(1, 6):
                            pa, pn = (k - 1) % 2, k % 2
                            for ui, (dr, c, d_) in enumerate(units):
                                ABp, ABn = d_["AB%d" % pa], d_["AB%d" % pn]
                                bk = pget()
                                if k < 5:
                                    kb.op("pe", lambda e, bk=bk, ABp=ABp: e.matmul(out=bk[0:64, 0:64], lhsT=ABp[:, 1, :], rhs=ABp[:, 0, :], start=True, stop=True), reads=[ABp.b], writes=[bk.b])
                                kb.op("pe", lambda e, bk=bk, ABp=ABp: e.matmul(out=bk[0:64, 64:128], lhsT=ABp[:, 0, :], rhs=ABp[:, 1, :], start=True, stop=True), reads=[ABp.b], writes=[bk.b])
                                lo = 0 if k < 5 else 64
                                if (ui + k) % 2 == 0:
                                    kb.op("act", lambda e, bk=bk, ABn=ABn, lo=lo: e.activation(out=ABn[:].rearrange("p a b -> p (a b)")[:, lo:128], in_=bk[0:64, lo:128], func=AF.Identity), writes=[ABn.b, bk.b])
                                else:
                                    kb.op("dve", lambda e, bk=bk, ABn=ABn, lo=lo: e.tensor_copy(out=ABn[:].rearrange("p a b -> p (a b)")[:, lo:128], in_=bk[0:64, lo:128]), writes=[ABn.b, bk.b])
                            for ui, (dr, c, d_) in enumerate(units):
                                ABn, Xp, Xn = d_["AB%d" % pn], d_["X%d" % pa], d_["X%d" % pn]
                                bk = pget()
                                kb.op("pe", lambda e, bk=bk, Xp=Xp: e.matmul(out=bk[0:64, 0:64], lhsT=I64, rhs=Xp[:], start=True, stop=False), reads=[Xp.b, ident.b], writes=[bk.b])
                                kb.op("pe", lambda e, bk=bk, Xp=Xp, ABn=ABn: e.matmul(out=bk[0:64, 0:64], lhsT=ABn[:, 1, :], rhs=Xp[:], start=False, stop=True), reads=[Xp.b, ABn.b], writes=[bk.b])
                                if (ui + k) % 2 == 1:
                                    kb.op("act", lambda e, bk=bk, Xn=Xn: e.activation(out=Xn[:], in_=bk[0:64, 0:64], func=AF.Identity), writes=[Xn.b, bk.b])
                                else:
                                    kb.op("dve", lambda e, bk=bk, Xn=Xn: e.tensor_copy(out=Xn[:], in_=bk[0:64, 0:64]), writes=[Xn.b, bk.b])
                        for (dr, c, d_) in units:
                            TT = d_["X1"]
                            bk = pget()
                            kb.op("pe", lambda e, bk=bk, d_=d_, TT=TT: e.matmul(out=bk[:, 0:64], lhsT=d_["kbg"][:], rhs=TT[:], start=True, stop=True), reads=[d_["kbg"].b, TT.b], writes=[bk.b])
                            kb.op("act", lambda e, bk=bk, d_=d_: e.activation(out=d_["nWT"][:], in_=bk[:, 0:64], func=AF.Identity, scale=-1.0), writes=[d_["nWT"].b, bk.b])
                        for u in range(G):
                            for dr in range(2):
                                if dr * G + u >= len(units):
                                    continue
                                dr_, c, d_ = units[dr * G + u]
                                r = dr * 6 + hd
                                S = Sd[dr]
                                t0 = c * 64
                                TT = d_["X1"]
                                bk = pget()
                                kb.op("pe", lambda e, bk=bk, d_=d_, TT=TT: e.matmul(out=bk[0:64, 0:128], lhsT=TT[:], rhs=d_["vb"][:], start=True, stop=False), reads=[d_["vb"].b, TT.b], writes=[bk.b])
                                kb.op("pe", lambda e, bk=bk, d_=d_, S=S: e.matmul(out=bk[0:64, 0:128], lhsT=d_["nWT"][:], rhs=S[:], start=False, stop=True), reads=[d_["nWT"].b, S.b], writes=[bk.b])
                                kb.op("act", lambda e, bk=bk, d_=d_: e.activation(out=d_["VN"][:], in_=bk[0:64, 0:128], func=AF.Identity), writes=[d_["VN"].b, bk.b])
                                bk = pget()
                                kb.op("pe", lambda e, bk=bk, t0=t0, dr=dr, S=S: e.matmul(out=bk[0:64, 0:128], lhsT=RAW[:, dr, t0:t0 + 64], rhs=S[:], start=True, stop=False), reads=[RAW.b, S.b], writes=[bk.b])
                                kb.op("pe", lambda e, bk=bk, d_=d_: e.matmul(out=bk[0:64, 0:128], lhsT=d_["E"][:, 2, :], rhs=d_["VN"][:], start=False, stop=True), reads=[d_["E"].b, d_["VN"].b], writes=[bk.b])
                                kb.op("dve", lambda e, bk=bk, c=c: e.tensor_tensor(out=OACC[:, c, :], in0=bk[0:64, 0:128], in1=OACC[:, c, :], op=ALU.add), writes=[OACC.b, bk.b])
                                bk = pget()
                                kb.op("pe", lambda e, bk=bk, d_=d_: e.matmul(out=bk[:, 0:128], lhsT=d_["kdec"][:], rhs=d_["VN"][:], start=True, stop=True), reads=[d_["kdec"].b, d_["VN"].b], writes=[bk.b])
                                kb.op("dve", lambda e, bk=bk, c=c, r=r, S=S: e.scalar_tensor_tensor(out=S[:], in0=S[:], scalar=EGLB[:, r, c:c + 1], in1=bk[:, 0:128], op0=ALU.mult, op1=ALU.add),
                                      reads=[EGLB.b], writes=[S.b, bk.b])
                    kb.dma("sp", RAW[:, 2, :], projT[ZOFF + hd * 128:ZOFF + (hd + 1) * 128, :], reads=[CV.b], writes=[RAW.b])
                    kb.op("act", lambda e: e.activation(out=RAW[:, 2, :], in_=RAW[:, 2, :], func=AF.Silu), reads=[RAW.b], writes=[RAW.b])
                    for c in range(NCK):
                        f_ = fin[c % 2]; s_ = fst[c % 2]
                        kb.op("act", lambda e, f_=f_, s_=s_, c=c: e.activation(out=f_[:], in_=OACC[:, c, :], func=AF.Square, accum_out=s_[:, 0:1]), reads=[OACC.b], writes=[f_.b, s_.b])
                        kb.op("act", lambda e, s_=s_: e.activation(out=s_[:, 1:2], in_=s_[:, 0:1], func=AF.Sqrt, bias=EPS, scale=1.0 / 128), reads=[s_.b], writes=[s_.b])
                        kb.op("dve", lambda e, s_=s_: e.reciprocal(out=s_[:, 2:3], in_=s_[:, 1:2]), reads=[s_.b], writes=[s_.b])
                        kb.op("dve", lambda e, f_=f_, s_=s_, c=c: e.scalar_tensor_tensor(out=f_[:], in0=OACC[:, c, :], scalar=s_[:, 2:3], in1=NORM[:], op0=ALU.mult, op1=ALU.mult),
                              reads=[OACC.b, s_.b, NORM.b], writes=[f_.b])
                        bk = pget()
                        kb.op("pe", lambda e, bk=bk, f_=f_: e.transpose(out=bk[:, 0:64], in_=f_[:], identity=I64), reads=[f_.b, ident.b], writes=[bk.b])
                        kb.op("dve", lambda e, bk=bk, c=c: e.tensor_tensor(out=OUT[:, c * 64:(c + 1) * 64], in0=bk[:, 0:64], in1=RAW[:, 2, c * 64:(c + 1) * 64], op=ALU.mult),
                              reads=[RAW.b], writes=[OUT.b, bk.b])
                    kb.dma("sp", mixT[512 + hd * 128:512 + (hd + 1) * 128, :], OUT[:], reads=[OUT.b])
                kb.barrier()
            if stage <= 3:
                break
            with ExitStack() as ph:
                MQ, MK, MV, MO, MI, MF = 3608, 3992, 4376, 5144, 5912, 5924
                NCK = T // 64
                SEL = sb(ph, "mSEL", [64, 12, 128]); NSEL = sb(ph, "mNSEL", [64, 12, 128])
                MB = sb(ph, "mMB", [64, 4, 64])
                NORM = sb(ph, "mNORM", [64, 128])
                MP = sb(ph, "mMP", [64, 2])
                kb.dma("sp", SEL[:], sel_d, writes=[SEL.b])
                kb.dma("sp", MB[:], mb_d, writes=[MB.b])
                kb.dma("sp", NORM[:], ml_norm_d[:, l * 128:(l + 1) * 128], writes=[NORM.b])
                kb.dma("sp", MP[:], ml_mp_d[:, l * 2:(l + 1) * 2], writes=[MP.b])
                kb.op("act", lambda e: e.activation(out=NSEL[:], in_=SEL[:], func=AF.Identity, scale=-1.0), reads=[SEL.b], writes=[NSEL.b])
                RI = sb(ph, "mRI", [64, T]); CM = sb(ph, "mCM", [64, T])
                COL = sb(ph, "mCOL", [64, NCK, 4, 12])
                AOB = sb(ph, "mAOB", [128, 2, 12, NCK])
                I64 = ident[0:64, 0:64]

                def seqperm(eng, dst, dstb, src, srcb, npart, p0=0):
                    kb.op(eng, lambda e: (e.tensor_copy(out=dst[p0:p0 + npart, 0:NCTX], in_=src[p0:p0 + npart, 0:NCTX]) if eng != "act" else
                                          e.activation(out=dst[p0:p0 + npart, 0:NCTX], in_=src[p0:p0 + npart, 0:NCTX], func=AF.Identity)), reads=[srcb], writes=[dstb])
                    ov = dst[p0:p0 + npart, NCTX:T].rearrange("p (c r) -> p c r", r=32)
                    iv = src[p0:p0 + npart, NCTX:T].rearrange("p (r c) -> p c r", c=64)
                    kb.op(eng, lambda e: (e.tensor_copy(out=ov, in_=iv) if eng != "act" else e.activation(out=ov, in_=iv, func=AF.Identity)), reads=[srcb], writes=[dstb])

                with ExitStack() as ph2:
                    RM = sb(ph2, "mRM", [64, T]); RMA = sb(ph2, "mRMA", [64, T])
                    RF = sb(ph2, "mRF", [64, T]); BC = sb(ph2, "mBC", [64, T]); X1 = sb(ph2, "mX1", [64, T]); X2 = sb(ph2, "mX2", [64, T])
                    CH = sb(ph2, "mCH", [64, 8, NCK])
                    kb.dma("sp", RM[:], rm_d, writes=[RM.b])
                    kb.dma("sp", RMA[:], rma_d, writes=[RMA.b])
                    kb.op("dve", lambda e: e.memset(X1[:], 0.0), writes=[X1.b])
                    kb.op("dve", lambda e: e.memset(X2[:], 0.0), writes=[X2.b])
                    for dr in range(2):
                        kb.dma("sp", X1[dr * 32:dr * 32 + 6, :], projT[MI + dr * 6:MI + dr * 6 + 6, :], writes=[X1.b])
                        kb.dma("sp", X2[dr * 32:dr * 32 + 6, :], projT[MF + dr * 6:MF + dr * 6 + 6, :], writes=[X2.b])
                    seqperm("dve", RI, RI.b, X1, X1.b, 64)
                    seqperm("dve", RF, RF.b, X2, X2.b, 64)
                    kb.op("act", lambda e: e.activation(out=RI[:], in_=RI[:], func=AF.Identity, bias=MP[:, 0:1]), reads=[RI.b, MP.b], writes=[RI.b])
                    kb.op("dve", lambda e: e.tensor_scalar(out=MP[:, 1:2], in0=MP[:, 1:2], scalar1=-1.0, scalar2=None, op0=ALU.mult), reads=[MP.b], writes=[MP.b])
                    kb.op("act", lambda e: e.activation(out=RF[:], in_=RF[:], func=AF.Exp, bias=MP[:, 1:2], scale=-1.0), reads=[RF.b, MP.b], writes=[RF.b])
                    kb.op("act", lambda e: e.activation(out=RF[:], in_=RF[:], func=AF.Ln, bias=1.0), reads=[RF.b], writes=[RF.b])
                    kb.op("dve", lambda e: e.tensor_scalar(out=RF[:], in0=RF[:], scalar1=-1.0, scalar2=None, op0=ALU.mult), reads=[RF.b], writes=[RF.b])
                    kb.op("dve", lambda e: e.tensor_tensor_scan(out=BC[0:32, :], data0=RM[0:32, :], data1=RF[0:32, :], initial=0.0, op0=ALU.mult, op1=ALU.add), reads=[RM.b, RF.b], writes=[BC.b])
                    kb.op("dve", lambda e: e.tensor_tensor_scan(out=BC[32:64, ::-1], data0=RM[32:64, ::-1], data1=RF[32:64, ::-1], initial=0.0, op0=ALU.mult, op1=ALU.add), reads=[RM.b, RF.b], writes=[BC.b])
                    kb.op("dve", lambda e: e.tensor_tensor(out=RI[:], in0=RI[:], in1=BC[:], op=ALU.subtract), reads=[RI.b, BC.b], writes=[RI.b])
                    kb.op("dve", lambda e: e.tensor_tensor_scan(out=CM[0:32, :], data0=RMA[0:32, :], data1=RI[0:32, :], initial=-1e30, op0=ALU.add, op1=ALU.max), reads=[RMA.b, RI.b], writes=[CM.b])
                    kb.op("dve", lambda e: e.tensor_tensor_scan(out=CM[32:64, ::-1], data0=RMA[32:64, ::-1], data1=RI[32:64, ::-1], initial=-1e30, op0=ALU.add, op1=ALU.max), reads=[RMA.b, RI.b], writes=[CM.b])
                    for dr in range(2):
                        pr = slice(dr * 32, dr * 32 + 32)
                        lastpos = 63 if dr == 0 else 0
                        kb.op("dve", lambda e, pr=pr, lastpos=lastpos: e.tensor_copy(out=CH[pr, 0, :], in_=BC[pr, :].rearrange("p (c s) -> p c s", s=64)[:, :, lastpos]), reads=[BC.b], writes=[CH.b])
                        kb.op("dve", lambda e, pr=pr, lastpos=lastpos: e.tensor_copy(out=CH[pr, 1, :], in_=CM[pr, :].rearrange("p (c s) -> p c s", s=64)[:, :, lastpos]), reads=[CM.b], writes=[CH.b])
                    B_ = [CH.b]
                    kb.op("dve", lambda e: e.tensor_tensor(out=CH[:, 2, :], in0=CH[:, 0, :], in1=CH[:, 1, :], op=ALU.add), reads=B_, writes=B_)
                    kb.op("dve", lambda e: e.tensor_tensor_scan(out=CH[0:32, 3, :], data0=CH[0:32, 0, :], data1=CH[0:32, 2, :], initial=0.0, op0=ALU.add, op1=ALU.max), reads=B_, writes=B_)
                    kb.op("dve", lambda e: e.tensor_tensor_scan(out=CH[32:64, 3, 0:4][:, ::-1], data0=CH[32:64, 0, 0:4][:, ::-1], data1=CH[32:64, 2, 0:4][:, ::-1], initial=0.0, op0=ALU.add, op1=ALU.max), reads=B_, writes=B_)
                    kb.op("dve", lambda e: e.tensor_tensor_scan(out=CH[32:64, 3, 4:NCK][:, ::-1], data0=CH[32:64, 0, 4:NCK][:, ::-1], data1=CH[32:64, 2, 4:NCK][:, ::-1], initial=CH[32:64, 3, 0:1], op0=ALU.add, op1=ALU.max), reads=B_, writes=B_)
                    kb.op("dve", lambda e: e.memset(CH[:, 4, :], 0.0), reads=B_, writes=B_)
                    kb.op("dve", lambda e: e.tensor_copy(out=CH[0:32, 4, 1:NCK], in_=CH[0:32, 3, 0:NCK - 1]), reads=B_, writes=B_)
                    kb.op("dve", lambda e: e.tensor_copy(out=CH[32:64, 4, 0:3], in_=CH[32:64, 3, 1:4]), reads=B_, writes=B_)
                    kb.op("dve", lambda e: e.tensor_copy(out=CH[32:64, 4, 4:NCK - 1], in_=CH[32:64, 3, 5:NCK]), reads=B_, writes=B_)
                    kb.op("dve", lambda e: e.tensor_copy(out=CH[32:64, 4, NCK - 1:NCK], in_=CH[32:64, 3, 0:1]), reads=B_, writes=B_)
                    kb.op("dve", lambda e: e.tensor_tensor(out=CH[:, 5, :], in0=CH[:, 0, :], in1=CH[:, 4, :], op=ALU.add), reads=B_, writes=B_)
                    kb.op("dve", lambda e: e.tensor_tensor(out=CH[:, 5, :], in0=CH[:, 5, :], in1=CH[:, 3, :], op=ALU.subtract), reads=B_, writes=B_)
                    kb.op("dve", lambda e: e.tensor_tensor(out=CH[:, 6, :], in0=CH[:, 2, :], in1=CH[:, 3, :], op=ALU.subtract), reads=B_, writes=B_)
                    kb.op("act", lambda e: e.activation(out=CH[:, 5:7, :], in_=CH[:, 5:7, :], func=AF.Exp), reads=B_, writes=B_)
                    for c in range(NCK):
                        kb.op("dve", lambda e, c=c: e.tensor_scalar(out=RF[:, c * 64:(c + 1) * 64], in0=CM[:, c * 64:(c + 1) * 64], scalar1=-1.0, scalar2=CH[:, 4, c:c + 1],
                                                                     op0=ALU.mult, op1=ALU.add), reads=[CM.b, CH.b], writes=[RF.b])
                        kb.op("dve", lambda e, c=c: e.tensor_scalar(out=X2[:, c * 64:(c + 1) * 64], in0=RI[:, c * 64:(c + 1) * 64], scalar1=CH[:, 1, c:c + 1], scalar2=None,
                                                                     op0=ALU.subtract), reads=[RI.b, CH.b], writes=[X2.b])
                    kb.op("act", lambda e: e.activation(out=X2[:], in_=X2[:], func=AF.Exp), reads=[X2.b], writes=[X2.b])
                    kb.op("dve", lambda e: e.tensor_tensor(out=BC[:], in0=BC[:], in1=CM[:], op=ALU.add), reads=[BC.b, CM.b], writes=[BC.b])
                    kb.op("dve", lambda e: e.scalar_tensor_tensor(out=BC[:], in0=RF[:], scalar=0.0, in1=BC[:], op0=ALU.max, op1=ALU.add), reads=[RF.b, BC.b], writes=[BC.b])
                    kb.op("act", lambda e: e.activation(out=BC[:], in_=BC[:], func=AF.Exp, scale=-1.0), reads=[BC.b], writes=[BC.b])
                    kb.op("dve", lambda e: e.tensor_scalar(out=X1[:], in0=RF[:], scalar1=0.0, scalar2=None, op0=ALU.min), reads=[RF.b], writes=[X1.b])
                    kb.op("act", lambda e: e.activation(out=X1[:], in_=X1[:], func=AF.Exp), reads=[X1.b], writes=[X1.b])
                    kb.op("dve", lambda e: e.tensor_scalar(out=RF[:], in0=RF[:], scalar1=-1.0, scalar2=0.0, op0=ALU.mult, op1=ALU.min), reads=[RF.b], writes=[RF.b])
                    kb.op("act", lambda e: e.activation(out=RF[:], in_=RF[:], func=AF.Exp), reads=[RF.b], writes=[RF.b])
                    for c in range(NCK):
                        pcol = banks[c % 4]
                        for qi, row in enumerate((X1, RF, BC, X2)):
                            kb.op("pe", lambda e, row=row, qi=qi, pcol=pcol, c=c: e.matmul(out=pcol[0:64, qi * 12:(qi + 1) * 12], lhsT=row[:, c * 64:(c + 1) * 64],
                                                                                       rhs=SEL[:, :, 0], start=True, stop=True), reads=[row.b, SEL.b], writes=[pcol.b])
                        kb.op("act", lambda e, pcol=pcol, c=c: e.activation(out=COL[:, c, :, :].rearrange("p a b -> p (a b)"), in_=pcol[0:64, 0:48], func=AF.Identity),
                              writes=[COL.b, pcol.b])
                    for qi in range(2):
                        for r in range(12):
                            pcol = banks[4 + (qi * 12 + r) % 2]
                            kb.op("pe", lambda e, r=r, pcol=pcol, qi=qi: e.matmul(out=pcol[:, 0:NCK], lhsT=SEL[:, r, :], rhs=CH[:, 5 + qi, :], start=True, stop=True), reads=[SEL.b, CH.b], writes=[pcol.b])
                            kb.op("act", lambda e, r=r, pcol=pcol, qi=qi: e.activation(out=AOB[:, qi, r, :], in_=pcol[:, 0:NCK], func=AF.Identity), writes=[AOB.b, pcol.b])
                    kb.barrier()
                psn = [0]

                def pget():
                    bk = banks[psn[0] % 8]
                    psn[0] += 1
                    return bk
                RAW = sb(ph, "mRAW", [128, T])
                QT = sb(ph, "mQT", [64, T]); KT = sb(ph, "mKT", [64, T]); VT = sb(ph, "mVT", [128, T])
                VA = sb(ph, "mVA", [64, NCK, 132])
                HACC = sb(ph, "mHACC", [64, NCK, 128]); OUT = sb(ph, "mOUT", [128, T], BF16)
                CA = sb(ph, "mCA", [64, 132])
                G = 4
                U = []
                for u in range(G):
                    d_ = {}
                    for nm, shp in (("E", [64, 64]), ("wk", [64, 64]), ("tmp", [64, 132]), ("comb", [64, 132]), ("sc", [64, 4])):
                        d_[nm] = sb(ph, "mu%d%s" % (u, nm), shp)
                    U.append(d_)
                fin = [sb(ph, "mfin%d" % i, [64, 128]) for i in range(2)]
                fst = [sb(ph, "mfst%d" % i, [64, 4]) for i in range(2)]
                kb.op("dve", lambda e: e.memset(VA[:], 1.0), writes=[VA.b])
                for hd in range(6):
                    kb.dma("sp", RAW[0:64, :], projT[MQ + hd * 64:MQ + (hd + 1) * 64, :], writes=[RAW.b])
                    seqperm("act", QT, QT.b, RAW, RAW.b, 64)
                    kb.op("act", lambda e: e.activation(out=QT[:], in_=QT[:], func=AF.Identity, scale=0.125), reads=[QT.b], writes=[QT.b])
                    kb.dma("sp", RAW[0:64, :], projT[MK + hd * 64:MK + (hd + 1) * 64, :], reads=[QT.b], writes=[RAW.b])
                    seqperm("dve", KT, KT.b, RAW, RAW.b, 64)
                    kb.dma("sp", RAW[:], projT[MV + hd * 128:MV + (hd + 1) * 128, :], reads=[KT.b], writes=[RAW.b])
                    seqperm("act", VT, VT.b, RAW, RAW.b, 128)
                    for c in range(NCK):
                        bk = pget()
                        kb.op("pe", lambda e, bk=bk, c=c: e.transpose(out=bk[0:64, 0:128], in_=VT[:, c * 64:(c + 1) * 64], identity=ident[:]), reads=[VT.b, ident.b], writes=[bk.b])
                        if c % 2 == 0:
                            kb.op("act", lambda e, bk=bk, c=c: e.activation(out=VA[:, c, 0:128], in_=bk[0:64, 0:128], func=AF.Identity), writes=[VA.b, bk.b])
                        else:
                            kb.op("dve", lambda e, bk=bk, c=c: e.tensor_copy(out=VA[:, c, 0:128], in_=bk[0:64, 0:128]), writes=[VA.b, bk.b])
                    for dr in range(2):
                        r = dr * 6 + hd
                        kb.op("dve", lambda e: e.memset(CA[:], 0.0), writes=[CA.b])
                        order = list(range(NCK)) if dr == 0 else list(range(3, -1, -1)) + list(range(NCK - 1, 3, -1))
                        m_it = 3 if dr == 0 else 2
                        for g0 in range(0, NCK, G):
                            grp = order[g0:g0 + G]
                            for u, c in enumerate(grp):
                                d_ = U[u]
                                ch = slice(c * 64, (c + 1) * 64)
                                be = pget()
                                kb.op("pe", lambda e, be=be, ch=ch: e.matmul(out=be[0:64, 0:64], lhsT=RI[:, ch], rhs=SEL[:, r, 0:64], start=True, stop=False), reads=[RI.b, SEL.b], writes=[be.b])
                                kb.op("pe", lambda e, be=be, ch=ch: e.matmul(out=be[0:64, 0:64], lhsT=NSEL[:, r, 0:64], rhs=CM[:, ch], start=False, stop=False), reads=[CM.b, NSEL.b], writes=[be.b])
                                kb.op("pe", lambda e, be=be: e.matmul(out=be[0:64, 0:64], lhsT=I64, rhs=MB[:, m_it, :], start=False, stop=True), reads=[MB.b, ident.b], writes=[be.b])
                                kb.op("act", lambda e, be=be, d_=d_: e.activation(out=d_["E"][:], in_=be[0:64, 0:64], func=AF.Exp), writes=[d_["E"].b, be.b])
                                bp = pget()
                                kb.op("pe", lambda e, bp=bp, ch=ch: e.matmul(out=bp[0:64, 0:64], lhsT=KT[:, ch], rhs=QT[:, ch], start=True, stop=True), reads=[KT.b, QT.b], writes=[bp.b])
                                kb.op("pe", lambda e, bp=bp, ch=ch: e.transpose(out=bp[0:64, 64:128], in_=KT[:, ch], identity=I64), reads=[KT.b, ident.b], writes=[bp.b])
                                kb.op("dve", lambda e, bp=bp, d_=d_: e.tensor_tensor(out=d_["E"][:], in0=bp[0:64, 0:64], in1=d_["E"][:], op=ALU.mult), reads=[d_["E"].b], writes=[d_["E"].b, bp.b])
                                kb.op("dve", lambda e, bp=bp, d_=d_, c=c: e.tensor_scalar(out=d_["wk"][:], in0=bp[0:64, 64:128], scalar1=COL[:, c, 3, r:r + 1], scalar2=None, op0=ALU.mult),
                                      reads=[COL.b], writes=[d_["wk"].b, bp.b])
                            for u, c in enumerate(grp):
                                d_ = U[u]
                                ch = slice(c * 64, (c + 1) * 64)
                                b1 = pget()
                                kb.op("pe", lambda e, b1=b1, ch=ch: e.matmul(out=b1[0:64, 0:129], lhsT=QT[:, ch], rhs=CA[:, 0:129], start=True, stop=True), reads=[QT.b, CA.b], writes=[b1.b])
                                kb.op("act", lambda e, b1=b1, d_=d_, c=c: e.activation(out=d_["tmp"][:, 0:129], in_=b1[0:64, 0:129], func=AF.Identity, scale=COL[:, c, 0, r:r + 1]),
                                      reads=[COL.b], writes=[d_["tmp"].b, b1.b])
                                b2 = pget()
                                kb.op("pe", lambda e, b2=b2, d_=d_, c=c: e.matmul(out=b2[0:64, 0:129], lhsT=d_["E"][:], rhs=VA[:, c, 0:129], start=True, stop=True), reads=[d_["E"].b, VA.b], writes=[b2.b])
                                kb.op("dve", lambda e, b2=b2, d_=d_, c=c: e.scalar_tensor_tensor(out=d_["comb"][:, 0:129], in0=b2[0:64, 0:129], scalar=COL[:, c, 1, r:r + 1], in1=d_["tmp"][:, 0:129],
                                                                                              op0=ALU.mult, op1=ALU.add), reads=[COL.b, d_["tmp"].b], writes=[d_["comb"].b, b2.b])
                                kb.op("act", lambda e, d_=d_: e.activation(out=d_["sc"][:, 2:3], in_=d_["comb"][:, 128:129], func=AF.Abs), reads=[d_["comb"].b], writes=[d_["sc"].b])
                                kb.op("dve", lambda e, d_=d_, c=c: e.tensor_scalar(out=d_["sc"][:, 0:1], in0=d_["sc"][:, 2:3], scalar1=COL[:, c, 2, r:r + 1], scalar2=None, op0=ALU.max),
                                      reads=[COL.b, d_["sc"].b], writes=[d_["sc"].b])
                                kb.op("dve", lambda e, d_=d_: e.reciprocal(out=d_["sc"][:, 1:2], in_=d_["sc"][:, 0:1]), reads=[d_["sc"].b], writes=[d_["sc"].b])
                                if dr == 0:
                                    kb.op("dve", lambda e, d_=d_, c=c: e.tensor_scalar(out=HACC[:, c, :], in0=d_["comb"][:, 0:128], scalar1=d_["sc"][:, 1:2], scalar2=None, op0=ALU.mult),
                                          reads=[d_["comb"].b, d_["sc"].b], writes=[HACC.b])
                                else:
                                    kb.op("dve", lambda e, d_=d_, c=c: e.scalar_tensor_tensor(out=HACC[:, c, :], in0=d_["comb"][:, 0:128], scalar=d_["sc"][:, 1:2], in1=HACC[:, c, :], op0=ALU.mult, op1=ALU.add),
                                          reads=[d_["comb"].b, d_["sc"].b, HACC.b], writes=[HACC.b])
                                b3 = pget()
                                kb.op("pe", lambda e, b3=b3, d_=d_, c=c: e.matmul(out=b3[0:64, 0:129], lhsT=d_["wk"][:], rhs=VA[:, c, 0:129], start=True, stop=True), reads=[d_["wk"].b, VA.b], writes=[b3.b])
                                kb.op("act", lambda e, c=c: e.activation(out=CA[:, 0:129], in_=CA[:, 0:129], func=AF.Identity, scale=AOB[0:64, 0, r, c:c + 1]), reads=[AOB.b, CA.b], writes=[CA.b])
                                kb.op("dve", lambda e, b3=b3, c=c: e.scalar_tensor_tensor(out=CA[:, 0:129], in0=b3[0:64, 0:129], scalar=AOB[0:64, 1, r, c:c + 1], in1=CA[:, 0:129], op0=ALU.mult, op1=ALU.add),
                                      reads=[AOB.b, CA.b], writes=[CA.b, b3.b])
                    kb.dma("sp", RAW[:], projT[MO + hd * 128:MO + (hd + 1) * 128, :], reads=[VT.b], writes=[RAW.b])
                    seqperm("act", VT, VT.b, RAW, RAW.b, 128)
                    kb.op("act", lambda e: e.activation(out=VT[:], in_=VT[:], func=AF.Sigmoid), reads=[VT.b], writes=[VT.b])
                    for c in range(NCK):
                        f_ = fin[c % 2]; s_ = fst[c % 2]
                        kb.op("act", lambda e, f_=f_, s_=s_, c=c: e.activation(out=f_[:], in_=HACC[:, c, :], func=AF.Square, accum_out=s_[:, 0:1]), reads=[HACC.b], writes=[f_.b, s_.b])
                        kb.op("act", lambda e, s_=s_: e.activation(out=s_[:, 1:2], in_=s_[:, 0:1], func=AF.Sqrt, bias=EPS, scale=1.0 / 128), reads=[s_.b], writes=[s_.b])
                        kb.op("dve", lambda e, s_=s_: e.reciprocal(out=s_[:, 2:3], in_=s_[:, 1:2]), reads=[s_.b], writes=[s_.b])
                        kb.op("dve", lambda e, f_=f_, s_=s_, c=c: e.scalar_tensor_tensor(out=f_[:], in0=HACC[:, c, :], scalar=s_[:, 2:3], in1=NORM[:], op0=ALU.mult, op1=ALU.mult),
                              reads=[HACC.b, s_.b, NORM.b], writes=[f_.b])
                        bk = pget()
                        kb.op("pe", lambda e, bk=bk, f_=f_: e.transpose(out=bk[:, 0:64], in_=f_[:], identity=I64), reads=[f_.b, ident.b], writes=[bk.b])
                        kb.op("dve", lambda e, bk=bk, c=c: e.tensor_tensor(out=RAW[:, c * 64:(c + 1) * 64], in0=bk[:, 0:64], in1=VT[:, c * 64:(c + 1) * 64], op=ALU.mult),
                              reads=[VT.b], writes=[RAW.b, bk.b])
                    kb.op("act", lambda e: e.activation(out=OUT[:, 0:NCTX], in_=RAW[:, 0:NCTX], func=AF.Identity), reads=[RAW.b], writes=[OUT.b])
                    kb.op("act", lambda e: e.activation(out=OUT[:, NCTX:T].rearrange("p (r c) -> p c r", c=64), in_=RAW[:, NCTX:T].rearrange("p (c r) -> p c r", r=32), func=AF.Identity),
                          reads=[RAW.b], writes=[OUT.b])
                    kb.dma("sp", mixT[1280 + hd * 128:1280 + (hd + 1) * 128, :], OUT[:], reads=[OUT.b])
                kb.barrier()
            if stage <= 4:
                break
            with ExitStack() as ph:
                last = (l == L - 1)
                mixb = sb(ph, "mixb", [128, KC, 512], BF16)
                yTb = sb(ph, "yTb", [128, KC, 512])
                h2T = sb(ph, "h2T", [128, KC, 512], BF16)
                hidT = sb(ph, "hidT", [128, 44, 512], BF16)
                xt = [sb(ph, "xt%d" % i, [128, D]) for i in range(2)]
                GG = [[sb(ph, "GG%d%d" % (a, w_), [128, D]) for w_ in range(2)] for a in range(2)]
                wk = [sb(ph, "wk%d" % i, [128, KC, 128], BF16) for i in range(4)]
                wd = [sb(ph, "wd%d" % i, [128, 44, 128], BF16) for i in range(2)]
                sgt = [sb(ph, "sgt%d" % i, [128, 512]) for i in range(2)]
                junk = sb(ph, "junk2", [128, 512], BF16)
                stt_ = [sb(ph, "st2%d" % i, [128, 8]) for i in range(2)]
                bc = sb(ph, "bc", [128, 128])
                for a, (mi, gi) in enumerate(((2, 1), (5, 3))):
                    for w_ in range(2):
                        for j in range(KC):
                            kb.op("dve", lambda e, mi=mi, gi=gi, w_=w_, j=j: e.tensor_tensor(
                                out=bc[:, 0:1], in0=modsT[:, mi * KC + j, w_:w_ + 1], in1=gT[:, (gi * L + l) * KC + j:(gi * L + l) * KC + j + 1],
                                op=ALU.mult), reads=[modsT.b, gT.b], writes=[bc.b])
                            kb.op("dve", lambda e: e.tensor_copy(out=bc[:, 1:128], in_=bc[:, 0:1].to_broadcast([128, 127])), reads=[bc.b], writes=[bc.b])
                            pb = banks[j % 4]
                            kb.op("pe", lambda e, pb=pb: e.transpose(out=pb[:, 0:128], in_=bc[:], identity=ident[:]), reads=[bc.b, ident.b], writes=[pb.b])
                            kb.op("act", lambda e, pb=pb, a=a, w_=w_, j=j: e.activation(out=GG[a][w_][:, j * 128:(j + 1) * 128], in_=pb[:, 0:128], func=AF.Identity),
                                  reads=[pb.b], writes=[GG[a][w_].b])
                wov = w_out[l].rearrange("(k p) c -> p k c", p=128)
                wgv = w_gate[l].rearrange("(k p) c -> p k c", p=128)
                wuv = w_up[l].rearrange("(k p) c -> p k c", p=128)
                wdv = w_down[l].rearrange("(h p) c -> p h c", p=128)
                mixv = mixT.rearrange("(k p) t -> p k t", p=128)
                tstart = NCTX if last else 0
                nwk = [0]
                nx = [0]

                def post_res(tglob, tloc, a, dst):
                    w_ = 1 if tglob < 2 else 0
                    x_ = xt[nx[0] % 2]
                    s_ = stt_[nx[0] % 2]
                    nx[0] += 1
                    kb.dma("sp", x_[:], xres[tglob * 128:(tglob + 1) * 128, :], writes=[x_.b])
                    for jb in range(4):
                        pb = banks[jb]
                        for jj in range(4):
                            j = jb * 4 + jj
                            kb.op("pe", lambda e, pb=pb, jj=jj, j=j: e.transpose(out=pb[:, jj * 128:(jj + 1) * 128], in_=yTb[:, j, tloc * 128:(tloc + 1) * 128],
                                                                              identity=ident[:]), reads=[yTb.b, ident.b], writes=[pb.b])
                        kb.op("act", lambda e, pb=pb, jb=jb, s_=s_: e.activation(out=junk[:], in_=pb[:], func=AF.Square, accum_out=s_[:, jb:jb + 1]),
                              reads=[pb.b], writes=[junk.b, s_.b])
                    kb.op("dve", lambda e, s_=s_: e.tensor_reduce(out=s_[:, 4:5], in_=s_[:, 0:4], axis=AX.X, op=ALU.add), reads=[s_.b], writes=[s_.b])
                    kb.op("act", lambda e, s_=s_: e.activation(out=s_[:, 5:6], in_=s_[:, 4:5], func=AF.Sqrt, bias=EPS, scale=1.0 / D), reads=[s_.b], writes=[s_.b])
                    kb.op("dve", lambda e, s_=s_: e.reciprocal(out=s_[:, 6:7], in_=s_[:, 5:6]), reads=[s_.b], writes=[s_.b])
                    for jb in range(4):
                        pb = banks[jb]
                        sg_ = sgt[jb % 2]
                        kb.op("dve", lambda e, pb=pb, sg_=sg_, jb=jb, s_=s_: e.scalar_tensor_tensor(
                            out=sg_[:], in0=pb[:], scalar=s_[:, 6:7], in1=GG[a][w_][:, jb * 512:(jb + 1) * 512], op0=ALU.mult, op1=ALU.mult),
                            reads=[pb.b, s_.b, GG[a][w_].b], writes=[sg_.b])
                        kb.op("dve", lambda e, sg_=sg_, jb=jb, x_=x_: e.tensor_tensor(out=x_[:, jb * 512:(jb + 1) * 512], in0=x_[:, jb * 512:(jb + 1) * 512], in1=sg_[:],
                                                                                op=ALU.add), reads=[sg_.b, x_.b], writes=[x_.b])
                    kb.dma("sp", dst, x_[:], reads=[x_.b])
                    return x_, s_, w_

                def dense(src, nk, wview, j0, nj, wbufs, t_nt, consume):
                    for j in range(j0, j0 + nj):
                        w = wbufs[nwk[0] % len(wbufs)]
                        nwk[0] += 1
                        kb.dma("pool", w[:, :nk, :], wview[:, :, j * 128:(j + 1) * 128], writes=[w.b])
                        pj = banks[4 + nwk[0] % 2]
                        for k in range(nk):
                            kb.op("pe", lambda e, pj=pj, w=w, k=k: e.matmul(out=pj[:, :t_nt], lhsT=w[:, k, :], rhs=src[:, k, :t_nt], start=(k == 0), stop=(k == nk - 1)),
                                  reads=[w.b, src.b], writes=[pj.b])
                        consume(j, pj)

                t0 = tstart
                while t0 < T:
                    nt = min(512, T - t0)
                    ntl = nt // 128
                    kb.dma("sp", mixb[:, :, :nt], mixv[:, :, t0:t0 + nt], writes=[mixb.b])

                    def cons_y(j, pj):
                        if j % 2 == 0:
                            kb.op("act", lambda e: e.activation(out=yTb[:, j, :nt], in_=pj[:, :nt], func=AF.Identity), reads=[pj.b], writes=[yTb.b])
                        else:
                            kb.op("dve", lambda e: e.tensor_copy(out=yTb[:, j, :nt], in_=pj[:, :nt]), reads=[pj.b], writes=[yTb.b])
                    dense(mixb, KC, wov, 0, KC, wk, nt, cons_y)
                    for tl in range(ntl):
                        tg = t0 // 128 + tl
                        x_, s_, w_ = post_res(tg, tl, 0, xres[tg * 128:(tg + 1) * 128, :])
                        kb.op("act", lambda e, x_=x_, s_=s_: e.activation(out=junk[:], in_=x_[:, 0:512], func=AF.Square, accum_out=s_[:, 0:1]), reads=[x_.b], writes=[junk.b, s_.b])
                        for q in range(1, 4):
                            kb.op("act", lambda e, x_=x_, s_=s_, q=q: e.activation(out=junk[:], in_=x_[:, q * 512:(q + 1) * 512], func=AF.Square, accum_out=s_[:, q:q + 1]),
                                  reads=[x_.b], writes=[junk.b, s_.b])
                        kb.op("dve", lambda e, s_=s_: e.tensor_reduce(out=s_[:, 4:5], in_=s_[:, 0:4], axis=AX.X, op=ALU.add), reads=[s_.b], writes=[s_.b])
                        kb.op("act", lambda e, s_=s_: e.activation(out=s_[:, 5:6], in_=s_[:, 4:5], func=AF.Sqrt, bias=EPS, scale=1.0 / D), reads=[s_.b], writes=[s_.b])
                        kb.op("dve", lambda e, s_=s_: e.reciprocal(out=s_[:, 6:7], in_=s_[:, 5:6]), reads=[s_.b], writes=[s_.b])
                        kb.op("dve", lambda e, x_=x_, s_=s_: e.tensor_scalar(out=x_[:], in0=x_[:], scalar1=s_[:, 6:7], scalar2=None, op0=ALU.mult), reads=[x_.b, s_.b], writes=[x_.b])
                        for jb in range(4):
                            pt = banks[jb]
                            for jj in range(4):
                                j = jb * 4 + jj
                                kb.op("pe", lambda e, pt=pt, x_=x_, jj=jj, j=j: e.transpose(out=pt[:, jj * 128:(jj + 1) * 128], in_=x_[:, j * 128:(j + 1) * 128], identity=ident[:]),
                                      reads=[x_.b, ident.b], writes=[pt.b])
                            for jj in range(4):
                                j = jb * 4 + jj
                                kb.op("act", lambda e, pt=pt, jj=jj, j=j, tl=tl, w_=w_: e.activation(
                                    out=h2T[:, j, tl * 128:(tl + 1) * 128], in_=pt[:, jj * 128:(jj + 1) * 128], func=AF.Identity,
                                    bias=modsT[:, 3 * KC + j, w_:w_ + 1], scale=gsf[:, j, w_:w_ + 1]), reads=[pt.b, modsT.b, gsf.b], writes=[h2T.b])
                    for hc in range(44):
                        got = {}

                        def cons_g(j, pj):
                            got["g"] = pj
                        dense(h2T, KC, wgv, hc, 1, wk, nt, cons_g)
                        pg = got["g"]
                        sg_ = sgt[hc % 2]
                        kb.op("act", lambda e, pg=pg, sg_=sg_: e.activation(out=sg_[:, :nt], in_=pg[:, :nt], func=AF.Silu), reads=[pg.b], writes=[sg_.b])

                        def cons_u(j, pj):
                            kb.op("dve", lambda e: e.tensor_tensor(out=hidT[:, hc, :nt], in0=pj[:, :nt], in1=sg_[:, :nt], op=ALU.mult), reads=[pj.b, sg_.b], writes=[hidT.b])
                        dense(h2T, KC, wuv, hc, 1, wk, nt, cons_u)
                    dense(hidT, 44, wdv, 0, KC, wd, nt, cons_y)
                    for tl in range(ntl):
                        tg = t0 // 128 + tl
                        dst = out_d[(tg - 2) * 128:(tg - 1) * 128, :] if last else xres[tg * 128:(tg + 1) * 128, :]
                        post_res(tg, tl, 1, dst)
                    t0 += nt
                kb.barrier()
            if stage == 5:
                kb.dma("sp", xres_o, xres)
                break
        kb.barrier()
    return nc


def kernel(**inp):
    inp = {k: np.asarray(v) for k, v in inp.items()}
    nc = build(99)
    base = host_prep(inp, 0)
    in_maps = []
    for core in range(8):
        b = core % 4
        m = dict(base)
        if b != 0:
            pb = host_prep_batch(inp, b)
            m.update(pb)
        in_maps.append(m)
    res = run_bass_kernel_spmd(nc, in_maps, core_ids=list(range(8)))
    out = np.stack([np.asarray(res.results[b]["out"]) for b in range(4)], 0).astype(np.float32)
    return out
```

```python
import numpy as np
from contextlib import ExitStack
import concourse.bass as bass
import concourse.mybir as mybir
from concourse.bass_utils import run_bass_kernel_spmd

F32 = mybir.dt.float32
BF16 = mybir.dt.bfloat16
ALU = mybir.AluOpType
AF = mybir.ActivationFunctionType
AX = mybir.AxisListType

D = 2048
T = 2304
NCTX = 256
NLAT = 2048
L = 2
KC = 16
IN_COLS = 5936
FFN = 5632
EPS = 1e-6
NEG = -30000.0
SEM_ROT = 30000
NSLOT = 6
LC = 128


class Ev:
    __slots__ = ("sem", "val")

    def __init__(self, sem, val):
        self.sem = sem
        self.val = val


class Buf:
    __slots__ = ("w", "r", "name")

    def __init__(self, name=""):
        self.w = None
        self.r = {}
        self.name = name


class Eng:
    def __init__(self, kb, name, h):
        self.kb = kb
        self.name = name
        self.h = h
        self.sem = kb.newsem("e_" + name)
        self.count = 0
        self.seen = {}
        self.n = 0


class Slot:
    def __init__(self, sem):
        self.sem = sem
        self.uses = 0


class KB:
    def __init__(self, nc, es):
        self.nc = nc
        self.es = es
        self.nsem = 0
        self.E = {}
        for name, h in (("pe", nc.tensor), ("act", nc.scalar), ("dve", nc.vector), ("pool", nc.gpsimd), ("sp", nc.sync)):
            self.E[name] = Eng(self, name, h)
        self.slots = {}
        self.rr = {}
        for q in ("sp", "pool", "act"):
            self.slots[q] = [Slot(self.newsem("d_%s%d" % (q, i))) for i in range(NSLOT)]
            self.rr[q] = 0

    def newsem(self, name):
        self.nsem += 1
        return self.es.enter_context(self.nc.semaphore("%s_%d" % (name, self.nsem)))

    def _wait(self, eng, ev):
        k = id(ev.sem)
        if eng.seen.get(k, 0) < ev.val:
            eng.h.wait_ge(ev.sem, ev.val)
            eng.seen[k] = ev.val

    def _deps(self, eng, reads, writes):
        need = {}

        def add(ev):
            k = id(ev.sem)
            if k not in need or need[k].val < ev.val:
                need[k] = ev

        for b in reads:
            if b.w is not None:
                add(b.w)
        for b in writes:
            if b.w is not None:
                add(b.w)
            for ev in b.r.values():
                add(ev)
        for ev in need.values():
            if eng.name == "pe" and ev.sem is eng.sem:
                continue
            self._wait(eng, ev)

    def _post(self, ev, reads, writes):
        k = id(ev.sem)
        for b in reads:
            b.r[k] = ev
        for b in writes:
            b.w = ev
            b.r = {}

    def op(self, e, fn, reads=(), writes=()):
        eng = self.E[e]
        self._deps(eng, reads, writes)
        inst = fn(eng.h)
        if eng.count >= SEM_ROT:
            eng.sem = self.newsem("e_" + eng.name)
            eng.count = 0
        eng.count += 1
        eng.n += 1
        inst.then_inc(eng.sem, 1)
        ev = Ev(eng.sem, eng.count)
        self._post(ev, reads, writes)
        return ev

    def dma(self, q, out, in_, reads=(), writes=(), **kw):
        eng = self.E[q]
        self._deps(eng, reads, writes)
        sl = self.slots[q][self.rr[q] % NSLOT]
        self.rr[q] += 1
        if sl.uses * 16 >= SEM_ROT:
            self._wait(eng, Ev(sl.sem, 16 * sl.uses))
            sl.sem = self.newsem("d_" + q)
            sl.uses = 0
        if sl.uses > 0:
            self._wait(eng, Ev(sl.sem, 16 * sl.uses))
        inst = eng.h.dma_start(out=out, in_=in_, **kw)
        sl.uses += 1
        inst.then_inc(sl.sem, 16)
        ev = Ev(sl.sem, 16 * sl.uses)
        self._post(ev, reads, writes)
        return ev

    def barrier(self, engines=("pe", "act", "dve", "pool", "sp")):
        evs = []
        for e in self.E.values():
            if e.count > 0:
                evs.append(Ev(e.sem, e.count))
        for q in self.slots:
            for sl in self.slots[q]:
                if sl.uses > 0:
                    evs.append(Ev(sl.sem, 16 * sl.uses))
        for en in engines:
            eng = self.E[en]
            for ev in evs:
                self._wait(eng, ev)


class Tl:
    def __init__(self, t, name=""):
        self.t = t
        self.b = Buf(name)

    def __getitem__(self, idx):
        return self.t[idx]


def host_prep_batch(inp, b):
    f = np.float32
    m = {}
    m["xin"] = np.ascontiguousarray(np.concatenate([inp["ctx"][b], inp["x"][b]], axis=0).astype(f))
    cv = np.stack([inp["c"][b].reshape(KC, 128).T, inp["c_ctx"].reshape(KC, 128).T], axis=-1)
    m["cv"] = np.ascontiguousarray(cv.astype(f))
    return m


def host_prep(inp, b):
    f = np.float32
    m = host_prep_batch(inp, b)
    m["ada_w"] = inp["ada_w"]
    m["ada_bT"] = np.ascontiguousarray(inp["ada_b"].reshape(L, 96, 128).transpose(2, 0, 1).reshape(128, L * 96).astype(f))
    g = np.stack([inp["norm_mix_pre"], inp["norm_mix_post"], inp["norm_ffn_pre"], inp["norm_ffn_post"]], 0)
    m["gT"] = np.ascontiguousarray(g.reshape(4, L, KC, 128).transpose(3, 0, 1, 2).reshape(128, 4 * L * KC).astype(f))
    m["w_in"] = inp["w_in"]
    for k_ in ("w_out", "ffn_w_gate", "ffn_w_up", "ffn_w_down"):
        m[k_] = inp[k_]
    m["ident"] = np.eye(128, dtype=f)
    def st_major(a):
        return np.ascontiguousarray(a.reshape(L, 2, 16, 128).transpose(3, 0, 1, 2).reshape(128, L * 2 * 16).astype(f))
    m["s5_lre"] = st_major(inp["s5_lam_re"].reshape(L, 2, 2048))
    m["s5_lim"] = st_major(inp["s5_lam_im"].reshape(L, 2, 2048))
    m["s5_ldt"] = st_major(np.repeat(inp["s5_log_dt"], 64, axis=-1))
    Bb = np.zeros((128, L, 2, 2, 16, 128), f)
    Cb = np.zeros((128, L, 2, 2, 16, 128), f)
    for ci, (bn, cn) in enumerate((("s5_b_re", "s5_c_re"), ("s5_b_im", "s5_c_im"))):
        bsrc = inp[bn]
        csrc = inp[cn]
        for g in range(32):
            st = g // 2
            r0 = (g % 8) * 16
            c0 = (g % 2) * 64
            Bb[r0:r0 + 16, :, :, ci, st, c0:c0 + 64] = bsrc[:, :, g].transpose(3, 0, 1, 2)
            Cb[c0:c0 + 64, :, :, ci, st, r0:r0 + 16] = csrc[:, :, g].transpose(3, 0, 1, 2)
    m["s5_Bb"] = np.ascontiguousarray(Bb.reshape(128, L * 2 * 2 * 16, 128))
    m["s5_Cb"] = np.ascontiguousarray(Cb.reshape(128, L * 2 * 2 * 16, 128))
    m["s5_dT"] = np.ascontiguousarray(inp["s5_d"].reshape(L, 4, 128).transpose(2, 0, 1).reshape(128, L * 4).astype(f))
    m["s5_gbT"] = np.ascontiguousarray(inp["s5_glu_b"].reshape(L, 4, 128).transpose(2, 0, 1).reshape(128, L * 4).astype(f))
    m["s5_glu_w"] = inp["s5_glu_w"]
    tt = np.arange(T)
    rm = np.ones((64, T), f)
    rm[0:32, tt % 64 == 0] = 0.0
    rm[32:64, tt % 64 == 63] = 0.0
    m["rm"] = rm
    sel = np.zeros((64, 12, 128), f)
    for r_ in range(12):
        sel[(r_ // 6) * 32 + r_ % 6, r_, :] = 1.0
    m["sel"] = sel
    a_ = np.arange(64)[:, None]; b_ = np.arange(64)[None, :]
    mb = np.stack([np.where(b_ < a_, 0.0, NEG), np.where(b_ > a_, 0.0, NEG), np.where(b_ <= a_, 0.0, NEG), np.where(b_ >= a_, 0.0, NEG)], 1).astype(f)
    m["mb"] = np.ascontiguousarray(mb)
    m["gdn_normr"] = np.ascontiguousarray(np.tile(inp["gdn_norm"].reshape(1, L * 128), (64, 1)).astype(f))
    gp = np.zeros((64, L, 2), f)
    for dr_ in range(2):
        gp[dr_ * 32:dr_ * 32 + 6, :, 0] = inp["gdn_a_log"][:, dr_, :].T
        gp[dr_ * 32:dr_ * 32 + 6, :, 1] = inp["gdn_dt_bias"][:, dr_, :].T
    m["gdn_gp"] = np.ascontiguousarray(gp.reshape(64, L * 2))
    cw = inp["gdn_conv_w"].reshape(L, 3, 18, 128).transpose(3, 0, 2, 1)
    m["gdn_cw"] = np.ascontiguousarray(cw.reshape(128, L * 54).astype(f))
    rma = np.zeros((64, T), f)
    rma[0:32, tt % 64 == 0] = -1e30
    rma[32:64, tt % 64 == 63] = -1e30
    m["rma"] = rma
    m["ml_normr"] = np.ascontiguousarray(np.tile(inp["mlstm_norm"].reshape(1, L * 128), (64, 1)).astype(f))
    mp = np.zeros((64, L, 2), f)
    for dr_ in range(2):
        mp[dr_ * 32:dr_ * 32 + 6, :, 0] = inp["mlstm_i_bias"][:, dr_, :].T
        mp[dr_ * 32:dr_ * 32 + 6, :, 1] = inp["mlstm_f_bias"][:, dr_, :].T
    m["ml_mp"] = np.ascontiguousarray(mp.reshape(64, L * 2))
    m["tau1"] = np.ascontiguousarray(np.tile(np.arange(1, LC + 1, dtype=f)[None, :], (128, 1)))
    return m


def build(stage=99):
    nc = bass.Bass("TRN2", target_bir_lowering=False)
    es = ExitStack()
    with es:
        def din(name, shape, dt=F32):
            return nc.dram_tensor(name, list(shape), dt, kind="ExternalInput").ap()

        def dout(name, shape, dt=F32):
            return nc.dram_tensor(name, list(shape), dt, kind="ExternalOutput").ap()

        def dscr(name, shape, dt=F32):
            return nc.dram_tensor(name, list(shape), dt, kind="Internal").ap()

        xin = din("xin", [T, D])
        cv_d = din("cv", [128, KC, 2])
        ada_w = din("ada_w", [L, D, 6 * D])
        ada_bT = din("ada_bT", [128, L * 96])
        gT_d = din("gT", [128, 4 * L * KC])
        w_in = din("w_in", [L, D, IN_COLS])
        ident_d = din("ident", [128, 128])
        s5_lre_d = din("s5_lre", [128, L * 32])
        s5_lim_d = din("s5_lim", [128, L * 32])
        s5_ldt_d = din("s5_ldt", [128, L * 32])
        s5_Bb_d = din("s5_Bb", [128, L * 64, 128])
        s5_Cb_d = din("s5_Cb", [128, L * 64, 128])
        s5_dT_d = din("s5_dT", [128, L * 4])
        s5_gbT_d = din("s5_gbT", [128, L * 4])
        s5_gluw_d = din("s5_glu_w", [L, 512, 512])
        tau1_d = din("tau1", [128, LC])
        rm_d = din("rm", [64, T])
        sel_d = din("sel", [64, 12, 128])
        mb_d = din("mb", [64, 4, 64])
        gdn_norm_d = din("gdn_normr", [64, L * 128])
        gdn_gp_d = din("gdn_gp", [64, L * 2])
        gdn_cw_d = din("gdn_cw", [128, L * 54])
        rma_d = din("rma", [64, T])
        ml_norm_d = din("ml_normr", [64, L * 128])
        ml_mp_d = din("ml_mp", [64, L * 2])
        w_out = din("w_out", [L, D, D])
        w_gate = din("ffn_w_gate", [L, D, FFN])
        w_up = din("ffn_w_up", [L, D, FFN])
        w_down = din("ffn_w_down", [L, FFN, D])
        out_d = dout("out", [NLAT, D])
        xres = dscr("xres", [T, D])
        if stage <= 1:
            projT = dout("projT", [47 * 128, T])
            mods_o = dout("mods_o", [128, 96 * 2])
        else:
            projT = dscr("projT", [47 * 128, T])
        if stage == 3:
            dbg_cv = dout("dbg_cv", [128, 3, T])
            dbg_rows = dout("dbg_rows", [64, 6, T])
            dbg_oacc = dout("dbg_oacc", [64, 36, 128])
            dbg_oaccf = dout("dbg_oaccf", [64, 36, 128])
        if stage == 5:
            mixT = din("mixT", [D, T], BF16)
            xres_o = dout("xres_o", [T, D])
        elif 2 <= stage <= 4:
            mixT = dout("mixT", [D, T], BF16)
        else:
            mixT = dscr("mixT", [D, T], BF16)

        kb = KB(nc, es)

        cnt = [0]

        def sb(st, name, shape, dt=F32):
            cnt[0] += 1
            nm = "s%d_%s" % (cnt[0], name)
            return Tl(st.enter_context(nc.sbuf_tensor(nm, list(shape), dt)), nm)

        def ps(st, name, shape, dt=F32):
            cnt[0] += 1
            nm = "p%d_%s" % (cnt[0], name)
            return Tl(st.enter_context(nc.psum_tensor(nm, list(shape), dt)), nm)

        ident = sb(es, "ident", [128, 128])
        gT = sb(es, "gT", [128, 4 * L * KC])
        abT = sb(es, "abT", [128, L * 96])
        cvs = sb(es, "cvs", [128, KC, 2])
        modsT = sb(es, "modsT", [128, 96, 2])
        gsm = sb(es, "gsm", [128, KC, 2])
        gsf = sb(es, "gsf", [128, KC, 2])
        kb.dma("sp", ident[:], ident_d, writes=[ident.b])
        kb.dma("sp", gT[:], gT_d, writes=[gT.b])
        kb.dma("sp", abT[:], ada_bT, writes=[abT.b])
        kb.dma("sp", cvs[:], cv_d, writes=[cvs.b])
        kb.op("act", lambda e: e.activation(out=cvs[:], in_=cvs[:], func=AF.Silu), reads=[cvs.b], writes=[cvs.b])

        banks = [ps(es, "bank%d" % i, [128, 512]) for i in range(8)]
        xres_b = Buf("xres")
        kb.dma("sp", xres, xin, writes=[xres_b])
        kb.barrier()

        for l in range(L):
            with ExitStack() as ph:
                wA = [sb(ph, "wA%d" % i, [128, KC, 512]) for i in range(2)]
                pm = banks[0]
                adv = ada_w[l].rearrange("(k p) c -> p k c", p=128)
                for cb in range(24):
                    w = wA[cb % 2]
                    kb.dma("sp", w[:], adv[:, :, cb * 512:(cb + 1) * 512], writes=[w.b])
                    for jj in range(4):
                        jo = cb * 4 + jj
                        for k in range(KC):
                            kb.op("pe", lambda e, w=w, k=k, jj=jj, jo=jo: e.matmul(
                                out=pm[:, jo * 2:jo * 2 + 2], lhsT=w[:, k, jj * 128:(jj + 1) * 128], rhs=cvs[:, k, :],
                                start=(k == 0), stop=(k == KC - 1)), reads=[w.b, cvs.b], writes=[pm.b])
                for wi in range(2):
                    kb.op("dve", lambda e, wi=wi: e.tensor_tensor(
                        out=modsT[:, :, wi], in0=pm[:, wi:192:2], in1=abT[:, l * 96:(l + 1) * 96], op=ALU.add),
                        reads=[pm.b, abT.b], writes=[modsT.b])
                for (gs, mi, gi) in ((gsm, 1, 0), (gsf, 4, 2)):
                    for wi in range(2):
                        kb.op("dve", lambda e, gs=gs, mi=mi, gi=gi, wi=wi: e.scalar_tensor_tensor(
                            out=gs[:, :, wi], in0=modsT[:, mi * KC:(mi + 1) * KC, wi], scalar=1.0,
                            in1=gT[:, (gi * L + l) * KC:(gi * L + l + 1) * KC], op0=ALU.add, op1=ALU.mult),
                            reads=[modsT.b, gT.b], writes=[gs.b])
                kb.barrier()
            if stage <= 1 and l == 0:
                kb.dma("sp", mods_o, modsT[:].rearrange("p a b -> p (a b)"), reads=[modsT.b])

            with ExitStack() as lay:
                hT = sb(lay, "hT", [128, KC, T], BF16)
                with ExitStack() as ph:
                    xt = [sb(ph, "xt%d" % i, [128, D]) for i in range(2)]
                    junk = sb(ph, "junk", [128, D], BF16)
                    st = [sb(ph, "st%d" % i, [128, 4]) for i in range(2)]
                    for i in range(T // 128):
                        x_ = xt[i % 2]
                        s_ = st[i % 2]
                        wsel = 1 if i < 2 else 0
                        kb.dma("sp", x_[:], xres[i * 128:(i + 1) * 128, :], writes=[x_.b])
                        kb.op("act", lambda e, x_=x_, s_=s_: e.activation(out=junk[:], in_=x_[:], func=AF.Square,
                                                                           accum_out=s_[:, 0:1]),
                              reads=[x_.b], writes=[junk.b, s_.b])
                        kb.op("act", lambda e, s_=s_: e.activation(out=s_[:, 1:2], in_=s_[:, 0:1], func=AF.Sqrt,
                                                                    bias=EPS, scale=1.0 / D), reads=[s_.b], writes=[s_.b])
                        kb.op("dve", lambda e, s_=s_: e.reciprocal(out=s_[:, 2:3], in_=s_[:, 1:2]), reads=[s_.b], writes=[s_.b])
                        kb.op("dve", lambda e, x_=x_, s_=s_: e.tensor_scalar(out=x_[:], in0=x_[:], scalar1=s_[:, 2:3], scalar2=None,
                                                                          op0=ALU.mult), reads=[x_.b, s_.b], writes=[x_.b])
                        for jb in range(4):
                            pt = banks[1 + (i * 4 + jb) % 4]
                            for jj in range(4):
                                j = jb * 4 + jj
                                kb.op("pe", lambda e, pt=pt, x_=x_, jj=jj, j=j: e.transpose(
                                    out=pt[:, jj * 128:(jj + 1) * 128], in_=x_[:, j * 128:(j + 1) * 128], identity=ident[:]),
                                    reads=[x_.b, ident.b], writes=[pt.b])
                            for jj in range(4):
                                j = jb * 4 + jj
                                kb.op("act", lambda e, pt=pt, jj=jj, j=j, i=i, wsel=wsel: e.activation(
                                    out=hT[:, j, i * 128:(i + 1) * 128], in_=pt[:, jj * 128:(jj + 1) * 128], func=AF.Identity,
                                    bias=modsT[:, 0 * KC + j, wsel:wsel + 1], scale=gsm[:, j, wsel:wsel + 1]),
                                    reads=[pt.b, modsT.b, gsm.b], writes=[hT.b])
                    kb.barrier()
                with ExitStack() as ph:
                    wC = [sb(ph, "wC%d" % i, [128, KC, 128], BF16) for i in range(2)]
                    sg = [sb(ph, "sg%d" % i, [128, 512]) for i in range(3)]
                    wv = w_in[l].rearrange("(k p) c -> p k c", p=128)
                    nev = 0
                    for cc in range(47):
                        c0 = cc * 128
                        n = min(128, IN_COLS - c0)
                        w = wC[cc % 2]
                        kb.dma("pool", w[:, :, :n], wv[:, :, c0:c0 + n], writes=[w.b])
                        for tb in range(5):
                            t0 = tb * 512
                            nt = min(512, T - t0)
                            pj = banks[5 + nev % 3]
                            for k in range(KC):
                                kb.op("pe", lambda e, pj=pj, w=w, k=k, n=n, t0=t0, nt=nt: e.matmul(
                                    out=pj[:n, :nt], lhsT=w[:, k, :n], rhs=hT[:, k, t0:t0 + nt],
                                    start=(k == 0), stop=(k == KC - 1)), reads=[w.b, hT.b], writes=[pj.b])
                            s_ = sg[nev % 3]
                            eng = "act" if nev % 2 == 0 else "dve"
                            if eng == "act":
                                kb.op("act", lambda e, s_=s_, pj=pj, n=n, nt=nt: e.activation(out=s_[:n, :nt], in_=pj[:n, :nt],
                                                                                            func=AF.Identity),
                                      reads=[pj.b], writes=[s_.b])
                            else:
                                kb.op("dve", lambda e, s_=s_, pj=pj, n=n, nt=nt: e.tensor_copy(out=s_[:n, :nt], in_=pj[:n, :nt]),
                                      reads=[pj.b], writes=[s_.b])
                            kb.dma("sp", projT[c0:c0 + n, t0:t0 + nt], s_[:n, :nt], reads=[s_.b])
                            nev += 1
                    kb.barrier()
            if stage <= 1:
                break
            wov = w_out[l].rearrange("(k p) c -> p k c", p=128)
            wgv = w_gate[l].rearrange("(k p) c -> p k c", p=128)
            wuv = w_up[l].rearrange("(k p) c -> p k c", p=128)
            wdv = w_down[l].rearrange("(h p) c -> p h c", p=128)
            pre = {}
            for nm_, view_, nch_, nk_ in (("wo", wov, KC, KC), ("wg", wgv, 44, KC), ("wu", wuv, 44, KC), ("wd", wdv, KC, 44)):
                scr_ = dscr("pc_%s%d" % (nm_, l), [nch_, 128, nk_ * 128], BF16)
                lst_ = []
                for j_ in range(nch_):
                    b_ = Buf()
                    dst_ = scr_[j_].rearrange("p (k c) -> p k c", c=128)
                    kb.dma("pool", dst_, view_[:, :, j_ * 128:(j_ + 1) * 128], writes=[b_])
                    lst_.append((dst_, b_))
                pre[nm_] = lst_
            with ExitStack() as ph:
                TWO_PI = 6.283185307179586
                C1 = 6.28125
                C2 = TWO_PI - C1
                PI = 3.141592653589793
                uT = sb(ph, "uT", [128, 4, T])
                yT = sb(ph, "yT", [128, 4, T])
                Bb = sb(ph, "Bb", [128, 32, 128])
                Cb = sb(ph, "Cb", [128, 32, 128])
                lre = sb(ph, "lre", [128, 32]); lim = sb(ph, "lim", [128, 32]); ldt = sb(ph, "ldt", [128, 32])
                tau1 = sb(ph, "tau1", [128, LC])
                dTt = sb(ph, "dTt", [128, L * 4]); gbT = sb(ph, "gbT", [128, L * 4])
                gluw = sb(ph, "gluw", [128, 4, 512])
                kb.dma("sp", uT[:], projT[0:512, :].rearrange("(c p) t -> p c t", p=128), writes=[uT.b])
                kb.dma("sp", lre[:], s5_lre_d[:, l * 32:(l + 1) * 32], writes=[lre.b])
                kb.dma("sp", lim[:], s5_lim_d[:, l * 32:(l + 1) * 32], writes=[lim.b])
                kb.dma("sp", ldt[:], s5_ldt_d[:, l * 32:(l + 1) * 32], writes=[ldt.b])
                kb.dma("sp", tau1[:], tau1_d, writes=[tau1.b])
                kb.dma("sp", dTt[:], s5_dT_d, writes=[dTt.b])
                kb.dma("sp", gbT[:], s5_gbT_d, writes=[gbT.b])
                kb.dma("sp", gluw[:], s5_gluw_d[l].rearrange("(c p) n -> p c n", p=128), writes=[gluw.b])

                def sincos(n, ang, o_sin, o_cos, tf, ti, tm, tb_):
                    B = [tb_]
                    kb.op("dve", lambda e: e.tensor_scalar(out=tf, in0=ang, scalar1=1.0 / TWO_PI, scalar2=None, op0=ALU.mult), reads=B, writes=B)
                    kb.op("dve", lambda e: e.tensor_copy(out=ti, in_=tf), reads=B, writes=B)
                    kb.op("dve", lambda e: e.tensor_copy(out=tf, in_=ti), reads=B, writes=B)
                    kb.op("dve", lambda e: e.scalar_tensor_tensor(out=tm, in0=tf, scalar=-C1, in1=ang, op0=ALU.mult, op1=ALU.add), reads=B, writes=B)
                    kb.op("dve", lambda e: e.scalar_tensor_tensor(out=tm, in0=tf, scalar=-C2, in1=tm, op0=ALU.mult, op1=ALU.add), reads=B, writes=B)

                    def wrap(y):
                        kb.op("dve", lambda e: e.tensor_scalar(out=tf, in0=y, scalar1=PI, scalar2=TWO_PI, op0=ALU.is_gt, op1=ALU.mult), reads=B, writes=B)
                        kb.op("dve", lambda e: e.tensor_tensor(out=y, in0=y, in1=tf, op=ALU.subtract), reads=B, writes=B)
                        kb.op("dve", lambda e: e.tensor_scalar(out=tf, in0=y, scalar1=-PI, scalar2=TWO_PI, op0=ALU.is_lt, op1=ALU.mult), reads=B, writes=B)
                        kb.op("dve", lambda e: e.tensor_tensor(out=y, in0=y, in1=tf, op=ALU.add), reads=B, writes=B)
                    wrap(tm)
                    kb.op("act", lambda e: e.activation(out=o_sin, in_=tm, func=AF.Sin), reads=B, writes=B)
                    kb.op("dve", lambda e: e.tensor_scalar(out=tm, in0=tm, scalar1=PI / 2, scalar2=None, op0=ALU.add), reads=B, writes=B)
                    wrap(tm)
                    kb.op("act", lambda e: e.activation(out=o_cos, in_=tm, func=AF.Sin), reads=B, writes=B)

                with ExitStack() as ph2:
                    tabs = sb(ph2, "tabs", [128, 4, 16 * LC])
                    tw = sb(ph2, "tw", [128, 3, 16 * LC])
                    twi = sb(ph2, "twi", [128, 16 * LC], mybir.dt.int32)
                    sp_ = sb(ph2, "s5par", [128, 16, 16])
                    spi = sb(ph2, "s5pari", [128, 16], mybir.dt.int32)
                    car = sb(ph2, "car", [128, 2, 16])
                    S5U = [sb(ph2, "s5u%d" % i, [128, 6, LC]) for i in range(8)]
                    S5B = [tabs.b, tw.b, twi.b, sp_.b, spi.b]

                    def P_(k):
                        return sp_[:, k, :]
                    for i in range(2):
                        o = (l * 2 + i) * 16
                        B = [sp_.b]
                        kb.dma("sp", Bb[:], s5_Bb_d[:, (l * 2 + i) * 32:(l * 2 + i + 1) * 32, :], writes=[Bb.b])
                        kb.dma("sp", Cb[:], s5_Cb_d[:, (l * 2 + i) * 32:(l * 2 + i + 1) * 32, :], writes=[Cb.b])
                        kb.op("act", lambda e: e.activation(out=Cb[:, 16:32, :], in_=Cb[:, 16:32, :], func=AF.Identity, scale=-1.0),
                              reads=[Cb.b], writes=[Cb.b])
                        kb.op("act", lambda e: e.activation(out=P_(0), in_=ldt[:, (l * 2 + i) * 16 - l * 32 + 0:(l * 2 + i) * 16 - l * 32 + 16], func=AF.Exp), reads=[ldt.b], writes=B)
                        kb.op("dve", lambda e: e.tensor_tensor(out=P_(1), in0=lre[:, i * 16:(i + 1) * 16], in1=P_(0), op=ALU.mult), reads=[lre.b] + B, writes=B)
                        kb.op("dve", lambda e: e.tensor_tensor(out=P_(2), in0=lim[:, i * 16:(i + 1) * 16], in1=P_(0), op=ALU.mult), reads=[lim.b] + B, writes=B)
                        kb.op("act", lambda e: e.activation(out=P_(3), in_=P_(1), func=AF.Exp), reads=B, writes=B)
                        sincos(16, P_(2), P_(4), P_(5), P_(6), spi[:], P_(7), sp_.b)
                        kb.op("dve", lambda e: e.tensor_tensor(out=P_(6), in0=P_(3), in1=P_(5), op=ALU.mult), reads=B, writes=B)
                        kb.op("dve", lambda e: e.tensor_scalar(out=P_(6), in0=P_(6), scalar1=-1.0, scalar2=None, op0=ALU.add), reads=B, writes=B)
                        kb.op("dve", lambda e: e.tensor_tensor(out=P_(7), in0=P_(3), in1=P_(4), op=ALU.mult), reads=B, writes=B)
                        kb.op("dve", lambda e: e.tensor_tensor(out=P_(8), in0=lre[:, i * 16:(i + 1) * 16], in1=lre[:, i * 16:(i + 1) * 16], op=ALU.mult), reads=[lre.b] + B, writes=B)
                        kb.op("dve", lambda e: e.tensor_tensor(out=P_(9), in0=lim[:, i * 16:(i + 1) * 16], in1=lim[:, i * 16:(i + 1) * 16], op=ALU.mult), reads=[lim.b] + B, writes=B)
                        kb.op("dve", lambda e: e.tensor_tensor(out=P_(8), in0=P_(8), in1=P_(9), op=ALU.add), reads=B, writes=B)
                        kb.op("dve", lambda e: e.reciprocal(out=P_(8), in_=P_(8)), reads=B, writes=B)
                        kb.op("dve", lambda e: e.tensor_tensor(out=P_(10), in0=P_(6), in1=lre[:, i * 16:(i + 1) * 16], op=ALU.mult), reads=[lre.b] + B, writes=B)
                        kb.op("dve", lambda e: e.tensor_tensor(out=P_(9), in0=P_(7), in1=lim[:, i * 16:(i + 1) * 16], op=ALU.mult), reads=[lim.b] + B, writes=B)
                        kb.op("dve", lambda e: e.tensor_tensor(out=P_(10), in0=P_(10), in1=P_(9), op=ALU.add), reads=B, writes=B)
                        kb.op("dve", lambda e: e.tensor_tensor(out=P_(10), in0=P_(10), in1=P_(8), op=ALU.mult), reads=B, writes=B)
                        kb.op("dve", lambda e: e.tensor_tensor(out=P_(11), in0=P_(7), in1=lre[:, i * 16:(i + 1) * 16], op=ALU.mult), reads=[lre.b] + B, writes=B)
                        kb.op("dve", lambda e: e.tensor_tensor(out=P_(9), in0=P_(6), in1=lim[:, i * 16:(i + 1) * 16], op=ALU.mult), reads=[lim.b] + B, writes=B)
                        kb.op("dve", lambda e: e.tensor_tensor(out=P_(11), in0=P_(11), in1=P_(9), op=ALU.subtract), reads=B, writes=B)
                        kb.op("dve", lambda e: e.tensor_tensor(out=P_(11), in0=P_(11), in1=P_(8), op=ALU.mult), reads=B, writes=B)
                        for st in range(16):
                            kb.op("dve", lambda e, st=st: e.tensor_scalar(out=tw[:, 0, st * LC:(st + 1) * LC], in0=tau1[:], scalar1=sp_[:, 2, st:st + 1],
                                                                         scalar2=None, op0=ALU.mult), reads=[tau1.b] + B, writes=[tw.b])
                        sincos(16 * LC, tw[:, 0, :], tabs[:, 3, :], tabs[:, 2, :], tw[:, 1, :], twi[:], tw[:, 2, :], tw.b)
                        kb.op("dve", lambda e: e.tensor_copy(out=tw[:, 0, 0:1], in_=tw[:, 0, 0:1]), reads=[tw.b, tabs.b], writes=[tw.b, tabs.b])
                        for st in range(16):
                            sl = slice(st * LC, (st + 1) * LC)
                            kb.op("dve", lambda e, st=st, sl=sl: e.tensor_scalar(out=tw[:, 1, sl], in0=tabs[:, 3, sl], scalar1=sp_[:, 11, st:st + 1], scalar2=None, op0=ALU.mult), reads=[tabs.b] + B, writes=[tw.b])
                            kb.op("dve", lambda e, st=st, sl=sl: e.scalar_tensor_tensor(out=tabs[:, 0, sl], in0=tabs[:, 2, sl], scalar=sp_[:, 10, st:st + 1], in1=tw[:, 1, sl], op0=ALU.mult, op1=ALU.add), reads=[tw.b] + B, writes=[tabs.b])
                            kb.op("dve", lambda e, st=st, sl=sl: e.tensor_scalar(out=tw[:, 1, sl], in0=tabs[:, 3, sl], scalar1=sp_[:, 10, st:st + 1], scalar2=None, op0=ALU.mult), reads=[tabs.b] + B, writes=[tw.b])
                            kb.op("dve", lambda e, st=st, sl=sl: e.scalar_tensor_tensor(out=tabs[:, 1, sl], in0=tabs[:, 2, sl], scalar=sp_[:, 11, st:st + 1], in1=tw[:, 1, sl], op0=ALU.mult, op1=ALU.subtract), reads=[tw.b] + B, writes=[tabs.b])
                        kb.op("dve", lambda e: e.memset(car[:], 0.0), writes=[car.b])
                        NCH = T // LC
                        NCC = NCTX // LC
                        order = list(range(NCH)) if i == 0 else list(range(NCC - 1, -1, -1)) + list(range(NCH - 1, NCC - 1, -1))

                        def rv(ap):
                            return ap[:, ::-1] if i == 1 else ap
                        lastc = LC - 1 if i == 0 else 0
                        for n in order:
                            t0 = n * LC
                            for hf in range(2):
                                for q in range(8):
                                    st = hf * 8 + q
                                    pb = banks[q // 2]
                                    for ci in range(2):
                                        c0 = (q % 2) * 2 * LC + ci * LC
                                        kb.op("pe", lambda e, ci=ci, pb=pb, st=st, c0=c0: e.matmul(out=pb[:, c0:c0 + LC], lhsT=Bb[:, ci * 16 + st, :], rhs=uT[:, st // 4, t0:t0 + LC],
                                                                                              start=True, stop=True), reads=[Bb.b, uT.b], writes=[pb.b])
                                for q in range(8):
                                    st = hf * 8 + q
                                    pb = banks[q // 2]
                                    sl = slice(st * LC, (st + 1) * LC)
                                    u_ = S5U[q]
                                    bre = rv(pb[:, (q % 2) * 2 * LC:(q % 2) * 2 * LC + LC]); bim = rv(pb[:, (q % 2) * 2 * LC + LC:(q % 2) * 2 * LC + 2 * LC])
                                    kb.op("dve", lambda e, u_=u_, bre=bre, sl=sl: e.tensor_tensor(out=u_[:, 0, :], in0=bre, in1=tabs[:, 0, sl], op=ALU.mult), reads=[pb.b, tabs.b], writes=[u_.b])
                                    kb.op("dve", lambda e, u_=u_, bim=bim, sl=sl: e.tensor_tensor(out=u_[:, 1, :], in0=bim, in1=tabs[:, 1, sl], op=ALU.mult), reads=[pb.b, tabs.b], writes=[u_.b])
                                    kb.op("dve", lambda e, u_=u_, bre=bre, sl=sl: e.tensor_tensor(out=u_[:, 2, :], in0=bre, in1=tabs[:, 1, sl], op=ALU.mult), reads=[pb.b, tabs.b], writes=[u_.b])
                                    kb.op("dve", lambda e, u_=u_, bim=bim, sl=sl: e.tensor_tensor(out=u_[:, 3, :], in0=bim, in1=tabs[:, 0, sl], op=ALU.mult), reads=[pb.b, tabs.b], writes=[u_.b])
                                for q in range(8):
                                    u_ = S5U[q]
                                    kb.op("dve", lambda e, u_=u_: e.tensor_tensor(out=u_[:, 0, :], in0=u_[:, 0, :], in1=u_[:, 1, :], op=ALU.subtract), reads=[u_.b], writes=[u_.b])
                                    kb.op("dve", lambda e, u_=u_: e.tensor_tensor(out=u_[:, 2, :], in0=u_[:, 2, :], in1=u_[:, 3, :], op=ALU.add), reads=[u_.b], writes=[u_.b])
                                for q in range(8):
                                    st = hf * 8 + q
                                    u_ = S5U[q]
                                    rb = sp_[:, 3, st:st + 1].to_broadcast([128, LC])
                                    kb.op("dve", lambda e, u_=u_, rb=rb, st=st: e.tensor_tensor_scan(out=u_[:, 1, :], data0=rb, data1=u_[:, 0, :], initial=car[:, 0, st:st + 1], op0=ALU.mult, op1=ALU.add),
                                          reads=[u_.b, sp_.b, car.b], writes=[u_.b])
                                    kb.op("dve", lambda e, u_=u_, rb=rb, st=st: e.tensor_tensor_scan(out=u_[:, 3, :], data0=rb, data1=u_[:, 2, :], initial=car[:, 1, st:st + 1], op0=ALU.mult, op1=ALU.add),
                                          reads=[u_.b, sp_.b, car.b], writes=[u_.b])
                                for q in range(8):
                                    st = hf * 8 + q
                                    sl = slice(st * LC, (st + 1) * LC)
                                    u_ = S5U[q]
                                    kb.op("pool", lambda e, u_=u_, sl=sl: e.tensor_tensor(out=u_[:, 0, :], in0=u_[:, 1, :], in1=tabs[:, 2, sl], op=ALU.mult), reads=[u_.b, tabs.b], writes=[u_.b])
                                    kb.op("pool", lambda e, u_=u_, sl=sl: e.tensor_tensor(out=u_[:, 2, :], in0=u_[:, 3, :], in1=tabs[:, 3, sl], op=ALU.mult), reads=[u_.b, tabs.b], writes=[u_.b])
                                    kb.op("pool", lambda e, u_=u_: e.tensor_tensor(out=rv(u_[:, 4, :]), in0=u_[:, 0, :], in1=u_[:, 2, :], op=ALU.subtract), reads=[u_.b], writes=[u_.b])
                                    kb.op("pool", lambda e, u_=u_, sl=sl: e.tensor_tensor(out=u_[:, 0, :], in0=u_[:, 1, :], in1=tabs[:, 3, sl], op=ALU.mult), reads=[u_.b, tabs.b], writes=[u_.b])
                                    kb.op("pool", lambda e, u_=u_, sl=sl: e.tensor_tensor(out=u_[:, 2, :], in0=u_[:, 3, :], in1=tabs[:, 2, sl], op=ALU.mult), reads=[u_.b, tabs.b], writes=[u_.b])
                                    kb.op("pool", lambda e, u_=u_: e.tensor_tensor(out=rv(u_[:, 5, :]), in0=u_[:, 0, :], in1=u_[:, 2, :], op=ALU.add), reads=[u_.b], writes=[u_.b])
                                for q in range(8):
                                    st = hf * 8 + q
                                    u_ = S5U[q]
                                    kb.op("act", lambda e, u_=u_, st=st: e.activation(out=car[:, 0, st:st + 1], in_=u_[:, 4, lastc:lastc + 1], func=AF.Identity), reads=[u_.b], writes=[car.b])
                                    kb.op("act", lambda e, u_=u_, st=st: e.activation(out=car[:, 1, st:st + 1], in_=u_[:, 5, lastc:lastc + 1], func=AF.Identity), reads=[u_.b], writes=[car.b])
                                for f2 in range(2):
                                    fc = hf * 2 + f2
                                    py = banks[4 + fc]
                                    for q4 in range(4):
                                        q = f2 * 4 + q4
                                        st = hf * 8 + q
                                        u_ = S5U[q]
                                        kb.op("pe", lambda e, py=py, st=st, u_=u_, q4=q4: e.matmul(out=py[:, 0:LC], lhsT=Cb[:, st, :], rhs=u_[:, 4, :], start=(q4 == 0), stop=False),
                                              reads=[Cb.b, u_.b], writes=[py.b])
                                        kb.op("pe", lambda e, py=py, st=st, u_=u_, q4=q4: e.matmul(out=py[:, 0:LC], lhsT=Cb[:, 16 + st, :], rhs=u_[:, 5, :], start=False, stop=(q4 == 3)),
                                              reads=[Cb.b, u_.b], writes=[py.b])
                                    if i == 0:
                                        kb.op("act", lambda e, py=py, fc=fc: e.activation(out=yT[:, fc, t0:t0 + LC], in_=py[:, 0:LC], func=AF.Identity), writes=[yT.b, py.b])
                                    else:
                                        kb.op("dve", lambda e, py=py, fc=fc: e.tensor_tensor(out=yT[:, fc, t0:t0 + LC], in0=py[:, 0:LC], in1=yT[:, fc, t0:t0 + LC], op=ALU.add),
                                              writes=[yT.b, py.b])
                    kb.barrier()
                with ExitStack() as ph2:
                    g1 = sb(ph2, "g1", [128, T]); g2 = sb(ph2, "g2", [128, T])
                    og = [sb(ph2, "og%d" % i, [128, 512], BF16) for i in range(2)]
                    sgl = [sb(ph2, "sgl%d" % i, [128, 512]) for i in range(2)]
                    for fc in range(4):
                        kb.op("dve", lambda e: e.scalar_tensor_tensor(out=yT[:, fc, :], in0=uT[:, fc, :], scalar=dTt[:, l * 4 + fc:l * 4 + fc + 1], in1=yT[:, fc, :],
                                                                      op0=ALU.mult, op1=ALU.add), reads=[uT.b, dTt.b, yT.b], writes=[yT.b])
                        kb.op("act", lambda e: e.activation(out=g1[:], in_=yT[:, fc, :], func=AF.Square), reads=[yT.b], writes=[g1.b])
                        kb.op("dve", lambda e: e.tensor_scalar(out=g1[:], in0=g1[:], scalar1=0.044715, scalar2=1.0, op0=ALU.mult, op1=ALU.add), reads=[g1.b], writes=[g1.b])
                        kb.op("dve", lambda e: e.tensor_tensor(out=g1[:], in0=g1[:], in1=yT[:, fc, :], op=ALU.mult), reads=[g1.b, yT.b], writes=[g1.b])
                        kb.op("act", lambda e: e.activation(out=g2[:], in_=g1[:], func=AF.Sigmoid, scale=1.5957691216057308), reads=[g1.b], writes=[g2.b])
                        kb.op("dve", lambda e: e.tensor_tensor(out=yT[:, fc, :], in0=yT[:, fc, :], in1=g2[:], op=ALU.mult), reads=[g2.b, yT.b], writes=[yT.b])
                    ne = 0
                    for fo in range(4):
                        for tb in range(5):
                            t0 = tb * 512
                            nt = min(512, T - t0)
                            pg = banks[4 + ne % 2]
                            for fi in range(4):
                                kb.op("pe", lambda e, fi=fi, pg=pg: e.matmul(out=pg[:, :nt], lhsT=gluw[:, fi, fo * 128:(fo + 1) * 128], rhs=yT[:, fi, t0:t0 + nt],
                                                                             start=(fi == 0), stop=(fi == 3)), reads=[gluw.b, yT.b], writes=[pg.b])
                            s_ = sgl[ne % 2]; o_ = og[ne % 2]
                            kb.op("act", lambda e, pg=pg, s_=s_: e.activation(out=s_[:, :nt], in_=pg[:, :nt], func=AF.Sigmoid, bias=gbT[:, l * 4 + fo:l * 4 + fo + 1]),
                                  reads=[pg.b, gbT.b], writes=[s_.b])
                            kb.op("dve", lambda e, s_=s_, o_=o_: e.tensor_tensor(out=o_[:, :nt], in0=s_[:, :nt], in1=yT[:, fo, t0:t0 + nt], op=ALU.mult),
                                  reads=[s_.b, yT.b], writes=[o_.b])
                            kb.dma("sp", mixT[fo * 128:(fo + 1) * 128, t0:t0 + nt], o_[:, :nt], reads=[o_.b])
                            ne += 1
                kb.barrier()
            if stage <= 2:
                break
            with ExitStack() as ph:
                QOFF, KOFF, VOFF, ZOFF, AOFF, BOFF = 512, 1280, 2048, 2816, 3584, 3596
                NCK = T // 64
                RM = sb(ph, "RM", [64, T])
                SEL = sb(ph, "SEL", [64, 12, 128]); NSEL = sb(ph, "NSEL", [64, 12, 128])
                MB = sb(ph, "MB", [64, 4, 64])
                ONES = sb(ph, "ONES", [128, 128])
                NORM = sb(ph, "NORM", [64, 128])
                GP = sb(ph, "GP", [64, 2])
                CW = sb(ph, "CW", [128, 18 * 3])
                kb.dma("sp", RM[:], rm_d, writes=[RM.b])
                kb.dma("sp", SEL[:], sel_d, writes=[SEL.b])
                kb.dma("sp", MB[:], mb_d, writes=[MB.b])
                kb.dma("sp", NORM[:], gdn_norm_d[:, l * 128:(l + 1) * 128], writes=[NORM.b])
                kb.dma("sp", GP[:], gdn_gp_d[:, l * 2:(l + 1) * 2], writes=[GP.b])
                kb.dma("sp", CW[:], gdn_cw_d[:, l * 54:(l + 1) * 54], writes=[CW.b])
                kb.op("act", lambda e: e.activation(out=NSEL[:], in_=SEL[:], func=AF.Identity, scale=-1.0), reads=[SEL.b], writes=[NSEL.b])
                kb.op("dve", lambda e: e.memset(ONES[:], 1.0), writes=[ONES.b])
                RA = sb(ph, "RA", [64, T]); RB = sb(ph, "RB", [64, T]); GC = sb(ph, "GC", [64, T])
                R1 = sb(ph, "R1", [64, T]); EG = sb(ph, "EG", [64, T]); BG = sb(ph, "BG", [64, T])
                EGL = sb(ph, "EGL", [64, NCK])
                COL = sb(ph, "COL", [64, NCK, 3, 12])
                EGLB = sb(ph, "EGLB", [128, 12, NCK])
                kb.op("dve", lambda e: e.memset(RA[:], 0.0), writes=[RA.b])
                kb.op("dve", lambda e: e.memset(RB[:], 0.0), writes=[RB.b])
                for dr in range(2):
                    kb.dma("sp", RA[dr * 32:dr * 32 + 6, :], projT[AOFF + dr * 6:AOFF + dr * 6 + 6, :], writes=[RA.b])
                    kb.dma("sp", RB[dr * 32:dr * 32 + 6, :], projT[BOFF + dr * 6:BOFF + dr * 6 + 6, :], writes=[RB.b])
                kb.op("act", lambda e: e.activation(out=RA[:], in_=RA[:], func=AF.Exp, bias=GP[:, 1:2]), reads=[RA.b, GP.b], writes=[RA.b])
                kb.op("act", lambda e: e.activation(out=RA[:], in_=RA[:], func=AF.Ln, bias=1.0), reads=[RA.b], writes=[RA.b])
                kb.op("act", lambda e: e.activation(out=GP[:, 0:1], in_=GP[:, 0:1], func=AF.Exp), reads=[GP.b], writes=[GP.b])
                kb.op("dve", lambda e: e.tensor_scalar(out=RA[:], in0=RA[:], scalar1=GP[:, 0:1], scalar2=-1.0, op0=ALU.mult, op1=ALU.mult), reads=[RA.b, GP.b], writes=[RA.b])
                kb.op("act", lambda e: e.activation(out=RB[:], in_=RB[:], func=AF.Sigmoid), reads=[RB.b], writes=[RB.b])
                kb.op("act", lambda e: e.activation(out=R1[:], in_=RB[:], func=AF.Ln), reads=[RB.b], writes=[R1.b])
                kb.op("dve", lambda e: e.tensor_tensor_scan(out=GC[0:32, :], data0=RM[0:32, :], data1=RA[0:32, :], initial=0.0, op0=ALU.mult, op1=ALU.add),
                      reads=[RM.b, RA.b], writes=[GC.b])
                kb.op("dve", lambda e: e.tensor_tensor_scan(out=GC[32:64, ::-1], data0=RM[32:64, ::-1], data1=RA[32:64, ::-1], initial=0.0, op0=ALU.mult, op1=ALU.add),
                      reads=[RM.b, RA.b], writes=[GC.b])
                kb.op("dve", lambda e: e.tensor_tensor(out=R1[:], in0=R1[:], in1=GC[:], op=ALU.add), reads=[R1.b, GC.b], writes=[R1.b])
                kb.op("act", lambda e: e.activation(out=EG[:], in_=GC[:], func=AF.Exp), reads=[GC.b], writes=[EG.b])
                kb.op("dve", lambda e: e.tensor_tensor(out=BG[:], in0=EG[:], in1=RB[:], op=ALU.mult), reads=[EG.b, RB.b], writes=[BG.b])
                GCL = sb(ph, "GCL", [64, NCK])
                for dr in range(2):
                    pr = slice(dr * 32, dr * 32 + 32)
                    lastpos = 63 if dr == 0 else 0
                    gv = GC[pr, :].rearrange("p (c s) -> p c s", s=64)
                    kb.op("dve", lambda e, pr=pr, gv=gv, lastpos=lastpos: e.tensor_copy(out=GCL[pr, :], in_=gv[:, :, lastpos]), reads=[GC.b], writes=[GCL.b])
                kb.op("act", lambda e: e.activation(out=EGL[:], in_=GCL[:], func=AF.Exp), reads=[GCL.b], writes=[EGL.b])
                for c in range(NCK):
                    kb.op("dve", lambda e, c=c: e.tensor_scalar(out=RA[:, c * 64:(c + 1) * 64], in0=GC[:, c * 64:(c + 1) * 64], scalar1=-1.0, scalar2=GCL[:, c:c + 1],
                                                                 op0=ALU.mult, op1=ALU.add), reads=[GC.b, GCL.b], writes=[RA.b])
                kb.op("act", lambda e: e.activation(out=RA[:], in_=RA[:], func=AF.Exp), reads=[RA.b], writes=[RA.b])
                for c in range(NCK):
                    pcol = banks[c % 4]
                    for qi, row in enumerate((BG, RA, RB)):
                        kb.op("pe", lambda e, row=row, qi=qi, pcol=pcol, c=c: e.matmul(out=pcol[0:64, qi * 12:(qi + 1) * 12], lhsT=row[:, c * 64:(c + 1) * 64],
                                                                                   rhs=SEL[:, :, 0], start=True, stop=True), reads=[row.b, SEL.b], writes=[pcol.b])
                    kb.op("act", lambda e, pcol=pcol, c=c: e.activation(out=COL[:, c, :, :].rearrange("p a b -> p (a b)"), in_=pcol[0:64, 0:36], func=AF.Identity),
                          reads=[pcol.b], writes=[COL.b])
                for r in range(12):
                    pcol = banks[4 + r % 2]
                    kb.op("pe", lambda e, r=r, pcol=pcol: e.matmul(out=pcol[:, 0:NCK], lhsT=SEL[:, r, :], rhs=EGL[:], start=True, stop=True), reads=[SEL.b, EGL.b], writes=[pcol.b])
                    kb.op("act", lambda e, r=r, pcol=pcol: e.activation(out=EGLB[:, r, :], in_=pcol[:, 0:NCK], func=AF.Identity), reads=[pcol.b], writes=[EGLB.b])
                kb.barrier()
                import os as _os
                DBG = int(_os.environ.get("DBG_GDN", "9"))
                if stage == 3:
                    for qi_, row_ in enumerate((GC, R1, EG, RA, RB, BG)):
                        kb.dma("sp", dbg_rows[:, qi_, :], row_[:], reads=[row_.b])
                psn = [0]

                def pget():
                    bk = banks[psn[0] % 8]
                    psn[0] += 1
                    return bk
                RAW = sb(ph, "RAW", [128, 3, T]); CV = sb(ph, "CV", [128, 3, T])
                OACC = sb(ph, "OACC", [64, NCK, 128]); OUT = sb(ph, "OUT", [128, T], BF16)
                Sd = [sb(ph, "Sst%d" % i, [128, 128]) for i in range(2)]
                G = 4
                U = []
                for u in range(2 * G):
                    d_ = {}
                    for nm, shp in (("AB0", [64, 2, 64]), ("AB1", [64, 2, 64]), ("X0", [64, 64]), ("X1", [64, 64]),
                                    ("E", [64, 3, 64]), ("kbg", [64, 128]), ("kdec", [64, 128]), ("vb", [64, 128]),
                                    ("nWT", [128, 64]), ("VN", [64, 128])):
                        d_[nm] = sb(ph, "u%d%s" % (u, nm), shp, BF16 if nm in ("AB0", "AB1", "X0", "X1", "kbg", "vb") else F32)
                    U.append(d_)
                fin = [sb(ph, "fin%d" % i, [64, 128]) for i in range(2)]
                fst = [sb(ph, "fst%d" % i, [64, 4]) for i in range(2)]
                I64 = ident[0:64, 0:64]
                I64b_t = sb(ph, "I64b", [64, 64], BF16)
                kb.op("dve", lambda e: e.tensor_copy(out=I64b_t[:], in_=ident[0:64, 0:64]), reads=[ident.b], writes=[I64b_t.b])
                I64b = I64b_t[:]
                for hd in range(6 if DBG >= 1 else 0):
                    for ci, off in enumerate((QOFF, KOFF, VOFF)):
                        kb.dma("sp", RAW[:, ci, :], projT[off + hd * 128:off + (hd + 1) * 128, :], writes=[RAW.b])
                    for ci in range(3):
                        cch = ci * 6 + hd
                        for (s0, s1) in ((0, NCTX), (NCTX, T)):
                            kb.op("dve", lambda e, ci=ci, cch=cch, s0=s0, s1=s1: e.tensor_scalar(out=CV[:, ci, s0:s1], in0=RAW[:, ci, s0:s1], scalar1=CW[:, cch * 3 + 1:cch * 3 + 2],
                                                                                              scalar2=None, op0=ALU.mult), reads=[RAW.b, CW.b], writes=[CV.b])
                            kb.op("dve", lambda e, ci=ci, cch=cch, s0=s0, s1=s1: e.scalar_tensor_tensor(out=CV[:, ci, s0 + 1:s1], in0=RAW[:, ci, s0:s1 - 1], scalar=CW[:, cch * 3:cch * 3 + 1],
                                                                                                     in1=CV[:, ci, s0 + 1:s1], op0=ALU.mult, op1=ALU.add), reads=[RAW.b, CW.b, CV.b], writes=[CV.b])
                            kb.op("dve", lambda e, ci=ci, cch=cch, s0=s0, s1=s1: e.scalar_tensor_tensor(out=CV[:, ci, s0:s1 - 1], in0=RAW[:, ci, s0 + 1:s1], scalar=CW[:, cch * 3 + 2:cch * 3 + 3],
                                                                                                     in1=CV[:, ci, s0:s1 - 1], op0=ALU.mult, op1=ALU.add), reads=[RAW.b, CW.b, CV.b], writes=[CV.b])
                    kb.op("act", lambda e: e.activation(out=CV[:], in_=CV[:], func=AF.Silu), reads=[CV.b], writes=[CV.b])
                    for ci in range(2):
                        kb.op("act", lambda e, ci=ci: e.activation(out=RAW[:, 0, :], in_=CV[:, ci, :], func=AF.Square), reads=[CV.b], writes=[RAW.b])
                        for tb in range(5):
                            t0 = tb * 512
                            nt = min(512, T - t0)
                            pb = banks[6 + tb % 2]
                            kb.op("pe", lambda e, pb=pb, t0=t0, nt=nt: e.matmul(out=pb[:, :nt], lhsT=ONES[:], rhs=RAW[:, 0, t0:t0 + nt], start=True, stop=True),
                                  reads=[ONES.b, RAW.b], writes=[pb.b])
                            kb.op("act", lambda e, pb=pb, t0=t0, nt=nt: e.activation(out=RAW[:, 1, t0:t0 + nt], in_=pb[:, :nt], func=AF.Sqrt, bias=EPS), reads=[RAW.b], writes=[RAW.b, pb.b])
                        kb.op("dve", lambda e: e.reciprocal(out=RAW[:, 1, :], in_=RAW[:, 1, :]), reads=[RAW.b], writes=[RAW.b])
                        sc_ = (128.0 ** -0.5) if ci == 0 else 1.0
                        kb.op("dve", lambda e, ci=ci, sc_=sc_: e.scalar_tensor_tensor(out=CV[:, ci, :], in0=RAW[:, 1, :], scalar=sc_, in1=CV[:, ci, :], op0=ALU.mult, op1=ALU.mult),
                              reads=[RAW.b, CV.b], writes=[CV.b])
                    QT = lambda a, b_: CV[:, 0, a:b_]
                    KT = lambda a, b_: CV[:, 1, a:b_]
                    VT = lambda a, b_: CV[:, 2, a:b_]
                    kb.op("dve", lambda e: e.memset(OACC[:], 0.0), writes=[OACC.b])
                    for dr in range(2):
                        r = dr * 6 + hd
                        for tb in range(5):
                            t0 = tb * 512
                            nt = min(512, T - t0)
                            pb = banks[6 + tb % 2]
                            kb.op("pe", lambda e, pb=pb, t0=t0, nt=nt, r=r: e.matmul(out=pb[:, :nt], lhsT=SEL[:, r, :], rhs=EG[:, t0:t0 + nt], start=True, stop=True),
                                  reads=[SEL.b, EG.b], writes=[pb.b])
                            kb.op("dve", lambda e, pb=pb, t0=t0, nt=nt, dr=dr: e.tensor_tensor(out=RAW[:, dr, t0:t0 + nt], in0=pb[:, :nt], in1=CV[:, 0, t0:t0 + nt], op=ALU.mult),
                                  reads=[CV.b], writes=[RAW.b, pb.b])
                        kb.op("dve", lambda e, dr=dr: e.memset(Sd[dr][:], 0.0), writes=[Sd[dr].b])
                    orders = [list(range(NCK)), list(range(3, -1, -1)) + list(range(NCK - 1, 3, -1))]
                    masks = [(0, 1, 3), (1, 0, 2)]
                    for g0 in range(0, NCK, G):
                        units = []
                        for dr in range(2):
                            for u, c in enumerate(orders[dr][g0:g0 + G]):
                                units.append((dr, c, U[dr * G + u]))
                        for (dr, c, d_) in units:
                            r = dr * 6 + hd
                            t0 = c * 64
                            bk = pget()
                            kb.op("pe", lambda e, bk=bk, t0=t0: e.transpose(out=bk[0:64, 0:128], in_=KT(t0, t0 + 64), identity=ident[:]), reads=[CV.b, ident.b], writes=[bk.b])
                            kb.op("pe", lambda e, bk=bk, t0=t0: e.transpose(out=bk[0:64, 128:256], in_=VT(t0, t0 + 64), identity=ident[:]), reads=[CV.b, ident.b], writes=[bk.b])
                            kb.op("act", lambda e, bk=bk, d_=d_, c=c, r=r: e.activation(out=d_["kbg"][:], in_=bk[0:64, 0:128], func=AF.Identity, scale=COL[:, c, 0, r:r + 1]), reads=[COL.b], writes=[d_["kbg"].b, bk.b])
                            kb.op("act", lambda e, bk=bk, d_=d_, c=c, r=r: e.activation(out=d_["kdec"][:], in_=bk[0:64, 0:128], func=AF.Identity, scale=COL[:, c, 1, r:r + 1]), reads=[COL.b], writes=[d_["kdec"].b, bk.b])
                            kb.op("act", lambda e, bk=bk, d_=d_, c=c, r=r: e.activation(out=d_["vb"][:], in_=bk[0:64, 128:256], func=AF.Identity, scale=COL[:, c, 2, r:r + 1]), reads=[COL.b], writes=[d_["vb"].b, bk.b])
                        for (dr, c, d_) in units:
                            r = dr * 6 + hd
                            m_s, m_st, m_it = masks[dr]
                            t0 = c * 64
                            ch = slice(t0, t0 + 64)
                            bp = pget()
                            kb.op("pe", lambda e, bp=bp, t0=t0: e.matmul(out=bp[0:64, 0:64], lhsT=KT(t0, t0 + 64), rhs=KT(t0, t0 + 64), start=True, stop=True), reads=[CV.b], writes=[bp.b])
                            kb.op("pe", lambda e, bp=bp, t0=t0: e.matmul(out=bp[0:64, 64:128], lhsT=KT(t0, t0 + 64), rhs=QT(t0, t0 + 64), start=True, stop=True), reads=[CV.b], writes=[bp.b])
                            be = pget()
                            kb.op("pe", lambda e, be=be, ch=ch, r=r: e.matmul(out=be[0:64, 0:64], lhsT=R1[:, ch], rhs=SEL[:, r, 0:64], start=True, stop=False), reads=[R1.b, SEL.b], writes=[be.b])
                            kb.op("pe", lambda e, be=be, ch=ch, r=r: e.matmul(out=be[0:64, 0:64], lhsT=NSEL[:, r, 0:64], rhs=GC[:, ch], start=False, stop=False), reads=[GC.b, NSEL.b], writes=[be.b])
                            kb.op("pe", lambda e, be=be, m_s=m_s: e.matmul(out=be[0:64, 0:64], lhsT=I64, rhs=MB[:, m_s, :], start=False, stop=True), reads=[MB.b, ident.b], writes=[be.b])
                            kb.op("pe", lambda e, be=be, ch=ch, r=r: e.matmul(out=be[0:64, 64:128], lhsT=GC[:, ch], rhs=NSEL[:, r, 0:64], start=True, stop=False), reads=[GC.b, NSEL.b], writes=[be.b])
                            kb.op("pe", lambda e, be=be, ch=ch, r=r: e.matmul(out=be[0:64, 64:128], lhsT=SEL[:, r, 0:64], rhs=R1[:, ch], start=False, stop=False), reads=[R1.b, SEL.b], writes=[be.b])
                            kb.op("pe", lambda e, be=be, m_st=m_st: e.matmul(out=be[0:64, 64:128], lhsT=I64, rhs=MB[:, m_st, :], start=False, stop=True), reads=[MB.b, ident.b], writes=[be.b])
                            kb.op("pe", lambda e, be=be, ch=ch, r=r: e.matmul(out=be[0:64, 128:192], lhsT=GC[:, ch], rhs=NSEL[:, r, 0:64], start=True, stop=False), reads=[GC.b, NSEL.b], writes=[be.b])
                            kb.op("pe", lambda e, be=be, ch=ch, r=r: e.matmul(out=be[0:64, 128:192], lhsT=SEL[:, r, 0:64], rhs=GC[:, ch], start=False, stop=False), reads=[GC.b, SEL.b], writes=[be.b])
                            kb.op("pe", lambda e, be=be, m_it=m_it: e.matmul(out=be[0:64, 128:192], lhsT=I64, rhs=MB[:, m_it, :], start=False, stop=True), reads=[MB.b, ident.b], writes=[be.b])
                            kb.op("act", lambda e, be=be, d_=d_: e.activation(out=d_["E"][:].rearrange("p a b -> p (a b)"), in_=be[0:64, 0:192], func=AF.Exp), writes=[d_["E"].b, be.b])
                            kb.op("dve", lambda e, bp=bp, d_=d_: e.tensor_tensor(out=d_["AB0"][:, 1, :], in0=bp[0:64, 0:64], in1=d_["E"][:, 0, :], op=ALU.mult), reads=[d_["E"].b], writes=[d_["AB0"].b, bp.b])
                            kb.op("dve", lambda e, bp=bp, d_=d_: e.tensor_tensor(out=d_["AB0"][:, 0, :], in0=bp[0:64, 0:64], in1=d_["E"][:, 1, :], op=ALU.mult), reads=[d_["E"].b], writes=[d_["AB0"].b, bp.b])
                            kb.op("dve", lambda e, bp=bp, d_=d_: e.tensor_tensor(out=d_["E"][:, 2, :], in0=bp[0:64, 64:128], in1=d_["E"][:, 2, :], op=ALU.mult), reads=[d_["E"].b], writes=[d_["E"].b, bp.b])
                            kb.op("dve", lambda e, d_=d_: e.tensor_tensor(out=d_["X0"][:], in0=I64, in1=d_["AB0"][:, 0, :], op=ALU.subtract), reads=[ident.b, d_["AB0"].b], writes=[d_["X0"].b])
                        for k in range(1, 6):
                            pa, pn = (k - 1) % 2, k % 2
                            for ui, (dr, c, d_) in enumerate(units):
                                ABp, ABn = d_["AB%d" % pa], d_["AB%d" % pn]
                                bk = pget()
                                if k < 5:
                                    kb.op("pe", lambda e, bk=bk, ABp=ABp: e.matmul(out=bk[0:64, 0:64], lhsT=ABp[:, 1, :], rhs=ABp[:, 0, :], start=True, stop=True), reads=[ABp.b], writes=[bk.b])
                                kb.op("pe", lambda e, bk=bk, ABp=ABp: e.matmul(out=bk[0:64, 64:128], lhsT=ABp[:, 0, :], rhs=ABp[:, 1, :], start=True, stop=True), reads=[ABp.b], writes=[bk.b])
                                lo = 0 if k < 5 else 64
                                if (ui + k) % 2 == 0:
                                    kb.op("act", lambda e, bk=bk, ABn=ABn, lo=lo: e.activation(out=ABn[:].rearrange("p a b -> p (a b)")[:, lo:128], in_=bk[0:64, lo:128], func=AF.Identity), writes=[ABn.b, bk.b])
                                else:
                                    kb.op("dve", lambda e, bk=bk, ABn=ABn, lo=lo: e.tensor_copy(out=ABn[:].rearrange("p a b -> p (a b)")[:, lo:128], in_=bk[0:64, lo:128]), writes=[ABn.b, bk.b])
                            for ui, (dr, c, d_) in enumerate(units):
                                ABn, Xp, Xn = d_["AB%d" % pn], d_["X%d" % pa], d_["X%d" % pn]
                                bk = pget()
                                kb.op("pe", lambda e, bk=bk, Xp=Xp: e.matmul(out=bk[0:64, 0:64], lhsT=I64b, rhs=Xp[:], start=True, stop=False), reads=[Xp.b, I64b_t.b], writes=[bk.b])
                                kb.op("pe", lambda e, bk=bk, Xp=Xp, ABn=ABn: e.matmul(out=bk[0:64, 0:64], lhsT=ABn[:, 1, :], rhs=Xp[:], start=False, stop=True), reads=[Xp.b, ABn.b], writes=[bk.b])
                                if (ui + k) % 2 == 1:
                                    kb.op("act", lambda e, bk=bk, Xn=Xn: e.activation(out=Xn[:], in_=bk[0:64, 0:64], func=AF.Identity), writes=[Xn.b, bk.b])
                                else:
                                    kb.op("dve", lambda e, bk=bk, Xn=Xn: e.tensor_copy(out=Xn[:], in_=bk[0:64, 0:64]), writes=[Xn.b, bk.b])
                        for (dr, c, d_) in units:
                            TT = d_["X1"]
                            bk = pget()
                            kb.op("pe", lambda e, bk=bk, d_=d_, TT=TT: e.matmul(out=bk[:, 0:64], lhsT=d_["kbg"][:], rhs=TT[:], start=True, stop=True), reads=[d_["kbg"].b, TT.b], writes=[bk.b])
                            kb.op("act", lambda e, bk=bk, d_=d_: e.activation(out=d_["nWT"][:], in_=bk[:, 0:64], func=AF.Identity, scale=-1.0), writes=[d_["nWT"].b, bk.b])
                        for u in range(G):
                            for dr in range(2):
                                if dr * G + u >= len(units):
                                    continue
                                dr_, c, d_ = units[dr * G + u]
                                r = dr * 6 + hd
                                S = Sd[dr]
                                t0 = c * 64
                                TT = d_["X1"]
                                bk = pget()
                                kb.op("pe", lambda e, bk=bk, d_=d_, TT=TT: e.matmul(out=bk[0:64, 0:128], lhsT=TT[:], rhs=d_["vb"][:], start=True, stop=False), reads=[d_["vb"].b, TT.b], writes=[bk.b])
                                kb.op("pe", lambda e, bk=bk, d_=d_, S=S: e.matmul(out=bk[0:64, 0:128], lhsT=d_["nWT"][:], rhs=S[:], start=False, stop=True), reads=[d_["nWT"].b, S.b], writes=[bk.b])
                                kb.op("act", lambda e, bk=bk, d_=d_: e.activation(out=d_["VN"][:], in_=bk[0:64, 0:128], func=AF.Identity), writes=[d_["VN"].b, bk.b])
                                bk = pget()
                                kb.op("pe", lambda e, bk=bk, t0=t0, dr=dr, S=S: e.matmul(out=bk[0:64, 0:128], lhsT=RAW[:, dr, t0:t0 + 64], rhs=S[:], start=True, stop=False), reads=[RAW.b, S.b], writes=[bk.b])
                                kb.op("pe", lambda e, bk=bk, d_=d_: e.matmul(out=bk[0:64, 0:128], lhsT=d_["E"][:, 2, :], rhs=d_["VN"][:], start=False, stop=True), reads=[d_["E"].b, d_["VN"].b], writes=[bk.b])
                                kb.op("dve", lambda e, bk=bk, c=c: e.tensor_tensor(out=OACC[:, c, :], in0=bk[0:64, 0:128], in1=OACC[:, c, :], op=ALU.add), writes=[OACC.b, bk.b])
                                bk = pget()
                                kb.op("pe", lambda e, bk=bk, d_=d_: e.matmul(out=bk[:, 0:128], lhsT=d_["kdec"][:], rhs=d_["VN"][:], start=True, stop=True), reads=[d_["kdec"].b, d_["VN"].b], writes=[bk.b])
                                kb.op("dve", lambda e, bk=bk, c=c, r=r, S=S: e.scalar_tensor_tensor(out=S[:], in0=S[:], scalar=EGLB[:, r, c:c + 1], in1=bk[:, 0:128], op0=ALU.mult, op1=ALU.add),
                                      reads=[EGLB.b], writes=[S.b, bk.b])
                    kb.dma("sp", RAW[:, 2, :], projT[ZOFF + hd * 128:ZOFF + (hd + 1) * 128, :], reads=[CV.b], writes=[RAW.b])
                    kb.op("act", lambda e: e.activation(out=RAW[:, 2, :], in_=RAW[:, 2, :], func=AF.Silu), reads=[RAW.b], writes=[RAW.b])
                    for c in range(NCK):
                        f_ = fin[c % 2]; s_ = fst[c % 2]
                        kb.op("act", lambda e, f_=f_, s_=s_, c=c: e.activation(out=f_[:], in_=OACC[:, c, :], func=AF.Square, accum_out=s_[:, 0:1]), reads=[OACC.b], writes=[f_.b, s_.b])
                        kb.op("act", lambda e, s_=s_: e.activation(out=s_[:, 1:2], in_=s_[:, 0:1], func=AF.Sqrt, bias=EPS, scale=1.0 / 128), reads=[s_.b], writes=[s_.b])
                        kb.op("dve", lambda e, s_=s_: e.reciprocal(out=s_[:, 2:3], in_=s_[:, 1:2]), reads=[s_.b], writes=[s_.b])
                        kb.op("dve", lambda e, f_=f_, s_=s_, c=c: e.scalar_tensor_tensor(out=f_[:], in0=OACC[:, c, :], scalar=s_[:, 2:3], in1=NORM[:], op0=ALU.mult, op1=ALU.mult),
                              reads=[OACC.b, s_.b, NORM.b], writes=[f_.b])
                        bk = pget()
                        kb.op("pe", lambda e, bk=bk, f_=f_: e.transpose(out=bk[:, 0:64], in_=f_[:], identity=I64), reads=[f_.b, ident.b], writes=[bk.b])
                        kb.op("dve", lambda e, bk=bk, c=c: e.tensor_tensor(out=OUT[:, c * 64:(c + 1) * 64], in0=bk[:, 0:64], in1=RAW[:, 2, c * 64:(c + 1) * 64], op=ALU.mult),
                              reads=[RAW.b], writes=[OUT.b, bk.b])
                    kb.dma("sp", mixT[512 + hd * 128:512 + (hd + 1) * 128, :], OUT[:], reads=[OUT.b])
                kb.barrier()
            if stage <= 3:
                break
            with ExitStack() as ph:
                MQ, MK, MV, MO, MI, MF = 3608, 3992, 4376, 5144, 5912, 5924
                NCK = T // 64
                SEL = sb(ph, "mSEL", [64, 12, 128]); NSEL = sb(ph, "mNSEL", [64, 12, 128])
                MB = sb(ph, "mMB", [64, 4, 64])
                NORM = sb(ph, "mNORM", [64, 128])
                MP = sb(ph, "mMP", [64, 2])
                kb.dma("sp", SEL[:], sel_d, writes=[SEL.b])
                kb.dma("sp", MB[:], mb_d, writes=[MB.b])
                kb.dma("sp", NORM[:], ml_norm_d[:, l * 128:(l + 1) * 128], writes=[NORM.b])
                kb.dma("sp", MP[:], ml_mp_d[:, l * 2:(l + 1) * 2], writes=[MP.b])
                kb.op("act", lambda e: e.activation(out=NSEL[:], in_=SEL[:], func=AF.Identity, scale=-1.0), reads=[SEL.b], writes=[NSEL.b])
                RI = sb(ph, "mRI", [64, T]); CM = sb(ph, "mCM", [64, T])
                COL = sb(ph, "mCOL", [64, NCK, 4, 12])
                AOB = sb(ph, "mAOB", [128, 2, 12, NCK])
                I64 = ident[0:64, 0:64]

                def seqperm(eng, dst, dstb, src, srcb, npart, p0=0):
                    kb.op(eng, lambda e: (e.tensor_copy(out=dst[p0:p0 + npart, 0:NCTX], in_=src[p0:p0 + npart, 0:NCTX]) if eng != "act" else
                                          e.activation(out=dst[p0:p0 + npart, 0:NCTX], in_=src[p0:p0 + npart, 0:NCTX], func=AF.Identity)), reads=[srcb], writes=[dstb])
                    ov = dst[p0:p0 + npart, NCTX:T].rearrange("p (c r) -> p c r", r=32)
                    iv = src[p0:p0 + npart, NCTX:T].rearrange("p (r c) -> p c r", c=64)
                    kb.op(eng, lambda e: (e.tensor_copy(out=ov, in_=iv) if eng != "act" else e.activation(out=ov, in_=iv, func=AF.Identity)), reads=[srcb], writes=[dstb])

                with ExitStack() as ph2:
                    RM = sb(ph2, "mRM", [64, T]); RMA = sb(ph2, "mRMA", [64, T])
                    RF = sb(ph2, "mRF", [64, T]); BC = sb(ph2, "mBC", [64, T]); X1 = sb(ph2, "mX1", [64, T]); X2 = sb(ph2, "mX2", [64, T])
                    CH = sb(ph2, "mCH", [64, 8, NCK])
                    kb.dma("sp", RM[:], rm_d, writes=[RM.b])
                    kb.dma("sp", RMA[:], rma_d, writes=[RMA.b])
                    kb.op("dve", lambda e: e.memset(X1[:], 0.0), writes=[X1.b])
                    kb.op("dve", lambda e: e.memset(X2[:], 0.0), writes=[X2.b])
                    for dr in range(2):
                        kb.dma("sp", X1[dr * 32:dr * 32 + 6, :], projT[MI + dr * 6:MI + dr * 6 + 6, :], writes=[X1.b])
                        kb.dma("sp", X2[dr * 32:dr * 32 + 6, :], projT[MF + dr * 6:MF + dr * 6 + 6, :], writes=[X2.b])
                    seqperm("dve", RI, RI.b, X1, X1.b, 64)
                    seqperm("dve", RF, RF.b, X2, X2.b, 64)
                    kb.op("act", lambda e: e.activation(out=RI[:], in_=RI[:], func=AF.Identity, bias=MP[:, 0:1]), reads=[RI.b, MP.b], writes=[RI.b])
                    kb.op("dve", lambda e: e.tensor_scalar(out=MP[:, 1:2], in0=MP[:, 1:2], scalar1=-1.0, scalar2=None, op0=ALU.mult), reads=[MP.b], writes=[MP.b])
                    kb.op("act", lambda e: e.activation(out=RF[:], in_=RF[:], func=AF.Exp, bias=MP[:, 1:2], scale=-1.0), reads=[RF.b, MP.b], writes=[RF.b])
                    kb.op("act", lambda e: e.activation(out=RF[:], in_=RF[:], func=AF.Ln, bias=1.0), reads=[RF.b], writes=[RF.b])
                    kb.op("dve", lambda e: e.tensor_scalar(out=RF[:], in0=RF[:], scalar1=-1.0, scalar2=None, op0=ALU.mult), reads=[RF.b], writes=[RF.b])
                    kb.op("dve", lambda e: e.tensor_tensor_scan(out=BC[0:32, :], data0=RM[0:32, :], data1=RF[0:32, :], initial=0.0, op0=ALU.mult, op1=ALU.add), reads=[RM.b, RF.b], writes=[BC.b])
                    kb.op("dve", lambda e: e.tensor_tensor_scan(out=BC[32:64, ::-1], data0=RM[32:64, ::-1], data1=RF[32:64, ::-1], initial=0.0, op0=ALU.mult, op1=ALU.add), reads=[RM.b, RF.b], writes=[BC.b])
                    kb.op("dve", lambda e: e.tensor_tensor(out=RI[:], in0=RI[:], in1=BC[:], op=ALU.subtract), reads=[RI.b, BC.b], writes=[RI.b])
                    kb.op("dve", lambda e: e.tensor_tensor_scan(out=CM[0:32, :], data0=RMA[0:32, :], data1=RI[0:32, :], initial=-1e30, op0=ALU.add, op1=ALU.max), reads=[RMA.b, RI.b], writes=[CM.b])
                    kb.op("dve", lambda e: e.tensor_tensor_scan(out=CM[32:64, ::-1], data0=RMA[32:64, ::-1], data1=RI[32:64, ::-1], initial=-1e30, op0=ALU.add, op1=ALU.max), reads=[RMA.b, RI.b], writes=[CM.b])
                    for dr in range(2):
                        pr = slice(dr * 32, dr * 32 + 32)
                        lastpos = 63 if dr == 0 else 0
                        kb.op("dve", lambda e, pr=pr, lastpos=lastpos: e.tensor_copy(out=CH[pr, 0, :], in_=BC[pr, :].rearrange("p (c s) -> p c s", s=64)[:, :, lastpos]), reads=[BC.b], writes=[CH.b])
                        kb.op("dve", lambda e, pr=pr, lastpos=lastpos: e.tensor_copy(out=CH[pr, 1, :], in_=CM[pr, :].rearrange("p (c s) -> p c s", s=64)[:, :, lastpos]), reads=[CM.b], writes=[CH.b])
                    B_ = [CH.b]
                    kb.op("dve", lambda e: e.tensor_tensor(out=CH[:, 2, :], in0=CH[:, 0, :], in1=CH[:, 1, :], op=ALU.add), reads=B_, writes=B_)
                    kb.op("dve", lambda e: e.tensor_tensor_scan(out=CH[0:32, 3, :], data0=CH[0:32, 0, :], data1=CH[0:32, 2, :], initial=0.0, op0=ALU.add, op1=ALU.max), reads=B_, writes=B_)
                    kb.op("dve", lambda e: e.tensor_tensor_scan(out=CH[32:64, 3, 0:4][:, ::-1], data0=CH[32:64, 0, 0:4][:, ::-1], data1=CH[32:64, 2, 0:4][:, ::-1], initial=0.0, op0=ALU.add, op1=ALU.max), reads=B_, writes=B_)
                    kb.op("dve", lambda e: e.tensor_tensor_scan(out=CH[32:64, 3, 4:NCK][:, ::-1], data0=CH[32:64, 0, 4:NCK][:, ::-1], data1=CH[32:64, 2, 4:NCK][:, ::-1], initial=CH[32:64, 3, 0:1], op0=ALU.add, op1=ALU.max), reads=B_, writes=B_)
                    kb.op("dve", lambda e: e.memset(CH[:, 4, :], 0.0), reads=B_, writes=B_)
                    kb.op("dve", lambda e: e.tensor_copy(out=CH[0:32, 4, 1:NCK], in_=CH[0:32, 3, 0:NCK - 1]), reads=B_, writes=B_)
                    kb.op("dve", lambda e: e.tensor_copy(out=CH[32:64, 4, 0:3], in_=CH[32:64, 3, 1:4]), reads=B_, writes=B_)
                    kb.op("dve", lambda e: e.tensor_copy(out=CH[32:64, 4, 4:NCK - 1], in_=CH[32:64, 3, 5:NCK]), reads=B_, writes=B_)
                    kb.op("dve", lambda e: e.tensor_copy(out=CH[32:64, 4, NCK - 1:NCK], in_=CH[32:64, 3, 0:1]), reads=B_, writes=B_)
                    kb.op("dve", lambda e: e.tensor_tensor(out=CH[:, 5, :], in0=CH[:, 0, :], in1=CH[:, 4, :], op=ALU.add), reads=B_, writes=B_)
                    kb.op("dve", lambda e: e.tensor_tensor(out=CH[:, 5, :], in0=CH[:, 5, :], in1=CH[:, 3, :], op=ALU.subtract), reads=B_, writes=B_)
                    kb.op("dve", lambda e: e.tensor_tensor(out=CH[:, 6, :], in0=CH[:, 2, :], in1=CH[:, 3, :], op=ALU.subtract), reads=B_, writes=B_)
                    kb.op("act", lambda e: e.activation(out=CH[:, 5:7, :], in_=CH[:, 5:7, :], func=AF.Exp), reads=B_, writes=B_)
                    for c in range(NCK):
                        kb.op("dve", lambda e, c=c: e.tensor_scalar(out=RF[:, c * 64:(c + 1) * 64], in0=CM[:, c * 64:(c + 1) * 64], scalar1=-1.0, scalar2=CH[:, 4, c:c + 1],
                                                                     op0=ALU.mult, op1=ALU.add), reads=[CM.b, CH.b], writes=[RF.b])
                        kb.op("dve", lambda e, c=c: e.tensor_scalar(out=X2[:, c * 64:(c + 1) * 64], in0=RI[:, c * 64:(c + 1) * 64], scalar1=CH[:, 1, c:c + 1], scalar2=None,
                                                                     op0=ALU.subtract), reads=[RI.b, CH.b], writes=[X2.b])
                    kb.op("act", lambda e: e.activation(out=X2[:], in_=X2[:], func=AF.Exp), reads=[X2.b], writes=[X2.b])
                    kb.op("dve", lambda e: e.tensor_tensor(out=BC[:], in0=BC[:], in1=CM[:], op=ALU.add), reads=[BC.b, CM.b], writes=[BC.b])
                    kb.op("dve", lambda e: e.scalar_tensor_tensor(out=BC[:], in0=RF[:], scalar=0.0, in1=BC[:], op0=ALU.max, op1=ALU.add), reads=[RF.b, BC.b], writes=[BC.b])
                    kb.op("act", lambda e: e.activation(out=BC[:], in_=BC[:], func=AF.Exp, scale=-1.0), reads=[BC.b], writes=[BC.b])
                    kb.op("dve", lambda e: e.tensor_scalar(out=X1[:], in0=RF[:], scalar1=0.0, scalar2=None, op0=ALU.min), reads=[RF.b], writes=[X1.b])
                    kb.op("act", lambda e: e.activation(out=X1[:], in_=X1[:], func=AF.Exp), reads=[X1.b], writes=[X1.b])
                    kb.op("dve", lambda e: e.tensor_scalar(out=RF[:], in0=RF[:], scalar1=-1.0, scalar2=0.0, op0=ALU.mult, op1=ALU.min), reads=[RF.b], writes=[RF.b])
                    kb.op("act", lambda e: e.activation(out=RF[:], in_=RF[:], func=AF.Exp), reads=[RF.b], writes=[RF.b])
                    for c in range(NCK):
                        pcol = banks[c % 4]
                        for qi, row in enumerate((X1, RF, BC, X2)):
                            kb.op("pe", lambda e, row=row, qi=qi, pcol=pcol, c=c: e.matmul(out=pcol[0:64, qi * 12:(qi + 1) * 12], lhsT=row[:, c * 64:(c + 1) * 64],
                                                                                       rhs=SEL[:, :, 0], start=True, stop=True), reads=[row.b, SEL.b], writes=[pcol.b])
                        kb.op("act", lambda e, pcol=pcol, c=c: e.activation(out=COL[:, c, :, :].rearrange("p a b -> p (a b)"), in_=pcol[0:64, 0:48], func=AF.Identity),
                              writes=[COL.b, pcol.b])
                    for qi in range(2):
                        for r in range(12):
                            pcol = banks[4 + (qi * 12 + r) % 2]
                            kb.op("pe", lambda e, r=r, pcol=pcol, qi=qi: e.matmul(out=pcol[:, 0:NCK], lhsT=SEL[:, r, :], rhs=CH[:, 5 + qi, :], start=True, stop=True), reads=[SEL.b, CH.b], writes=[pcol.b])
                            kb.op("act", lambda e, r=r, pcol=pcol, qi=qi: e.activation(out=AOB[:, qi, r, :], in_=pcol[:, 0:NCK], func=AF.Identity), writes=[AOB.b, pcol.b])
                    kb.barrier()
                psn = [0]

                def pget():
                    bk = banks[psn[0] % 8]
                    psn[0] += 1
                    return bk
                RAW = sb(ph, "mRAW", [128, T])
                QT = sb(ph, "mQT", [64, T]); KT = sb(ph, "mKT", [64, T]); VT = sb(ph, "mVT", [128, T])
                VA = sb(ph, "mVA", [64, NCK, 132])
                HACC = sb(ph, "mHACC", [64, NCK, 128]); OUT = sb(ph, "mOUT", [128, T], BF16)
                CA = sb(ph, "mCA", [64, 132])
                G = 4
                U = []
                for u in range(G):
                    d_ = {}
                    for nm, shp in (("E", [64, 64]), ("wk", [64, 64]), ("tmp", [64, 132]), ("comb", [64, 132]), ("sc", [64, 4])):
                        d_[nm] = sb(ph, "mu%d%s" % (u, nm), shp)
                    U.append(d_)
                fin = [sb(ph, "mfin%d" % i, [64, 128]) for i in range(2)]
                fst = [sb(ph, "mfst%d" % i, [64, 4]) for i in range(2)]
                kb.op("dve", lambda e: e.memset(VA[:], 1.0), writes=[VA.b])
                for hd in range(6):
                    kb.dma("sp", RAW[0:64, :], projT[MQ + hd * 64:MQ + (hd + 1) * 64, :], writes=[RAW.b])
                    seqperm("act", QT, QT.b, RAW, RAW.b, 64)
                    kb.op("act", lambda e: e.activation(out=QT[:], in_=QT[:], func=AF.Identity, scale=0.125), reads=[QT.b], writes=[QT.b])
                    kb.dma("sp", RAW[0:64, :], projT[MK + hd * 64:MK + (hd + 1) * 64, :], reads=[QT.b], writes=[RAW.b])
                    seqperm("dve", KT, KT.b, RAW, RAW.b, 64)
                    kb.dma("sp", RAW[:], projT[MV + hd * 128:MV + (hd + 1) * 128, :], reads=[KT.b], writes=[RAW.b])
                    seqperm("act", VT, VT.b, RAW, RAW.b, 128)
                    for c in range(NCK):
                        bk = pget()
                        kb.op("pe", lambda e, bk=bk, c=c: e.transpose(out=bk[0:64, 0:128], in_=VT[:, c * 64:(c + 1) * 64], identity=ident[:]), reads=[VT.b, ident.b], writes=[bk.b])
                        if c % 2 == 0:
                            kb.op("act", lambda e, bk=bk, c=c: e.activation(out=VA[:, c, 0:128], in_=bk[0:64, 0:128], func=AF.Identity), writes=[VA.b, bk.b])
                        else:
                            kb.op("dve", lambda e, bk=bk, c=c: e.tensor_copy(out=VA[:, c, 0:128], in_=bk[0:64, 0:128]), writes=[VA.b, bk.b])
                    for dr in range(2):
                        r = dr * 6 + hd
                        kb.op("dve", lambda e: e.memset(CA[:], 0.0), writes=[CA.b])
                        order = list(range(NCK)) if dr == 0 else list(range(3, -1, -1)) + list(range(NCK - 1, 3, -1))
                        m_it = 3 if dr == 0 else 2
                        for g0 in range(0, NCK, G):
                            grp = order[g0:g0 + G]
                            for u, c in enumerate(grp):
                                d_ = U[u]
                                ch = slice(c * 64, (c + 1) * 64)
                                be = pget()
                                kb.op("pe", lambda e, be=be, ch=ch: e.matmul(out=be[0:64, 0:64], lhsT=RI[:, ch], rhs=SEL[:, r, 0:64], start=True, stop=False), reads=[RI.b, SEL.b], writes=[be.b])
                                kb.op("pe", lambda e, be=be, ch=ch: e.matmul(out=be[0:64, 0:64], lhsT=NSEL[:, r, 0:64], rhs=CM[:, ch], start=False, stop=False), reads=[CM.b, NSEL.b], writes=[be.b])
                                kb.op("pe", lambda e, be=be: e.matmul(out=be[0:64, 0:64], lhsT=I64, rhs=MB[:, m_it, :], start=False, stop=True), reads=[MB.b, ident.b], writes=[be.b])
                                kb.op("act", lambda e, be=be, d_=d_: e.activation(out=d_["E"][:], in_=be[0:64, 0:64], func=AF.Exp), writes=[d_["E"].b, be.b])
                                bp = pget()
                                kb.op("pe", lambda e, bp=bp, ch=ch: e.matmul(out=bp[0:64, 0:64], lhsT=KT[:, ch], rhs=QT[:, ch], start=True, stop=True), reads=[KT.b, QT.b], writes=[bp.b])
                                kb.op("pe", lambda e, bp=bp, ch=ch: e.transpose(out=bp[0:64, 64:128], in_=KT[:, ch], identity=I64), reads=[KT.b, ident.b], writes=[bp.b])
                                kb.op("dve", lambda e, bp=bp, d_=d_: e.tensor_tensor(out=d_["E"][:], in0=bp[0:64, 0:64], in1=d_["E"][:], op=ALU.mult), reads=[d_["E"].b], writes=[d_["E"].b, bp.b])
                                kb.op("dve", lambda e, bp=bp, d_=d_, c=c: e.tensor_scalar(out=d_["wk"][:], in0=bp[0:64, 64:128], scalar1=COL[:, c, 3, r:r + 1], scalar2=None, op0=ALU.mult),
                                      reads=[COL.b], writes=[d_["wk"].b, bp.b])
                            for u, c in enumerate(grp):
                                d_ = U[u]
                                ch = slice(c * 64, (c + 1) * 64)
                                b1 = pget()
                                kb.op("pe", lambda e, b1=b1, ch=ch: e.matmul(out=b1[0:64, 0:129], lhsT=QT[:, ch], rhs=CA[:, 0:129], start=True, stop=True), reads=[QT.b, CA.b], writes=[b1.b])
                                kb.op("act", lambda e, b1=b1, d_=d_, c=c: e.activation(out=d_["tmp"][:, 0:129], in_=b1[0:64, 0:129], func=AF.Identity, scale=COL[:, c, 0, r:r + 1]),
                                      reads=[COL.b], writes=[d_["tmp"].b, b1.b])
                                b2 = pget()
                                kb.op("pe", lambda e, b2=b2, d_=d_, c=c: e.matmul(out=b2[0:64, 0:129], lhsT=d_["E"][:], rhs=VA[:, c, 0:129], start=True, stop=True), reads=[d_["E"].b, VA.b], writes=[b2.b])
                                kb.op("dve", lambda e, b2=b2, d_=d_, c=c: e.scalar_tensor_tensor(out=d_["comb"][:, 0:129], in0=b2[0:64, 0:129], scalar=COL[:, c, 1, r:r + 1], in1=d_["tmp"][:, 0:129],
                                                                                              op0=ALU.mult, op1=ALU.add), reads=[COL.b, d_["tmp"].b], writes=[d_["comb"].b, b2.b])
                                kb.op("act", lambda e, d_=d_: e.activation(out=d_["sc"][:, 2:3], in_=d_["comb"][:, 128:129], func=AF.Abs), reads=[d_["comb"].b], writes=[d_["sc"].b])
                                kb.op("dve", lambda e, d_=d_, c=c: e.tensor_scalar(out=d_["sc"][:, 0:1], in0=d_["sc"][:, 2:3], scalar1=COL[:, c, 2, r:r + 1], scalar2=None, op0=ALU.max),
                                      reads=[COL.b, d_["sc"].b], writes=[d_["sc"].b])
                                kb.op("dve", lambda e, d_=d_: e.reciprocal(out=d_["sc"][:, 1:2], in_=d_["sc"][:, 0:1]), reads=[d_["sc"].b], writes=[d_["sc"].b])
                                if dr == 0:
                                    kb.op("dve", lambda e, d_=d_, c=c: e.tensor_scalar(out=HACC[:, c, :], in0=d_["comb"][:, 0:128], scalar1=d_["sc"][:, 1:2], scalar2=None, op0=ALU.mult),
                                          reads=[d_["comb"].b, d_["sc"].b], writes=[HACC.b])
                                else:
                                    kb.op("dve", lambda e, d_=d_, c=c: e.scalar_tensor_tensor(out=HACC[:, c, :], in0=d_["comb"][:, 0:128], scalar=d_["sc"][:, 1:2], in1=HACC[:, c, :], op0=ALU.mult, op1=ALU.add),
                                          reads=[d_["comb"].b, d_["sc"].b, HACC.b], writes=[HACC.b])
                                b3 = pget()
                                kb.op("pe", lambda e, b3=b3, d_=d_, c=c: e.matmul(out=b3[0:64, 0:129], lhsT=d_["wk"][:], rhs=VA[:, c, 0:129], start=True, stop=True), reads=[d_["wk"].b, VA.b], writes=[b3.b])
                                kb.op("act", lambda e, c=c: e.activation(out=CA[:, 0:129], in_=CA[:, 0:129], func=AF.Identity, scale=AOB[0:64, 0, r, c:c + 1]), reads=[AOB.b, CA.b], writes=[CA.b])
                                kb.op("dve", lambda e, b3=b3, c=c: e.scalar_tensor_tensor(out=CA[:, 0:129], in0=b3[0:64, 0:129], scalar=AOB[0:64, 1, r, c:c + 1], in1=CA[:, 0:129], op0=ALU.mult, op1=ALU.add),
                                      reads=[AOB.b, CA.b], writes=[CA.b, b3.b])
                    kb.dma("sp", RAW[:], projT[MO + hd * 128:MO + (hd + 1) * 128, :], reads=[VT.b], writes=[RAW.b])
                    seqperm("act", VT, VT.b, RAW, RAW.b, 128)
                    kb.op("act", lambda e: e.activation(out=VT[:], in_=VT[:], func=AF.Sigmoid), reads=[VT.b], writes=[VT.b])
                    for c in range(NCK):
                        f_ = fin[c % 2]; s_ = fst[c % 2]
                        kb.op("act", lambda e, f_=f_, s_=s_, c=c: e.activation(out=f_[:], in_=HACC[:, c, :], func=AF.Square, accum_out=s_[:, 0:1]), reads=[HACC.b], writes=[f_.b, s_.b])
                        kb.op("act", lambda e, s_=s_: e.activation(out=s_[:, 1:2], in_=s_[:, 0:1], func=AF.Sqrt, bias=EPS, scale=1.0 / 128), reads=[s_.b], writes=[s_.b])
                        kb.op("dve", lambda e, s_=s_: e.reciprocal(out=s_[:, 2:3], in_=s_[:, 1:2]), reads=[s_.b], writes=[s_.b])
                        kb.op("dve", lambda e, f_=f_, s_=s_, c=c: e.scalar_tensor_tensor(out=f_[:], in0=HACC[:, c, :], scalar=s_[:, 2:3], in1=NORM[:], op0=ALU.mult, op1=ALU.mult),
                              reads=[HACC.b, s_.b, NORM.b], writes=[f_.b])
                        bk = pget()
                        kb.op("pe", lambda e, bk=bk, f_=f_: e.transpose(out=bk[:, 0:64], in_=f_[:], identity=I64), reads=[f_.b, ident.b], writes=[bk.b])
                        kb.op("dve", lambda e, bk=bk, c=c: e.tensor_tensor(out=RAW[:, c * 64:(c + 1) * 64], in0=bk[:, 0:64], in1=VT[:, c * 64:(c + 1) * 64], op=ALU.mult),
                              reads=[VT.b], writes=[RAW.b, bk.b])
                    kb.op("act", lambda e: e.activation(out=OUT[:, 0:NCTX], in_=RAW[:, 0:NCTX], func=AF.Identity), reads=[RAW.b], writes=[OUT.b])
                    kb.op("act", lambda e: e.activation(out=OUT[:, NCTX:T].rearrange("p (r c) -> p c r", c=64), in_=RAW[:, NCTX:T].rearrange("p (c r) -> p c r", r=32), func=AF.Identity),
                          reads=[RAW.b], writes=[OUT.b])
                    kb.dma("sp", mixT[1280 + hd * 128:1280 + (hd + 1) * 128, :], OUT[:], reads=[OUT.b])
                kb.barrier()
            if stage <= 4:
                break
            with ExitStack() as ph:
                last = (l == L - 1)
                mixb = sb(ph, "mixb", [128, KC, 512], BF16)
                yTb = sb(ph, "yTb", [128, KC, 512])
                h2T = sb(ph, "h2T", [128, KC, 512], BF16)
                hidT = sb(ph, "hidT", [128, 44, 512], BF16)
                xt = [sb(ph, "xt%d" % i, [128, D]) for i in range(2)]
                GG = [[sb(ph, "GG%d%d" % (a, w_), [128, D]) for w_ in range(2)] for a in range(2)]
                wk = [sb(ph, "wk%d" % i, [128, KC, 128], BF16) for i in range(4)]
                wd = [sb(ph, "wd%d" % i, [128, 44, 128], BF16) for i in range(2)]
                sgt = [sb(ph, "sgt%d" % i, [128, 512]) for i in range(2)]
                junk = sb(ph, "junk2", [128, 512], BF16)
                stt_ = [sb(ph, "st2%d" % i, [128, 8]) for i in range(2)]
                bc = sb(ph, "bc", [128, 128])
                for a, (mi, gi) in enumerate(((2, 1), (5, 3))):
                    for w_ in range(2):
                        for j in range(KC):
                            kb.op("dve", lambda e, mi=mi, gi=gi, w_=w_, j=j: e.tensor_tensor(
                                out=bc[:, 0:1], in0=modsT[:, mi * KC + j, w_:w_ + 1], in1=gT[:, (gi * L + l) * KC + j:(gi * L + l) * KC + j + 1],
                                op=ALU.mult), reads=[modsT.b, gT.b], writes=[bc.b])
                            kb.op("dve", lambda e: e.tensor_copy(out=bc[:, 1:128], in_=bc[:, 0:1].to_broadcast([128, 127])), reads=[bc.b], writes=[bc.b])
                            pb = banks[j % 4]
                            kb.op("pe", lambda e, pb=pb: e.transpose(out=pb[:, 0:128], in_=bc[:], identity=ident[:]), reads=[bc.b, ident.b], writes=[pb.b])
                            kb.op("act", lambda e, pb=pb, a=a, w_=w_, j=j: e.activation(out=GG[a][w_][:, j * 128:(j + 1) * 128], in_=pb[:, 0:128], func=AF.Identity),
                                  reads=[pb.b], writes=[GG[a][w_].b])
                mixv = mixT.rearrange("(k p) t -> p k t", p=128)
                tstart = NCTX if last else 0
                nwk = [0]
                nx = [0]

                def post_res(tglob, tloc, a, dst):
                    w_ = 1 if tglob < 2 else 0
                    x_ = xt[nx[0] % 2]
                    s_ = stt_[nx[0] % 2]
                    nx[0] += 1
                    kb.dma("sp", x_[:], xres[tglob * 128:(tglob + 1) * 128, :], writes=[x_.b])
                    for jb in range(4):
                        pb = banks[jb]
                        for jj in range(4):
                            j = jb * 4 + jj
                            kb.op("pe", lambda e, pb=pb, jj=jj, j=j: e.transpose(out=pb[:, jj * 128:(jj + 1) * 128], in_=yTb[:, j, tloc * 128:(tloc + 1) * 128],
                                                                              identity=ident[:]), reads=[yTb.b, ident.b], writes=[pb.b])
                        kb.op("act", lambda e, pb=pb, jb=jb, s_=s_: e.activation(out=junk[:], in_=pb[:], func=AF.Square, accum_out=s_[:, jb:jb + 1]),
                              reads=[pb.b], writes=[junk.b, s_.b])
                    kb.op("dve", lambda e, s_=s_: e.tensor_reduce(out=s_[:, 4:5], in_=s_[:, 0:4], axis=AX.X, op=ALU.add), reads=[s_.b], writes=[s_.b])
                    kb.op("act", lambda e, s_=s_: e.activation(out=s_[:, 5:6], in_=s_[:, 4:5], func=AF.Sqrt, bias=EPS, scale=1.0 / D), reads=[s_.b], writes=[s_.b])
                    kb.op("dve", lambda e, s_=s_: e.reciprocal(out=s_[:, 6:7], in_=s_[:, 5:6]), reads=[s_.b], writes=[s_.b])
                    for jb in range(4):
                        pb = banks[jb]
                        sg_ = sgt[jb % 2]
                        kb.op("dve", lambda e, pb=pb, sg_=sg_, jb=jb, s_=s_: e.scalar_tensor_tensor(
                            out=sg_[:], in0=pb[:], scalar=s_[:, 6:7], in1=GG[a][w_][:, jb * 512:(jb + 1) * 512], op0=ALU.mult, op1=ALU.mult),
                            reads=[pb.b, s_.b, GG[a][w_].b], writes=[sg_.b])
                        kb.op("dve", lambda e, sg_=sg_, jb=jb, x_=x_: e.tensor_tensor(out=x_[:, jb * 512:(jb + 1) * 512], in0=x_[:, jb * 512:(jb + 1) * 512], in1=sg_[:],
                                                                                op=ALU.add), reads=[sg_.b, x_.b], writes=[x_.b])
                    kb.dma("sp", dst, x_[:], reads=[x_.b])
                    return x_, s_, w_

                def dense(src, nk, wview, j0, nj, wbufs, t_nt, consume):
                    for j in range(j0, j0 + nj):
                        w = wbufs[nwk[0] % len(wbufs)]
                        nwk[0] += 1
                        src_ap, src_b = wview[j]
                        kb.dma("sp" if nwk[0] % 2 == 0 else "act", w[:, :nk, :], src_ap, reads=[src_b], writes=[w.b])
                        pj = banks[4 + nwk[0] % 2]
                        for k in range(nk):
                            kb.op("pe", lambda e, pj=pj, w=w, k=k: e.matmul(out=pj[:, :t_nt], lhsT=w[:, k, :], rhs=src[:, k, :t_nt], start=(k == 0), stop=(k == nk - 1)),
                                  reads=[w.b, src.b], writes=[pj.b])
                        consume(j, pj)

                t0 = tstart
                while t0 < T:
                    nt = min(512, T - t0)
                    ntl = nt // 128
                    kb.dma("sp", mixb[:, :, :nt], mixv[:, :, t0:t0 + nt], writes=[mixb.b])

                    def cons_y(j, pj):
                        if j % 2 == 0:
                            kb.op("act", lambda e: e.activation(out=yTb[:, j, :nt], in_=pj[:, :nt], func=AF.Identity), reads=[pj.b], writes=[yTb.b])
                        else:
                            kb.op("dve", lambda e: e.tensor_copy(out=yTb[:, j, :nt], in_=pj[:, :nt]), reads=[pj.b], writes=[yTb.b])
                    dense(mixb, KC, pre["wo"], 0, KC, wk, nt, cons_y)
                    for tl in range(ntl):
                        tg = t0 // 128 + tl
                        x_, s_, w_ = post_res(tg, tl, 0, xres[tg * 128:(tg + 1) * 128, :])
                        kb.op("act", lambda e, x_=x_, s_=s_: e.activation(out=junk[:], in_=x_[:, 0:512], func=AF.Square, accum_out=s_[:, 0:1]), reads=[x_.b], writes=[junk.b, s_.b])
                        for q in range(1, 4):
                            kb.op("act", lambda e, x_=x_, s_=s_, q=q: e.activation(out=junk[:], in_=x_[:, q * 512:(q + 1) * 512], func=AF.Square, accum_out=s_[:, q:q + 1]),
                                  reads=[x_.b], writes=[junk.b, s_.b])
                        kb.op("dve", lambda e, s_=s_: e.tensor_reduce(out=s_[:, 4:5], in_=s_[:, 0:4], axis=AX.X, op=ALU.add), reads=[s_.b], writes=[s_.b])
                        kb.op("act", lambda e, s_=s_: e.activation(out=s_[:, 5:6], in_=s_[:, 4:5], func=AF.Sqrt, bias=EPS, scale=1.0 / D), reads=[s_.b], writes=[s_.b])
                        kb.op("dve", lambda e, s_=s_: e.reciprocal(out=s_[:, 6:7], in_=s_[:, 5:6]), reads=[s_.b], writes=[s_.b])
                        kb.op("dve", lambda e, x_=x_, s_=s_: e.tensor_scalar(out=x_[:], in0=x_[:], scalar1=s_[:, 6:7], scalar2=None, op0=ALU.mult), reads=[x_.b, s_.b], writes=[x_.b])
                        for jb in range(4):
                            pt = banks[jb]
                            for jj in range(4):
                                j = jb * 4 + jj
                                kb.op("pe", lambda e, pt=pt, x_=x_, jj=jj, j=j: e.transpose(out=pt[:, jj * 128:(jj + 1) * 128], in_=x_[:, j * 128:(j + 1) * 128], identity=ident[:]),
                                      reads=[x_.b, ident.b], writes=[pt.b])
                            for jj in range(4):
                                j = jb * 4 + jj
                                kb.op("act", lambda e, pt=pt, jj=jj, j=j, tl=tl, w_=w_: e.activation(
                                    out=h2T[:, j, tl * 128:(tl + 1) * 128], in_=pt[:, jj * 128:(jj + 1) * 128], func=AF.Identity,
                                    bias=modsT[:, 3 * KC + j, w_:w_ + 1], scale=gsf[:, j, w_:w_ + 1]), reads=[pt.b, modsT.b, gsf.b], writes=[h2T.b])
                    for hc in range(44):
                        got = {}

                        def cons_g(j, pj):
                            got["g"] = pj
                        dense(h2T, KC, pre["wg"], hc, 1, wk, nt, cons_g)
                        pg = got["g"]
                        sg_ = sgt[hc % 2]
                        kb.op("act", lambda e, pg=pg, sg_=sg_: e.activation(out=sg_[:, :nt], in_=pg[:, :nt], func=AF.Silu), reads=[pg.b], writes=[sg_.b])

                        def cons_u(j, pj):
                            kb.op("dve", lambda e: e.tensor_tensor(out=hidT[:, hc, :nt], in0=pj[:, :nt], in1=sg_[:, :nt], op=ALU.mult), reads=[pj.b, sg_.b], writes=[hidT.b])
                        dense(h2T, KC, pre["wu"], hc, 1, wk, nt, cons_u)
                    dense(hidT, 44, pre["wd"], 0, KC, wd, nt, cons_y)
                    for tl in range(ntl):
                        tg = t0 // 128 + tl
                        dst = out_d[(tg - 2) * 128:(tg - 1) * 128, :] if last else xres[tg * 128:(tg + 1) * 128, :]
                        post_res(tg, tl, 1, dst)
                    t0 += nt
                kb.barrier()
            if stage == 5:
                kb.dma("sp", xres_o, xres)
                break
        kb.barrier()
    return nc


def kernel(**inp):
    inp = {k: np.asarray(v) for k, v in inp.items()}
    nc = build(99)
    base = host_prep(inp, 0)
    in_maps = []
    for core in range(8):
        b = core % 4
        m = dict(base)
        if b != 0:
            pb = host_prep_batch(inp, b)
            m.update(pb)
        in_maps.append(m)
    res = run_bass_kernel_spmd(nc, in_maps, core_ids=list(range(8)))
    out = np.stack([np.asarray(res.results[b]["out"]) for b in range(4)], 0).astype(np.float32)
    return out
```

```python
import numpy as np
from contextlib import ExitStack
import concourse.bass as bass
import concourse.mybir as mybir
from concourse.bass_utils import run_bass_kernel_spmd

F32 = mybir.dt.float32
BF16 = mybir.dt.bfloat16
ALU = mybir.AluOpType
AF = mybir.ActivationFunctionType
AX = mybir.AxisListType

D = 2048
T = 2304
NCTX = 256
NLAT = 2048
L = 2
KC = 16
IN_COLS = 5936
FFN = 5632
EPS = 1e-6
NEG = -30000.0
SEM_ROT = 30000
NSLOT = 6
LC = 128


class Ev:
    __slots__ = ("sem", "val")

    def __init__(self, sem, val):
        self.sem = sem
        self.val = val


class Buf:
    __slots__ = ("w", "r", "name")

    def __init__(self, name=""):
        self.w = None
        self.r = {}
        self.name = name


class Eng:
    def __init__(self, kb, name, h):
        self.kb = kb
        self.name = name
        self.h = h
        self.sem = kb.newsem("e_" + name)
        self.count = 0
        self.seen = {}
        self.n = 0
        self.pending = 0


class Slot:
    def __init__(self, sem):
        self.sem = sem
        self.uses = 0


class KB:
    def __init__(self, nc, es):
        self.nc = nc
        self.es = es
        self.nsem = 0
        self.E = {}
        for name, h in (("pe", nc.tensor), ("act", nc.scalar), ("dve", nc.vector), ("pool", nc.gpsimd), ("sp", nc.sync)):
            self.E[name] = Eng(self, name, h)
        self.slots = {}
        self.rr = {}
        for q in ("sp", "pool", "act"):
            self.slots[q] = [Slot(self.newsem("d_%s%d" % (q, i))) for i in range(NSLOT)]
            self.rr[q] = 0

    def newsem(self, name):
        self.nsem += 1
        return self.es.enter_context(self.nc.semaphore("%s_%d" % (name, self.nsem)))

    def _wait(self, eng, ev):
        k = id(ev.sem)
        if eng.seen.get(k, 0) < ev.val:
            eng.h.wait_ge(ev.sem, ev.val)
            eng.seen[k] = ev.val

    def _deps(self, eng, reads, writes):
        need = {}

        def add(ev):
            k = id(ev.sem)
            if k not in need or need[k].val < ev.val:
                need[k] = ev

        for b in reads:
            if b.w is not None:
                add(b.w)
        for b in writes:
            if b.w is not None:
                add(b.w)
            for ev in b.r.values():
                add(ev)
        for ev in need.values():
            if eng.name == "pe" and ev.sem is eng.sem:
                continue
            self._wait(eng, ev)

    def _post(self, ev, reads, writes):
        k = id(ev.sem)
        for b in reads:
            b.r[k] = ev
        for b in writes:
            b.w = ev
            b.r = {}

    def op(self, e, fn, reads=(), writes=(), sig=True):
        eng = self.E[e]
        self._deps(eng, reads, writes)
        if eng.count >= SEM_ROT and eng.pending == 0:
            eng.sem = self.newsem("e_" + eng.name)
            eng.count = 0
        inst = fn(eng.h)
        eng.n += 1
        if sig:
            eng.count += 1
            eng.pending = 0
            inst.then_inc(eng.sem, 1)
            ev = Ev(eng.sem, eng.count)
        else:
            assert e == "pe"
            eng.pending += 1
            ev = Ev(eng.sem, eng.count + 1)
        self._post(ev, reads, writes)
        return ev

    def dma(self, q, out, in_, reads=(), writes=(), **kw):
        eng = self.E[q]
        self._deps(eng, reads, writes)
        sl = self.slots[q][self.rr[q] % NSLOT]
        self.rr[q] += 1
        if sl.uses * 16 >= SEM_ROT:
            self._wait(eng, Ev(sl.sem, 16 * sl.uses))
            sl.sem = self.newsem("d_" + q)
            sl.uses = 0
        if sl.uses > 0:
            self._wait(eng, Ev(sl.sem, 16 * sl.uses))
        inst = eng.h.dma_start(out=out, in_=in_, **kw)
        sl.uses += 1
        inst.then_inc(sl.sem, 16)
        ev = Ev(sl.sem, 16 * sl.uses)
        self._post(ev, reads, writes)
        return ev

    def barrier(self, engines=("pe", "act", "dve", "pool", "sp")):
        evs = []
        for e in self.E.values():
            assert e.pending == 0
            if e.count > 0:
                evs.append(Ev(e.sem, e.count))
        for q in self.slots:
            for sl in self.slots[q]:
                if sl.uses > 0:
                    evs.append(Ev(sl.sem, 16 * sl.uses))
        for en in engines:
            eng = self.E[en]
            for ev in evs:
                self._wait(eng, ev)


class Tl:
    def __init__(self, t, name=""):
        self.t = t
        self.b = Buf(name)

    def __getitem__(self, idx):
        return self.t[idx]


def host_prep_batch(inp, b):
    f = np.float32
    m = {}
    m["xin"] = np.ascontiguousarray(np.concatenate([inp["ctx"][b], inp["x"][b]], axis=0).astype(f))
    cv = np.stack([inp["c"][b].reshape(KC, 128).T, inp["c_ctx"].reshape(KC, 128).T], axis=-1)
    m["cv"] = np.ascontiguousarray(cv.astype(f))
    return m


def host_prep(inp, b):
    f = np.float32
    m = host_prep_batch(inp, b)
    m["ada_w"] = inp["ada_w"]
    m["ada_bT"] = np.ascontiguousarray(inp["ada_b"].reshape(L, 96, 128).transpose(2, 0, 1).reshape(128, L * 96).astype(f))
    g = np.stack([inp["norm_mix_pre"], inp["norm_mix_post"], inp["norm_ffn_pre"], inp["norm_ffn_post"]], 0)
    m["gT"] = np.ascontiguousarray(g.reshape(4, L, KC, 128).transpose(3, 0, 1, 2).reshape(128, 4 * L * KC).astype(f))
    m["w_in"] = inp["w_in"]
    for k_ in ("w_out", "ffn_w_gate", "ffn_w_up", "ffn_w_down"):
        m[k_] = inp[k_]
    m["ident"] = np.eye(128, dtype=f)
    def st_major(a):
        return np.ascontiguousarray(a.reshape(L, 2, 16, 128).transpose(3, 0, 1, 2).reshape(128, L * 2 * 16).astype(f))
    m["s5_lre"] = st_major(inp["s5_lam_re"].reshape(L, 2, 2048))
    m["s5_lim"] = st_major(inp["s5_lam_im"].reshape(L, 2, 2048))
    m["s5_ldt"] = st_major(np.repeat(inp["s5_log_dt"], 64, axis=-1))
    Bb = np.zeros((128, L, 2, 2, 16, 128), f)
    Cb = np.zeros((128, L, 2, 2, 16, 128), f)
    for ci, (bn, cn) in enumerate((("s5_b_re", "s5_c_re"), ("s5_b_im", "s5_c_im"))):
        bsrc = inp[bn]
        csrc = inp[cn]
        for g in range(32):
            st = g // 2
            r0 = (g % 8) * 16
            c0 = (g % 2) * 64
            Bb[r0:r0 + 16, :, :, ci, st, c0:c0 + 64] = bsrc[:, :, g].transpose(3, 0, 1, 2)
            Cb[c0:c0 + 64, :, :, ci, st, r0:r0 + 16] = csrc[:, :, g].transpose(3, 0, 1, 2)
    m["s5_Bb"] = np.ascontiguousarray(Bb.reshape(128, L * 2 * 2 * 16, 128))
    m["s5_Cb"] = np.ascontiguousarray(Cb.reshape(128, L * 2 * 2 * 16, 128))
    m["s5_dT"] = np.ascontiguousarray(inp["s5_d"].reshape(L, 4, 128).transpose(2, 0, 1).reshape(128, L * 4).astype(f))
    m["s5_gbT"] = np.ascontiguousarray(inp["s5_glu_b"].reshape(L, 4, 128).transpose(2, 0, 1).reshape(128, L * 4).astype(f))
    m["s5_glu_w"] = inp["s5_glu_w"]
    tt = np.arange(T)
    rm = np.ones((64, T), f)
    rm[0:32, tt % 64 == 0] = 0.0
    rm[32:64, tt % 64 == 63] = 0.0
    m["rm"] = rm
    sel = np.zeros((64, 12, 128), f)
    for r_ in range(12):
        sel[(r_ // 6) * 32 + r_ % 6, r_, :] = 1.0
    m["sel"] = sel
    a_ = np.arange(64)[:, None]; b_ = np.arange(64)[None, :]
    mb = np.stack([np.where(b_ < a_, 0.0, NEG), np.where(b_ > a_, 0.0, NEG), np.where(b_ <= a_, 0.0, NEG), np.where(b_ >= a_, 0.0, NEG)], 1).astype(f)
    m["mb"] = np.ascontiguousarray(mb)
    m["gdn_normr"] = np.ascontiguousarray(np.tile(inp["gdn_norm"].reshape(1, L * 128), (64, 1)).astype(f))
    gp = np.zeros((64, L, 2), f)
    for dr_ in range(2):
        gp[dr_ * 32:dr_ * 32 + 6, :, 0] = inp["gdn_a_log"][:, dr_, :].T
        gp[dr_ * 32:dr_ * 32 + 6, :, 1] = inp["gdn_dt_bias"][:, dr_, :].T
    m["gdn_gp"] = np.ascontiguousarray(gp.reshape(64, L * 2))
    cw = inp["gdn_conv_w"].reshape(L, 3, 18, 128).transpose(3, 0, 2, 1)
    m["gdn_cw"] = np.ascontiguousarray(cw.reshape(128, L * 54).astype(f))
    rma = np.zeros((64, T), f)
    rma[0:32, tt % 64 == 0] = -1e30
    rma[32:64, tt % 64 == 63] = -1e30
    m["rma"] = rma
    m["ml_normr"] = np.ascontiguousarray(np.tile(inp["mlstm_norm"].reshape(1, L * 128), (64, 1)).astype(f))
    mp = np.zeros((64, L, 2), f)
    for dr_ in range(2):
        mp[dr_ * 32:dr_ * 32 + 6, :, 0] = inp["mlstm_i_bias"][:, dr_, :].T
        mp[dr_ * 32:dr_ * 32 + 6, :, 1] = inp["mlstm_f_bias"][:, dr_, :].T
    m["ml_mp"] = np.ascontiguousarray(mp.reshape(64, L * 2))
    m["tau1"] = np.ascontiguousarray(np.tile(np.arange(1, LC + 1, dtype=f)[None, :], (128, 1)))
    return m


def build(stage=99):
    nc = bass.Bass("TRN2", target_bir_lowering=False)
    es = ExitStack()
    with es:
        def din(name, shape, dt=F32):
            return nc.dram_tensor(name, list(shape), dt, kind="ExternalInput").ap()

        def dout(name, shape, dt=F32):
            return nc.dram_tensor(name, list(shape), dt, kind="ExternalOutput").ap()

        def dscr(name, shape, dt=F32):
            return nc.dram_tensor(name, list(shape), dt, kind="Internal").ap()

        xin = din("xin", [T, D])
        cv_d = din("cv", [128, KC, 2])
        ada_w = din("ada_w", [L, D, 6 * D])
        ada_bT = din("ada_bT", [128, L * 96])
        gT_d = din("gT", [128, 4 * L * KC])
        w_in = din("w_in", [L, D, IN_COLS])
        ident_d = din("ident", [128, 128])
        s5_lre_d = din("s5_lre", [128, L * 32])
        s5_lim_d = din("s5_lim", [128, L * 32])
        s5_ldt_d = din("s5_ldt", [128, L * 32])
        s5_Bb_d = din("s5_Bb", [128, L * 64, 128])
        s5_Cb_d = din("s5_Cb", [128, L * 64, 128])
        s5_dT_d = din("s5_dT", [128, L * 4])
        s5_gbT_d = din("s5_gbT", [128, L * 4])
        s5_gluw_d = din("s5_glu_w", [L, 512, 512])
        tau1_d = din("tau1", [128, LC])
        rm_d = din("rm", [64, T])
        sel_d = din("sel", [64, 12, 128])
        mb_d = din("mb", [64, 4, 64])
        gdn_norm_d = din("gdn_normr", [64, L * 128])
        gdn_gp_d = din("gdn_gp", [64, L * 2])
        gdn_cw_d = din("gdn_cw", [128, L * 54])
        rma_d = din("rma", [64, T])
        ml_norm_d = din("ml_normr", [64, L * 128])
        ml_mp_d = din("ml_mp", [64, L * 2])
        w_out = din("w_out", [L, D, D])
        w_gate = din("ffn_w_gate", [L, D, FFN])
        w_up = din("ffn_w_up", [L, D, FFN])
        w_down = din("ffn_w_down", [L, FFN, D])
        out_d = dout("out", [NLAT, D])
        xres = dscr("xres", [T, D])
        if stage <= 1:
            projT = dout("projT", [47 * 128, T])
            mods_o = dout("mods_o", [128, 96 * 2])
        else:
            projT = dscr("projT", [47 * 128, T])
        if stage == 3:
            dbg_cv = dout("dbg_cv", [128, 3, T])
            dbg_rows = dout("dbg_rows", [64, 6, T])
            dbg_oacc = dout("dbg_oacc", [64, 36, 128])
            dbg_oaccf = dout("dbg_oaccf", [64, 36, 128])
        if stage == 5:
            mixT = din("mixT", [D, T], BF16)
            xres_o = dout("xres_o", [T, D])
        elif 2 <= stage <= 4:
            mixT = dout("mixT", [D, T], BF16)
        else:
            mixT = dscr("mixT", [D, T], BF16)

        kb = KB(nc, es)

        cnt = [0]

        def sb(st, name, shape, dt=F32):
            cnt[0] += 1
            nm = "s%d_%s" % (cnt[0], name)
            return Tl(st.enter_context(nc.sbuf_tensor(nm, list(shape), dt)), nm)

        def ps(st, name, shape, dt=F32):
            cnt[0] += 1
            nm = "p%d_%s" % (cnt[0], name)
            return Tl(st.enter_context(nc.psum_tensor(nm, list(shape), dt)), nm)

        ident = sb(es, "ident", [128, 128])
        gT = sb(es, "gT", [128, 4 * L * KC])
        abT = sb(es, "abT", [128, L * 96])
        cvs = sb(es, "cvs", [128, KC, 2])
        modsT = sb(es, "modsT", [128, 96, 2])
        gsm = sb(es, "gsm", [128, KC, 2])
        gsf = sb(es, "gsf", [128, KC, 2])
        kb.dma("sp", ident[:], ident_d, writes=[ident.b])
        kb.dma("sp", gT[:], gT_d, writes=[gT.b])
        kb.dma("sp", abT[:], ada_bT, writes=[abT.b])
        kb.dma("sp", cvs[:], cv_d, writes=[cvs.b])
        kb.op("act", lambda e: e.activation(out=cvs[:], in_=cvs[:], func=AF.Silu), reads=[cvs.b], writes=[cvs.b])

        banks = [ps(es, "bank%d" % i, [128, 512]) for i in range(8)]
        xres_b = Buf("xres")
        kb.dma("sp", xres, xin, writes=[xres_b])
        kb.barrier()

        for l in range(L):
            with ExitStack() as ph:
                wA = [sb(ph, "wA%d" % i, [128, KC, 512]) for i in range(2)]
                pm = banks[0]
                adv = ada_w[l].rearrange("(k p) c -> p k c", p=128)
                for cb in range(24):
                    w = wA[cb % 2]
                    kb.dma("sp", w[:], adv[:, :, cb * 512:(cb + 1) * 512], writes=[w.b])
                    for jj in range(4):
                        jo = cb * 4 + jj
                        for k in range(KC):
                            kb.op("pe", lambda e, w=w, k=k, jj=jj, jo=jo: e.matmul(
                                out=pm[:, jo * 2:jo * 2 + 2], lhsT=w[:, k, jj * 128:(jj + 1) * 128], rhs=cvs[:, k, :],
                                start=(k == 0), stop=(k == KC - 1)), reads=[w.b, cvs.b], writes=[pm.b], sig=(k == KC - 1))
                for wi in range(2):
                    kb.op("dve", lambda e, wi=wi: e.tensor_tensor(
                        out=modsT[:, :, wi], in0=pm[:, wi:192:2], in1=abT[:, l * 96:(l + 1) * 96], op=ALU.add),
                        reads=[pm.b, abT.b], writes=[modsT.b])
                for (gs, mi, gi) in ((gsm, 1, 0), (gsf, 4, 2)):
                    for wi in range(2):
                        kb.op("dve", lambda e, gs=gs, mi=mi, gi=gi, wi=wi: e.scalar_tensor_tensor(
                            out=gs[:, :, wi], in0=modsT[:, mi * KC:(mi + 1) * KC, wi], scalar=1.0,
                            in1=gT[:, (gi * L + l) * KC:(gi * L + l + 1) * KC], op0=ALU.add, op1=ALU.mult),
                            reads=[modsT.b, gT.b], writes=[gs.b])
                kb.barrier()
            if stage <= 1 and l == 0:
                kb.dma("sp", mods_o, modsT[:].rearrange("p a b -> p (a b)"), reads=[modsT.b])

            with ExitStack() as lay:
                hT = sb(lay, "hT", [128, KC, T], BF16)
                with ExitStack() as ph:
                    xt = [sb(ph, "xt%d" % i, [128, D]) for i in range(2)]
                    junk = sb(ph, "junk", [128, D], BF16)
                    st = [sb(ph, "st%d" % i, [128, 4]) for i in range(2)]
                    for i in range(T // 128):
                        x_ = xt[i % 2]
                        s_ = st[i % 2]
                        wsel = 1 if i < 2 else 0
                        kb.dma("sp", x_[:], xres[i * 128:(i + 1) * 128, :], writes=[x_.b])
                        kb.op("act", lambda e, x_=x_, s_=s_: e.activation(out=junk[:], in_=x_[:], func=AF.Square,
                                                                           accum_out=s_[:, 0:1]),
                              reads=[x_.b], writes=[junk.b, s_.b])
                        kb.op("act", lambda e, s_=s_: e.activation(out=s_[:, 1:2], in_=s_[:, 0:1], func=AF.Sqrt,
                                                                    bias=EPS, scale=1.0 / D), reads=[s_.b], writes=[s_.b])
                        kb.op("dve", lambda e, s_=s_: e.reciprocal(out=s_[:, 2:3], in_=s_[:, 1:2]), reads=[s_.b], writes=[s_.b])
                        kb.op("dve", lambda e, x_=x_, s_=s_: e.tensor_scalar(out=x_[:], in0=x_[:], scalar1=s_[:, 2:3], scalar2=None,
                                                                          op0=ALU.mult), reads=[x_.b, s_.b], writes=[x_.b])
                        for jb in range(4):
                            pt = banks[1 + (i * 4 + jb) % 4]
                            for jj in range(4):
                                j = jb * 4 + jj
                                kb.op("pe", lambda e, pt=pt, x_=x_, jj=jj, j=j: e.transpose(
                                    out=pt[:, jj * 128:(jj + 1) * 128], in_=x_[:, j * 128:(j + 1) * 128], identity=ident[:]),
                                    reads=[x_.b, ident.b], writes=[pt.b])
                            for jj in range(4):
                                j = jb * 4 + jj
                                kb.op("act", lambda e, pt=pt, jj=jj, j=j, i=i, wsel=wsel: e.activation(
                                    out=hT[:, j, i * 128:(i + 1) * 128], in_=pt[:, jj * 128:(jj + 1) * 128], func=AF.Identity,
                                    bias=modsT[:, 0 * KC + j, wsel:wsel + 1], scale=gsm[:, j, wsel:wsel + 1]),
                                    reads=[pt.b, modsT.b, gsm.b], writes=[hT.b])
                    kb.barrier()
                with ExitStack() as ph:
                    wC = [sb(ph, "wC%d" % i, [128, KC, 128], BF16) for i in range(2)]
                    sg = [sb(ph, "sg%d" % i, [128, 512]) for i in range(3)]
                    wv = w_in[l].rearrange("(k p) c -> p k c", p=128)
                    nev = 0
                    for cc in range(47):
                        c0 = cc * 128
                        n = min(128, IN_COLS - c0)
                        w = wC[cc % 2]
                        kb.dma("pool", w[:, :, :n], wv[:, :, c0:c0 + n], writes=[w.b])
                        for tb in range(5):
                            t0 = tb * 512
                            nt = min(512, T - t0)
                            pj = banks[5 + nev % 3]
                            for k in range(KC):
                                kb.op("pe", lambda e, pj=pj, w=w, k=k, n=n, t0=t0, nt=nt: e.matmul(
                                    out=pj[:n, :nt], lhsT=w[:, k, :n], rhs=hT[:, k, t0:t0 + nt],
                                    start=(k == 0), stop=(k == KC - 1)), reads=[w.b, hT.b], writes=[pj.b], sig=(k == KC - 1))
                            s_ = sg[nev % 3]
                            eng = "act" if nev % 2 == 0 else "dve"
                            if eng == "act":
                                kb.op("act", lambda e, s_=s_, pj=pj, n=n, nt=nt: e.activation(out=s_[:n, :nt], in_=pj[:n, :nt],
                                                                                            func=AF.Identity),
                                      reads=[pj.b], writes=[s_.b])
                            else:
                                kb.op("dve", lambda e, s_=s_, pj=pj, n=n, nt=nt: e.tensor_copy(out=s_[:n, :nt], in_=pj[:n, :nt]),
                                      reads=[pj.b], writes=[s_.b])
                            kb.dma("sp", projT[c0:c0 + n, t0:t0 + nt], s_[:n, :nt], reads=[s_.b])
                            nev += 1
                    kb.barrier()
            if stage <= 1:
                break
            wov = w_out[l].rearrange("(k p) c -> p k c", p=128)
            wgv = w_gate[l].rearrange("(k p) c -> p k c", p=128)
            wuv = w_up[l].rearrange("(k p) c -> p k c", p=128)
            wdv = w_down[l].rearrange("(h p) c -> p h c", p=128)
            pre = {}
            for nm_, view_, nch_, nk_ in (("wo", wov, KC, KC), ("wg", wgv, 44, KC), ("wu", wuv, 44, KC), ("wd", wdv, KC, 44)):
                scr_ = dscr("pc_%s%d" % (nm_, l), [nch_, 128, nk_ * 128], BF16)
                lst_ = []
                for j_ in range(nch_):
                    b_ = Buf()
                    dst_ = scr_[j_].rearrange("p (k c) -> p k c", c=128)
                    kb.dma("pool", dst_, view_[:, :, j_ * 128:(j_ + 1) * 128], writes=[b_])
                    lst_.append((dst_, b_))
                pre[nm_] = lst_
            with ExitStack() as ph:
                TWO_PI = 6.283185307179586
                C1 = 6.28125
                C2 = TWO_PI - C1
                PI = 3.141592653589793
                uT = sb(ph, "uT", [128, 4, T])
                yT = sb(ph, "yT", [128, 4, T])
                Bb = sb(ph, "Bb", [128, 32, 128])
                Cb = sb(ph, "Cb", [128, 32, 128])
                lre = sb(ph, "lre", [128, 32]); lim = sb(ph, "lim", [128, 32]); ldt = sb(ph, "ldt", [128, 32])
                tau1 = sb(ph, "tau1", [128, LC])
                dTt = sb(ph, "dTt", [128, L * 4]); gbT = sb(ph, "gbT", [128, L * 4])
                gluw = sb(ph, "gluw", [128, 4, 512])
                kb.dma("sp", uT[:], projT[0:512, :].rearrange("(c p) t -> p c t", p=128), writes=[uT.b])
                kb.dma("sp", lre[:], s5_lre_d[:, l * 32:(l + 1) * 32], writes=[lre.b])
                kb.dma("sp", lim[:], s5_lim_d[:, l * 32:(l + 1) * 32], writes=[lim.b])
                kb.dma("sp", ldt[:], s5_ldt_d[:, l * 32:(l + 1) * 32], writes=[ldt.b])
                kb.dma("sp", tau1[:], tau1_d, writes=[tau1.b])
                kb.dma("sp", dTt[:], s5_dT_d, writes=[dTt.b])
                kb.dma("sp", gbT[:], s5_gbT_d, writes=[gbT.b])
                kb.dma("sp", gluw[:], s5_gluw_d[l].rearrange("(c p) n -> p c n", p=128), writes=[gluw.b])

                def sincos(n, ang, o_sin, o_cos, tf, ti, tm, tb_):
                    B = [tb_]
                    kb.op("dve", lambda e: e.tensor_scalar(out=tf, in0=ang, scalar1=1.0 / TWO_PI, scalar2=None, op0=ALU.mult), reads=B, writes=B)
                    kb.op("dve", lambda e: e.tensor_copy(out=ti, in_=tf), reads=B, writes=B)
                    kb.op("dve", lambda e: e.tensor_copy(out=tf, in_=ti), reads=B, writes=B)
                    kb.op("dve", lambda e: e.scalar_tensor_tensor(out=tm, in0=tf, scalar=-C1, in1=ang, op0=ALU.mult, op1=ALU.add), reads=B, writes=B)
                    kb.op("dve", lambda e: e.scalar_tensor_tensor(out=tm, in0=tf, scalar=-C2, in1=tm, op0=ALU.mult, op1=ALU.add), reads=B, writes=B)

                    def wrap(y):
                        kb.op("dve", lambda e: e.tensor_scalar(out=tf, in0=y, scalar1=PI, scalar2=TWO_PI, op0=ALU.is_gt, op1=ALU.mult), reads=B, writes=B)
                        kb.op("dve", lambda e: e.tensor_tensor(out=y, in0=y, in1=tf, op=ALU.subtract), reads=B, writes=B)
                        kb.op("dve", lambda e: e.tensor_scalar(out=tf, in0=y, scalar1=-PI, scalar2=TWO_PI, op0=ALU.is_lt, op1=ALU.mult), reads=B, writes=B)
                        kb.op("dve", lambda e: e.tensor_tensor(out=y, in0=y, in1=tf, op=ALU.add), reads=B, writes=B)
                    wrap(tm)
                    kb.op("act", lambda e: e.activation(out=o_sin, in_=tm, func=AF.Sin), reads=B, writes=B)
                    kb.op("dve", lambda e: e.tensor_scalar(out=tm, in0=tm, scalar1=PI / 2, scalar2=None, op0=ALU.add), reads=B, writes=B)
                    wrap(tm)
                    kb.op("act", lambda e: e.activation(out=o_cos, in_=tm, func=AF.Sin), reads=B, writes=B)

                with ExitStack() as ph2:
                    tabs = sb(ph2, "tabs", [128, 4, 16 * LC])
                    tw = sb(ph2, "tw", [128, 3, 16 * LC])
                    twi = sb(ph2, "twi", [128, 16 * LC], mybir.dt.int32)
                    sp_ = sb(ph2, "s5par", [128, 16, 16])
                    spi = sb(ph2, "s5pari", [128, 16], mybir.dt.int32)
                    car = sb(ph2, "car", [128, 2, 16])
                    S5U = [sb(ph2, "s5u%d" % i, [128, 6, LC]) for i in range(8)]
                    S5B = [tabs.b, tw.b, twi.b, sp_.b, spi.b]

                    def P_(k):
                        return sp_[:, k, :]
                    for i in range(2):
                        o = (l * 2 + i) * 16
                        B = [sp_.b]
                        kb.dma("sp", Bb[:], s5_Bb_d[:, (l * 2 + i) * 32:(l * 2 + i + 1) * 32, :], writes=[Bb.b])
                        kb.dma("sp", Cb[:], s5_Cb_d[:, (l * 2 + i) * 32:(l * 2 + i + 1) * 32, :], writes=[Cb.b])
                        kb.op("act", lambda e: e.activation(out=Cb[:, 16:32, :], in_=Cb[:, 16:32, :], func=AF.Identity, scale=-1.0),
                              reads=[Cb.b], writes=[Cb.b])
                        kb.op("act", lambda e: e.activation(out=P_(0), in_=ldt[:, (l * 2 + i) * 16 - l * 32 + 0:(l * 2 + i) * 16 - l * 32 + 16], func=AF.Exp), reads=[ldt.b], writes=B)
                        kb.op("dve", lambda e: e.tensor_tensor(out=P_(1), in0=lre[:, i * 16:(i + 1) * 16], in1=P_(0), op=ALU.mult), reads=[lre.b] + B, writes=B)
                        kb.op("dve", lambda e: e.tensor_tensor(out=P_(2), in0=lim[:, i * 16:(i + 1) * 16], in1=P_(0), op=ALU.mult), reads=[lim.b] + B, writes=B)
                        kb.op("act", lambda e: e.activation(out=P_(3), in_=P_(1), func=AF.Exp), reads=B, writes=B)
                        sincos(16, P_(2), P_(4), P_(5), P_(6), spi[:], P_(7), sp_.b)
                        kb.op("dve", lambda e: e.tensor_tensor(out=P_(6), in0=P_(3), in1=P_(5), op=ALU.mult), reads=B, writes=B)
                        kb.op("dve", lambda e: e.tensor_scalar(out=P_(6), in0=P_(6), scalar1=-1.0, scalar2=None, op0=ALU.add), reads=B, writes=B)
                        kb.op("dve", lambda e: e.tensor_tensor(out=P_(7), in0=P_(3), in1=P_(4), op=ALU.mult), reads=B, writes=B)
                        kb.op("dve", lambda e: e.tensor_tensor(out=P_(8), in0=lre[:, i * 16:(i + 1) * 16], in1=lre[:, i * 16:(i + 1) * 16], op=ALU.mult), reads=[lre.b] + B, writes=B)
                        kb.op("dve", lambda e: e.tensor_tensor(out=P_(9), in0=lim[:, i * 16:(i + 1) * 16], in1=lim[:, i * 16:(i + 1) * 16], op=ALU.mult), reads=[lim.b] + B, writes=B)
                        kb.op("dve", lambda e: e.tensor_tensor(out=P_(8), in0=P_(8), in1=P_(9), op=ALU.add), reads=B, writes=B)
                        kb.op("dve", lambda e: e.reciprocal(out=P_(8), in_=P_(8)), reads=B, writes=B)
                        kb.op("dve", lambda e: e.tensor_tensor(out=P_(10), in0=P_(6), in1=lre[:, i * 16:(i + 1) * 16], op=ALU.mult), reads=[lre.b] + B, writes=B)
                        kb.op("dve", lambda e: e.tensor_tensor(out=P_(9), in0=P_(7), in1=lim[:, i * 16:(i + 1) * 16], op=ALU.mult), reads=[lim.b] + B, writes=B)
                        kb.op("dve", lambda e: e.tensor_tensor(out=P_(10), in0=P_(10), in1=P_(9), op=ALU.add), reads=B, writes=B)
                        kb.op("dve", lambda e: e.tensor_tensor(out=P_(10), in0=P_(10), in1=P_(8), op=ALU.mult), reads=B, writes=B)
                        kb.op("dve", lambda e: e.tensor_tensor(out=P_(11), in0=P_(7), in1=lre[:, i * 16:(i + 1) * 16], op=ALU.mult), reads=[lre.b] + B, writes=B)
                        kb.op("dve", lambda e: e.tensor_tensor(out=P_(9), in0=P_(6), in1=lim[:, i * 16:(i + 1) * 16], op=ALU.mult), reads=[lim.b] + B, writes=B)
                        kb.op("dve", lambda e: e.tensor_tensor(out=P_(11), in0=P_(11), in1=P_(9), op=ALU.subtract), reads=B, writes=B)
                        kb.op("dve", lambda e: e.tensor_tensor(out=P_(11), in0=P_(11), in1=P_(8), op=ALU.mult), reads=B, writes=B)
                        for st in range(16):
                            kb.op("dve", lambda e, st=st: e.tensor_scalar(out=tw[:, 0, st * LC:(st + 1) * LC], in0=tau1[:], scalar1=sp_[:, 2, st:st + 1],
                                                                         scalar2=None, op0=ALU.mult), reads=[tau1.b] + B, writes=[tw.b])
                        sincos(16 * LC, tw[:, 0, :], tabs[:, 3, :], tabs[:, 2, :], tw[:, 1, :], twi[:], tw[:, 2, :], tw.b)
                        kb.op("dve", lambda e: e.tensor_copy(out=tw[:, 0, 0:1], in_=tw[:, 0, 0:1]), reads=[tw.b, tabs.b], writes=[tw.b, tabs.b])
                        for st in range(16):
                            sl = slice(st * LC, (st + 1) * LC)
                            kb.op("dve", lambda e, st=st, sl=sl: e.tensor_scalar(out=tw[:, 1, sl], in0=tabs[:, 3, sl], scalar1=sp_[:, 11, st:st + 1], scalar2=None, op0=ALU.mult), reads=[tabs.b] + B, writes=[tw.b])
                            kb.op("dve", lambda e, st=st, sl=sl: e.scalar_tensor_tensor(out=tabs[:, 0, sl], in0=tabs[:, 2, sl], scalar=sp_[:, 10, st:st + 1], in1=tw[:, 1, sl], op0=ALU.mult, op1=ALU.add), reads=[tw.b] + B, writes=[tabs.b])
                            kb.op("dve", lambda e, st=st, sl=sl: e.tensor_scalar(out=tw[:, 1, sl], in0=tabs[:, 3, sl], scalar1=sp_[:, 10, st:st + 1], scalar2=None, op0=ALU.mult), reads=[tabs.b] + B, writes=[tw.b])
                            kb.op("dve", lambda e, st=st, sl=sl: e.scalar_tensor_tensor(out=tabs[:, 1, sl], in0=tabs[:, 2, sl], scalar=sp_[:, 11, st:st + 1], in1=tw[:, 1, sl], op0=ALU.mult, op1=ALU.subtract), reads=[tw.b] + B, writes=[tabs.b])
                        kb.op("dve", lambda e: e.memset(car[:], 0.0), writes=[car.b])
                        NCH = T // LC
                        NCC = NCTX // LC
                        order = list(range(NCH)) if i == 0 else list(range(NCC - 1, -1, -1)) + list(range(NCH - 1, NCC - 1, -1))

                        def rv(ap):
                            return ap[:, ::-1] if i == 1 else ap
                        lastc = LC - 1 if i == 0 else 0
                        for n in order:
                            t0 = n * LC
                            for hf in range(2):
                                for q in range(8):
                                    st = hf * 8 + q
                                    pb = banks[q // 2]
                                    for ci in range(2):
                                        c0 = (q % 2) * 2 * LC + ci * LC
                                        kb.op("pe", lambda e, ci=ci, pb=pb, st=st, c0=c0: e.matmul(out=pb[:, c0:c0 + LC], lhsT=Bb[:, ci * 16 + st, :], rhs=uT[:, st // 4, t0:t0 + LC],
                                                                                              start=True, stop=True), reads=[Bb.b, uT.b], writes=[pb.b])
                                for q in range(8):
                                    st = hf * 8 + q
                                    pb = banks[q // 2]
                                    sl = slice(st * LC, (st + 1) * LC)
                                    u_ = S5U[q]
                                    bre = rv(pb[:, (q % 2) * 2 * LC:(q % 2) * 2 * LC + LC]); bim = rv(pb[:, (q % 2) * 2 * LC + LC:(q % 2) * 2 * LC + 2 * LC])
                                    kb.op("dve", lambda e, u_=u_, bre=bre, sl=sl: e.tensor_tensor(out=u_[:, 0, :], in0=bre, in1=tabs[:, 0, sl], op=ALU.mult), reads=[pb.b, tabs.b], writes=[u_.b])
                                    kb.op("dve", lambda e, u_=u_, bim=bim, sl=sl: e.tensor_tensor(out=u_[:, 1, :], in0=bim, in1=tabs[:, 1, sl], op=ALU.mult), reads=[pb.b, tabs.b], writes=[u_.b])
                                    kb.op("dve", lambda e, u_=u_, bre=bre, sl=sl: e.tensor_tensor(out=u_[:, 2, :], in0=bre, in1=tabs[:, 1, sl], op=ALU.mult), reads=[pb.b, tabs.b], writes=[u_.b])
                                    kb.op("dve", lambda e, u_=u_, bim=bim, sl=sl: e.tensor_tensor(out=u_[:, 3, :], in0=bim, in1=tabs[:, 0, sl], op=ALU.mult), reads=[pb.b, tabs.b], writes=[u_.b])
                                for q in range(8):
                                    u_ = S5U[q]
                                    kb.op("dve", lambda e, u_=u_: e.tensor_tensor(out=u_[:, 0, :], in0=u_[:, 0, :], in1=u_[:, 1, :], op=ALU.subtract), reads=[u_.b], writes=[u_.b])
                                    kb.op("dve", lambda e, u_=u_: e.tensor_tensor(out=u_[:, 2, :], in0=u_[:, 2, :], in1=u_[:, 3, :], op=ALU.add), reads=[u_.b], writes=[u_.b])
                                for q in range(8):
                                    st = hf * 8 + q
                                    u_ = S5U[q]
                                    rb = sp_[:, 3, st:st + 1].to_broadcast([128, LC])
                                    kb.op("dve", lambda e, u_=u_, rb=rb, st=st: e.tensor_tensor_scan(out=u_[:, 1, :], data0=rb, data1=u_[:, 0, :], initial=car[:, 0, st:st + 1], op0=ALU.mult, op1=ALU.add),
                                          reads=[u_.b, sp_.b, car.b], writes=[u_.b])
                                    kb.op("dve", lambda e, u_=u_, rb=rb, st=st: e.tensor_tensor_scan(out=u_[:, 3, :], data0=rb, data1=u_[:, 2, :], initial=car[:, 1, st:st + 1], op0=ALU.mult, op1=ALU.add),
                                          reads=[u_.b, sp_.b, car.b], writes=[u_.b])
                                for q in range(8):
                                    st = hf * 8 + q
                                    sl = slice(st * LC, (st + 1) * LC)
                                    u_ = S5U[q]
                                    kb.op("pool", lambda e, u_=u_, sl=sl: e.tensor_tensor(out=u_[:, 0, :], in0=u_[:, 1, :], in1=tabs[:, 2, sl], op=ALU.mult), reads=[u_.b, tabs.b], writes=[u_.b])
                                    kb.op("pool", lambda e, u_=u_, sl=sl: e.tensor_tensor(out=u_[:, 2, :], in0=u_[:, 3, :], in1=tabs[:, 3, sl], op=ALU.mult), reads=[u_.b, tabs.b], writes=[u_.b])
                                    kb.op("pool", lambda e, u_=u_: e.tensor_tensor(out=rv(u_[:, 4, :]), in0=u_[:, 0, :], in1=u_[:, 2, :], op=ALU.subtract), reads=[u_.b], writes=[u_.b])
                                    kb.op("pool", lambda e, u_=u_, sl=sl: e.tensor_tensor(out=u_[:, 0, :], in0=u_[:, 1, :], in1=tabs[:, 3, sl], op=ALU.mult), reads=[u_.b, tabs.b], writes=[u_.b])
                                    kb.op("pool", lambda e, u_=u_, sl=sl: e.tensor_tensor(out=u_[:, 2, :], in0=u_[:, 3, :], in1=tabs[:, 2, sl], op=ALU.mult), reads=[u_.b, tabs.b], writes=[u_.b])
                                    kb.op("pool", lambda e, u_=u_: e.tensor_tensor(out=rv(u_[:, 5, :]), in0=u_[:, 0, :], in1=u_[:, 2, :], op=ALU.add), reads=[u_.b], writes=[u_.b])
                                for q in range(8):
                                    st = hf * 8 + q
                                    u_ = S5U[q]
                                    kb.op("act", lambda e, u_=u_, st=st: e.activation(out=car[:, 0, st:st + 1], in_=u_[:, 4, lastc:lastc + 1], func=AF.Identity), reads=[u_.b], writes=[car.b])
                                    kb.op("act", lambda e, u_=u_, st=st: e.activation(out=car[:, 1, st:st + 1], in_=u_[:, 5, lastc:lastc + 1], func=AF.Identity), reads=[u_.b], writes=[car.b])
                                for f2 in range(2):
                                    fc = hf * 2 + f2
                                    py = banks[4 + fc]
                                    for q4 in range(4):
                                        q = f2 * 4 + q4
                                        st = hf * 8 + q
                                        u_ = S5U[q]
                                        kb.op("pe", lambda e, py=py, st=st, u_=u_, q4=q4: e.matmul(out=py[:, 0:LC], lhsT=Cb[:, st, :], rhs=u_[:, 4, :], start=(q4 == 0), stop=False),
                                              reads=[Cb.b, u_.b], writes=[py.b], sig=False)
                                        kb.op("pe", lambda e, py=py, st=st, u_=u_, q4=q4: e.matmul(out=py[:, 0:LC], lhsT=Cb[:, 16 + st, :], rhs=u_[:, 5, :], start=False, stop=(q4 == 3)),
                                              reads=[Cb.b, u_.b], writes=[py.b])
                                    if i == 0:
                                        kb.op("act", lambda e, py=py, fc=fc: e.activation(out=yT[:, fc, t0:t0 + LC], in_=py[:, 0:LC], func=AF.Identity), writes=[yT.b, py.b])
                                    else:
                                        kb.op("dve", lambda e, py=py, fc=fc: e.tensor_tensor(out=yT[:, fc, t0:t0 + LC], in0=py[:, 0:LC], in1=yT[:, fc, t0:t0 + LC], op=ALU.add),
                                              writes=[yT.b, py.b])
                    kb.barrier()
                with ExitStack() as ph2:
                    g1 = sb(ph2, "g1", [128, T]); g2 = sb(ph2, "g2", [128, T])
                    og = [sb(ph2, "og%d" % i, [128, 512], BF16) for i in range(2)]
                    sgl = [sb(ph2, "sgl%d" % i, [128, 512]) for i in range(2)]
                    for fc in range(4):
                        kb.op("dve", lambda e: e.scalar_tensor_tensor(out=yT[:, fc, :], in0=uT[:, fc, :], scalar=dTt[:, l * 4 + fc:l * 4 + fc + 1], in1=yT[:, fc, :],
                                                                      op0=ALU.mult, op1=ALU.add), reads=[uT.b, dTt.b, yT.b], writes=[yT.b])
                        kb.op("act", lambda e: e.activation(out=g1[:], in_=yT[:, fc, :], func=AF.Square), reads=[yT.b], writes=[g1.b])
                        kb.op("dve", lambda e: e.tensor_scalar(out=g1[:], in0=g1[:], scalar1=0.044715, scalar2=1.0, op0=ALU.mult, op1=ALU.add), reads=[g1.b], writes=[g1.b])
                        kb.op("dve", lambda e: e.tensor_tensor(out=g1[:], in0=g1[:], in1=yT[:, fc, :], op=ALU.mult), reads=[g1.b, yT.b], writes=[g1.b])
                        kb.op("act", lambda e: e.activation(out=g2[:], in_=g1[:], func=AF.Sigmoid, scale=1.5957691216057308), reads=[g1.b], writes=[g2.b])
                        kb.op("dve", lambda e: e.tensor_tensor(out=yT[:, fc, :], in0=yT[:, fc, :], in1=g2[:], op=ALU.mult), reads=[g2.b, yT.b], writes=[yT.b])
                    ne = 0
                    for fo in range(4):
                        for tb in range(5):
                            t0 = tb * 512
                            nt = min(512, T - t0)
                            pg = banks[4 + ne % 2]
                            for fi in range(4):
                                kb.op("pe", lambda e, fi=fi, pg=pg: e.matmul(out=pg[:, :nt], lhsT=gluw[:, fi, fo * 128:(fo + 1) * 128], rhs=yT[:, fi, t0:t0 + nt],
                                                                             start=(fi == 0), stop=(fi == 3)), reads=[gluw.b, yT.b], writes=[pg.b], sig=(fi == 3))
                            s_ = sgl[ne % 2]; o_ = og[ne % 2]
                            kb.op("act", lambda e, pg=pg, s_=s_: e.activation(out=s_[:, :nt], in_=pg[:, :nt], func=AF.Sigmoid, bias=gbT[:, l * 4 + fo:l * 4 + fo + 1]),
                                  reads=[pg.b, gbT.b], writes=[s_.b])
                            kb.op("dve", lambda e, s_=s_, o_=o_: e.tensor_tensor(out=o_[:, :nt], in0=s_[:, :nt], in1=yT[:, fo, t0:t0 + nt], op=ALU.mult),
                                  reads=[s_.b, yT.b], writes=[o_.b])
                            kb.dma("sp", mixT[fo * 128:(fo + 1) * 128, t0:t0 + nt], o_[:, :nt], reads=[o_.b])
                            ne += 1
                kb.barrier()
            if stage <= 2:
                break
            with ExitStack() as ph:
                QOFF, KOFF, VOFF, ZOFF, AOFF, BOFF = 512, 1280, 2048, 2816, 3584, 3596
                NCK = T // 64
                RM = sb(ph, "RM", [64, T])
                SEL = sb(ph, "SEL", [64, 12, 128]); NSEL = sb(ph, "NSEL", [64, 12, 128])
                MB = sb(ph, "MB", [64, 4, 64])
                ONES = sb(ph, "ONES", [128, 128])
                NORM = sb(ph, "NORM", [64, 128])
                GP = sb(ph, "GP", [64, 2])
                CW = sb(ph, "CW", [128, 18 * 3])
                kb.dma("sp", RM[:], rm_d, writes=[RM.b])
                kb.dma("sp", SEL[:], sel_d, writes=[SEL.b])
                kb.dma("sp", MB[:], mb_d, writes=[MB.b])
                kb.dma("sp", NORM[:], gdn_norm_d[:, l * 128:(l + 1) * 128], writes=[NORM.b])
                kb.dma("sp", GP[:], gdn_gp_d[:, l * 2:(l + 1) * 2], writes=[GP.b])
                kb.dma("sp", CW[:], gdn_cw_d[:, l * 54:(l + 1) * 54], writes=[CW.b])
                kb.op("act", lambda e: e.activation(out=NSEL[:], in_=SEL[:], func=AF.Identity, scale=-1.0), reads=[SEL.b], writes=[NSEL.b])
                kb.op("dve", lambda e: e.memset(ONES[:], 1.0), writes=[ONES.b])
                RA = sb(ph, "RA", [64, T]); RB = sb(ph, "RB", [64, T]); GC = sb(ph, "GC", [64, T])
                R1 = sb(ph, "R1", [64, T]); EG = sb(ph, "EG", [64, T]); BG = sb(ph, "BG", [64, T])
                EGL = sb(ph, "EGL", [64, NCK])
                COL = sb(ph, "COL", [64, NCK, 3, 12])
                EGLB = sb(ph, "EGLB", [128, 12, NCK])
                kb.op("dve", lambda e: e.memset(RA[:], 0.0), writes=[RA.b])
                kb.op("dve", lambda e: e.memset(RB[:], 0.0), writes=[RB.b])
                for dr in range(2):
                    kb.dma("sp", RA[dr * 32:dr * 32 + 6, :], projT[AOFF + dr * 6:AOFF + dr * 6 + 6, :], writes=[RA.b])
                    kb.dma("sp", RB[dr * 32:dr * 32 + 6, :], projT[BOFF + dr * 6:BOFF + dr * 6 + 6, :], writes=[RB.b])
                kb.op("act", lambda e: e.activation(out=RA[:], in_=RA[:], func=AF.Exp, bias=GP[:, 1:2]), reads=[RA.b, GP.b], writes=[RA.b])
                kb.op("act", lambda e: e.activation(out=RA[:], in_=RA[:], func=AF.Ln, bias=1.0), reads=[RA.b], writes=[RA.b])
                kb.op("act", lambda e: e.activation(out=GP[:, 0:1], in_=GP[:, 0:1], func=AF.Exp), reads=[GP.b], writes=[GP.b])
                kb.op("dve", lambda e: e.tensor_scalar(out=RA[:], in0=RA[:], scalar1=GP[:, 0:1], scalar2=-1.0, op0=ALU.mult, op1=ALU.mult), reads=[RA.b, GP.b], writes=[RA.b])
                kb.op("act", lambda e: e.activation(out=RB[:], in_=RB[:], func=AF.Sigmoid), reads=[RB.b], writes=[RB.b])
                kb.op("act", lambda e: e.activation(out=R1[:], in_=RB[:], func=AF.Ln), reads=[RB.b], writes=[R1.b])
                kb.op("dve", lambda e: e.tensor_tensor_scan(out=GC[0:32, :], data0=RM[0:32, :], data1=RA[0:32, :], initial=0.0, op0=ALU.mult, op1=ALU.add),
                      reads=[RM.b, RA.b], writes=[GC.b])
                kb.op("dve", lambda e: e.tensor_tensor_scan(out=GC[32:64, ::-1], data0=RM[32:64, ::-1], data1=RA[32:64, ::-1], initial=0.0, op0=ALU.mult, op1=ALU.add),
                      reads=[RM.b, RA.b], writes=[GC.b])
                kb.op("dve", lambda e: e.tensor_tensor(out=R1[:], in0=R1[:], in1=GC[:], op=ALU.add), reads=[R1.b, GC.b], writes=[R1.b])
                kb.op("act", lambda e: e.activation(out=EG[:], in_=GC[:], func=AF.Exp), reads=[GC.b], writes=[EG.b])
                kb.op("dve", lambda e: e.tensor_tensor(out=BG[:], in0=EG[:], in1=RB[:], op=ALU.mult), reads=[EG.b, RB.b], writes=[BG.b])
                GCL = sb(ph, "GCL", [64, NCK])
                for dr in range(2):
                    pr = slice(dr * 32, dr * 32 + 32)
                    lastpos = 63 if dr == 0 else 0
                    gv = GC[pr, :].rearrange("p (c s) -> p c s", s=64)
                    kb.op("dve", lambda e, pr=pr, gv=gv, lastpos=lastpos: e.tensor_copy(out=GCL[pr, :], in_=gv[:, :, lastpos]), reads=[GC.b], writes=[GCL.b])
                kb.op("act", lambda e: e.activation(out=EGL[:], in_=GCL[:], func=AF.Exp), reads=[GCL.b], writes=[EGL.b])
                for c in range(NCK):
                    kb.op("dve", lambda e, c=c: e.tensor_scalar(out=RA[:, c * 64:(c + 1) * 64], in0=GC[:, c * 64:(c + 1) * 64], scalar1=-1.0, scalar2=GCL[:, c:c + 1],
                                                                 op0=ALU.mult, op1=ALU.add), reads=[GC.b, GCL.b], writes=[RA.b])
                kb.op("act", lambda e: e.activation(out=RA[:], in_=RA[:], func=AF.Exp), reads=[RA.b], writes=[RA.b])
                for c in range(NCK):
                    pcol = banks[c % 4]
                    for qi, row in enumerate((BG, RA, RB)):
                        kb.op("pe", lambda e, row=row, qi=qi, pcol=pcol, c=c: e.matmul(out=pcol[0:64, qi * 12:(qi + 1) * 12], lhsT=row[:, c * 64:(c + 1) * 64],
                                                                                   rhs=SEL[:, :, 0], start=True, stop=True), reads=[row.b, SEL.b], writes=[pcol.b])
                    kb.op("act", lambda e, pcol=pcol, c=c: e.activation(out=COL[:, c, :, :].rearrange("p a b -> p (a b)"), in_=pcol[0:64, 0:36], func=AF.Identity),
                          reads=[pcol.b], writes=[COL.b])
                for r in range(12):
                    pcol = banks[4 + r % 2]
                    kb.op("pe", lambda e, r=r, pcol=pcol: e.matmul(out=pcol[:, 0:NCK], lhsT=SEL[:, r, :], rhs=EGL[:], start=True, stop=True), reads=[SEL.b, EGL.b], writes=[pcol.b])
                    kb.op("act", lambda e, r=r, pcol=pcol: e.activation(out=EGLB[:, r, :], in_=pcol[:, 0:NCK], func=AF.Identity), reads=[pcol.b], writes=[EGLB.b])
                kb.barrier()
                import os as _os
                DBG = int(_os.environ.get("DBG_GDN", "9"))
                if stage == 3:
                    for qi_, row_ in enumerate((GC, R1, EG, RA, RB, BG)):
                        kb.dma("sp", dbg_rows[:, qi_, :], row_[:], reads=[row_.b])
                psn = [0]

                def pget():
                    bk = banks[psn[0] % 8]
                    psn[0] += 1
                    return bk
                RAW = sb(ph, "RAW", [128, 3, T]); CV = sb(ph, "CV", [128, 3, T])
                OACC = sb(ph, "OACC", [64, NCK, 128]); OUT = sb(ph, "OUT", [128, T], BF16)
                Sd = [sb(ph, "Sst%d" % i, [128, 128]) for i in range(2)]
                G = 4
                U = []
                for u in range(2 * G):
                    d_ = {}
                    for nm, shp in (("AB0", [64, 2, 64]), ("AB1", [64, 2, 64]), ("X0", [64, 64]), ("X1", [64, 64]),
                                    ("E", [64, 3, 64]), ("kbg", [64, 128]), ("kdec", [64, 128]), ("vb", [64, 128]),
                                    ("nWT", [128, 64]), ("VN", [64, 128])):
                        d_[nm] = sb(ph, "u%d%s" % (u, nm), shp, BF16 if nm in ("AB0", "AB1", "X0", "X1", "kbg", "vb") else F32)
                    U.append(d_)
                fin = [sb(ph, "fin%d" % i, [64, 128]) for i in range(2)]
                fst = [sb(ph, "fst%d" % i, [64, 4]) for i in range(2)]
                I64 = ident[0:64, 0:64]
                I64b_t = sb(ph, "I64b", [64, 64], BF16)
                kb.op("dve", lambda e: e.tensor_copy(out=I64b_t[:], in_=ident[0:64, 0:64]), reads=[ident.b], writes=[I64b_t.b])
                I64b = I64b_t[:]
                for hd in range(6 if DBG >= 1 else 0):
                    for ci, off in enumerate((QOFF, KOFF, VOFF)):
                        kb.dma("sp", RAW[:, ci, :], projT[off + hd * 128:off + (hd + 1) * 128, :], writes=[RAW.b])
                    for ci in range(3):
                        cch = ci * 6 + hd
                        for (s0, s1) in ((0, NCTX), (NCTX, T)):
                            kb.op("dve", lambda e, ci=ci, cch=cch, s0=s0, s1=s1: e.tensor_scalar(out=CV[:, ci, s0:s1], in0=RAW[:, ci, s0:s1], scalar1=CW[:, cch * 3 + 1:cch * 3 + 2],
                                                                                              scalar2=None, op0=ALU.mult), reads=[RAW.b, CW.b], writes=[CV.b])
                            kb.op("dve", lambda e, ci=ci, cch=cch, s0=s0, s1=s1: e.scalar_tensor_tensor(out=CV[:, ci, s0 + 1:s1], in0=RAW[:, ci, s0:s1 - 1], scalar=CW[:, cch * 3:cch * 3 + 1],
                                                                                                     in1=CV[:, ci, s0 + 1:s1], op0=ALU.mult, op1=ALU.add), reads=[RAW.b, CW.b, CV.b], writes=[CV.b])
                            kb.op("dve", lambda e, ci=ci, cch=cch, s0=s0, s1=s1: e.scalar_tensor_tensor(out=CV[:, ci, s0:s1 - 1], in0=RAW[:, ci, s0 + 1:s1], scalar=CW[:, cch * 3 + 2:cch * 3 + 3],
                                                                                                     in1=CV[:, ci, s0:s1 - 1], op0=ALU.mult, op1=ALU.add), reads=[RAW.b, CW.b, CV.b], writes=[CV.b])
                    kb.op("act", lambda e: e.activation(out=CV[:], in_=CV[:], func=AF.Silu), reads=[CV.b], writes=[CV.b])
                    for ci in range(2):
                        kb.op("act", lambda e, ci=ci: e.activation(out=RAW[:, 0, :], in_=CV[:, ci, :], func=AF.Square), reads=[CV.b], writes=[RAW.b])
                        for tb in range(5):
                            t0 = tb * 512
                            nt = min(512, T - t0)
                            pb = banks[6 + tb % 2]
                            kb.op("pe", lambda e, pb=pb, t0=t0, nt=nt: e.matmul(out=pb[:, :nt], lhsT=ONES[:], rhs=RAW[:, 0, t0:t0 + nt], start=True, stop=True),
                                  reads=[ONES.b, RAW.b], writes=[pb.b])
                            kb.op("act", lambda e, pb=pb, t0=t0, nt=nt: e.activation(out=RAW[:, 1, t0:t0 + nt], in_=pb[:, :nt], func=AF.Sqrt, bias=EPS), reads=[RAW.b], writes=[RAW.b, pb.b])
                        kb.op("dve", lambda e: e.reciprocal(out=RAW[:, 1, :], in_=RAW[:, 1, :]), reads=[RAW.b], writes=[RAW.b])
                        sc_ = (128.0 ** -0.5) if ci == 0 else 1.0
                        kb.op("dve", lambda e, ci=ci, sc_=sc_: e.scalar_tensor_tensor(out=CV[:, ci, :], in0=RAW[:, 1, :], scalar=sc_, in1=CV[:, ci, :], op0=ALU.mult, op1=ALU.mult),
                              reads=[RAW.b, CV.b], writes=[CV.b])
                    QT = lambda a, b_: CV[:, 0, a:b_]
                    KT = lambda a, b_: CV[:, 1, a:b_]
                    VT = lambda a, b_: CV[:, 2, a:b_]
                    kb.op("dve", lambda e: e.memset(OACC[:], 0.0), writes=[OACC.b])
                    for dr in range(2):
                        r = dr * 6 + hd
                        for tb in range(5):
                            t0 = tb * 512
                            nt = min(512, T - t0)
                            pb = banks[6 + tb % 2]
                            kb.op("pe", lambda e, pb=pb, t0=t0, nt=nt, r=r: e.matmul(out=pb[:, :nt], lhsT=SEL[:, r, :], rhs=EG[:, t0:t0 + nt], start=True, stop=True),
                                  reads=[SEL.b, EG.b], writes=[pb.b])
                            kb.op("dve", lambda e, pb=pb, t0=t0, nt=nt, dr=dr: e.tensor_tensor(out=RAW[:, dr, t0:t0 + nt], in0=pb[:, :nt], in1=CV[:, 0, t0:t0 + nt], op=ALU.mult),
                                  reads=[CV.b], writes=[RAW.b, pb.b])
                        kb.op("dve", lambda e, dr=dr: e.memset(Sd[dr][:], 0.0), writes=[Sd[dr].b])
                    orders = [list(range(NCK)), list(range(3, -1, -1)) + list(range(NCK - 1, 3, -1))]
                    masks = [(0, 1, 3), (1, 0, 2)]
                    for g0 in range(0, NCK, G):
                        units = []
                        for dr in range(2):
                            for u, c in enumerate(orders[dr][g0:g0 + G]):
                                units.append((dr, c, U[dr * G + u]))
                        for (dr, c, d_) in units:
                            r = dr * 6 + hd
                            t0 = c * 64
                            bk = pget()
                            kb.op("pe", lambda e, bk=bk, t0=t0: e.transpose(out=bk[0:64, 0:128], in_=KT(t0, t0 + 64), identity=ident[:]), reads=[CV.b, ident.b], writes=[bk.b])
                            kb.op("pe", lambda e, bk=bk, t0=t0: e.transpose(out=bk[0:64, 128:256], in_=VT(t0, t0 + 64), identity=ident[:]), reads=[CV.b, ident.b], writes=[bk.b])
                            kb.op("act", lambda e, bk=bk, d_=d_, c=c, r=r: e.activation(out=d_["kbg"][:], in_=bk[0:64, 0:128], func=AF.Identity, scale=COL[:, c, 0, r:r + 1]), reads=[COL.b], writes=[d_["kbg"].b, bk.b])
                            kb.op("act", lambda e, bk=bk, d_=d_, c=c, r=r: e.activation(out=d_["kdec"][:], in_=bk[0:64, 0:128], func=AF.Identity, scale=COL[:, c, 1, r:r + 1]), reads=[COL.b], writes=[d_["kdec"].b, bk.b])
                            kb.op("act", lambda e, bk=bk, d_=d_, c=c, r=r: e.activation(out=d_["vb"][:], in_=bk[0:64, 128:256], func=AF.Identity, scale=COL[:, c, 2, r:r + 1]), reads=[COL.b], writes=[d_["vb"].b, bk.b])
                        for (dr, c, d_) in units:
                            r = dr * 6 + hd
                            m_s, m_st, m_it = masks[dr]
                            t0 = c * 64
                            ch = slice(t0, t0 + 64)
                            bp = pget()
                            kb.op("pe", lambda e, bp=bp, t0=t0: e.matmul(out=bp[0:64, 0:64], lhsT=KT(t0, t0 + 64), rhs=KT(t0, t0 + 64), start=True, stop=True), reads=[CV.b], writes=[bp.b])
                            kb.op("pe", lambda e, bp=bp, t0=t0: e.matmul(out=bp[0:64, 64:128], lhsT=KT(t0, t0 + 64), rhs=QT(t0, t0 + 64), start=True, stop=True), reads=[CV.b], writes=[bp.b])
                            be = pget()
                            kb.op("pe", lambda e, be=be, ch=ch, r=r: e.matmul(out=be[0:64, 0:64], lhsT=R1[:, ch], rhs=SEL[:, r, 0:64], start=True, stop=False), reads=[R1.b, SEL.b], writes=[be.b], sig=False)
                            kb.op("pe", lambda e, be=be, ch=ch, r=r: e.matmul(out=be[0:64, 0:64], lhsT=NSEL[:, r, 0:64], rhs=GC[:, ch], start=False, stop=False), reads=[GC.b, NSEL.b], writes=[be.b], sig=False)
                            kb.op("pe", lambda e, be=be, m_s=m_s: e.matmul(out=be[0:64, 0:64], lhsT=I64, rhs=MB[:, m_s, :], start=False, stop=True), reads=[MB.b, ident.b], writes=[be.b])
                            kb.op("pe", lambda e, be=be, ch=ch, r=r: e.matmul(out=be[0:64, 64:128], lhsT=GC[:, ch], rhs=NSEL[:, r, 0:64], start=True, stop=False), reads=[GC.b, NSEL.b], writes=[be.b], sig=False)
                            kb.op("pe", lambda e, be=be, ch=ch, r=r: e.matmul(out=be[0:64, 64:128], lhsT=SEL[:, r, 0:64], rhs=R1[:, ch], start=False, stop=False), reads=[R1.b, SEL.b], writes=[be.b], sig=False)
                            kb.op("pe", lambda e, be=be, m_st=m_st: e.matmul(out=be[0:64, 64:128], lhsT=I64, rhs=MB[:, m_st, :], start=False, stop=True), reads=[MB.b, ident.b], writes=[be.b])
                            kb.op("pe", lambda e, be=be, ch=ch, r=r: e.matmul(out=be[0:64, 128:192], lhsT=GC[:, ch], rhs=NSEL[:, r, 0:64], start=True, stop=False), reads=[GC.b, NSEL.b], writes=[be.b], sig=False)
                            kb.op("pe", lambda e, be=be, ch=ch, r=r: e.matmul(out=be[0:64, 128:192], lhsT=SEL[:, r, 0:64], rhs=GC[:, ch], start=False, stop=False), reads=[GC.b, SEL.b], writes=[be.b], sig=False)
                            kb.op("pe", lambda e, be=be, m_it=m_it: e.matmul(out=be[0:64, 128:192], lhsT=I64, rhs=MB[:, m_it, :], start=False, stop=True), reads=[MB.b, ident.b], writes=[be.b])
                            kb.op("act", lambda e, be=be, d_=d_: e.activation(out=d_["E"][:].rearrange("p a b -> p (a b)"), in_=be[0:64, 0:192], func=AF.Exp), writes=[d_["E"].b, be.b])
                            kb.op("dve", lambda e, bp=bp, d_=d_: e.tensor_tensor(out=d_["AB0"][:, 1, :], in0=bp[0:64, 0:64], in1=d_["E"][:, 0, :], op=ALU.mult), reads=[d_["E"].b], writes=[d_["AB0"].b, bp.b])
                            kb.op("dve", lambda e, bp=bp, d_=d_: e.tensor_tensor(out=d_["AB0"][:, 0, :], in0=bp[0:64, 0:64], in1=d_["E"][:, 1, :], op=ALU.mult), reads=[d_["E"].b], writes=[d_["AB0"].b, bp.b])
                            kb.op("dve", lambda e, bp=bp, d_=d_: e.tensor_tensor(out=d_["E"][:, 2, :], in0=bp[0:64, 64:128], in1=d_["E"][:, 2, :], op=ALU.mult), reads=[d_["E"].b], writes=[d_["E"].b, bp.b])
                            kb.op("dve", lambda e, d_=d_: e.tensor_tensor(out=d_["X0"][:], in0=I64, in1=d_["AB0"][:, 0, :], op=ALU.subtract), reads=[ident.b, d_["AB0"].b], writes=[d_["X0"].b])
                        for k in range(1, 6):
                            pa, pn = (k - 1) % 2, k % 2
                            for ui, (dr, c, d_) in enumerate(units):
                                ABp, ABn = d_["AB%d" % pa], d_["AB%d" % pn]
                                bk = pget()
                                if k < 5:
                                    kb.op("pe", lambda e, bk=bk, ABp=ABp: e.matmul(out=bk[0:64, 0:64], lhsT=ABp[:, 1, :], rhs=ABp[:, 0, :], start=True, stop=True), reads=[ABp.b], writes=[bk.b])
                                kb.op("pe", lambda e, bk=bk, ABp=ABp: e.matmul(out=bk[0:64, 64:128], lhsT=ABp[:, 0, :], rhs=ABp[:, 1, :], start=True, stop=True), reads=[ABp.b], writes=[bk.b])
                                lo = 0 if k < 5 else 64
                                if (ui + k) % 2 == 0:
                                    kb.op("act", lambda e, bk=bk, ABn=ABn, lo=lo: e.activation(out=ABn[:].rearrange("p a b -> p (a b)")[:, lo:128], in_=bk[0:64, lo:128], func=AF.Identity), writes=[ABn.b, bk.b])
                                else:
                                    kb.op("dve", lambda e, bk=bk, ABn=ABn, lo=lo: e.tensor_copy(out=ABn[:].rearrange("p a b -> p (a b)")[:, lo:128], in_=bk[0:64, lo:128]), writes=[ABn.b, bk.b])
                            for ui, (dr, c, d_) in enumerate(units):
                                ABn, Xp, Xn = d_["AB%d" % pn], d_["X%d" % pa], d_["X%d" % pn]
                                bk = pget()
                                kb.op("pe", lambda e, bk=bk, Xp=Xp: e.matmul(out=bk[0:64, 0:64], lhsT=I64b, rhs=Xp[:], start=True, stop=False), reads=[Xp.b, I64b_t.b], writes=[bk.b], sig=False)
                                kb.op("pe", lambda e, bk=bk, Xp=Xp, ABn=ABn: e.matmul(out=bk[0:64, 0:64], lhsT=ABn[:, 1, :], rhs=Xp[:], start=False, stop=True), reads=[Xp.b, ABn.b], writes=[bk.b])
                                if (ui + k) % 2 == 1:
                                    kb.op("act", lambda e, bk=bk, Xn=Xn: e.activation(out=Xn[:], in_=bk[0:64, 0:64], func=AF.Identity), writes=[Xn.b, bk.b])
                                else:
                                    kb.op("dve", lambda e, bk=bk, Xn=Xn: e.tensor_copy(out=Xn[:], in_=bk[0:64, 0:64]), writes=[Xn.b, bk.b])
                        for (dr, c, d_) in units:
                            TT = d_["X1"]
                            bk = pget()
                            kb.op("pe", lambda e, bk=bk, d_=d_, TT=TT: e.matmul(out=bk[:, 0:64], lhsT=d_["kbg"][:], rhs=TT[:], start=True, stop=True), reads=[d_["kbg"].b, TT.b], writes=[bk.b])
                            kb.op("act", lambda e, bk=bk, d_=d_: e.activation(out=d_["nWT"][:], in_=bk[:, 0:64], func=AF.Identity, scale=-1.0), writes=[d_["nWT"].b, bk.b])
                        for u in range(G):
                            for dr in range(2):
                                if dr * G + u >= len(units):
                                    continue
                                dr_, c, d_ = units[dr * G + u]
                                r = dr * 6 + hd
                                S = Sd[dr]
                                t0 = c * 64
                                TT = d_["X1"]
                                bk = pget()
                                kb.op("pe", lambda e, bk=bk, d_=d_, TT=TT: e.matmul(out=bk[0:64, 0:128], lhsT=TT[:], rhs=d_["vb"][:], start=True, stop=False), reads=[d_["vb"].b, TT.b], writes=[bk.b], sig=False)
                                kb.op("pe", lambda e, bk=bk, d_=d_, S=S: e.matmul(out=bk[0:64, 0:128], lhsT=d_["nWT"][:], rhs=S[:], start=False, stop=True), reads=[d_["nWT"].b, S.b], writes=[bk.b])
                                kb.op("act", lambda e, bk=bk, d_=d_: e.activation(out=d_["VN"][:], in_=bk[0:64, 0:128], func=AF.Identity), writes=[d_["VN"].b, bk.b])
                                bk = pget()
                                kb.op("pe", lambda e, bk=bk, t0=t0, dr=dr, S=S: e.matmul(out=bk[0:64, 0:128], lhsT=RAW[:, dr, t0:t0 + 64], rhs=S[:], start=True, stop=False), reads=[RAW.b, S.b], writes=[bk.b], sig=False)
                                kb.op("pe", lambda e, bk=bk, d_=d_: e.matmul(out=bk[0:64, 0:128], lhsT=d_["E"][:, 2, :], rhs=d_["VN"][:], start=False, stop=True), reads=[d_["E"].b, d_["VN"].b], writes=[bk.b])
                                kb.op("dve", lambda e, bk=bk, c=c: e.tensor_tensor(out=OACC[:, c, :], in0=bk[0:64, 0:128], in1=OACC[:, c, :], op=ALU.add), writes=[OACC.b, bk.b])
                                bk = pget()
                                kb.op("pe", lambda e, bk=bk, d_=d_: e.matmul(out=bk[:, 0:128], lhsT=d_["kdec"][:], rhs=d_["VN"][:], start=True, stop=True), reads=[d_["kdec"].b, d_["VN"].b], writes=[bk.b])
                                kb.op("dve", lambda e, bk=bk, c=c, r=r, S=S: e.scalar_tensor_tensor(out=S[:], in0=S[:], scalar=EGLB[:, r, c:c + 1], in1=bk[:, 0:128], op0=ALU.mult, op1=ALU.add),
                                      reads=[EGLB.b], writes=[S.b, bk.b])
                    kb.dma("sp", RAW[:, 2, :], projT[ZOFF + hd * 128:ZOFF + (hd + 1) * 128, :], reads=[CV.b], writes=[RAW.b])
                    kb.op("act", lambda e: e.activation(out=RAW[:, 2, :], in_=RAW[:, 2, :], func=AF.Silu), reads=[RAW.b], writes=[RAW.b])
                    for c in range(NCK):
                        f_ = fin[c % 2]; s_ = fst[c % 2]
                        kb.op("act", lambda e, f_=f_, s_=s_, c=c: e.activation(out=f_[:], in_=OACC[:, c, :], func=AF.Square, accum_out=s_[:, 0:1]), reads=[OACC.b], writes=[f_.b, s_.b])
                        kb.op("act", lambda e, s_=s_: e.activation(out=s_[:, 1:2], in_=s_[:, 0:1], func=AF.Sqrt, bias=EPS, scale=1.0 / 128), reads=[s_.b], writes=[s_.b])
                        kb.op("dve", lambda e, s_=s_: e.reciprocal(out=s_[:, 2:3], in_=s_[:, 1:2]), reads=[s_.b], writes=[s_.b])
                        kb.op("dve", lambda e, f_=f_, s_=s_, c=c: e.scalar_tensor_tensor(out=f_[:], in0=OACC[:, c, :], scalar=s_[:, 2:3], in1=NORM[:], op0=ALU.mult, op1=ALU.mult),
                              reads=[OACC.b, s_.b, NORM.b], writes=[f_.b])
                        bk = pget()
                        kb.op("pe", lambda e, bk=bk, f_=f_: e.transpose(out=bk[:, 0:64], in_=f_[:], identity=I64), reads=[f_.b, ident.b], writes=[bk.b])
                        kb.op("dve", lambda e, bk=bk, c=c: e.tensor_tensor(out=OUT[:, c * 64:(c + 1) * 64], in0=bk[:, 0:64], in1=RAW[:, 2, c * 64:(c + 1) * 64], op=ALU.mult),
                              reads=[RAW.b], writes=[OUT.b, bk.b])
                    kb.dma("sp", mixT[512 + hd * 128:512 + (hd + 1) * 128, :], OUT[:], reads=[OUT.b])
                kb.barrier()
            if stage <= 3:
                break
            with ExitStack() as ph:
                MQ, MK, MV, MO, MI, MF = 3608, 3992, 4376, 5144, 5912, 5924
                NCK = T // 64
                SEL = sb(ph, "mSEL", [64, 12, 128]); NSEL = sb(ph, "mNSEL", [64, 12, 128])
                MB = sb(ph, "mMB", [64, 4, 64])
                NORM = sb(ph, "mNORM", [64, 128])
                MP = sb(ph, "mMP", [64, 2])
                kb.dma("sp", SEL[:], sel_d, writes=[SEL.b])
                kb.dma("sp", MB[:], mb_d, writes=[MB.b])
                kb.dma("sp", NORM[:], ml_norm_d[:, l * 128:(l + 1) * 128], writes=[NORM.b])
                kb.dma("sp", MP[:], ml_mp_d[:, l * 2:(l + 1) * 2], writes=[MP.b])
                kb.op("act", lambda e: e.activation(out=NSEL[:], in_=SEL[:], func=AF.Identity, scale=-1.0), reads=[SEL.b], writes=[NSEL.b])
                RI = sb(ph, "mRI", [64, T]); CM = sb(ph, "mCM", [64, T])
                COL = sb(ph, "mCOL", [64, NCK, 4, 12])
                AOB = sb(ph, "mAOB", [128, 2, 12, NCK])
                I64 = ident[0:64, 0:64]

                def seqperm(eng, dst, dstb, src, srcb, npart, p0=0):
                    kb.op(eng, lambda e: (e.tensor_copy(out=dst[p0:p0 + npart, 0:NCTX], in_=src[p0:p0 + npart, 0:NCTX]) if eng != "act" else
                                          e.activation(out=dst[p0:p0 + npart, 0:NCTX], in_=src[p0:p0 + npart, 0:NCTX], func=AF.Identity)), reads=[srcb], writes=[dstb])
                    ov = dst[p0:p0 + npart, NCTX:T].rearrange("p (c r) -> p c r", r=32)
                    iv = src[p0:p0 + npart, NCTX:T].rearrange("p (r c) -> p c r", c=64)
                    kb.op(eng, lambda e: (e.tensor_copy(out=ov, in_=iv) if eng != "act" else e.activation(out=ov, in_=iv, func=AF.Identity)), reads=[srcb], writes=[dstb])

                with ExitStack() as ph2:
                    RM = sb(ph2, "mRM", [64, T]); RMA = sb(ph2, "mRMA", [64, T])
                    RF = sb(ph2, "mRF", [64, T]); BC = sb(ph2, "mBC", [64, T]); X1 = sb(ph2, "mX1", [64, T]); X2 = sb(ph2, "mX2", [64, T])
                    CH = sb(ph2, "mCH", [64, 8, NCK])
                    kb.dma("sp", RM[:], rm_d, writes=[RM.b])
                    kb.dma("sp", RMA[:], rma_d, writes=[RMA.b])
                    kb.op("dve", lambda e: e.memset(X1[:], 0.0), writes=[X1.b])
                    kb.op("dve", lambda e: e.memset(X2[:], 0.0), writes=[X2.b])
                    for dr in range(2):
                        kb.dma("sp", X1[dr * 32:dr * 32 + 6, :], projT[MI + dr * 6:MI + dr * 6 + 6, :], writes=[X1.b])
                        kb.dma("sp", X2[dr * 32:dr * 32 + 6, :], projT[MF + dr * 6:MF + dr * 6 + 6, :], writes=[X2.b])
                    seqperm("dve", RI, RI.b, X1, X1.b, 64)
                    seqperm("dve", RF, RF.b, X2, X2.b, 64)
                    kb.op("act", lambda e: e.activation(out=RI[:], in_=RI[:], func=AF.Identity, bias=MP[:, 0:1]), reads=[RI.b, MP.b], writes=[RI.b])
                    kb.op("dve", lambda e: e.tensor_scalar(out=MP[:, 1:2], in0=MP[:, 1:2], scalar1=-1.0, scalar2=None, op0=ALU.mult), reads=[MP.b], writes=[MP.b])
                    kb.op("act", lambda e: e.activation(out=RF[:], in_=RF[:], func=AF.Exp, bias=MP[:, 1:2], scale=-1.0), reads=[RF.b, MP.b], writes=[RF.b])
                    kb.op("act", lambda e: e.activation(out=RF[:], in_=RF[:], func=AF.Ln, bias=1.0), reads=[RF.b], writes=[RF.b])
                    kb.op("dve", lambda e: e.tensor_scalar(out=RF[:], in0=RF[:], scalar1=-1.0, scalar2=None, op0=ALU.mult), reads=[RF.b], writes=[RF.b])
                    kb.op("dve", lambda e: e.tensor_tensor_scan(out=BC[0:32, :], data0=RM[0:32, :], data1=RF[0:32, :], initial=0.0, op0=ALU.mult, op1=ALU.add), reads=[RM.b, RF.b], writes=[BC.b])
                    kb.op("dve", lambda e: e.tensor_tensor_scan(out=BC[32:64, ::-1], data0=RM[32:64, ::-1], data1=RF[32:64, ::-1], initial=0.0, op0=ALU.mult, op1=ALU.add), reads=[RM.b, RF.b], writes=[BC.b])
                    kb.op("dve", lambda e: e.tensor_tensor(out=RI[:], in0=RI[:], in1=BC[:], op=ALU.subtract), reads=[RI.b, BC.b], writes=[RI.b])
                    kb.op("dve", lambda e: e.tensor_tensor_scan(out=CM[0:32, :], data0=RMA[0:32, :], data1=RI[0:32, :], initial=-1e30, op0=ALU.add, op1=ALU.max), reads=[RMA.b, RI.b], writes=[CM.b])
                    kb.op("dve", lambda e: e.tensor_tensor_scan(out=CM[32:64, ::-1], data0=RMA[32:64, ::-1], data1=RI[32:64, ::-1], initial=-1e30, op0=ALU.add, op1=ALU.max), reads=[RMA.b, RI.b], writes=[CM.b])
                    for dr in range(2):
                        pr = slice(dr * 32, dr * 32 + 32)
                        lastpos = 63 if dr == 0 else 0
                        kb.op("dve", lambda e, pr=pr, lastpos=lastpos: e.tensor_copy(out=CH[pr, 0, :], in_=BC[pr, :].rearrange("p (c s) -> p c s", s=64)[:, :, lastpos]), reads=[BC.b], writes=[CH.b])
                        kb.op("dve", lambda e, pr=pr, lastpos=lastpos: e.tensor_copy(out=CH[pr, 1, :], in_=CM[pr, :].rearrange("p (c s) -> p c s", s=64)[:, :, lastpos]), reads=[CM.b], writes=[CH.b])
                    B_ = [CH.b]
                    kb.op("dve", lambda e: e.tensor_tensor(out=CH[:, 2, :], in0=CH[:, 0, :], in1=CH[:, 1, :], op=ALU.add), reads=B_, writes=B_)
                    kb.op("dve", lambda e: e.tensor_tensor_scan(out=CH[0:32, 3, :], data0=CH[0:32, 0, :], data1=CH[0:32, 2, :], initial=0.0, op0=ALU.add, op1=ALU.max), reads=B_, writes=B_)
                    kb.op("dve", lambda e: e.tensor_tensor_scan(out=CH[32:64, 3, 0:4][:, ::-1], data0=CH[32:64, 0, 0:4][:, ::-1], data1=CH[32:64, 2, 0:4][:, ::-1], initial=0.0, op0=ALU.add, op1=ALU.max), reads=B_, writes=B_)
                    kb.op("dve", lambda e: e.tensor_tensor_scan(out=CH[32:64, 3, 4:NCK][:, ::-1], data0=CH[32:64, 0, 4:NCK][:, ::-1], data1=CH[32:64, 2, 4:NCK][:, ::-1], initial=CH[32:64, 3, 0:1], op0=ALU.add, op1=ALU.max), reads=B_, writes=B_)
                    kb.op("dve", lambda e: e.memset(CH[:, 4, :], 0.0), reads=B_, writes=B_)
                    kb.op("dve", lambda e: e.tensor_copy(out=CH[0:32, 4, 1:NCK], in_=CH[0:32, 3, 0:NCK - 1]), reads=B_, writes=B_)
                    kb.op("dve", lambda e: e.tensor_copy(out=CH[32:64, 4, 0:3], in_=CH[32:64, 3, 1:4]), reads=B_, writes=B_)
                    kb.op("dve", lambda e: e.tensor_copy(out=CH[32:64, 4, 4:NCK - 1], in_=CH[32:64, 3, 5:NCK]), reads=B_, writes=B_)
                    kb.op("dve", lambda e: e.tensor_copy(out=CH[32:64, 4, NCK - 1:NCK], in_=CH[32:64, 3, 0:1]), reads=B_, writes=B_)
                    kb.op("dve", lambda e: e.tensor_tensor(out=CH[:, 5, :], in0=CH[:, 0, :], in1=CH[:, 4, :], op=ALU.add), reads=B_, writes=B_)
                    kb.op("dve", lambda e: e.tensor_tensor(out=CH[:, 5, :], in0=CH[:, 5, :], in1=CH[:, 3, :], op=ALU.subtract), reads=B_, writes=B_)
                    kb.op("dve", lambda e: e.tensor_tensor(out=CH[:, 6, :], in0=CH[:, 2, :], in1=CH[:, 3, :], op=ALU.subtract), reads=B_, writes=B_)
                    kb.op("act", lambda e: e.activation(out=CH[:, 5:7, :], in_=CH[:, 5:7, :], func=AF.Exp), reads=B_, writes=B_)
                    for c in range(NCK):
                        kb.op("dve", lambda e, c=c: e.tensor_scalar(out=RF[:, c * 64:(c + 1) * 64], in0=CM[:, c * 64:(c + 1) * 64], scalar1=-1.0, scalar2=CH[:, 4, c:c + 1],
                                                                     op0=ALU.mult, op1=ALU.add), reads=[CM.b, CH.b], writes=[RF.b])
                        kb.op("dve", lambda e, c=c: e.tensor_scalar(out=X2[:, c * 64:(c + 1) * 64], in0=RI[:, c * 64:(c + 1) * 64], scalar1=CH[:, 1, c:c + 1], scalar2=None,
                                                                     op0=ALU.subtract), reads=[RI.b, CH.b], writes=[X2.b])
                    kb.op("act", lambda e: e.activation(out=X2[:], in_=X2[:], func=AF.Exp), reads=[X2.b], writes=[X2.b])
                    kb.op("dve", lambda e: e.tensor_tensor(out=BC[:], in0=BC[:], in1=CM[:], op=ALU.add), reads=[BC.b, CM.b], writes=[BC.b])
                    kb.op("dve", lambda e: e.scalar_tensor_tensor(out=BC[:], in0=RF[:], scalar=0.0, in1=BC[:], op0=ALU.max, op1=ALU.add), reads=[RF.b, BC.b], writes=[BC.b])
                    kb.op("act", lambda e: e.activation(out=BC[:], in_=BC[:], func=AF.Exp, scale=-1.0), reads=[BC.b], writes=[BC.b])
                    kb.op("dve", lambda e: e.tensor_scalar(out=X1[:], in0=RF[:], scalar1=0.0, scalar2=None, op0=ALU.min), reads=[RF.b], writes=[X1.b])
                    kb.op("act", lambda e: e.activation(out=X1[:], in_=X1[:], func=AF.Exp), reads=[X1.b], writes=[X1.b])
                    kb.op("dve", lambda e: e.tensor_scalar(out=RF[:], in0=RF[:], scalar1=-1.0, scalar2=0.0, op0=ALU.mult, op1=ALU.min), reads=[RF.b], writes=[RF.b])
                    kb.op("act", lambda e: e.activation(out=RF[:], in_=RF[:], func=AF.Exp), reads=[RF.b], writes=[RF.b])
                    for c in range(NCK):
                        pcol = banks[c % 4]
                        for qi, row in enumerate((X1, RF, BC, X2)):
                            kb.op("pe", lambda e, row=row, qi=qi, pcol=pcol, c=c: e.matmul(out=pcol[0:64, qi * 12:(qi + 1) * 12], lhsT=row[:, c * 64:(c + 1) * 64],
                                                                                       rhs=SEL[:, :, 0], start=True, stop=True), reads=[row.b, SEL.b], writes=[pcol.b])
                        kb.op("act", lambda e, pcol=pcol, c=c: e.activation(out=COL[:, c, :, :].rearrange("p a b -> p (a b)"), in_=pcol[0:64, 0:48], func=AF.Identity),
                              writes=[COL.b, pcol.b])
                    for qi in range(2):
                        for r in range(12):
                            pcol = banks[4 + (qi * 12 + r) % 2]
                            kb.op("pe", lambda e, r=r, pcol=pcol, qi=qi: e.matmul(out=pcol[:, 0:NCK], lhsT=SEL[:, r, :], rhs=CH[:, 5 + qi, :], start=True, stop=True), reads=[SEL.b, CH.b], writes=[pcol.b])
                            kb.op("act", lambda e, r=r, pcol=pcol, qi=qi: e.activation(out=AOB[:, qi, r, :], in_=pcol[:, 0:NCK], func=AF.Identity), writes=[AOB.b, pcol.b])
                    kb.barrier()
                psn = [0]

                def pget():
                    bk = banks[psn[0] % 8]
                    psn[0] += 1
                    return bk
                RAW = sb(ph, "mRAW", [128, T])
                QT = sb(ph, "mQT", [64, T]); KT = sb(ph, "mKT", [64, T]); VT = sb(ph, "mVT", [128, T])
                VA = sb(ph, "mVA", [64, NCK, 132])
                HACC = sb(ph, "mHACC", [64, NCK, 128]); OUT = sb(ph, "mOUT", [128, T], BF16)
                CAd = [sb(ph, "mCA%d" % i, [64, 132]) for i in range(2)]
                G = 4
                U = []
                for u in range(2 * G):
                    d_ = {}
                    for nm, shp in (("E", [64, 64]), ("wk", [64, 64]), ("tmp", [64, 132]), ("comb", [64, 132]), ("sc", [64, 4])):
                        d_[nm] = sb(ph, "mu%d%s" % (u, nm), shp)
                    U.append(d_)
                fin = [sb(ph, "mfin%d" % i, [64, 128]) for i in range(2)]
                fst = [sb(ph, "mfst%d" % i, [64, 4]) for i in range(2)]
                kb.op("dve", lambda e: e.memset(VA[:], 1.0), writes=[VA.b])
                for hd in range(6):
                    kb.dma("sp", RAW[0:64, :], projT[MQ + hd * 64:MQ + (hd + 1) * 64, :], writes=[RAW.b])
                    seqperm("act", QT, QT.b, RAW, RAW.b, 64)
                    kb.op("act", lambda e: e.activation(out=QT[:], in_=QT[:], func=AF.Identity, scale=0.125), reads=[QT.b], writes=[QT.b])
                    kb.dma("sp", RAW[0:64, :], projT[MK + hd * 64:MK + (hd + 1) * 64, :], reads=[QT.b], writes=[RAW.b])
                    seqperm("dve", KT, KT.b, RAW, RAW.b, 64)
                    kb.dma("sp", RAW[:], projT[MV + hd * 128:MV + (hd + 1) * 128, :], reads=[KT.b], writes=[RAW.b])
                    seqperm("act", VT, VT.b, RAW, RAW.b, 128)
                    for c in range(NCK):
                        bk = pget()
                        kb.op("pe", lambda e, bk=bk, c=c: e.transpose(out=bk[0:64, 0:128], in_=VT[:, c * 64:(c + 1) * 64], identity=ident[:]), reads=[VT.b, ident.b], writes=[bk.b])
                        if c % 2 == 0:
                            kb.op("act", lambda e, bk=bk, c=c: e.activation(out=VA[:, c, 0:128], in_=bk[0:64, 0:128], func=AF.Identity), writes=[VA.b, bk.b])
                        else:
                            kb.op("dve", lambda e, bk=bk, c=c: e.tensor_copy(out=VA[:, c, 0:128], in_=bk[0:64, 0:128]), writes=[VA.b, bk.b])
                    kb.op("dve", lambda e: e.memset(HACC[:], 0.0), writes=[HACC.b])
                    for dr in range(2):
                        kb.op("dve", lambda e, dr=dr: e.memset(CAd[dr][:], 0.0), writes=[CAd[dr].b])
                    orders = [list(range(NCK)), list(range(3, -1, -1)) + list(range(NCK - 1, 3, -1))]
                    for g0 in range(0, NCK, G):
                        units = []
                        for dr in range(2):
                            for u, c in enumerate(orders[dr][g0:g0 + G]):
                                units.append((dr, c, U[dr * G + u]))
                        for (dr, c, d_) in units:
                            r = dr * 6 + hd
                            m_it = 3 if dr == 0 else 2
                            ch = slice(c * 64, (c + 1) * 64)
                            be = pget()
                            kb.op("pe", lambda e, be=be, ch=ch, r=r: e.matmul(out=be[0:64, 0:64], lhsT=RI[:, ch], rhs=SEL[:, r, 0:64], start=True, stop=False), reads=[RI.b, SEL.b], writes=[be.b], sig=False)
                            kb.op("pe", lambda e, be=be, ch=ch, r=r: e.matmul(out=be[0:64, 0:64], lhsT=NSEL[:, r, 0:64], rhs=CM[:, ch], start=False, stop=False), reads=[CM.b, NSEL.b], writes=[be.b], sig=False)
                            kb.op("pe", lambda e, be=be, m_it=m_it: e.matmul(out=be[0:64, 0:64], lhsT=I64, rhs=MB[:, m_it, :], start=False, stop=True), reads=[MB.b, ident.b], writes=[be.b])
                            kb.op("act", lambda e, be=be, d_=d_: e.activation(out=d_["E"][:], in_=be[0:64, 0:64], func=AF.Exp), writes=[d_["E"].b, be.b])
                            bp = pget()
                            kb.op("pe", lambda e, bp=bp, ch=ch: e.matmul(out=bp[0:64, 0:64], lhsT=KT[:, ch], rhs=QT[:, ch], start=True, stop=True), reads=[KT.b, QT.b], writes=[bp.b])
                            kb.op("pe", lambda e, bp=bp, ch=ch: e.transpose(out=bp[0:64, 64:128], in_=KT[:, ch], identity=I64), reads=[KT.b, ident.b], writes=[bp.b])
                            kb.op("dve", lambda e, bp=bp, d_=d_: e.tensor_tensor(out=d_["E"][:], in0=bp[0:64, 0:64], in1=d_["E"][:], op=ALU.mult), reads=[d_["E"].b], writes=[d_["E"].b, bp.b])
                            kb.op("dve", lambda e, bp=bp, d_=d_, c=c, r=r: e.tensor_scalar(out=d_["wk"][:], in0=bp[0:64, 64:128], scalar1=COL[:, c, 3, r:r + 1], scalar2=None, op0=ALU.mult),
                                  reads=[COL.b], writes=[d_["wk"].b, bp.b])
                        for u in range(G):
                            for dr in range(2):
                                if dr * G + u >= len(units):
                                    continue
                                dr_, c, d_ = units[dr * G + u]
                                r = dr * 6 + hd
                                CA = CAd[dr]
                                ch = slice(c * 64, (c + 1) * 64)
                                b1 = pget()
                                kb.op("pe", lambda e, b1=b1, ch=ch, CA=CA: e.matmul(out=b1[0:64, 0:129], lhsT=QT[:, ch], rhs=CA[:, 0:129], start=True, stop=True), reads=[QT.b, CA.b], writes=[b1.b])
                                kb.op("act", lambda e, b1=b1, d_=d_, c=c, r=r: e.activation(out=d_["tmp"][:, 0:129], in_=b1[0:64, 0:129], func=AF.Identity, scale=COL[:, c, 0, r:r + 1]),
                                      reads=[COL.b], writes=[d_["tmp"].b, b1.b])
                                b2 = pget()
                                kb.op("pe", lambda e, b2=b2, d_=d_, c=c: e.matmul(out=b2[0:64, 0:129], lhsT=d_["E"][:], rhs=VA[:, c, 0:129], start=True, stop=True), reads=[d_["E"].b, VA.b], writes=[b2.b])
                                kb.op("dve", lambda e, b2=b2, d_=d_, c=c, r=r: e.scalar_tensor_tensor(out=d_["comb"][:, 0:129], in0=b2[0:64, 0:129], scalar=COL[:, c, 1, r:r + 1], in1=d_["tmp"][:, 0:129],
                                                                                              op0=ALU.mult, op1=ALU.add), reads=[COL.b, d_["tmp"].b], writes=[d_["comb"].b, b2.b])
                                kb.op("act", lambda e, d_=d_: e.activation(out=d_["sc"][:, 2:3], in_=d_["comb"][:, 128:129], func=AF.Abs), reads=[d_["comb"].b], writes=[d_["sc"].b])
                                kb.op("dve", lambda e, d_=d_, c=c, r=r: e.tensor_scalar(out=d_["sc"][:, 0:1], in0=d_["sc"][:, 2:3], scalar1=COL[:, c, 2, r:r + 1], scalar2=None, op0=ALU.max),
                                      reads=[COL.b, d_["sc"].b], writes=[d_["sc"].b])
                                kb.op("dve", lambda e, d_=d_: e.reciprocal(out=d_["sc"][:, 1:2], in_=d_["sc"][:, 0:1]), reads=[d_["sc"].b], writes=[d_["sc"].b])
                                kb.op("dve", lambda e, d_=d_, c=c: e.scalar_tensor_tensor(out=HACC[:, c, :], in0=d_["comb"][:, 0:128], scalar=d_["sc"][:, 1:2], in1=HACC[:, c, :], op0=ALU.mult, op1=ALU.add),
                                      reads=[d_["comb"].b, d_["sc"].b, HACC.b], writes=[HACC.b])
                                b3 = pget()
                                kb.op("pe", lambda e, b3=b3, d_=d_, c=c: e.matmul(out=b3[0:64, 0:129], lhsT=d_["wk"][:], rhs=VA[:, c, 0:129], start=True, stop=True), reads=[d_["wk"].b, VA.b], writes=[b3.b])
                                kb.op("act", lambda e, c=c, r=r, CA=CA: e.activation(out=CA[:, 0:129], in_=CA[:, 0:129], func=AF.Identity, scale=AOB[0:64, 0, r, c:c + 1]), reads=[AOB.b, CA.b], writes=[CA.b])
                                kb.op("dve", lambda e, b3=b3, c=c, r=r, CA=CA: e.scalar_tensor_tensor(out=CA[:, 0:129], in0=b3[0:64, 0:129], scalar=AOB[0:64, 1, r, c:c + 1], in1=CA[:, 0:129], op0=ALU.mult, op1=ALU.add),
                                      reads=[AOB.b, CA.b], writes=[CA.b, b3.b])
                    kb.dma("sp", RAW[:], projT[MO + hd * 128:MO + (hd + 1) * 128, :], reads=[VT.b], writes=[RAW.b])
                    seqperm("act", VT, VT.b, RAW, RAW.b, 128)
                    kb.op("act", lambda e: e.activation(out=VT[:], in_=VT[:], func=AF.Sigmoid), reads=[VT.b], writes=[VT.b])
                    for c in range(NCK):
                        f_ = fin[c % 2]; s_ = fst[c % 2]
                        kb.op("act", lambda e, f_=f_, s_=s_, c=c: e.activation(out=f_[:], in_=HACC[:, c, :], func=AF.Square, accum_out=s_[:, 0:1]), reads=[HACC.b], writes=[f_.b, s_.b])
                        kb.op("act", lambda e, s_=s_: e.activation(out=s_[:, 1:2], in_=s_[:, 0:1], func=AF.Sqrt, bias=EPS, scale=1.0 / 128), reads=[s_.b], writes=[s_.b])
                        kb.op("dve", lambda e, s_=s_: e.reciprocal(out=s_[:, 2:3], in_=s_[:, 1:2]), reads=[s_.b], writes=[s_.b])
                        kb.op("dve", lambda e, f_=f_, s_=s_, c=c: e.scalar_tensor_tensor(out=f_[:], in0=HACC[:, c, :], scalar=s_[:, 2:3], in1=NORM[:], op0=ALU.mult, op1=ALU.mult),
                              reads=[HACC.b, s_.b, NORM.b], writes=[f_.b])
                        bk = pget()
                        kb.op("pe", lambda e, bk=bk, f_=f_: e.transpose(out=bk[:, 0:64], in_=f_[:], identity=I64), reads=[f_.b, ident.b], writes=[bk.b])
                        kb.op("dve", lambda e, bk=bk, c=c: e.tensor_tensor(out=RAW[:, c * 64:(c + 1) * 64], in0=bk[:, 0:64], in1=VT[:, c * 64:(c + 1) * 64], op=ALU.mult),
                              reads=[VT.b], writes=[RAW.b, bk.b])
                    kb.op("act", lambda e: e.activation(out=OUT[:, 0:NCTX], in_=RAW[:, 0:NCTX], func=AF.Identity), reads=[RAW.b], writes=[OUT.b])
                    kb.op("act", lambda e: e.activation(out=OUT[:, NCTX:T].rearrange("p (r c) -> p c r", c=64), in_=RAW[:, NCTX:T].rearrange("p (c r) -> p c r", r=32), func=AF.Identity),
                          reads=[RAW.b], writes=[OUT.b])
                    kb.dma("sp", mixT[1280 + hd * 128:1280 + (hd + 1) * 128, :], OUT[:], reads=[OUT.b])
                kb.barrier()
            if stage <= 4:
                break
            with ExitStack() as ph:
                last = (l == L - 1)
                mixb = sb(ph, "mixb", [128, KC, 512], BF16)
                yTb = sb(ph, "yTb", [128, KC, 512])
                h2T = sb(ph, "h2T", [128, KC, 512], BF16)
                hidT = sb(ph, "hidT", [128, 44, 512], BF16)
                xt = [sb(ph, "xt%d" % i, [128, D]) for i in range(2)]
                GG = [[sb(ph, "GG%d%d" % (a, w_), [128, D]) for w_ in range(2)] for a in range(2)]
                wk = [sb(ph, "wk%d" % i, [128, KC, 128], BF16) for i in range(4)]
                wd = [sb(ph, "wd%d" % i, [128, 44, 128], BF16) for i in range(2)]
                sgt = [sb(ph, "sgt%d" % i, [128, 512]) for i in range(2)]
                junk = sb(ph, "junk2", [128, 512], BF16)
                stt_ = [sb(ph, "st2%d" % i, [128, 8]) for i in range(2)]
                bc = sb(ph, "bc", [128, 128])
                for a, (mi, gi) in enumerate(((2, 1), (5, 3))):
                    for w_ in range(2):
                        for j in range(KC):
                            kb.op("dve", lambda e, mi=mi, gi=gi, w_=w_, j=j: e.tensor_tensor(
                                out=bc[:, 0:1], in0=modsT[:, mi * KC + j, w_:w_ + 1], in1=gT[:, (gi * L + l) * KC + j:(gi * L + l) * KC + j + 1],
                                op=ALU.mult), reads=[modsT.b, gT.b], writes=[bc.b])
                            kb.op("dve", lambda e: e.tensor_copy(out=bc[:, 1:128], in_=bc[:, 0:1].to_broadcast([128, 127])), reads=[bc.b], writes=[bc.b])
                            pb = banks[j % 4]
                            kb.op("pe", lambda e, pb=pb: e.transpose(out=pb[:, 0:128], in_=bc[:], identity=ident[:]), reads=[bc.b, ident.b], writes=[pb.b])
                            kb.op("act", lambda e, pb=pb, a=a, w_=w_, j=j: e.activation(out=GG[a][w_][:, j * 128:(j + 1) * 128], in_=pb[:, 0:128], func=AF.Identity),
                                  reads=[pb.b], writes=[GG[a][w_].b])
                mixv = mixT.rearrange("(k p) t -> p k t", p=128)
                tstart = NCTX if last else 0
                nwk = [0]
                nx = [0]

                def post_res(tglob, tloc, a, dst):
                    w_ = 1 if tglob < 2 else 0
                    x_ = xt[nx[0] % 2]
                    s_ = stt_[nx[0] % 2]
                    nx[0] += 1
                    kb.dma("sp", x_[:], xres[tglob * 128:(tglob + 1) * 128, :], writes=[x_.b])
                    for jb in range(4):
                        pb = banks[jb]
                        for jj in range(4):
                            j = jb * 4 + jj
                            kb.op("pe", lambda e, pb=pb, jj=jj, j=j: e.transpose(out=pb[:, jj * 128:(jj + 1) * 128], in_=yTb[:, j, tloc * 128:(tloc + 1) * 128],
                                                                              identity=ident[:]), reads=[yTb.b, ident.b], writes=[pb.b])
                        kb.op("act", lambda e, pb=pb, jb=jb, s_=s_: e.activation(out=junk[:], in_=pb[:], func=AF.Square, accum_out=s_[:, jb:jb + 1]),
                              reads=[pb.b], writes=[junk.b, s_.b])
                    kb.op("dve", lambda e, s_=s_: e.tensor_reduce(out=s_[:, 4:5], in_=s_[:, 0:4], axis=AX.X, op=ALU.add), reads=[s_.b], writes=[s_.b])
                    kb.op("act", lambda e, s_=s_: e.activation(out=s_[:, 5:6], in_=s_[:, 4:5], func=AF.Sqrt, bias=EPS, scale=1.0 / D), reads=[s_.b], writes=[s_.b])
                    kb.op("dve", lambda e, s_=s_: e.reciprocal(out=s_[:, 6:7], in_=s_[:, 5:6]), reads=[s_.b], writes=[s_.b])
                    for jb in range(4):
                        pb = banks[jb]
                        sg_ = sgt[jb % 2]
                        kb.op("dve", lambda e, pb=pb, sg_=sg_, jb=jb, s_=s_: e.scalar_tensor_tensor(
                            out=sg_[:], in0=pb[:], scalar=s_[:, 6:7], in1=GG[a][w_][:, jb * 512:(jb + 1) * 512], op0=ALU.mult, op1=ALU.mult),
                            reads=[pb.b, s_.b, GG[a][w_].b], writes=[sg_.b])
                        kb.op("dve", lambda e, sg_=sg_, jb=jb, x_=x_: e.tensor_tensor(out=x_[:, jb * 512:(jb + 1) * 512], in0=x_[:, jb * 512:(jb + 1) * 512], in1=sg_[:],
                                                                                op=ALU.add), reads=[sg_.b, x_.b], writes=[x_.b])
                    kb.dma("sp", dst, x_[:], reads=[x_.b])
                    return x_, s_, w_

                def dense(src, nk, wview, j0, nj, wbufs, t_nt, consume):
                    for j in range(j0, j0 + nj):
                        w = wbufs[nwk[0] % len(wbufs)]
                        nwk[0] += 1
                        src_ap, src_b = wview[j]
                        kb.dma("sp" if nwk[0] % 2 == 0 else "act", w[:, :nk, :], src_ap, reads=[src_b], writes=[w.b])
                        pj = banks[4 + nwk[0] % 2]
                        for k in range(nk):
                            kb.op("pe", lambda e, pj=pj, w=w, k=k: e.matmul(out=pj[:, :t_nt], lhsT=w[:, k, :], rhs=src[:, k, :t_nt], start=(k == 0), stop=(k == nk - 1)),
                                  reads=[w.b, src.b], writes=[pj.b], sig=(k == nk - 1))
                        consume(j, pj)

                t0 = tstart
                while t0 < T:
                    nt = min(512, T - t0)
                    ntl = nt // 128
                    kb.dma("sp", mixb[:, :, :nt], mixv[:, :, t0:t0 + nt], writes=[mixb.b])

                    def cons_y(j, pj):
                        if j % 2 == 0:
                            kb.op("act", lambda e: e.activation(out=yTb[:, j, :nt], in_=pj[:, :nt], func=AF.Identity), reads=[pj.b], writes=[yTb.b])
                        else:
                            kb.op("dve", lambda e: e.tensor_copy(out=yTb[:, j, :nt], in_=pj[:, :nt]), reads=[pj.b], writes=[yTb.b])
                    dense(mixb, KC, pre["wo"], 0, KC, wk, nt, cons_y)
                    for tl in range(ntl):
                        tg = t0 // 128 + tl
                        x_, s_, w_ = post_res(tg, tl, 0, xres[tg * 128:(tg + 1) * 128, :])
                        kb.op("act", lambda e, x_=x_, s_=s_: e.activation(out=junk[:], in_=x_[:, 0:512], func=AF.Square, accum_out=s_[:, 0:1]), reads=[x_.b], writes=[junk.b, s_.b])
                        for q in range(1, 4):
                            kb.op("act", lambda e, x_=x_, s_=s_, q=q: e.activation(out=junk[:], in_=x_[:, q * 512:(q + 1) * 512], func=AF.Square, accum_out=s_[:, q:q + 1]),
                                  reads=[x_.b], writes=[junk.b, s_.b])
                        kb.op("dve", lambda e, s_=s_: e.tensor_reduce(out=s_[:, 4:5], in_=s_[:, 0:4], axis=AX.X, op=ALU.add), reads=[s_.b], writes=[s_.b])
                        kb.op("act", lambda e, s_=s_: e.activation(out=s_[:, 5:6], in_=s_[:, 4:5], func=AF.Sqrt, bias=EPS, scale=1.0 / D), reads=[s_.b], writes=[s_.b])
                        kb.op("dve", lambda e, s_=s_: e.reciprocal(out=s_[:, 6:7], in_=s_[:, 5:6]), reads=[s_.b], writes=[s_.b])
                        kb.op("dve", lambda e, x_=x_, s_=s_: e.tensor_scalar(out=x_[:], in0=x_[:], scalar1=s_[:, 6:7], scalar2=None, op0=ALU.mult), reads=[x_.b, s_.b], writes=[x_.b])
                        for jb in range(4):
                            pt = banks[jb]
                            for jj in range(4):
                                j = jb * 4 + jj
                                kb.op("pe", lambda e, pt=pt, x_=x_, jj=jj, j=j: e.transpose(out=pt[:, jj * 128:(jj + 1) * 128], in_=x_[:, j * 128:(j + 1) * 128], identity=ident[:]),
                                      reads=[x_.b, ident.b], writes=[pt.b])
                            for jj in range(4):
                                j = jb * 4 + jj
                                kb.op("act", lambda e, pt=pt, jj=jj, j=j, tl=tl, w_=w_: e.activation(
                                    out=h2T[:, j, tl * 128:(tl + 1) * 128], in_=pt[:, jj * 128:(jj + 1) * 128], func=AF.Identity,
                                    bias=modsT[:, 3 * KC + j, w_:w_ + 1], scale=gsf[:, j, w_:w_ + 1]), reads=[pt.b, modsT.b, gsf.b], writes=[h2T.b])
                    for hc in range(44):
                        got = {}

                        def cons_g(j, pj):
                            got["g"] = pj
                        dense(h2T, KC, pre["wg"], hc, 1, wk, nt, cons_g)
                        pg = got["g"]
                        sg_ = sgt[hc % 2]
                        kb.op("act", lambda e, pg=pg, sg_=sg_: e.activation(out=sg_[:, :nt], in_=pg[:, :nt], func=AF.Silu), reads=[pg.b], writes=[sg_.b])

                        def cons_u(j, pj):
                            kb.op("dve", lambda e: e.tensor_tensor(out=hidT[:, hc, :nt], in0=pj[:, :nt], in1=sg_[:, :nt], op=ALU.mult), reads=[pj.b, sg_.b], writes=[hidT.b])
                        dense(h2T, KC, pre["wu"], hc, 1, wk, nt, cons_u)
                    dense(hidT, 44, pre["wd"], 0, KC, wd, nt, cons_y)
                    for tl in range(ntl):
                        tg = t0 // 128 + tl
                        dst = out_d[(tg - 2) * 128:(tg - 1) * 128, :] if last else xres[tg * 128:(tg + 1) * 128, :]
                        post_res(tg, tl, 1, dst)
                    t0 += nt
                kb.barrier()
            if stage == 5:
                kb.dma("sp", xres_o, xres)
                break
        kb.barrier()
    return nc


def kernel(**inp):
    inp = {k: np.asarray(v) for k, v in inp.items()}
    nc = build(99)
    base = host_prep(inp, 0)
    in_maps = []
    for core in range(8):
        b = core % 4
        m = dict(base)
        if b != 0:
            pb = host_prep_batch(inp, b)
            m.update(pb)
        in_maps.append(m)
    res = run_bass_kernel_spmd(nc, in_maps, core_ids=list(range(8)))
    out = np.stack([np.asarray(res.results[b]["out"]) for b in range(4)], 0).astype(np.float32)
    return out
```

```python
import numpy as np
from contextlib import ExitStack
import concourse.bass as bass
import concourse.mybir as mybir
from concourse.bass_utils import run_bass_kernel_spmd

F32 = mybir.dt.float32
BF16 = mybir.dt.bfloat16
ALU = mybir.AluOpType
AF = mybir.ActivationFunctionType
AX = mybir.AxisListType

D = 2048
T = 2304
NCTX = 256
NLAT = 2048
L = 2
KC = 16
IN_COLS = 5936
FFN = 5632
EPS = 1e-6
NEG = -30000.0
SEM_ROT = 30000
NSLOT = 6
LC = 128


class Ev:
    __slots__ = ("sem", "val")

    def __init__(self, sem, val):
        self.sem = sem
        self.val = val


class Buf:
    __slots__ = ("w", "r", "name")

    def __init__(self, name=""):
        self.w = None
        self.r = {}
        self.name = name


class Eng:
    def __init__(self, kb, name, h):
        self.kb = kb
        self.name = name
        self.h = h
        self.sem = kb.newsem("e_" + name)
        self.count = 0
        self.seen = {}
        self.n = 0
        self.pending = 0


class Slot:
    def __init__(self, sem):
        self.sem = sem
        self.uses = 0


class KB:
    def __init__(self, nc, es):
        self.nc = nc
        self.es = es
        self.nsem = 0
        self.E = {}
        for name, h in (("pe", nc.tensor), ("act", nc.scalar), ("dve", nc.vector), ("pool", nc.gpsimd), ("sp", nc.sync)):
            self.E[name] = Eng(self, name, h)
        self.slots = {}
        self.rr = {}
        for q in ("sp", "pool", "act"):
            self.slots[q] = [Slot(self.newsem("d_%s%d" % (q, i))) for i in range(NSLOT)]
            self.rr[q] = 0

    def newsem(self, name):
        self.nsem += 1
        return self.es.enter_context(self.nc.semaphore("%s_%d" % (name, self.nsem)))

    def _wait(self, eng, ev):
        k = id(ev.sem)
        if eng.seen.get(k, 0) < ev.val:
            eng.h.wait_ge(ev.sem, ev.val)
            eng.seen[k] = ev.val

    def _deps(self, eng, reads, writes):
        need = {}

        def add(ev):
            k = id(ev.sem)
            if k not in need or need[k].val < ev.val:
                need[k] = ev

        for b in reads:
            if b.w is not None:
                add(b.w)
        for b in writes:
            if b.w is not None:
                add(b.w)
            for ev in b.r.values():
                add(ev)
        for ev in need.values():
            if eng.name == "pe" and ev.sem is eng.sem:
                continue
            self._wait(eng, ev)

    def _post(self, ev, reads, writes):
        k = id(ev.sem)
        for b in reads:
            b.r[k] = ev
        for b in writes:
            b.w = ev
            b.r = {}

    def op(self, e, fn, reads=(), writes=(), sig=True):
        eng = self.E[e]
        self._deps(eng, reads, writes)
        if eng.count >= SEM_ROT and eng.pending == 0:
            eng.sem = self.newsem("e_" + eng.name)
            eng.count = 0
        inst = fn(eng.h)
        eng.n += 1
        if sig:
            eng.count += 1
            eng.pending = 0
            inst.then_inc(eng.sem, 1)
            ev = Ev(eng.sem, eng.count)
        else:
            assert e == "pe"
            eng.pending += 1
            ev = Ev(eng.sem, eng.count + 1)
        self._post(ev, reads, writes)
        return ev

    def dma(self, q, out, in_, reads=(), writes=(), **kw):
        eng = self.E[q]
        self._deps(eng, reads, writes)
        sl = self.slots[q][self.rr[q] % NSLOT]
        self.rr[q] += 1
        if sl.uses * 16 >= SEM_ROT:
            self._wait(eng, Ev(sl.sem, 16 * sl.uses))
            sl.sem = self.newsem("d_" + q)
            sl.uses = 0
        if sl.uses > 0:
            self._wait(eng, Ev(sl.sem, 16 * sl.uses))
        inst = eng.h.dma_start(out=out, in_=in_, **kw)
        sl.uses += 1
        inst.then_inc(sl.sem, 16)
        ev = Ev(sl.sem, 16 * sl.uses)
        self._post(ev, reads, writes)
        return ev

    def barrier(self, engines=("pe", "act", "dve", "pool", "sp")):
        evs = []
        for e in self.E.values():
            assert e.pending == 0
            if e.count > 0:
                evs.append(Ev(e.sem, e.count))
        for q in self.slots:
            for sl in self.slots[q]:
                if sl.uses > 0:
                    evs.append(Ev(sl.sem, 16 * sl.uses))
        for en in engines:
            eng = self.E[en]
            for ev in evs:
                self._wait(eng, ev)


class Tl:
    def __init__(self, t, name=""):
        self.t = t
        self.b = Buf(name)

    def __getitem__(self, idx):
        return self.t[idx]


def host_prep_batch(inp, b):
    f = np.float32
    m = {}
    m["xin"] = np.ascontiguousarray(np.concatenate([inp["ctx"][b], inp["x"][b]], axis=0).astype(f))
    cv = np.stack([inp["c"][b].reshape(KC, 128).T, inp["c_ctx"].reshape(KC, 128).T], axis=-1)
    m["cv"] = np.ascontiguousarray(cv.astype(f))
    return m


def host_prep(inp, b):
    f = np.float32
    m = host_prep_batch(inp, b)
    m["ada_w"] = inp["ada_w"]
    m["ada_bT"] = np.ascontiguousarray(inp["ada_b"].reshape(L, 96, 128).transpose(2, 0, 1).reshape(128, L * 96).astype(f))
    g = np.stack([inp["norm_mix_pre"], inp["norm_mix_post"], inp["norm_ffn_pre"], inp["norm_ffn_post"]], 0)
    m["gT"] = np.ascontiguousarray(g.reshape(4, L, KC, 128).transpose(3, 0, 1, 2).reshape(128, 4 * L * KC).astype(f))
    m["w_in"] = inp["w_in"]
    for k_ in ("w_out", "ffn_w_gate", "ffn_w_up", "ffn_w_down"):
        m[k_] = inp[k_]
    m["ident"] = np.eye(128, dtype=f)
    def st_major(a):
        return np.ascontiguousarray(a.reshape(L, 2, 16, 128).transpose(3, 0, 1, 2).reshape(128, L * 2 * 16).astype(f))
    m["s5_lre"] = st_major(inp["s5_lam_re"].reshape(L, 2, 2048))
    m["s5_lim"] = st_major(inp["s5_lam_im"].reshape(L, 2, 2048))
    m["s5_ldt"] = st_major(np.repeat(inp["s5_log_dt"], 64, axis=-1))
    Bb = np.zeros((128, L, 2, 2, 16, 128), f)
    Cb = np.zeros((128, L, 2, 2, 16, 128), f)
    for ci, (bn, cn) in enumerate((("s5_b_re", "s5_c_re"), ("s5_b_im", "s5_c_im"))):
        bsrc = inp[bn]
        csrc = inp[cn]
        for g in range(32):
            st = g // 2
            r0 = (g % 8) * 16
            c0 = (g % 2) * 64
            Bb[r0:r0 + 16, :, :, ci, st, c0:c0 + 64] = bsrc[:, :, g].transpose(3, 0, 1, 2)
            Cb[c0:c0 + 64, :, :, ci, st, r0:r0 + 16] = csrc[:, :, g].transpose(3, 0, 1, 2)
    m["s5_Bb"] = np.ascontiguousarray(Bb.reshape(128, L * 2 * 2 * 16, 128))
    m["s5_Cb"] = np.ascontiguousarray(Cb.reshape(128, L * 2 * 2 * 16, 128))
    m["s5_dT"] = np.ascontiguousarray(inp["s5_d"].reshape(L, 4, 128).transpose(2, 0, 1).reshape(128, L * 4).astype(f))
    m["s5_gbT"] = np.ascontiguousarray(inp["s5_glu_b"].reshape(L, 4, 128).transpose(2, 0, 1).reshape(128, L * 4).astype(f))
    m["s5_glu_w"] = inp["s5_glu_w"]
    tt = np.arange(T)
    rm = np.ones((64, T), f)
    rm[0:32, tt % 64 == 0] = 0.0
    rm[32:64, tt % 64 == 63] = 0.0
    m["rm"] = rm
    sel = np.zeros((64, 12, 128), f)
    for r_ in range(12):
        sel[(r_ // 6) * 32 + r_ % 6, r_, :] = 1.0
    m["sel"] = sel
    a_ = np.arange(64)[:, None]; b_ = np.arange(64)[None, :]
    mb = np.stack([np.where(b_ < a_, 0.0, NEG), np.where(b_ > a_, 0.0, NEG), np.where(b_ <= a_, 0.0, NEG), np.where(b_ >= a_, 0.0, NEG)], 1).astype(f)
    m["mb"] = np.ascontiguousarray(mb)
    m["gdn_normr"] = np.ascontiguousarray(np.tile(inp["gdn_norm"].reshape(1, L * 128), (64, 1)).astype(f))
    gp = np.zeros((64, L, 2), f)
    for dr_ in range(2):
        gp[dr_ * 32:dr_ * 32 + 6, :, 0] = inp["gdn_a_log"][:, dr_, :].T
        gp[dr_ * 32:dr_ * 32 + 6, :, 1] = inp["gdn_dt_bias"][:, dr_, :].T
    m["gdn_gp"] = np.ascontiguousarray(gp.reshape(64, L * 2))
    cw = inp["gdn_conv_w"].reshape(L, 3, 18, 128).transpose(3, 0, 2, 1)
    m["gdn_cw"] = np.ascontiguousarray(cw.reshape(128, L * 54).astype(f))
    rma = np.zeros((64, T), f)
    rma[0:32, tt % 64 == 0] = -1e30
    rma[32:64, tt % 64 == 63] = -1e30
    m["rma"] = rma
    m["ml_normr"] = np.ascontiguousarray(np.tile(inp["mlstm_norm"].reshape(1, L * 128), (64, 1)).astype(f))
    mp = np.zeros((64, L, 2), f)
    for dr_ in range(2):
        mp[dr_ * 32:dr_ * 32 + 6, :, 0] = inp["mlstm_i_bias"][:, dr_, :].T
        mp[dr_ * 32:dr_ * 32 + 6, :, 1] = inp["mlstm_f_bias"][:, dr_, :].T
    m["ml_mp"] = np.ascontiguousarray(mp.reshape(64, L * 2))
    m["tau1"] = np.ascontiguousarray(np.tile(np.arange(1, LC + 1, dtype=f)[None, :], (128, 1)))
    return m


def build(stage=99):
    nc = bass.Bass("TRN2", target_bir_lowering=False)
    es = ExitStack()
    with es:
        def din(name, shape, dt=F32):
            return nc.dram_tensor(name, list(shape), dt, kind="ExternalInput").ap()

        def dout(name, shape, dt=F32):
            return nc.dram_tensor(name, list(shape), dt, kind="ExternalOutput").ap()

        def dscr(name, shape, dt=F32):
            return nc.dram_tensor(name, list(shape), dt, kind="Internal").ap()

        xin = din("xin", [T, D])
        cv_d = din("cv", [128, KC, 2])
        ada_w = din("ada_w", [L, D, 6 * D])
        ada_bT = din("ada_bT", [128, L * 96])
        gT_d = din("gT", [128, 4 * L * KC])
        w_in = din("w_in", [L, D, IN_COLS])
        ident_d = din("ident", [128, 128])
        s5_lre_d = din("s5_lre", [128, L * 32])
        s5_lim_d = din("s5_lim", [128, L * 32])
        s5_ldt_d = din("s5_ldt", [128, L * 32])
        s5_Bb_d = din("s5_Bb", [128, L * 64, 128])
        s5_Cb_d = din("s5_Cb", [128, L * 64, 128])
        s5_dT_d = din("s5_dT", [128, L * 4])
        s5_gbT_d = din("s5_gbT", [128, L * 4])
        s5_gluw_d = din("s5_glu_w", [L, 512, 512])
        tau1_d = din("tau1", [128, LC])
        rm_d = din("rm", [64, T])
        sel_d = din("sel", [64, 12, 128])
        mb_d = din("mb", [64, 4, 64])
        gdn_norm_d = din("gdn_normr", [64, L * 128])
        gdn_gp_d = din("gdn_gp", [64, L * 2])
        gdn_cw_d = din("gdn_cw", [128, L * 54])
        rma_d = din("rma", [64, T])
        ml_norm_d = din("ml_normr", [64, L * 128])
        ml_mp_d = din("ml_mp", [64, L * 2])
        w_out = din("w_out", [L, D, D])
        w_gate = din("ffn_w_gate", [L, D, FFN])
        w_up = din("ffn_w_up", [L, D, FFN])
        w_down = din("ffn_w_down", [L, FFN, D])
        out_d = dout("out", [NLAT, D])
        xres = dscr("xres", [T, D])
        if stage <= 1:
            projT = dout("projT", [47 * 128, T])
            mods_o = dout("mods_o", [128, 96 * 2])
        else:
            projT = dscr("projT", [47 * 128, T])
        if stage == 3:
            dbg_cv = dout("dbg_cv", [128, 3, T])
            dbg_rows = dout("dbg_rows", [64, 6, T])
            dbg_oacc = dout("dbg_oacc", [64, 36, 128])
            dbg_oaccf = dout("dbg_oaccf", [64, 36, 128])
        if stage == 5:
            mixT = din("mixT", [D, T], BF16)
            xres_o = dout("xres_o", [T, D])
        elif 2 <= stage <= 4:
            mixT = dout("mixT", [D, T], BF16)
        else:
            mixT = dscr("mixT", [D, T], BF16)

        kb = KB(nc, es)

        cnt = [0]

        def sb(st, name, shape, dt=F32):
            cnt[0] += 1
            nm = "s%d_%s" % (cnt[0], name)
            return Tl(st.enter_context(nc.sbuf_tensor(nm, list(shape), dt)), nm)

        def ps(st, name, shape, dt=F32):
            cnt[0] += 1
            nm = "p%d_%s" % (cnt[0], name)
            return Tl(st.enter_context(nc.psum_tensor(nm, list(shape), dt)), nm)

        ident = sb(es, "ident", [128, 128])
        gT = sb(es, "gT", [128, 4 * L * KC])
        abT = sb(es, "abT", [128, L * 96])
        cvs = sb(es, "cvs", [128, KC, 2])
        modsT = sb(es, "modsT", [128, 96, 2])
        gsm = sb(es, "gsm", [128, KC, 2])
        gsf = sb(es, "gsf", [128, KC, 2])
        kb.dma("sp", ident[:], ident_d, writes=[ident.b])
        kb.dma("sp", gT[:], gT_d, writes=[gT.b])
        kb.dma("sp", abT[:], ada_bT, writes=[abT.b])
        kb.dma("sp", cvs[:], cv_d, writes=[cvs.b])
        kb.op("act", lambda e: e.activation(out=cvs[:], in_=cvs[:], func=AF.Silu), reads=[cvs.b], writes=[cvs.b])

        banks = [ps(es, "bank%d" % i, [128, 512]) for i in range(8)]
        xres_b = Buf("xres")
        kb.dma("sp", xres, xin, writes=[xres_b])
        kb.barrier()

        for l in range(L):
            with ExitStack() as ph:
                wA = [sb(ph, "wA%d" % i, [128, KC, 512]) for i in range(2)]
                pm = banks[0]
                adv = ada_w[l].rearrange("(k p) c -> p k c", p=128)
                for cb in range(24):
                    w = wA[cb % 2]
                    kb.dma("sp", w[:], adv[:, :, cb * 512:(cb + 1) * 512], writes=[w.b])
                    for jj in range(4):
                        jo = cb * 4 + jj
                        for k in range(KC):
                            kb.op("pe", lambda e, w=w, k=k, jj=jj, jo=jo: e.matmul(
                                out=pm[:, jo * 2:jo * 2 + 2], lhsT=w[:, k, jj * 128:(jj + 1) * 128], rhs=cvs[:, k, :],
                                start=(k == 0), stop=(k == KC - 1)), reads=[w.b, cvs.b], writes=[pm.b], sig=(k == KC - 1))
                for wi in range(2):
                    kb.op("dve", lambda e, wi=wi: e.tensor_tensor(
                        out=modsT[:, :, wi], in0=pm[:, wi:192:2], in1=abT[:, l * 96:(l + 1) * 96], op=ALU.add),
                        reads=[pm.b, abT.b], writes=[modsT.b])
                for (gs, mi, gi) in ((gsm, 1, 0), (gsf, 4, 2)):
                    for wi in range(2):
                        kb.op("dve", lambda e, gs=gs, mi=mi, gi=gi, wi=wi: e.scalar_tensor_tensor(
                            out=gs[:, :, wi], in0=modsT[:, mi * KC:(mi + 1) * KC, wi], scalar=1.0,
                            in1=gT[:, (gi * L + l) * KC:(gi * L + l + 1) * KC], op0=ALU.add, op1=ALU.mult),
                            reads=[modsT.b, gT.b], writes=[gs.b])
                kb.barrier()
            if stage <= 1 and l == 0:
                kb.dma("sp", mods_o, modsT[:].rearrange("p a b -> p (a b)"), reads=[modsT.b])

            with ExitStack() as lay:
                hT = sb(lay, "hT", [128, KC, T], BF16)
                with ExitStack() as ph:
                    xt = [sb(ph, "xt%d" % i, [128, D]) for i in range(2)]
                    junk = sb(ph, "junk", [128, D], BF16)
                    st = [sb(ph, "st%d" % i, [128, 4]) for i in range(2)]
                    for i in range(T // 128):
                        x_ = xt[i % 2]
                        s_ = st[i % 2]
                        wsel = 1 if i < 2 else 0
                        kb.dma("sp", x_[:], xres[i * 128:(i + 1) * 128, :], writes=[x_.b])
                        kb.op("act", lambda e, x_=x_, s_=s_: e.activation(out=junk[:], in_=x_[:], func=AF.Square,
                                                                           accum_out=s_[:, 0:1]),
                              reads=[x_.b], writes=[junk.b, s_.b])
                        kb.op("act", lambda e, s_=s_: e.activation(out=s_[:, 1:2], in_=s_[:, 0:1], func=AF.Sqrt,
                                                                    bias=EPS, scale=1.0 / D), reads=[s_.b], writes=[s_.b])
                        kb.op("dve", lambda e, s_=s_: e.reciprocal(out=s_[:, 2:3], in_=s_[:, 1:2]), reads=[s_.b], writes=[s_.b])
                        kb.op("dve", lambda e, x_=x_, s_=s_: e.tensor_scalar(out=x_[:], in0=x_[:], scalar1=s_[:, 2:3], scalar2=None,
                                                                          op0=ALU.mult), reads=[x_.b, s_.b], writes=[x_.b])
                        for jb in range(4):
                            pt = banks[1 + (i * 4 + jb) % 4]
                            for jj in range(4):
                                j = jb * 4 + jj
                                kb.op("pe", lambda e, pt=pt, x_=x_, jj=jj, j=j: e.transpose(
                                    out=pt[:, jj * 128:(jj + 1) * 128], in_=x_[:, j * 128:(j + 1) * 128], identity=ident[:]),
                                    reads=[x_.b, ident.b], writes=[pt.b])
                            for jj in range(4):
                                j = jb * 4 + jj
                                kb.op("act", lambda e, pt=pt, jj=jj, j=j, i=i, wsel=wsel: e.activation(
                                    out=hT[:, j, i * 128:(i + 1) * 128], in_=pt[:, jj * 128:(jj + 1) * 128], func=AF.Identity,
                                    bias=modsT[:, 0 * KC + j, wsel:wsel + 1], scale=gsm[:, j, wsel:wsel + 1]),
                                    reads=[pt.b, modsT.b, gsm.b], writes=[hT.b])
                    kb.barrier()
                with ExitStack() as ph:
                    wC = [sb(ph, "wC%d" % i, [128, KC, 128], BF16) for i in range(2)]
                    sg = [sb(ph, "sg%d" % i, [128, 512]) for i in range(3)]
                    wv = w_in[l].rearrange("(k p) c -> p k c", p=128)
                    nev = 0
                    for cc in range(47):
                        c0 = cc * 128
                        n = min(128, IN_COLS - c0)
                        w = wC[cc % 2]
                        kb.dma("pool", w[:, :, :n], wv[:, :, c0:c0 + n], writes=[w.b])
                        for tb in range(5):
                            t0 = tb * 512
                            nt = min(512, T - t0)
                            pj = banks[5 + nev % 3]
                            for k in range(KC):
                                kb.op("pe", lambda e, pj=pj, w=w, k=k, n=n, t0=t0, nt=nt: e.matmul(
                                    out=pj[:n, :nt], lhsT=w[:, k, :n], rhs=hT[:, k, t0:t0 + nt],
                                    start=(k == 0), stop=(k == KC - 1)), reads=[w.b, hT.b], writes=[pj.b], sig=(k == KC - 1))
                            s_ = sg[nev % 3]
                            eng = "act" if nev % 2 == 0 else "dve"
                            if eng == "act":
                                kb.op("act", lambda e, s_=s_, pj=pj, n=n, nt=nt: e.activation(out=s_[:n, :nt], in_=pj[:n, :nt],
                                                                                            func=AF.Identity),
                                      reads=[pj.b], writes=[s_.b])
                            else:
                                kb.op("dve", lambda e, s_=s_, pj=pj, n=n, nt=nt: e.tensor_copy(out=s_[:n, :nt], in_=pj[:n, :nt]),
                                      reads=[pj.b], writes=[s_.b])
                            kb.dma("sp", projT[c0:c0 + n, t0:t0 + nt], s_[:n, :nt], reads=[s_.b])
                            nev += 1
                    kb.barrier()
            if stage <= 1:
                break
            wov = w_out[l].rearrange("(k p) c -> p k c", p=128)
            wgv = w_gate[l].rearrange("(k p) c -> p k c", p=128)
            wuv = w_up[l].rearrange("(k p) c -> p k c", p=128)
            wdv = w_down[l].rearrange("(h p) c -> p h c", p=128)
            pre = {}
            for nm_, view_, nch_, nk_ in (("wo", wov, KC, KC), ("wg", wgv, 44, KC), ("wu", wuv, 44, KC), ("wd", wdv, KC, 44)):
                scr_ = dscr("pc_%s%d" % (nm_, l), [nch_, 128, nk_ * 128], BF16)
                lst_ = []
                for j_ in range(nch_):
                    b_ = Buf()
                    dst_ = scr_[j_].rearrange("p (k c) -> p k c", c=128)
                    kb.dma("pool", dst_, view_[:, :, j_ * 128:(j_ + 1) * 128], writes=[b_])
                    lst_.append((dst_, b_))
                pre[nm_] = lst_
            with ExitStack() as ph:
                TWO_PI = 6.283185307179586
                C1 = 6.28125
                C2 = TWO_PI - C1
                PI = 3.141592653589793
                uT = sb(ph, "uT", [128, 4, T])
                yT = sb(ph, "yT", [128, 4, T])
                Bb = sb(ph, "Bb", [128, 32, 128])
                Cb = sb(ph, "Cb", [128, 32, 128])
                lre = sb(ph, "lre", [128, 32]); lim = sb(ph, "lim", [128, 32]); ldt = sb(ph, "ldt", [128, 32])
                tau1 = sb(ph, "tau1", [128, LC])
                dTt = sb(ph, "dTt", [128, L * 4]); gbT = sb(ph, "gbT", [128, L * 4])
                gluw = sb(ph, "gluw", [128, 4, 512])
                kb.dma("sp", uT[:], projT[0:512, :].rearrange("(c p) t -> p c t", p=128), writes=[uT.b])
                kb.dma("sp", lre[:], s5_lre_d[:, l * 32:(l + 1) * 32], writes=[lre.b])
                kb.dma("sp", lim[:], s5_lim_d[:, l * 32:(l + 1) * 32], writes=[lim.b])
                kb.dma("sp", ldt[:], s5_ldt_d[:, l * 32:(l + 1) * 32], writes=[ldt.b])
                kb.dma("sp", tau1[:], tau1_d, writes=[tau1.b])
                kb.dma("sp", dTt[:], s5_dT_d, writes=[dTt.b])
                kb.dma("sp", gbT[:], s5_gbT_d, writes=[gbT.b])
                kb.dma("sp", gluw[:], s5_gluw_d[l].rearrange("(c p) n -> p c n", p=128), writes=[gluw.b])

                def sincos(n, ang, o_sin, o_cos, tf, ti, tm, tb_):
                    B = [tb_]
                    kb.op("dve", lambda e: e.tensor_scalar(out=tf, in0=ang, scalar1=1.0 / TWO_PI, scalar2=None, op0=ALU.mult), reads=B, writes=B)
                    kb.op("dve", lambda e: e.tensor_copy(out=ti, in_=tf), reads=B, writes=B)
                    kb.op("dve", lambda e: e.tensor_copy(out=tf, in_=ti), reads=B, writes=B)
                    kb.op("dve", lambda e: e.scalar_tensor_tensor(out=tm, in0=tf, scalar=-C1, in1=ang, op0=ALU.mult, op1=ALU.add), reads=B, writes=B)
                    kb.op("dve", lambda e: e.scalar_tensor_tensor(out=tm, in0=tf, scalar=-C2, in1=tm, op0=ALU.mult, op1=ALU.add), reads=B, writes=B)

                    def wrap(y):
                        kb.op("dve", lambda e: e.tensor_scalar(out=tf, in0=y, scalar1=PI, scalar2=TWO_PI, op0=ALU.is_gt, op1=ALU.mult), reads=B, writes=B)
                        kb.op("dve", lambda e: e.tensor_tensor(out=y, in0=y, in1=tf, op=ALU.subtract), reads=B, writes=B)
                        kb.op("dve", lambda e: e.tensor_scalar(out=tf, in0=y, scalar1=-PI, scalar2=TWO_PI, op0=ALU.is_lt, op1=ALU.mult), reads=B, writes=B)
                        kb.op("dve", lambda e: e.tensor_tensor(out=y, in0=y, in1=tf, op=ALU.add), reads=B, writes=B)
                    wrap(tm)
                    kb.op("act", lambda e: e.activation(out=o_sin, in_=tm, func=AF.Sin), reads=B, writes=B)
                    kb.op("dve", lambda e: e.tensor_scalar(out=tm, in0=tm, scalar1=PI / 2, scalar2=None, op0=ALU.add), reads=B, writes=B)
                    wrap(tm)
                    kb.op("act", lambda e: e.activation(out=o_cos, in_=tm, func=AF.Sin), reads=B, writes=B)

                with ExitStack() as ph2:
                    tabs = sb(ph2, "tabs", [128, 4, 16 * LC])
                    tw = sb(ph2, "tw", [128, 3, 16 * LC])
                    twi = sb(ph2, "twi", [128, 16 * LC], mybir.dt.int32)
                    sp_ = sb(ph2, "s5par", [128, 16, 16])
                    spi = sb(ph2, "s5pari", [128, 16], mybir.dt.int32)
                    car = sb(ph2, "car", [128, 2, 16])
                    S5U = [sb(ph2, "s5u%d" % i, [128, 6, LC]) for i in range(8)]
                    S5UB = [[Buf("s5u%d_%d" % (i, k_)) for k_ in range(6)] for i in range(8)]
                    S5B = [tabs.b, tw.b, twi.b, sp_.b, spi.b]

                    def P_(k):
                        return sp_[:, k, :]
                    for i in range(2):
                        o = (l * 2 + i) * 16
                        B = [sp_.b]
                        kb.dma("sp", Bb[:], s5_Bb_d[:, (l * 2 + i) * 32:(l * 2 + i + 1) * 32, :], writes=[Bb.b])
                        kb.dma("sp", Cb[:], s5_Cb_d[:, (l * 2 + i) * 32:(l * 2 + i + 1) * 32, :], writes=[Cb.b])
                        kb.op("act", lambda e: e.activation(out=Cb[:, 16:32, :], in_=Cb[:, 16:32, :], func=AF.Identity, scale=-1.0),
                              reads=[Cb.b], writes=[Cb.b])
                        kb.op("act", lambda e: e.activation(out=P_(0), in_=ldt[:, (l * 2 + i) * 16 - l * 32 + 0:(l * 2 + i) * 16 - l * 32 + 16], func=AF.Exp), reads=[ldt.b], writes=B)
                        kb.op("dve", lambda e: e.tensor_tensor(out=P_(1), in0=lre[:, i * 16:(i + 1) * 16], in1=P_(0), op=ALU.mult), reads=[lre.b] + B, writes=B)
                        kb.op("dve", lambda e: e.tensor_tensor(out=P_(2), in0=lim[:, i * 16:(i + 1) * 16], in1=P_(0), op=ALU.mult), reads=[lim.b] + B, writes=B)
                        kb.op("act", lambda e: e.activation(out=P_(3), in_=P_(1), func=AF.Exp), reads=B, writes=B)
                        sincos(16, P_(2), P_(4), P_(5), P_(6), spi[:], P_(7), sp_.b)
                        kb.op("dve", lambda e: e.tensor_tensor(out=P_(6), in0=P_(3), in1=P_(5), op=ALU.mult), reads=B, writes=B)
                        kb.op("dve", lambda e: e.tensor_scalar(out=P_(6), in0=P_(6), scalar1=-1.0, scalar2=None, op0=ALU.add), reads=B, writes=B)
                        kb.op("dve", lambda e: e.tensor_tensor(out=P_(7), in0=P_(3), in1=P_(4), op=ALU.mult), reads=B, writes=B)
                        kb.op("dve", lambda e: e.tensor_tensor(out=P_(8), in0=lre[:, i * 16:(i + 1) * 16], in1=lre[:, i * 16:(i + 1) * 16], op=ALU.mult), reads=[lre.b] + B, writes=B)
                        kb.op("dve", lambda e: e.tensor_tensor(out=P_(9), in0=lim[:, i * 16:(i + 1) * 16], in1=lim[:, i * 16:(i + 1) * 16], op=ALU.mult), reads=[lim.b] + B, writes=B)
                        kb.op("dve", lambda e: e.tensor_tensor(out=P_(8), in0=P_(8), in1=P_(9), op=ALU.add), reads=B, writes=B)
                        kb.op("dve", lambda e: e.reciprocal(out=P_(8), in_=P_(8)), reads=B, writes=B)
                        kb.op("dve", lambda e: e.tensor_tensor(out=P_(10), in0=P_(6), in1=lre[:, i * 16:(i + 1) * 16], op=ALU.mult), reads=[lre.b] + B, writes=B)
                        kb.op("dve", lambda e: e.tensor_tensor(out=P_(9), in0=P_(7), in1=lim[:, i * 16:(i + 1) * 16], op=ALU.mult), reads=[lim.b] + B, writes=B)
                        kb.op("dve", lambda e: e.tensor_tensor(out=P_(10), in0=P_(10), in1=P_(9), op=ALU.add), reads=B, writes=B)
                        kb.op("dve", lambda e: e.tensor_tensor(out=P_(10), in0=P_(10), in1=P_(8), op=ALU.mult), reads=B, writes=B)
                        kb.op("dve", lambda e: e.tensor_tensor(out=P_(11), in0=P_(7), in1=lre[:, i * 16:(i + 1) * 16], op=ALU.mult), reads=[lre.b] + B, writes=B)
                        kb.op("dve", lambda e: e.tensor_tensor(out=P_(9), in0=P_(6), in1=lim[:, i * 16:(i + 1) * 16], op=ALU.mult), reads=[lim.b] + B, writes=B)
                        kb.op("dve", lambda e: e.tensor_tensor(out=P_(11), in0=P_(11), in1=P_(9), op=ALU.subtract), reads=B, writes=B)
                        kb.op("dve", lambda e: e.tensor_tensor(out=P_(11), in0=P_(11), in1=P_(8), op=ALU.mult), reads=B, writes=B)
                        for st in range(16):
                            kb.op("dve", lambda e, st=st: e.tensor_scalar(out=tw[:, 0, st * LC:(st + 1) * LC], in0=tau1[:], scalar1=sp_[:, 2, st:st + 1],
                                                                         scalar2=None, op0=ALU.mult), reads=[tau1.b] + B, writes=[tw.b])
                        sincos(16 * LC, tw[:, 0, :], tabs[:, 3, :], tabs[:, 2, :], tw[:, 1, :], twi[:], tw[:, 2, :], tw.b)
                        kb.op("dve", lambda e: e.tensor_copy(out=tw[:, 0, 0:1], in_=tw[:, 0, 0:1]), reads=[tw.b, tabs.b], writes=[tw.b, tabs.b])
                        for st in range(16):
                            sl = slice(st * LC, (st + 1) * LC)
                            kb.op("dve", lambda e, st=st, sl=sl: e.tensor_scalar(out=tw[:, 1, sl], in0=tabs[:, 3, sl], scalar1=sp_[:, 11, st:st + 1], scalar2=None, op0=ALU.mult), reads=[tabs.b] + B, writes=[tw.b])
                            kb.op("dve", lambda e, st=st, sl=sl: e.scalar_tensor_tensor(out=tabs[:, 0, sl], in0=tabs[:, 2, sl], scalar=sp_[:, 10, st:st + 1], in1=tw[:, 1, sl], op0=ALU.mult, op1=ALU.add), reads=[tw.b] + B, writes=[tabs.b])
                            kb.op("dve", lambda e, st=st, sl=sl: e.tensor_scalar(out=tw[:, 1, sl], in0=tabs[:, 3, sl], scalar1=sp_[:, 10, st:st + 1], scalar2=None, op0=ALU.mult), reads=[tabs.b] + B, writes=[tw.b])
                            kb.op("dve", lambda e, st=st, sl=sl: e.scalar_tensor_tensor(out=tabs[:, 1, sl], in0=tabs[:, 2, sl], scalar=sp_[:, 11, st:st + 1], in1=tw[:, 1, sl], op0=ALU.mult, op1=ALU.subtract), reads=[tw.b] + B, writes=[tabs.b])
                        kb.op("dve", lambda e: e.memset(car[:], 0.0), writes=[car.b])
                        NCH = T // LC
                        NCC = NCTX // LC
                        order = list(range(NCH)) if i == 0 else list(range(NCC - 1, -1, -1)) + list(range(NCH - 1, NCC - 1, -1))

                        def rv(ap):
                            return ap[:, ::-1] if i == 1 else ap
                        lastc = LC - 1 if i == 0 else 0
                        for n in order:
                            t0 = n * LC
                            for hf in range(2):
                                for q in range(8):
                                    st = hf * 8 + q
                                    pb = banks[q // 2]
                                    for ci in range(2):
                                        c0 = (q % 2) * 2 * LC + ci * LC
                                        kb.op("pe", lambda e, ci=ci, pb=pb, st=st, c0=c0: e.matmul(out=pb[:, c0:c0 + LC], lhsT=Bb[:, ci * 16 + st, :], rhs=uT[:, st // 4, t0:t0 + LC],
                                                                                              start=True, stop=True), reads=[Bb.b, uT.b], writes=[pb.b])
                                for q in range(8):
                                    st = hf * 8 + q
                                    pb = banks[q // 2]
                                    sl = slice(st * LC, (st + 1) * LC)
                                    u_ = S5U[q]
                                    ub_ = S5UB[q]
                                    bre = rv(pb[:, (q % 2) * 2 * LC:(q % 2) * 2 * LC + LC]); bim = rv(pb[:, (q % 2) * 2 * LC + LC:(q % 2) * 2 * LC + 2 * LC])
                                    kb.op("dve", lambda e, u_=u_, bre=bre, sl=sl: e.tensor_tensor(out=u_[:, 0, :], in0=bre, in1=tabs[:, 0, sl], op=ALU.mult), reads=[pb.b, tabs.b], writes=[ub_[0]])
                                    kb.op("dve", lambda e, u_=u_, bim=bim, sl=sl: e.tensor_tensor(out=u_[:, 1, :], in0=bim, in1=tabs[:, 1, sl], op=ALU.mult), reads=[pb.b, tabs.b], writes=[ub_[1]])
                                    kb.op("dve", lambda e, u_=u_, bre=bre, sl=sl: e.tensor_tensor(out=u_[:, 2, :], in0=bre, in1=tabs[:, 1, sl], op=ALU.mult), reads=[pb.b, tabs.b], writes=[ub_[2]])
                                    kb.op("dve", lambda e, u_=u_, bim=bim, sl=sl: e.tensor_tensor(out=u_[:, 3, :], in0=bim, in1=tabs[:, 0, sl], op=ALU.mult), reads=[pb.b, tabs.b], writes=[ub_[3]])
                                for q in range(8):
                                    u_ = S5U[q]
                                    ub_ = S5UB[q]
                                    kb.op("dve", lambda e, u_=u_: e.tensor_tensor(out=u_[:, 0, :], in0=u_[:, 0, :], in1=u_[:, 1, :], op=ALU.subtract), reads=[ub_[1]], writes=[ub_[0]])
                                    kb.op("dve", lambda e, u_=u_: e.tensor_tensor(out=u_[:, 2, :], in0=u_[:, 2, :], in1=u_[:, 3, :], op=ALU.add), reads=[ub_[3]], writes=[ub_[2]])
                                for q in range(8):
                                    st = hf * 8 + q
                                    u_ = S5U[q]
                                    ub_ = S5UB[q]
                                    rb = sp_[:, 3, st:st + 1].to_broadcast([128, LC])
                                    kb.op("dve", lambda e, u_=u_, rb=rb, st=st: e.tensor_tensor_scan(out=u_[:, 1, :], data0=rb, data1=u_[:, 0, :], initial=car[:, 0, st:st + 1], op0=ALU.mult, op1=ALU.add),
                                          reads=[ub_[0], sp_.b, car.b], writes=[ub_[1]])
                                    kb.op("dve", lambda e, u_=u_, rb=rb, st=st: e.tensor_tensor_scan(out=u_[:, 3, :], data0=rb, data1=u_[:, 2, :], initial=car[:, 1, st:st + 1], op0=ALU.mult, op1=ALU.add),
                                          reads=[ub_[2], sp_.b, car.b], writes=[ub_[3]])
                                for q in range(8):
                                    st = hf * 8 + q
                                    sl = slice(st * LC, (st + 1) * LC)
                                    u_ = S5U[q]
                                    ub_ = S5UB[q]
                                    kb.op("pool", lambda e, u_=u_, sl=sl: e.tensor_tensor(out=u_[:, 0, :], in0=u_[:, 1, :], in1=tabs[:, 2, sl], op=ALU.mult), reads=[ub_[1], tabs.b], writes=[ub_[0]])
                                    kb.op("pool", lambda e, u_=u_, sl=sl: e.tensor_tensor(out=u_[:, 2, :], in0=u_[:, 3, :], in1=tabs[:, 3, sl], op=ALU.mult), reads=[ub_[3], tabs.b], writes=[ub_[2]])
                                    kb.op("pool", lambda e, u_=u_: e.tensor_tensor(out=rv(u_[:, 4, :]), in0=u_[:, 0, :], in1=u_[:, 2, :], op=ALU.subtract), reads=[ub_[0], ub_[2]], writes=[ub_[4]])
                                    kb.op("pool", lambda e, u_=u_, sl=sl: e.tensor_tensor(out=u_[:, 0, :], in0=u_[:, 1, :], in1=tabs[:, 3, sl], op=ALU.mult), reads=[ub_[1], tabs.b], writes=[ub_[0]])
                                    kb.op("pool", lambda e, u_=u_, sl=sl: e.tensor_tensor(out=u_[:, 2, :], in0=u_[:, 3, :], in1=tabs[:, 2, sl], op=ALU.mult), reads=[ub_[3], tabs.b], writes=[ub_[2]])
                                    kb.op("pool", lambda e, u_=u_: e.tensor_tensor(out=rv(u_[:, 5, :]), in0=u_[:, 0, :], in1=u_[:, 2, :], op=ALU.add), reads=[ub_[0], ub_[2]], writes=[ub_[5]])
                                for q in range(8):
                                    st = hf * 8 + q
                                    u_ = S5U[q]
                                    ub_ = S5UB[q]
                                    kb.op("act", lambda e, u_=u_, st=st: e.activation(out=car[:, 0, st:st + 1], in_=u_[:, 4, lastc:lastc + 1], func=AF.Identity), reads=[ub_[4]], writes=[car.b])
                                    kb.op("act", lambda e, u_=u_, st=st: e.activation(out=car[:, 1, st:st + 1], in_=u_[:, 5, lastc:lastc + 1], func=AF.Identity), reads=[ub_[5]], writes=[car.b])
                                for f2 in range(2):
                                    fc = hf * 2 + f2
                                    py = banks[4 + fc]
                                    for q4 in range(4):
                                        q = f2 * 4 + q4
                                        st = hf * 8 + q
                                        u_ = S5U[q]
                                        ub_ = S5UB[q]
                                        kb.op("pe", lambda e, py=py, st=st, u_=u_, q4=q4: e.matmul(out=py[:, 0:LC], lhsT=Cb[:, st, :], rhs=u_[:, 4, :], start=(q4 == 0), stop=False),
                                              reads=[Cb.b, ub_[4]], writes=[py.b], sig=False)
                                        kb.op("pe", lambda e, py=py, st=st, u_=u_, q4=q4: e.matmul(out=py[:, 0:LC], lhsT=Cb[:, 16 + st, :], rhs=u_[:, 5, :], start=False, stop=(q4 == 3)),
                                              reads=[Cb.b, ub_[5]], writes=[py.b])
                                    if i == 0:
                                        kb.op("act", lambda e, py=py, fc=fc: e.activation(out=yT[:, fc, t0:t0 + LC], in_=py[:, 0:LC], func=AF.Identity), writes=[yT.b, py.b])
                                    else:
                                        kb.op("dve", lambda e, py=py, fc=fc: e.tensor_tensor(out=yT[:, fc, t0:t0 + LC], in0=py[:, 0:LC], in1=yT[:, fc, t0:t0 + LC], op=ALU.add),
                                              writes=[yT.b, py.b])
                    kb.barrier()
                with ExitStack() as ph2:
                    g1 = sb(ph2, "g1", [128, T]); g2 = sb(ph2, "g2", [128, T])
                    og = [sb(ph2, "og%d" % i, [128, 512], BF16) for i in range(2)]
                    sgl = [sb(ph2, "sgl%d" % i, [128, 512]) for i in range(2)]
                    for fc in range(4):
                        kb.op("dve", lambda e: e.scalar_tensor_tensor(out=yT[:, fc, :], in0=uT[:, fc, :], scalar=dTt[:, l * 4 + fc:l * 4 + fc + 1], in1=yT[:, fc, :],
                                                                      op0=ALU.mult, op1=ALU.add), reads=[uT.b, dTt.b, yT.b], writes=[yT.b])
                        kb.op("act", lambda e: e.activation(out=g1[:], in_=yT[:, fc, :], func=AF.Square), reads=[yT.b], writes=[g1.b])
                        kb.op("dve", lambda e: e.tensor_scalar(out=g1[:], in0=g1[:], scalar1=0.044715, scalar2=1.0, op0=ALU.mult, op1=ALU.add), reads=[g1.b], writes=[g1.b])
                        kb.op("dve", lambda e: e.tensor_tensor(out=g1[:], in0=g1[:], in1=yT[:, fc, :], op=ALU.mult), reads=[g1.b, yT.b], writes=[g1.b])
                        kb.op("act", lambda e: e.activation(out=g2[:], in_=g1[:], func=AF.Sigmoid, scale=1.5957691216057308), reads=[g1.b], writes=[g2.b])
                        kb.op("dve", lambda e: e.tensor_tensor(out=yT[:, fc, :], in0=yT[:, fc, :], in1=g2[:], op=ALU.mult), reads=[g2.b, yT.b], writes=[yT.b])
                    ne = 0
                    for fo in range(4):
                        for tb in range(5):
                            t0 = tb * 512
                            nt = min(512, T - t0)
                            pg = banks[4 + ne % 2]
                            for fi in range(4):
                                kb.op("pe", lambda e, fi=fi, pg=pg: e.matmul(out=pg[:, :nt], lhsT=gluw[:, fi, fo * 128:(fo + 1) * 128], rhs=yT[:, fi, t0:t0 + nt],
                                                                             start=(fi == 0), stop=(fi == 3)), reads=[gluw.b, yT.b], writes=[pg.b], sig=(fi == 3))
                            s_ = sgl[ne % 2]; o_ = og[ne % 2]
                            kb.op("act", lambda e, pg=pg, s_=s_: e.activation(out=s_[:, :nt], in_=pg[:, :nt], func=AF.Sigmoid, bias=gbT[:, l * 4 + fo:l * 4 + fo + 1]),
                                  reads=[pg.b, gbT.b], writes=[s_.b])
                            kb.op("dve", lambda e, s_=s_, o_=o_: e.tensor_tensor(out=o_[:, :nt], in0=s_[:, :nt], in1=yT[:, fo, t0:t0 + nt], op=ALU.mult),
                                  reads=[s_.b, yT.b], writes=[o_.b])
                            kb.dma("sp", mixT[fo * 128:(fo + 1) * 128, t0:t0 + nt], o_[:, :nt], reads=[o_.b])
                            ne += 1
                kb.barrier()
            if stage <= 2:
                break
            with ExitStack() as ph:
                QOFF, KOFF, VOFF, ZOFF, AOFF, BOFF = 512, 1280, 2048, 2816, 3584, 3596
                NCK = T // 64
                RM = sb(ph, "RM", [64, T])
                SEL = sb(ph, "SEL", [64, 12, 128]); NSEL = sb(ph, "NSEL", [64, 12, 128])
                MB = sb(ph, "MB", [64, 4, 64])
                ONES = sb(ph, "ONES", [128, 128])
                NORM = sb(ph, "NORM", [64, 128])
                GP = sb(ph, "GP", [64, 2])
                CW = sb(ph, "CW", [128, 18 * 3])
                kb.dma("sp", RM[:], rm_d, writes=[RM.b])
                kb.dma("sp", SEL[:], sel_d, writes=[SEL.b])
                kb.dma("sp", MB[:], mb_d, writes=[MB.b])
                kb.dma("sp", NORM[:], gdn_norm_d[:, l * 128:(l + 1) * 128], writes=[NORM.b])
                kb.dma("sp", GP[:], gdn_gp_d[:, l * 2:(l + 1) * 2], writes=[GP.b])
                kb.dma("sp", CW[:], gdn_cw_d[:, l * 54:(l + 1) * 54], writes=[CW.b])
                kb.op("act", lambda e: e.activation(out=NSEL[:], in_=SEL[:], func=AF.Identity, scale=-1.0), reads=[SEL.b], writes=[NSEL.b])
                kb.op("dve", lambda e: e.memset(ONES[:], 1.0), writes=[ONES.b])
                RA = sb(ph, "RA", [64, T]); RB = sb(ph, "RB", [64, T]); GC = sb(ph, "GC", [64, T])
                R1 = sb(ph, "R1", [64, T]); EG = sb(ph, "EG", [64, T]); BG = sb(ph, "BG", [64, T])
                EGL = sb(ph, "EGL", [64, NCK])
                COL = sb(ph, "COL", [64, NCK, 3, 12])
                EGLB = sb(ph, "EGLB", [128, 12, NCK])
                kb.op("dve", lambda e: e.memset(RA[:], 0.0), writes=[RA.b])
                kb.op("dve", lambda e: e.memset(RB[:], 0.0), writes=[RB.b])
                for dr in range(2):
                    kb.dma("sp", RA[dr * 32:dr * 32 + 6, :], projT[AOFF + dr * 6:AOFF + dr * 6 + 6, :], writes=[RA.b])
                    kb.dma("sp", RB[dr * 32:dr * 32 + 6, :], projT[BOFF + dr * 6:BOFF + dr * 6 + 6, :], writes=[RB.b])
                kb.op("act", lambda e: e.activation(out=RA[:], in_=RA[:], func=AF.Exp, bias=GP[:, 1:2]), reads=[RA.b, GP.b], writes=[RA.b])
                kb.op("act", lambda e: e.activation(out=RA[:], in_=RA[:], func=AF.Ln, bias=1.0), reads=[RA.b], writes=[RA.b])
                kb.op("act", lambda e: e.activation(out=GP[:, 0:1], in_=GP[:, 0:1], func=AF.Exp), reads=[GP.b], writes=[GP.b])
                kb.op("dve", lambda e: e.tensor_scalar(out=RA[:], in0=RA[:], scalar1=GP[:, 0:1], scalar2=-1.0, op0=ALU.mult, op1=ALU.mult), reads=[RA.b, GP.b], writes=[RA.b])
                kb.op("act", lambda e: e.activation(out=RB[:], in_=RB[:], func=AF.Sigmoid), reads=[RB.b], writes=[RB.b])
                kb.op("act", lambda e: e.activation(out=R1[:], in_=RB[:], func=AF.Ln), reads=[RB.b], writes=[R1.b])
                kb.op("dve", lambda e: e.tensor_tensor_scan(out=GC[0:32, :], data0=RM[0:32, :], data1=RA[0:32, :], initial=0.0, op0=ALU.mult, op1=ALU.add),
                      reads=[RM.b, RA.b], writes=[GC.b])
                kb.op("dve", lambda e: e.tensor_tensor_scan(out=GC[32:64, ::-1], data0=RM[32:64, ::-1], data1=RA[32:64, ::-1], initial=0.0, op0=ALU.mult, op1=ALU.add),
                      reads=[RM.b, RA.b], writes=[GC.b])
                kb.op("dve", lambda e: e.tensor_tensor(out=R1[:], in0=R1[:], in1=GC[:], op=ALU.add), reads=[R1.b, GC.b], writes=[R1.b])
                kb.op("act", lambda e: e.activation(out=EG[:], in_=GC[:], func=AF.Exp), reads=[GC.b], writes=[EG.b])
                kb.op("dve", lambda e: e.tensor_tensor(out=BG[:], in0=EG[:], in1=RB[:], op=ALU.mult), reads=[EG.b, RB.b], writes=[BG.b])
                GCL = sb(ph, "GCL", [64, NCK])
                for dr in range(2):
                    pr = slice(dr * 32, dr * 32 + 32)
                    lastpos = 63 if dr == 0 else 0
                    gv = GC[pr, :].rearrange("p (c s) -> p c s", s=64)
                    kb.op("dve", lambda e, pr=pr, gv=gv, lastpos=lastpos: e.tensor_copy(out=GCL[pr, :], in_=gv[:, :, lastpos]), reads=[GC.b], writes=[GCL.b])
                kb.op("act", lambda e: e.activation(out=EGL[:], in_=GCL[:], func=AF.Exp), reads=[GCL.b], writes=[EGL.b])
                for c in range(NCK):
                    kb.op("dve", lambda e, c=c: e.tensor_scalar(out=RA[:, c * 64:(c + 1) * 64], in0=GC[:, c * 64:(c + 1) * 64], scalar1=-1.0, scalar2=GCL[:, c:c + 1],
                                                                 op0=ALU.mult, op1=ALU.add), reads=[GC.b, GCL.b], writes=[RA.b])
                kb.op("act", lambda e: e.activation(out=RA[:], in_=RA[:], func=AF.Exp), reads=[RA.b], writes=[RA.b])
                for c in range(NCK):
                    pcol = banks[c % 4]
                    for qi, row in enumerate((BG, RA, RB)):
                        kb.op("pe", lambda e, row=row, qi=qi, pcol=pcol, c=c: e.matmul(out=pcol[0:64, qi * 12:(qi + 1) * 12], lhsT=row[:, c * 64:(c + 1) * 64],
                                                                                   rhs=SEL[:, :, 0], start=True, stop=True), reads=[row.b, SEL.b], writes=[pcol.b])
                    kb.op("act", lambda e, pcol=pcol, c=c: e.activation(out=COL[:, c, :, :].rearrange("p a b -> p (a b)"), in_=pcol[0:64, 0:36], func=AF.Identity),
                          reads=[pcol.b], writes=[COL.b])
                for r in range(12):
                    pcol = banks[4 + r % 2]
                    kb.op("pe", lambda e, r=r, pcol=pcol: e.matmul(out=pcol[:, 0:NCK], lhsT=SEL[:, r, :], rhs=EGL[:], start=True, stop=True), reads=[SEL.b, EGL.b], writes=[pcol.b])
                    kb.op("act", lambda e, r=r, pcol=pcol: e.activation(out=EGLB[:, r, :], in_=pcol[:, 0:NCK], func=AF.Identity), reads=[pcol.b], writes=[EGLB.b])
                kb.barrier()
                DBG = 9
                if stage == 3:
                    for qi_, row_ in enumerate((GC, R1, EG, RA, RB, BG)):
                        kb.dma("sp", dbg_rows[:, qi_, :], row_[:], reads=[row_.b])
                psn = [0]

                def pget():
                    bk = banks[psn[0] % 8]
                    psn[0] += 1
                    return bk
                RAW = sb(ph, "RAW", [128, 3, T]); CV = sb(ph, "CV", [128, 3, T])
                OACC = sb(ph, "OACC", [64, NCK, 128]); OUT = sb(ph, "OUT", [128, T], BF16)
                Sd = [sb(ph, "Sst%d" % i, [128, 128]) for i in range(2)]
                G = 4
                U = []
                for u in range(2 * G):
                    d_ = {}
                    for nm, shp in (("AB0", [64, 2, 64]), ("AB1", [64, 2, 64]), ("X0", [64, 64]), ("X1", [64, 64]),
                                    ("E", [64, 3, 64]), ("kbg", [64, 128]), ("kdec", [64, 128]), ("vb", [64, 128]),
                                    ("nWT", [128, 64]), ("VN", [64, 128])):
                        d_[nm] = sb(ph, "u%d%s" % (u, nm), shp, BF16 if nm in ("AB0", "AB1", "X0", "X1", "kbg", "vb") else F32)
                    U.append(d_)
                fin = [sb(ph, "fin%d" % i, [64, 128]) for i in range(2)]
                fst = [sb(ph, "fst%d" % i, [64, 4]) for i in range(2)]
                I64 = ident[0:64, 0:64]
                I64b_t = sb(ph, "I64b", [64, 64], BF16)
                kb.op("dve", lambda e: e.tensor_copy(out=I64b_t[:], in_=ident[0:64, 0:64]), reads=[ident.b], writes=[I64b_t.b])
                I64b = I64b_t[:]
                for hd in range(6 if DBG >= 1 else 0):
                    for ci, off in enumerate((QOFF, KOFF, VOFF)):
                        kb.dma("sp", RAW[:, ci, :], projT[off + hd * 128:off + (hd + 1) * 128, :], writes=[RAW.b])
                    for ci in range(3):
                        cch = ci * 6 + hd
                        for (s0, s1) in ((0, NCTX), (NCTX, T)):
                            kb.op("dve", lambda e, ci=ci, cch=cch, s0=s0, s1=s1: e.tensor_scalar(out=CV[:, ci, s0:s1], in0=RAW[:, ci, s0:s1], scalar1=CW[:, cch * 3 + 1:cch * 3 + 2],
                                                                                              scalar2=None, op0=ALU.mult), reads=[RAW.b, CW.b], writes=[CV.b])
                            kb.op("dve", lambda e, ci=ci, cch=cch, s0=s0, s1=s1: e.scalar_tensor_tensor(out=CV[:, ci, s0 + 1:s1], in0=RAW[:, ci, s0:s1 - 1], scalar=CW[:, cch * 3:cch * 3 + 1],
                                                                                                     in1=CV[:, ci, s0 + 1:s1], op0=ALU.mult, op1=ALU.add), reads=[RAW.b, CW.b, CV.b], writes=[CV.b])
                            kb.op("dve", lambda e, ci=ci, cch=cch, s0=s0, s1=s1: e.scalar_tensor_tensor(out=CV[:, ci, s0:s1 - 1], in0=RAW[:, ci, s0 + 1:s1], scalar=CW[:, cch * 3 + 2:cch * 3 + 3],
                                                                                                     in1=CV[:, ci, s0:s1 - 1], op0=ALU.mult, op1=ALU.add), reads=[RAW.b, CW.b, CV.b], writes=[CV.b])
                    kb.op("act", lambda e: e.activation(out=CV[:], in_=CV[:], func=AF.Silu), reads=[CV.b], writes=[CV.b])
                    for ci in range(2):
                        kb.op("act", lambda e, ci=ci: e.activation(out=RAW[:, 0, :], in_=CV[:, ci, :], func=AF.Square), reads=[CV.b], writes=[RAW.b])
                        for tb in range(5):
                            t0 = tb * 512
                            nt = min(512, T - t0)
                            pb = banks[6 + tb % 2]
                            kb.op("pe", lambda e, pb=pb, t0=t0, nt=nt: e.matmul(out=pb[:, :nt], lhsT=ONES[:], rhs=RAW[:, 0, t0:t0 + nt], start=True, stop=True),
                                  reads=[ONES.b, RAW.b], writes=[pb.b])
                            kb.op("act", lambda e, pb=pb, t0=t0, nt=nt: e.activation(out=RAW[:, 1, t0:t0 + nt], in_=pb[:, :nt], func=AF.Sqrt, bias=EPS), reads=[RAW.b], writes=[RAW.b, pb.b])
                        kb.op("dve", lambda e: e.reciprocal(out=RAW[:, 1, :], in_=RAW[:, 1, :]), reads=[RAW.b], writes=[RAW.b])
                        sc_ = (128.0 ** -0.5) if ci == 0 else 1.0
                        kb.op("dve", lambda e, ci=ci, sc_=sc_: e.scalar_tensor_tensor(out=CV[:, ci, :], in0=RAW[:, 1, :], scalar=sc_, in1=CV[:, ci, :], op0=ALU.mult, op1=ALU.mult),
                              reads=[RAW.b, CV.b], writes=[CV.b])
                    QT = lambda a, b_: CV[:, 0, a:b_]
                    KT = lambda a, b_: CV[:, 1, a:b_]
                    VT = lambda a, b_: CV[:, 2, a:b_]
                    kb.op("dve", lambda e: e.memset(OACC[:], 0.0), writes=[OACC.b])
                    for dr in range(2):
                        r = dr * 6 + hd
                        for tb in range(5):
                            t0 = tb * 512
                            nt = min(512, T - t0)
                            pb = banks[6 + tb % 2]
                            kb.op("pe", lambda e, pb=pb, t0=t0, nt=nt, r=r: e.matmul(out=pb[:, :nt], lhsT=SEL[:, r, :], rhs=EG[:, t0:t0 + nt], start=True, stop=True),
                                  reads=[SEL.b, EG.b], writes=[pb.b])
                            kb.op("dve", lambda e, pb=pb, t0=t0, nt=nt, dr=dr: e.tensor_tensor(out=RAW[:, dr, t0:t0 + nt], in0=pb[:, :nt], in1=CV[:, 0, t0:t0 + nt], op=ALU.mult),
                                  reads=[CV.b], writes=[RAW.b, pb.b])
                        kb.op("dve", lambda e, dr=dr: e.memset(Sd[dr][:], 0.0), writes=[Sd[dr].b])
                    orders = [list(range(NCK)), list(range(3, -1, -1)) + list(range(NCK - 1, 3, -1))]
                    masks = [(0, 1, 3), (1, 0, 2)]
                    for g0 in range(0, NCK, G):
                        units = []
                        for dr in range(2):
                            for u, c in enumerate(orders[dr][g0:g0 + G]):
                                units.append((dr, c, U[dr * G + u]))
                        for (dr, c, d_) in units:
                            r = dr * 6 + hd
                            t0 = c * 64
                            bk = pget()
                            kb.op("pe", lambda e, bk=bk, t0=t0: e.transpose(out=bk[0:64, 0:128], in_=KT(t0, t0 + 64), identity=ident[:]), reads=[CV.b, ident.b], writes=[bk.b])
                            kb.op("pe", lambda e, bk=bk, t0=t0: e.transpose(out=bk[0:64, 128:256], in_=VT(t0, t0 + 64), identity=ident[:]), reads=[CV.b, ident.b], writes=[bk.b])
                            kb.op("act", lambda e, bk=bk, d_=d_, c=c, r=r: e.activation(out=d_["kbg"][:], in_=bk[0:64, 0:128], func=AF.Identity, scale=COL[:, c, 0, r:r + 1]), reads=[COL.b], writes=[d_["kbg"].b, bk.b])
                            kb.op("act", lambda e, bk=bk, d_=d_, c=c, r=r: e.activation(out=d_["kdec"][:], in_=bk[0:64, 0:128], func=AF.Identity, scale=COL[:, c, 1, r:r + 1]), reads=[COL.b], writes=[d_["kdec"].b, bk.b])
                            kb.op("act", lambda e, bk=bk, d_=d_, c=c, r=r: e.activation(out=d_["vb"][:], in_=bk[0:64, 128:256], func=AF.Identity, scale=COL[:, c, 2, r:r + 1]), reads=[COL.b], writes=[d_["vb"].b, bk.b])
                        for (dr, c, d_) in units:
                            r = dr * 6 + hd
                            m_s, m_st, m_it = masks[dr]
                            t0 = c * 64
                            ch = slice(t0, t0 + 64)
                            bp = pget()
                            kb.op("pe", lambda e, bp=bp, t0=t0: e.matmul(out=bp[0:64, 0:64], lhsT=KT(t0, t0 + 64), rhs=KT(t0, t0 + 64), start=True, stop=True), reads=[CV.b], writes=[bp.b])
                            kb.op("pe", lambda e, bp=bp, t0=t0: e.matmul(out=bp[0:64, 64:128], lhsT=KT(t0, t0 + 64), rhs=QT(t0, t0 + 64), start=True, stop=True), reads=[CV.b], writes=[bp.b])
                            be = pget()
                            kb.op("pe", lambda e, be=be, ch=ch, r=r: e.matmul(out=be[0:64, 0:64], lhsT=R1[:, ch], rhs=SEL[:, r, 0:64], start=True, stop=False), reads=[R1.b, SEL.b], writes=[be.b], sig=False)
                            kb.op("pe", lambda e, be=be, ch=ch, r=r: e.matmul(out=be[0:64, 0:64], lhsT=NSEL[:, r, 0:64], rhs=GC[:, ch], start=False, stop=False), reads=[GC.b, NSEL.b], writes=[be.b], sig=False)
                            kb.op("pe", lambda e, be=be, m_s=m_s: e.matmul(out=be[0:64, 0:64], lhsT=I64, rhs=MB[:, m_s, :], start=False, stop=True), reads=[MB.b, ident.b], writes=[be.b])
                            kb.op("pe", lambda e, be=be, ch=ch, r=r: e.matmul(out=be[0:64, 64:128], lhsT=GC[:, ch], rhs=NSEL[:, r, 0:64], start=True, stop=False), reads=[GC.b, NSEL.b], writes=[be.b], sig=False)
                            kb.op("pe", lambda e, be=be, ch=ch, r=r: e.matmul(out=be[0:64, 64:128], lhsT=SEL[:, r, 0:64], rhs=R1[:, ch], start=False, stop=False), reads=[R1.b, SEL.b], writes=[be.b], sig=False)
                            kb.op("pe", lambda e, be=be, m_st=m_st: e.matmul(out=be[0:64, 64:128], lhsT=I64, rhs=MB[:, m_st, :], start=False, stop=True), reads=[MB.b, ident.b], writes=[be.b])
                            kb.op("pe", lambda e, be=be, ch=ch, r=r: e.matmul(out=be[0:64, 128:192], lhsT=GC[:, ch], rhs=NSEL[:, r, 0:64], start=True, stop=False), reads=[GC.b, NSEL.b], writes=[be.b], sig=False)
                            kb.op("pe", lambda e, be=be, ch=ch, r=r: e.matmul(out=be[0:64, 128:192], lhsT=SEL[:, r, 0:64], rhs=GC[:, ch], start=False, stop=False), reads=[GC.b, SEL.b], writes=[be.b], sig=False)
                            kb.op("pe", lambda e, be=be, m_it=m_it: e.matmul(out=be[0:64, 128:192], lhsT=I64, rhs=MB[:, m_it, :], start=False, stop=True), reads=[MB.b, ident.b], writes=[be.b])
                            kb.op("act", lambda e, be=be, d_=d_: e.activation(out=d_["E"][:].rearrange("p a b -> p (a b)"), in_=be[0:64, 0:192], func=AF.Exp), writes=[d_["E"].b, be.b])
                            kb.op("dve", lambda e, bp=bp, d_=d_: e.tensor_tensor(out=d_["AB0"][:, 1, :], in0=bp[0:64, 0:64], in1=d_["E"][:, 0, :], op=ALU.mult), reads=[d_["E"].b], writes=[d_["AB0"].b, bp.b])
                            kb.op("dve", lambda e, bp=bp, d_=d_: e.tensor_tensor(out=d_["AB0"][:, 0, :], in0=bp[0:64, 0:64], in1=d_["E"][:, 1, :], op=ALU.mult), reads=[d_["E"].b], writes=[d_["AB0"].b, bp.b])
                            kb.op("dve", lambda e, bp=bp, d_=d_: e.tensor_tensor(out=d_["E"][:, 2, :], in0=bp[0:64, 64:128], in1=d_["E"][:, 2, :], op=ALU.mult), reads=[d_["E"].b], writes=[d_["E"].b, bp.b])
                            kb.op("dve", lambda e, d_=d_: e.tensor_tensor(out=d_["X0"][:], in0=I64, in1=d_["AB0"][:, 0, :], op=ALU.subtract), reads=[ident.b, d_["AB0"].b], writes=[d_["X0"].b])
                        for k in range(1, 6):
                            pa, pn = (k - 1) % 2, k % 2
                            for ui, (dr, c, d_) in enumerate(units):
                                ABp, ABn = d_["AB%d" % pa], d_["AB%d" % pn]
                                bk = pget()
                                if k < 5:
                                    kb.op("pe", lambda e, bk=bk, ABp=ABp: e.matmul(out=bk[0:64, 0:64], lhsT=ABp[:, 1, :], rhs=ABp[:, 0, :], start=True, stop=True), reads=[ABp.b], writes=[bk.b])
                                kb.op("pe", lambda e, bk=bk, ABp=ABp: e.matmul(out=bk[0:64, 64:128], lhsT=ABp[:, 0, :], rhs=ABp[:, 1, :], start=True, stop=True), reads=[ABp.b], writes=[bk.b])
                                lo = 0 if k < 5 else 64
                                if (ui + k) % 2 == 0:
                                    kb.op("act", lambda e, bk=bk, ABn=ABn, lo=lo: e.activation(out=ABn[:].rearrange("p a b -> p (a b)")[:, lo:128], in_=bk[0:64, lo:128], func=AF.Identity), writes=[ABn.b, bk.b])
                                else:
                                    kb.op("dve", lambda e, bk=bk, ABn=ABn, lo=lo: e.tensor_copy(out=ABn[:].rearrange("p a b -> p (a b)")[:, lo:128], in_=bk[0:64, lo:128]), writes=[ABn.b, bk.b])
                            for ui, (dr, c, d_) in enumerate(units):
                                ABn, Xp, Xn = d_["AB%d" % pn], d_["X%d" % pa], d_["X%d" % pn]
                                bk = pget()
                                kb.op("pe", lambda e, bk=bk, Xp=Xp: e.matmul(out=bk[0:64, 0:64], lhsT=I64b, rhs=Xp[:], start=True, stop=False), reads=[Xp.b, I64b_t.b], writes=[bk.b], sig=False)
                                kb.op("pe", lambda e, bk=bk, Xp=Xp, ABn=ABn: e.matmul(out=bk[0:64, 0:64], lhsT=ABn[:, 1, :], rhs=Xp[:], start=False, stop=True), reads=[Xp.b, ABn.b], writes=[bk.b])
                                if (ui + k) % 2 == 1:
                                    kb.op("act", lambda e, bk=bk, Xn=Xn: e.activation(out=Xn[:], in_=bk[0:64, 0:64], func=AF.Identity), writes=[Xn.b, bk.b])
                                else:
                                    kb.op("dve", lambda e, bk=bk, Xn=Xn: e.tensor_copy(out=Xn[:], in_=bk[0:64, 0:64]), writes=[Xn.b, bk.b])
                        for (dr, c, d_) in units:
                            TT = d_["X1"]
                            bk = pget()
                            kb.op("pe", lambda e, bk=bk, d_=d_, TT=TT: e.matmul(out=bk[:, 0:64], lhsT=d_["kbg"][:], rhs=TT[:], start=True, stop=True), reads=[d_["kbg"].b, TT.b], writes=[bk.b])
                            kb.op("act", lambda e, bk=bk, d_=d_: e.activation(out=d_["nWT"][:], in_=bk[:, 0:64], func=AF.Identity, scale=-1.0), writes=[d_["nWT"].b, bk.b])
                        for u in range(G):
                            for dr in range(2):
                                if dr * G + u >= len(units):
                                    continue
                                dr_, c, d_ = units[dr * G + u]
                                r = dr * 6 + hd
                                S = Sd[dr]
                                t0 = c * 64
                                TT = d_["X1"]
                                bk = pget()
                                kb.op("pe", lambda e, bk=bk, d_=d_, TT=TT: e.matmul(out=bk[0:64, 0:128], lhsT=TT[:], rhs=d_["vb"][:], start=True, stop=False), reads=[d_["vb"].b, TT.b], writes=[bk.b], sig=False)
                                kb.op("pe", lambda e, bk=bk, d_=d_, S=S: e.matmul(out=bk[0:64, 0:128], lhsT=d_["nWT"][:], rhs=S[:], start=False, stop=True), reads=[d_["nWT"].b, S.b], writes=[bk.b])
                                kb.op("act", lambda e, bk=bk, d_=d_: e.activation(out=d_["VN"][:], in_=bk[0:64, 0:128], func=AF.Identity), writes=[d_["VN"].b, bk.b])
                                bk = pget()
                                kb.op("pe", lambda e, bk=bk, t0=t0, dr=dr, S=S: e.matmul(out=bk[0:64, 0:128], lhsT=RAW[:, dr, t0:t0 + 64], rhs=S[:], start=True, stop=False), reads=[RAW.b, S.b], writes=[bk.b], sig=False)
                                kb.op("pe", lambda e, bk=bk, d_=d_: e.matmul(out=bk[0:64, 0:128], lhsT=d_["E"][:, 2, :], rhs=d_["VN"][:], start=False, stop=True), reads=[d_["E"].b, d_["VN"].b], writes=[bk.b])
                                kb.op("dve", lambda e, bk=bk, c=c: e.tensor_tensor(out=OACC[:, c, :], in0=bk[0:64, 0:128], in1=OACC[:, c, :], op=ALU.add), writes=[OACC.b, bk.b])
                                bk = pget()
                                kb.op("pe", lambda e, bk=bk, d_=d_: e.matmul(out=bk[:, 0:128], lhsT=d_["kdec"][:], rhs=d_["VN"][:], start=True, stop=True), reads=[d_["kdec"].b, d_["VN"].b], writes=[bk.b])
                                kb.op("dve", lambda e, bk=bk, c=c, r=r, S=S: e.scalar_tensor_tensor(out=S[:], in0=S[:], scalar=EGLB[:, r, c:c + 1], in1=bk[:, 0:128], op0=ALU.mult, op1=ALU.add),
                                      reads=[EGLB.b], writes=[S.b, bk.b])
                    kb.dma("sp", RAW[:, 2, :], projT[ZOFF + hd * 128:ZOFF + (hd + 1) * 128, :], reads=[CV.b], writes=[RAW.b])
                    kb.op("act", lambda e: e.activation(out=RAW[:, 2, :], in_=RAW[:, 2, :], func=AF.Silu), reads=[RAW.b], writes=[RAW.b])
                    for c in range(NCK):
                        f_ = fin[c % 2]; s_ = fst[c % 2]
                        kb.op("act", lambda e, f_=f_, s_=s_, c=c: e.activation(out=f_[:], in_=OACC[:, c, :], func=AF.Square, accum_out=s_[:, 0:1]), reads=[OACC.b], writes=[f_.b, s_.b])
                        kb.op("act", lambda e, s_=s_: e.activation(out=s_[:, 1:2], in_=s_[:, 0:1], func=AF.Sqrt, bias=EPS, scale=1.0 / 128), reads=[s_.b], writes=[s_.b])
                        kb.op("dve", lambda e, s_=s_: e.reciprocal(out=s_[:, 2:3], in_=s_[:, 1:2]), reads=[s_.b], writes=[s_.b])
                        kb.op("dve", lambda e, f_=f_, s_=s_, c=c: e.scalar_tensor_tensor(out=f_[:], in0=OACC[:, c, :], scalar=s_[:, 2:3], in1=NORM[:], op0=ALU.mult, op1=ALU.mult),
                              reads=[OACC.b, s_.b, NORM.b], writes=[f_.b])
                        bk = pget()
                        kb.op("pe", lambda e, bk=bk, f_=f_: e.transpose(out=bk[:, 0:64], in_=f_[:], identity=I64), reads=[f_.b, ident.b], writes=[bk.b])
                        kb.op("dve", lambda e, bk=bk, c=c: e.tensor_tensor(out=OUT[:, c * 64:(c + 1) * 64], in0=bk[:, 0:64], in1=RAW[:, 2, c * 64:(c + 1) * 64], op=ALU.mult),
                              reads=[RAW.b], writes=[OUT.b, bk.b])
                    kb.dma("sp", mixT[512 + hd * 128:512 + (hd + 1) * 128, :], OUT[:], reads=[OUT.b])
                kb.barrier()
            if stage <= 3:
                break
            with ExitStack() as ph:
                MQ, MK, MV, MO, MI, MF = 3608, 3992, 4376, 5144, 5912, 5924
                NCK = T // 64
                SEL = sb(ph, "mSEL", [64, 12, 128]); NSEL = sb(ph, "mNSEL", [64, 12, 128])
                MB = sb(ph, "mMB", [64, 4, 64])
                NORM = sb(ph, "mNORM", [64, 128])
                MP = sb(ph, "mMP", [64, 2])
                kb.dma("sp", SEL[:], sel_d, writes=[SEL.b])
                kb.dma("sp", MB[:], mb_d, writes=[MB.b])
                kb.dma("sp", NORM[:], ml_norm_d[:, l * 128:(l + 1) * 128], writes=[NORM.b])
                kb.dma("sp", MP[:], ml_mp_d[:, l * 2:(l + 1) * 2], writes=[MP.b])
                kb.op("act", lambda e: e.activation(out=NSEL[:], in_=SEL[:], func=AF.Identity, scale=-1.0), reads=[SEL.b], writes=[NSEL.b])
                RI = sb(ph, "mRI", [64, T]); CM = sb(ph, "mCM", [64, T])
                COL = sb(ph, "mCOL", [64, NCK, 4, 12])
                AOB = sb(ph, "mAOB", [128, 2, 12, NCK])
                I64 = ident[0:64, 0:64]

                def seqperm(eng, dst, dstb, src, srcb, npart, p0=0):
                    kb.op(eng, lambda e: (e.tensor_copy(out=dst[p0:p0 + npart, 0:NCTX], in_=src[p0:p0 + npart, 0:NCTX]) if eng != "act" else
                                          e.activation(out=dst[p0:p0 + npart, 0:NCTX], in_=src[p0:p0 + npart, 0:NCTX], func=AF.Identity)), reads=[srcb], writes=[dstb])
                    ov = dst[p0:p0 + npart, NCTX:T].rearrange("p (c r) -> p c r", r=32)
                    iv = src[p0:p0 + npart, NCTX:T].rearrange("p (r c) -> p c r", c=64)
                    kb.op(eng, lambda e: (e.tensor_copy(out=ov, in_=iv) if eng != "act" else e.activation(out=ov, in_=iv, func=AF.Identity)), reads=[srcb], writes=[dstb])

                with ExitStack() as ph2:
                    RM = sb(ph2, "mRM", [64, T]); RMA = sb(ph2, "mRMA", [64, T])
                    RF = sb(ph2, "mRF", [64, T]); BC = sb(ph2, "mBC", [64, T]); X1 = sb(ph2, "mX1", [64, T]); X2 = sb(ph2, "mX2", [64, T])
                    CH = sb(ph2, "mCH", [64, 8, NCK])
                    kb.dma("sp", RM[:], rm_d, writes=[RM.b])
                    kb.dma("sp", RMA[:], rma_d, writes=[RMA.b])
                    kb.op("dve", lambda e: e.memset(X1[:], 0.0), writes=[X1.b])
                    kb.op("dve", lambda e: e.memset(X2[:], 0.0), writes=[X2.b])
                    for dr in range(2):
                        kb.dma("sp", X1[dr * 32:dr * 32 + 6, :], projT[MI + dr * 6:MI + dr * 6 + 6, :], writes=[X1.b])
                        kb.dma("sp", X2[dr * 32:dr * 32 + 6, :], projT[MF + dr * 6:MF + dr * 6 + 6, :], writes=[X2.b])
                    seqperm("dve", RI, RI.b, X1, X1.b, 64)
                    seqperm("dve", RF, RF.b, X2, X2.b, 64)
                    kb.op("act", lambda e: e.activation(out=RI[:], in_=RI[:], func=AF.Identity, bias=MP[:, 0:1]), reads=[RI.b, MP.b], writes=[RI.b])
                    kb.op("dve", lambda e: e.tensor_scalar(out=MP[:, 1:2], in0=MP[:, 1:2], scalar1=-1.0, scalar2=None, op0=ALU.mult), reads=[MP.b], writes=[MP.b])
                    kb.op("act", lambda e: e.activation(out=RF[:], in_=RF[:], func=AF.Exp, bias=MP[:, 1:2], scale=-1.0), reads=[RF.b, MP.b], writes=[RF.b])
                    kb.op("act", lambda e: e.activation(out=RF[:], in_=RF[:], func=AF.Ln, bias=1.0), reads=[RF.b], writes=[RF.b])
                    kb.op("dve", lambda e: e.tensor_scalar(out=RF[:], in0=RF[:], scalar1=-1.0, scalar2=None, op0=ALU.mult), reads=[RF.b], writes=[RF.b])
                    kb.op("dve", lambda e: e.tensor_tensor_scan(out=BC[0:32, :], data0=RM[0:32, :], data1=RF[0:32, :], initial=0.0, op0=ALU.mult, op1=ALU.add), reads=[RM.b, RF.b], writes=[BC.b])
                    kb.op("dve", lambda e: e.tensor_tensor_scan(out=BC[32:64, ::-1], data0=RM[32:64, ::-1], data1=RF[32:64, ::-1], initial=0.0, op0=ALU.mult, op1=ALU.add), reads=[RM.b, RF.b], writes=[BC.b])
                    kb.op("dve", lambda e: e.tensor_tensor(out=RI[:], in0=RI[:], in1=BC[:], op=ALU.subtract), reads=[RI.b, BC.b], writes=[RI.b])
                    kb.op("dve", lambda e: e.tensor_tensor_scan(out=CM[0:32, :], data0=RMA[0:32, :], data1=RI[0:32, :], initial=-1e30, op0=ALU.add, op1=ALU.max), reads=[RMA.b, RI.b], writes=[CM.b])
                    kb.op("dve", lambda e: e.tensor_tensor_scan(out=CM[32:64, ::-1], data0=RMA[32:64, ::-1], data1=RI[32:64, ::-1], initial=-1e30, op0=ALU.add, op1=ALU.max), reads=[RMA.b, RI.b], writes=[CM.b])
                    for dr in range(2):
                        pr = slice(dr * 32, dr * 32 + 32)
                        lastpos = 63 if dr == 0 else 0
                        kb.op("dve", lambda e, pr=pr, lastpos=lastpos: e.tensor_copy(out=CH[pr, 0, :], in_=BC[pr, :].rearrange("p (c s) -> p c s", s=64)[:, :, lastpos]), reads=[BC.b], writes=[CH.b])
                        kb.op("dve", lambda e, pr=pr, lastpos=lastpos: e.tensor_copy(out=CH[pr, 1, :], in_=CM[pr, :].rearrange("p (c s) -> p c s", s=64)[:, :, lastpos]), reads=[CM.b], writes=[CH.b])
                    B_ = [CH.b]
                    kb.op("dve", lambda e: e.tensor_tensor(out=CH[:, 2, :], in0=CH[:, 0, :], in1=CH[:, 1, :], op=ALU.add), reads=B_, writes=B_)
                    kb.op("dve", lambda e: e.tensor_tensor_scan(out=CH[0:32, 3, :], data0=CH[0:32, 0, :], data1=CH[0:32, 2, :], initial=0.0, op0=ALU.add, op1=ALU.max), reads=B_, writes=B_)
                    kb.op("dve", lambda e: e.tensor_tensor_scan(out=CH[32:64, 3, 0:4][:, ::-1], data0=CH[32:64, 0, 0:4][:, ::-1], data1=CH[32:64, 2, 0:4][:, ::-1], initial=0.0, op0=ALU.add, op1=ALU.max), reads=B_, writes=B_)
                    kb.op("dve", lambda e: e.tensor_tensor_scan(out=CH[32:64, 3, 4:NCK][:, ::-1], data0=CH[32:64, 0, 4:NCK][:, ::-1], data1=CH[32:64, 2, 4:NCK][:, ::-1], initial=CH[32:64, 3, 0:1], op0=ALU.add, op1=ALU.max), reads=B_, writes=B_)
                    kb.op("dve", lambda e: e.memset(CH[:, 4, :], 0.0), reads=B_, writes=B_)
                    kb.op("dve", lambda e: e.tensor_copy(out=CH[0:32, 4, 1:NCK], in_=CH[0:32, 3, 0:NCK - 1]), reads=B_, writes=B_)
                    kb.op("dve", lambda e: e.tensor_copy(out=CH[32:64, 4, 0:3], in_=CH[32:64, 3, 1:4]), reads=B_, writes=B_)
                    kb.op("dve", lambda e: e.tensor_copy(out=CH[32:64, 4, 4:NCK - 1], in_=CH[32:64, 3, 5:NCK]), reads=B_, writes=B_)
                    kb.op("dve", lambda e: e.tensor_copy(out=CH[32:64, 4, NCK - 1:NCK], in_=CH[32:64, 3, 0:1]), reads=B_, writes=B_)
                    kb.op("dve", lambda e: e.tensor_tensor(out=CH[:, 5, :], in0=CH[:, 0, :], in1=CH[:, 4, :], op=ALU.add), reads=B_, writes=B_)
                    kb.op("dve", lambda e: e.tensor_tensor(out=CH[:, 5, :], in0=CH[:, 5, :], in1=CH[:, 3, :], op=ALU.subtract), reads=B_, writes=B_)
                    kb.op("dve", lambda e: e.tensor_tensor(out=CH[:, 6, :], in0=CH[:, 2, :], in1=CH[:, 3, :], op=ALU.subtract), reads=B_, writes=B_)
                    kb.op("act", lambda e: e.activation(out=CH[:, 5:7, :], in_=CH[:, 5:7, :], func=AF.Exp), reads=B_, writes=B_)
                    for c in range(NCK):
                        kb.op("dve", lambda e, c=c: e.tensor_scalar(out=RF[:, c * 64:(c + 1) * 64], in0=CM[:, c * 64:(c + 1) * 64], scalar1=-1.0, scalar2=CH[:, 4, c:c + 1],
                                                                     op0=ALU.mult, op1=ALU.add), reads=[CM.b, CH.b], writes=[RF.b])
                        kb.op("dve", lambda e, c=c: e.tensor_scalar(out=X2[:, c * 64:(c + 1) * 64], in0=RI[:, c * 64:(c + 1) * 64], scalar1=CH[:, 1, c:c + 1], scalar2=None,
                                                                     op0=ALU.subtract), reads=[RI.b, CH.b], writes=[X2.b])
                    kb.op("act", lambda e: e.activation(out=X2[:], in_=X2[:], func=AF.Exp), reads=[X2.b], writes=[X2.b])
                    kb.op("dve", lambda e: e.tensor_tensor(out=BC[:], in0=BC[:], in1=CM[:], op=ALU.add), reads=[BC.b, CM.b], writes=[BC.b])
                    kb.op("dve", lambda e: e.scalar_tensor_tensor(out=BC[:], in0=RF[:], scalar=0.0, in1=BC[:], op0=ALU.max, op1=ALU.add), reads=[RF.b, BC.b], writes=[BC.b])
                    kb.op("act", lambda e: e.activation(out=BC[:], in_=BC[:], func=AF.Exp, scale=-1.0), reads=[BC.b], writes=[BC.b])
                    kb.op("dve", lambda e: e.tensor_scalar(out=X1[:], in0=RF[:], scalar1=0.0, scalar2=None, op0=ALU.min), reads=[RF.b], writes=[X1.b])
                    kb.op("act", lambda e: e.activation(out=X1[:], in_=X1[:], func=AF.Exp), reads=[X1.b], writes=[X1.b])
                    kb.op("dve", lambda e: e.tensor_scalar(out=RF[:], in0=RF[:], scalar1=-1.0, scalar2=0.0, op0=ALU.mult, op1=ALU.min), reads=[RF.b], writes=[RF.b])
                    kb.op("act", lambda e: e.activation(out=RF[:], in_=RF[:], func=AF.Exp), reads=[RF.b], writes=[RF.b])
                    for c in range(NCK):
                        pcol = banks[c % 4]
                        for qi, row in enumerate((X1, RF, BC, X2)):
                            kb.op("pe", lambda e, row=row, qi=qi, pcol=pcol, c=c: e.matmul(out=pcol[0:64, qi * 12:(qi + 1) * 12], lhsT=row[:, c * 64:(c + 1) * 64],
                                                                                       rhs=SEL[:, :, 0], start=True, stop=True), reads=[row.b, SEL.b], writes=[pcol.b])
                        kb.op("act", lambda e, pcol=pcol, c=c: e.activation(out=COL[:, c, :, :].rearrange("p a b -> p (a b)"), in_=pcol[0:64, 0:48], func=AF.Identity),
                              writes=[COL.b, pcol.b])
                    for qi in range(2):
                        for r in range(12):
                            pcol = banks[4 + (qi * 12 + r) % 2]
                            kb.op("pe", lambda e, r=r, pcol=pcol, qi=qi: e.matmul(out=pcol[:, 0:NCK], lhsT=SEL[:, r, :], rhs=CH[:, 5 + qi, :], start=True, stop=True), reads=[SEL.b, CH.b], writes=[pcol.b])
                            kb.op("act", lambda e, r=r, pcol=pcol, qi=qi: e.activation(out=AOB[:, qi, r, :], in_=pcol[:, 0:NCK], func=AF.Identity), writes=[AOB.b, pcol.b])
                    kb.barrier()
                psn = [0]

                def pget():
                    bk = banks[psn[0] % 8]
                    psn[0] += 1
                    return bk
                RAW = sb(ph, "mRAW", [128, T])
                QT = sb(ph, "mQT", [64, T]); KT = sb(ph, "mKT", [64, T]); VT = sb(ph, "mVT", [128, T])
                VA = sb(ph, "mVA", [64, NCK, 132])
                HACC = sb(ph, "mHACC", [64, NCK, 128]); OUT = sb(ph, "mOUT", [128, T], BF16)
                CAd = [sb(ph, "mCA%d" % i, [64, 132]) for i in range(2)]
                G = 4
                U = []
                for u in range(2 * G):
                    d_ = {}
                    for nm, shp in (("E", [64, 64]), ("wk", [64, 64]), ("tmp", [64, 132]), ("comb", [64, 132]), ("sc", [64, 4])):
                        d_[nm] = sb(ph, "mu%d%s" % (u, nm), shp)
                    U.append(d_)
                fin = [sb(ph, "mfin%d" % i, [64, 128]) for i in range(2)]
                fst = [sb(ph, "mfst%d" % i, [64, 4]) for i in range(2)]
                kb.op("dve", lambda e: e.memset(VA[:], 1.0), writes=[VA.b])
                for hd in range(6):
                    kb.dma("sp", RAW[0:64, :], projT[MQ + hd * 64:MQ + (hd + 1) * 64, :], writes=[RAW.b])
                    seqperm("act", QT, QT.b, RAW, RAW.b, 64)
                    kb.op("act", lambda e: e.activation(out=QT[:], in_=QT[:], func=AF.Identity, scale=0.125), reads=[QT.b], writes=[QT.b])
                    kb.dma("sp", RAW[0:64, :], projT[MK + hd * 64:MK + (hd + 1) * 64, :], reads=[QT.b], writes=[RAW.b])
                    seqperm("dve", KT, KT.b, RAW, RAW.b, 64)
                    kb.dma("sp", RAW[:], projT[MV + hd * 128:MV + (hd + 1) * 128, :], reads=[KT.b], writes=[RAW.b])
                    seqperm("act", VT, VT.b, RAW, RAW.b, 128)
                    for c in range(NCK):
                        bk = pget()
                        kb.op("pe", lambda e, bk=bk, c=c: e.transpose(out=bk[0:64, 0:128], in_=VT[:, c * 64:(c + 1) * 64], identity=ident[:]), reads=[VT.b, ident.b], writes=[bk.b])
                        if c % 2 == 0:
                            kb.op("act", lambda e, bk=bk, c=c: e.activation(out=VA[:, c, 0:128], in_=bk[0:64, 0:128], func=AF.Identity), writes=[VA.b, bk.b])
                        else:
                            kb.op("dve", lambda e, bk=bk, c=c: e.tensor_copy(out=VA[:, c, 0:128], in_=bk[0:64, 0:128]), writes=[VA.b, bk.b])
                    kb.op("dve", lambda e: e.memset(HACC[:], 0.0), writes=[HACC.b])
                    for dr in range(2):
                        kb.op("dve", lambda e, dr=dr: e.memset(CAd[dr][:], 0.0), writes=[CAd[dr].b])
                    orders = [list(range(NCK)), list(range(3, -1, -1)) + list(range(NCK - 1, 3, -1))]
                    for g0 in range(0, NCK, G):
                        units = []
                        for dr in range(2):
                            for u, c in enumerate(orders[dr][g0:g0 + G]):
                                units.append((dr, c, U[dr * G + u]))
                        for (dr, c, d_) in units:
                            r = dr * 6 + hd
                            m_it = 3 if dr == 0 else 2
                            ch = slice(c * 64, (c + 1) * 64)
                            be = pget()
                            kb.op("pe", lambda e, be=be, ch=ch, r=r: e.matmul(out=be[0:64, 0:64], lhsT=RI[:, ch], rhs=SEL[:, r, 0:64], start=True, stop=False), reads=[RI.b, SEL.b], writes=[be.b], sig=False)
                            kb.op("pe", lambda e, be=be, ch=ch, r=r: e.matmul(out=be[0:64, 0:64], lhsT=NSEL[:, r, 0:64], rhs=CM[:, ch], start=False, stop=False), reads=[CM.b, NSEL.b], writes=[be.b], sig=False)
                            kb.op("pe", lambda e, be=be, m_it=m_it: e.matmul(out=be[0:64, 0:64], lhsT=I64, rhs=MB[:, m_it, :], start=False, stop=True), reads=[MB.b, ident.b], writes=[be.b])
                            kb.op("act", lambda e, be=be, d_=d_: e.activation(out=d_["E"][:], in_=be[0:64, 0:64], func=AF.Exp), writes=[d_["E"].b, be.b])
                            bp = pget()
                            kb.op("pe", lambda e, bp=bp, ch=ch: e.matmul(out=bp[0:64, 0:64], lhsT=KT[:, ch], rhs=QT[:, ch], start=True, stop=True), reads=[KT.b, QT.b], writes=[bp.b])
                            kb.op("pe", lambda e, bp=bp, ch=ch: e.transpose(out=bp[0:64, 64:128], in_=KT[:, ch], identity=I64), reads=[KT.b, ident.b], writes=[bp.b])
                            kb.op("dve", lambda e, bp=bp, d_=d_: e.tensor_tensor(out=d_["E"][:], in0=bp[0:64, 0:64], in1=d_["E"][:], op=ALU.mult), reads=[d_["E"].b], writes=[d_["E"].b, bp.b])
                            kb.op("dve", lambda e, bp=bp, d_=d_, c=c, r=r: e.tensor_scalar(out=d_["wk"][:], in0=bp[0:64, 64:128], scalar1=COL[:, c, 3, r:r + 1], scalar2=None, op0=ALU.mult),
                                  reads=[COL.b], writes=[d_["wk"].b, bp.b])
                        for u in range(G):
                            for dr in range(2):
                                if dr * G + u >= len(units):
                                    continue
                                dr_, c, d_ = units[dr * G + u]
                                r = dr * 6 + hd
                                CA = CAd[dr]
                                ch = slice(c * 64, (c + 1) * 64)
                                b1 = pget()
                                kb.op("pe", lambda e, b1=b1, ch=ch, CA=CA: e.matmul(out=b1[0:64, 0:129], lhsT=QT[:, ch], rhs=CA[:, 0:129], start=True, stop=True), reads=[QT.b, CA.b], writes=[b1.b])
                                kb.op("act", lambda e, b1=b1, d_=d_, c=c, r=r: e.activation(out=d_["tmp"][:, 0:129], in_=b1[0:64, 0:129], func=AF.Identity, scale=COL[:, c, 0, r:r + 1]),
                                      reads=[COL.b], writes=[d_["tmp"].b, b1.b])
                                b2 = pget()
                                kb.op("pe", lambda e, b2=b2, d_=d_, c=c: e.matmul(out=b2[0:64, 0:129], lhsT=d_["E"][:], rhs=VA[:, c, 0:129], start=True, stop=True), reads=[d_["E"].b, VA.b], writes=[b2.b])
                                kb.op("dve", lambda e, b2=b2, d_=d_, c=c, r=r: e.scalar_tensor_tensor(out=d_["comb"][:, 0:129], in0=b2[0:64, 0:129], scalar=COL[:, c, 1, r:r + 1], in1=d_["tmp"][:, 0:129],
                                                                                              op0=ALU.mult, op1=ALU.add), reads=[COL.b, d_["tmp"].b], writes=[d_["comb"].b, b2.b])
                                kb.op("act", lambda e, d_=d_: e.activation(out=d_["sc"][:, 2:3], in_=d_["comb"][:, 128:129], func=AF.Abs), reads=[d_["comb"].b], writes=[d_["sc"].b])
                                kb.op("dve", lambda e, d_=d_, c=c, r=r: e.tensor_scalar(out=d_["sc"][:, 0:1], in0=d_["sc"][:, 2:3], scalar1=COL[:, c, 2, r:r + 1], scalar2=None, op0=ALU.max),
                                      reads=[COL.b, d_["sc"].b], writes=[d_["sc"].b])
                                kb.op("dve", lambda e, d_=d_: e.reciprocal(out=d_["sc"][:, 1:2], in_=d_["sc"][:, 0:1]), reads=[d_["sc"].b], writes=[d_["sc"].b])
                                kb.op("dve", lambda e, d_=d_, c=c: e.scalar_tensor_tensor(out=HACC[:, c, :], in0=d_["comb"][:, 0:128], scalar=d_["sc"][:, 1:2], in1=HACC[:, c, :], op0=ALU.mult, op1=ALU.add),
                                      reads=[d_["comb"].b, d_["sc"].b, HACC.b], writes=[HACC.b])
                                b3 = pget()
                                kb.op("pe", lambda e, b3=b3, d_=d_, c=c: e.matmul(out=b3[0:64, 0:129], lhsT=d_["wk"][:], rhs=VA[:, c, 0:129], start=True, stop=True), reads=[d_["wk"].b, VA.b], writes=[b3.b])
                                kb.op("act", lambda e, c=c, r=r, CA=CA: e.activation(out=CA[:, 0:129], in_=CA[:, 0:129], func=AF.Identity, scale=AOB[0:64, 0, r, c:c + 1]), reads=[AOB.b, CA.b], writes=[CA.b])
                                kb.op("dve", lambda e, b3=b3, c=c, r=r, CA=CA: e.scalar_tensor_tensor(out=CA[:, 0:129], in0=b3[0:64, 0:129], scalar=AOB[0:64, 1, r, c:c + 1], in1=CA[:, 0:129], op0=ALU.mult, op1=ALU.add),
                                      reads=[AOB.b, CA.b], writes=[CA.b, b3.b])
                    kb.dma("sp", RAW[:], projT[MO + hd * 128:MO + (hd + 1) * 128, :], reads=[VT.b], writes=[RAW.b])
                    seqperm("act", VT, VT.b, RAW, RAW.b, 128)
                    kb.op("act", lambda e: e.activation(out=VT[:], in_=VT[:], func=AF.Sigmoid), reads=[VT.b], writes=[VT.b])
                    for c in range(NCK):
                        f_ = fin[c % 2]; s_ = fst[c % 2]
                        kb.op("act", lambda e, f_=f_, s_=s_, c=c: e.activation(out=f_[:], in_=HACC[:, c, :], func=AF.Square, accum_out=s_[:, 0:1]), reads=[HACC.b], writes=[f_.b, s_.b])
                        kb.op("act", lambda e, s_=s_: e.activation(out=s_[:, 1:2], in_=s_[:, 0:1], func=AF.Sqrt, bias=EPS, scale=1.0 / 128), reads=[s_.b], writes=[s_.b])
                        kb.op("dve", lambda e, s_=s_: e.reciprocal(out=s_[:, 2:3], in_=s_[:, 1:2]), reads=[s_.b], writes=[s_.b])
                        kb.op("dve", lambda e, f_=f_, s_=s_, c=c: e.scalar_tensor_tensor(out=f_[:], in0=HACC[:, c, :], scalar=s_[:, 2:3], in1=NORM[:], op0=ALU.mult, op1=ALU.mult),
                              reads=[HACC.b, s_.b, NORM.b], writes=[f_.b])
                        bk = pget()
                        kb.op("pe", lambda e, bk=bk, f_=f_: e.transpose(out=bk[:, 0:64], in_=f_[:], identity=I64), reads=[f_.b, ident.b], writes=[bk.b])
                        kb.op("dve", lambda e, bk=bk, c=c: e.tensor_tensor(out=RAW[:, c * 64:(c + 1) * 64], in0=bk[:, 0:64], in1=VT[:, c * 64:(c + 1) * 64], op=ALU.mult),
                              reads=[VT.b], writes=[RAW.b, bk.b])
                    kb.op("act", lambda e: e.activation(out=OUT[:, 0:NCTX], in_=RAW[:, 0:NCTX], func=AF.Identity), reads=[RAW.b], writes=[OUT.b])
                    kb.op("act", lambda e: e.activation(out=OUT[:, NCTX:T].rearrange("p (r c) -> p c r", c=64), in_=RAW[:, NCTX:T].rearrange("p (c r) -> p c r", r=32), func=AF.Identity),
                          reads=[RAW.b], writes=[OUT.b])
                    kb.dma("sp", mixT[1280 + hd * 128:1280 + (hd + 1) * 128, :], OUT[:], reads=[OUT.b])
                kb.barrier()
            if stage <= 4:
                break
            with ExitStack() as ph:
                last = (l == L - 1)
                mixb = sb(ph, "mixb", [128, KC, 512], BF16)
                yTb = sb(ph, "yTb", [128, KC, 512])
                h2T = sb(ph, "h2T", [128, KC, 512], BF16)
                hidT = sb(ph, "hidT", [128, 44, 512], BF16)
                xt = [sb(ph, "xt%d" % i, [128, D]) for i in range(2)]
                GG = [[sb(ph, "GG%d%d" % (a, w_), [128, D]) for w_ in range(2)] for a in range(2)]
                wk = [sb(ph, "wk%d" % i, [128, KC, 128], BF16) for i in range(4)]
                wd = [sb(ph, "wd%d" % i, [128, 44, 128], BF16) for i in range(2)]
                sgt = [sb(ph, "sgt%d" % i, [128, 512]) for i in range(2)]
                junk = sb(ph, "junk2", [128, 512], BF16)
                stt_ = [sb(ph, "st2%d" % i, [128, 8]) for i in range(2)]
                bc = sb(ph, "bc", [128, 128])
                for a, (mi, gi) in enumerate(((2, 1), (5, 3))):
                    for w_ in range(2):
                        for j in range(KC):
                            kb.op("dve", lambda e, mi=mi, gi=gi, w_=w_, j=j: e.tensor_tensor(
                                out=bc[:, 0:1], in0=modsT[:, mi * KC + j, w_:w_ + 1], in1=gT[:, (gi * L + l) * KC + j:(gi * L + l) * KC + j + 1],
                                op=ALU.mult), reads=[modsT.b, gT.b], writes=[bc.b])
                            kb.op("dve", lambda e: e.tensor_copy(out=bc[:, 1:128], in_=bc[:, 0:1].to_broadcast([128, 127])), reads=[bc.b], writes=[bc.b])
                            pb = banks[j % 4]
                            kb.op("pe", lambda e, pb=pb: e.transpose(out=pb[:, 0:128], in_=bc[:], identity=ident[:]), reads=[bc.b, ident.b], writes=[pb.b])
                            kb.op("act", lambda e, pb=pb, a=a, w_=w_, j=j: e.activation(out=GG[a][w_][:, j * 128:(j + 1) * 128], in_=pb[:, 0:128], func=AF.Identity),
                                  reads=[pb.b], writes=[GG[a][w_].b])
                mixv = mixT.rearrange("(k p) t -> p k t", p=128)
                tstart = NCTX if last else 0
                nwk = [0]
                nx = [0]

                def post_res(tglob, tloc, a, dst):
                    w_ = 1 if tglob < 2 else 0
                    x_ = xt[nx[0] % 2]
                    s_ = stt_[nx[0] % 2]
                    nx[0] += 1
                    kb.dma("sp", x_[:], xres[tglob * 128:(tglob + 1) * 128, :], writes=[x_.b])
                    for jb in range(4):
                        pb = banks[jb]
                        for jj in range(4):
                            j = jb * 4 + jj
                            kb.op("pe", lambda e, pb=pb, jj=jj, j=j: e.transpose(out=pb[:, jj * 128:(jj + 1) * 128], in_=yTb[:, j, tloc * 128:(tloc + 1) * 128],
                                                                              identity=ident[:]), reads=[yTb.b, ident.b], writes=[pb.b])
                        kb.op("act", lambda e, pb=pb, jb=jb, s_=s_: e.activation(out=junk[:], in_=pb[:], func=AF.Square, accum_out=s_[:, jb:jb + 1]),
                              reads=[pb.b], writes=[junk.b, s_.b])
                    kb.op("dve", lambda e, s_=s_: e.tensor_reduce(out=s_[:, 4:5], in_=s_[:, 0:4], axis=AX.X, op=ALU.add), reads=[s_.b], writes=[s_.b])
                    kb.op("act", lambda e, s_=s_: e.activation(out=s_[:, 5:6], in_=s_[:, 4:5], func=AF.Sqrt, bias=EPS, scale=1.0 / D), reads=[s_.b], writes=[s_.b])
                    kb.op("dve", lambda e, s_=s_: e.reciprocal(out=s_[:, 6:7], in_=s_[:, 5:6]), reads=[s_.b], writes=[s_.b])
                    for jb in range(4):
                        pb = banks[jb]
                        sg_ = sgt[jb % 2]
                        kb.op("dve", lambda e, pb=pb, sg_=sg_, jb=jb, s_=s_: e.scalar_tensor_tensor(
                            out=sg_[:], in0=pb[:], scalar=s_[:, 6:7], in1=GG[a][w_][:, jb * 512:(jb + 1) * 512], op0=ALU.mult, op1=ALU.mult),
                            reads=[pb.b, s_.b, GG[a][w_].b], writes=[sg_.b])
                        kb.op("dve", lambda e, sg_=sg_, jb=jb, x_=x_: e.tensor_tensor(out=x_[:, jb * 512:(jb + 1) * 512], in0=x_[:, jb * 512:(jb + 1) * 512], in1=sg_[:],
                                                                                op=ALU.add), reads=[sg_.b, x_.b], writes=[x_.b])
                    kb.dma("sp", dst, x_[:], reads=[x_.b])
                    return x_, s_, w_

                def dense(src, nk, wview, j0, nj, wbufs, t_nt, consume):
                    for j in range(j0, j0 + nj):
                        w = wbufs[nwk[0] % len(wbufs)]
                        nwk[0] += 1
                        src_ap, src_b = wview[j]
                        kb.dma("sp" if nwk[0] % 2 == 0 else "act", w[:, :nk, :], src_ap, reads=[src_b], writes=[w.b])
                        pj = banks[4 + nwk[0] % 2]
                        for k in range(nk):
                            kb.op("pe", lambda e, pj=pj, w=w, k=k: e.matmul(out=pj[:, :t_nt], lhsT=w[:, k, :], rhs=src[:, k, :t_nt], start=(k == 0), stop=(k == nk - 1)),
                                  reads=[w.b, src.b], writes=[pj.b], sig=(k == nk - 1))
                        consume(j, pj)

                t0 = tstart
                while t0 < T:
                    nt = min(512, T - t0)
                    ntl = nt // 128
                    kb.dma("sp", mixb[:, :, :nt], mixv[:, :, t0:t0 + nt], writes=[mixb.b])

                    def cons_y(j, pj):
                        if j % 2 == 0:
                            kb.op("act", lambda e: e.activation(out=yTb[:, j, :nt], in_=pj[:, :nt], func=AF.Identity), reads=[pj.b], writes=[yTb.b])
                        else:
                            kb.op("dve", lambda e: e.tensor_copy(out=yTb[:, j, :nt], in_=pj[:, :nt]), reads=[pj.b], writes=[yTb.b])
                    dense(mixb, KC, pre["wo"], 0, KC, wk, nt, cons_y)
                    for tl in range(ntl):
                        tg = t0 // 128 + tl
                        x_, s_, w_ = post_res(tg, tl, 0, xres[tg * 128:(tg + 1) * 128, :])
                        kb.op("act", lambda e, x_=x_, s_=s_: e.activation(out=junk[:], in_=x_[:, 0:512], func=AF.Square, accum_out=s_[:, 0:1]), reads=[x_.b], writes=[junk.b, s_.b])
                        for q in range(1, 4):
                            kb.op("act", lambda e, x_=x_, s_=s_, q=q: e.activation(out=junk[:], in_=x_[:, q * 512:(q + 1) * 512], func=AF.Square, accum_out=s_[:, q:q + 1]),
                                  reads=[x_.b], writes=[junk.b, s_.b])
                        kb.op("dve", lambda e, s_=s_: e.tensor_reduce(out=s_[:, 4:5], in_=s_[:, 0:4], axis=AX.X, op=ALU.add), reads=[s_.b], writes=[s_.b])
                        kb.op("act", lambda e, s_=s_: e.activation(out=s_[:, 5:6], in_=s_[:, 4:5], func=AF.Sqrt, bias=EPS, scale=1.0 / D), reads=[s_.b], writes=[s_.b])
                        kb.op("dve", lambda e, s_=s_: e.reciprocal(out=s_[:, 6:7], in_=s_[:, 5:6]), reads=[s_.b], writes=[s_.b])
                        kb.op("dve", lambda e, x_=x_, s_=s_: e.tensor_scalar(out=x_[:], in0=x_[:], scalar1=s_[:, 6:7], scalar2=None, op0=ALU.mult), reads=[x_.b, s_.b], writes=[x_.b])
                        for jb in range(4):
                            pt = banks[jb]
                            for jj in range(4):
                                j = jb * 4 + jj
                                kb.op("pe", lambda e, pt=pt, x_=x_, jj=jj, j=j: e.transpose(out=pt[:, jj * 128:(jj + 1) * 128], in_=x_[:, j * 128:(j + 1) * 128], identity=ident[:]),
                                      reads=[x_.b, ident.b], writes=[pt.b])
                            for jj in range(4):
                                j = jb * 4 + jj
                                kb.op("act", lambda e, pt=pt, jj=jj, j=j, tl=tl, w_=w_: e.activation(
                                    out=h2T[:, j, tl * 128:(tl + 1) * 128], in_=pt[:, jj * 128:(jj + 1) * 128], func=AF.Identity,
                                    bias=modsT[:, 3 * KC + j, w_:w_ + 1], scale=gsf[:, j, w_:w_ + 1]), reads=[pt.b, modsT.b, gsf.b], writes=[h2T.b])
                    for hc in range(44):
                        got = {}

                        def cons_g(j, pj):
                            got["g"] = pj
                        dense(h2T, KC, pre["wg"], hc, 1, wk, nt, cons_g)
                        pg = got["g"]
                        sg_ = sgt[hc % 2]
                        kb.op("act", lambda e, pg=pg, sg_=sg_: e.activation(out=sg_[:, :nt], in_=pg[:, :nt], func=AF.Silu), reads=[pg.b], writes=[sg_.b])

                        def cons_u(j, pj):
                            kb.op("dve", lambda e: e.tensor_tensor(out=hidT[:, hc, :nt], in0=pj[:, :nt], in1=sg_[:, :nt], op=ALU.mult), reads=[pj.b, sg_.b], writes=[hidT.b])
                        dense(h2T, KC, pre["wu"], hc, 1, wk, nt, cons_u)
                    dense(hidT, 44, pre["wd"], 0, KC, wd, nt, cons_y)
                    for tl in range(ntl):
                        tg = t0 // 128 + tl
                        dst = out_d[(tg - 2) * 128:(tg - 1) * 128, :] if last else xres[tg * 128:(tg + 1) * 128, :]
                        post_res(tg, tl, 1, dst)
                    t0 += nt
                kb.barrier()
            if stage == 5:
                kb.dma("sp", xres_o, xres)
                break
        kb.barrier()
    return nc


def kernel(**inp):
    inp = {k: np.asarray(v) for k, v in inp.items()}
    nc = build(99)
    base = host_prep(inp, 0)
    in_maps = []
    for core in range(8):
        b = core % 4
        m = dict(base)
        if b != 0:
            pb = host_prep_batch(inp, b)
            m.update(pb)
        in_maps.append(m)
    res = run_bass_kernel_spmd(nc, in_maps, core_ids=list(range(8)))
    out = np.stack([np.asarray(res.results[b]["out"]) for b in range(4)], 0).astype(np.float32)
    return out
```

```python
import numpy as np
from contextlib import ExitStack
import concourse.bass as bass
import concourse.mybir as mybir
from concourse.bass_utils import run_bass_kernel_spmd

F32 = mybir.dt.float32
BF16 = mybir.dt.bfloat16
ALU = mybir.AluOpType
AF = mybir.ActivationFunctionType
AX = mybir.AxisListType

D = 2048
T = 2304
NCTX = 256
NLAT = 2048
L = 2
KC = 16
IN_COLS = 5936
FFN = 5632
EPS = 1e-6
NEG = -30000.0
SEM_ROT = 30000
NSLOT = 6
LC = 128


class Ev:
    __slots__ = ("sem", "val")

    def __init__(self, sem, val):
        self.sem = sem
        self.val = val


class Buf:
    __slots__ = ("w", "r", "name")

    def __init__(self, name=""):
        self.w = None
        self.r = {}
        self.name = name


class Eng:
    def __init__(self, kb, name, h):
        self.kb = kb
        self.name = name
        self.h = h
        self.sem = kb.newsem("e_" + name)
        self.count = 0
        self.seen = {}
        self.n = 0
        self.pending = 0


class Slot:
    def __init__(self, sem):
        self.sem = sem
        self.uses = 0


class KB:
    def __init__(self, nc, es):
        self.nc = nc
        self.es = es
        self.nsem = 0
        self.E = {}
        for name, h in (("pe", nc.tensor), ("act", nc.scalar), ("dve", nc.vector), ("pool", nc.gpsimd), ("sp", nc.sync)):
            self.E[name] = Eng(self, name, h)
        self.slots = {}
        self.rr = {}
        for q in ("sp", "pool", "act"):
            self.slots[q] = [Slot(self.newsem("d_%s%d" % (q, i))) for i in range(NSLOT)]
            self.rr[q] = 0

    def newsem(self, name):
        self.nsem += 1
        return self.es.enter_context(self.nc.semaphore("%s_%d" % (name, self.nsem)))

    def _wait(self, eng, ev):
        k = id(ev.sem)
        if eng.seen.get(k, 0) < ev.val:
            eng.h.wait_ge(ev.sem, ev.val)
            eng.seen[k] = ev.val

    def _deps(self, eng, reads, writes):
        need = {}

        def add(ev):
            k = id(ev.sem)
            if k not in need or need[k].val < ev.val:
                need[k] = ev

        for b in reads:
            if b.w is not None:
                add(b.w)
        for b in writes:
            if b.w is not None:
                add(b.w)
            for ev in b.r.values():
                add(ev)
        for ev in need.values():
            if eng.name == "pe" and ev.sem is eng.sem:
                continue
            self._wait(eng, ev)

    def _post(self, ev, reads, writes):
        k = id(ev.sem)
        for b in reads:
            b.r[k] = ev
        for b in writes:
            b.w = ev
            b.r = {}

    def op(self, e, fn, reads=(), writes=(), sig=True):
        eng = self.E[e]
        self._deps(eng, reads, writes)
        if eng.count >= SEM_ROT and eng.pending == 0:
            eng.sem = self.newsem("e_" + eng.name)
            eng.count = 0
        inst = fn(eng.h)
        eng.n += 1
        if sig:
            eng.count += 1
            eng.pending = 0
            inst.then_inc(eng.sem, 1)
            ev = Ev(eng.sem, eng.count)
        else:
            assert e == "pe"
            eng.pending += 1
            ev = Ev(eng.sem, eng.count + 1)
        self._post(ev, reads, writes)
        return ev

    def dma(self, q, out, in_, reads=(), writes=(), **kw):
        eng = self.E[q]
        self._deps(eng, reads, writes)
        sl = self.slots[q][self.rr[q] % NSLOT]
        self.rr[q] += 1
        if sl.uses * 16 >= SEM_ROT:
            self._wait(eng, Ev(sl.sem, 16 * sl.uses))
            sl.sem = self.newsem("d_" + q)
            sl.uses = 0
        if sl.uses > 0:
            self._wait(eng, Ev(sl.sem, 16 * sl.uses))
        inst = eng.h.dma_start(out=out, in_=in_, **kw)
        sl.uses += 1
        inst.then_inc(sl.sem, 16)
        ev = Ev(sl.sem, 16 * sl.uses)
        self._post(ev, reads, writes)
        return ev

    def barrier(self, engines=("pe", "act", "dve", "pool", "sp")):
        evs = []
        for e in self.E.values():
            assert e.pending == 0
            if e.count > 0:
                evs.append(Ev(e.sem, e.count))
        for q in self.slots:
            for sl in self.slots[q]:
                if sl.uses > 0:
                    evs.append(Ev(sl.sem, 16 * sl.uses))
        for en in engines:
            eng = self.E[en]
            for ev in evs:
                self._wait(eng, ev)


class Tl:
    def __init__(self, t, name=""):
        self.t = t
        self.b = Buf(name)

    def __getitem__(self, idx):
        return self.t[idx]


def host_prep_batch(inp, b):
    f = np.float32
    m = {}
    m["xin"] = np.ascontiguousarray(np.concatenate([inp["ctx"][b], inp["x"][b]], axis=0).astype(f))
    cv = np.stack([inp["c"][b].reshape(KC, 128).T, inp["c_ctx"].reshape(KC, 128).T], axis=-1)
    m["cv"] = np.ascontiguousarray(cv.astype(f))
    return m


def host_prep(inp, b):
    f = np.float32
    m = host_prep_batch(inp, b)
    m["ada_w"] = inp["ada_w"]
    m["ada_bT"] = np.ascontiguousarray(inp["ada_b"].reshape(L, 96, 128).transpose(2, 0, 1).reshape(128, L * 96).astype(f))
    g = np.stack([inp["norm_mix_pre"], inp["norm_mix_post"], inp["norm_ffn_pre"], inp["norm_ffn_post"]], 0)
    m["gT"] = np.ascontiguousarray(g.reshape(4, L, KC, 128).transpose(3, 0, 1, 2).reshape(128, 4 * L * KC).astype(f))
    m["w_in"] = inp["w_in"]
    for k_ in ("w_out", "ffn_w_gate", "ffn_w_up", "ffn_w_down"):
        m[k_] = inp[k_]
    m["ident"] = np.eye(128, dtype=f)
    def st_major(a):
        return np.ascontiguousarray(a.reshape(L, 2, 16, 128).transpose(3, 0, 1, 2).reshape(128, L * 2 * 16).astype(f))
    m["s5_lre"] = st_major(inp["s5_lam_re"].reshape(L, 2, 2048))
    m["s5_lim"] = st_major(inp["s5_lam_im"].reshape(L, 2, 2048))
    m["s5_ldt"] = st_major(np.repeat(inp["s5_log_dt"], 64, axis=-1))
    Bb = np.zeros((128, L, 2, 2, 16, 128), f)
    Cb = np.zeros((128, L, 2, 2, 16, 128), f)
    for ci, (bn, cn) in enumerate((("s5_b_re", "s5_c_re"), ("s5_b_im", "s5_c_im"))):
        bsrc = inp[bn]
        csrc = inp[cn]
        for g in range(32):
            st = g // 2
            r0 = (g % 8) * 16
            c0 = (g % 2) * 64
            Bb[r0:r0 + 16, :, :, ci, st, c0:c0 + 64] = bsrc[:, :, g].transpose(3, 0, 1, 2)
            Cb[c0:c0 + 64, :, :, ci, st, r0:r0 + 16] = csrc[:, :, g].transpose(3, 0, 1, 2)
    m["s5_Bb"] = np.ascontiguousarray(Bb.reshape(128, L * 2 * 2 * 16, 128))
    m["s5_Cb"] = np.ascontiguousarray(Cb.reshape(128, L * 2 * 2 * 16, 128))
    m["s5_dT"] = np.ascontiguousarray(inp["s5_d"].reshape(L, 4, 128).transpose(2, 0, 1).reshape(128, L * 4).astype(f))
    m["s5_gbT"] = np.ascontiguousarray(inp["s5_glu_b"].reshape(L, 4, 128).transpose(2, 0, 1).reshape(128, L * 4).astype(f))
    m["s5_glu_w"] = inp["s5_glu_w"]
    tt = np.arange(T)
    rm = np.ones((64, T), f)
    rm[0:32, tt % 64 == 0] = 0.0
    rm[32:64, tt % 64 == 63] = 0.0
    m["rm"] = rm
    sel = np.zeros((64, 12, 128), f)
    for r_ in range(12):
        sel[(r_ // 6) * 32 + r_ % 6, r_, :] = 1.0
    m["sel"] = sel
    a_ = np.arange(64)[:, None]; b_ = np.arange(64)[None, :]
    mb = np.stack([np.where(b_ < a_, 0.0, NEG), np.where(b_ > a_, 0.0, NEG), np.where(b_ <= a_, 0.0, NEG), np.where(b_ >= a_, 0.0, NEG)], 1).astype(f)
    m["mb"] = np.ascontiguousarray(mb)
    m["gdn_normr"] = np.ascontiguousarray(np.tile(inp["gdn_norm"].reshape(1, L * 128), (64, 1)).astype(f))
    gp = np.zeros((64, L, 2), f)
    for dr_ in range(2):
        gp[dr_ * 32:dr_ * 32 + 6, :, 0] = inp["gdn_a_log"][:, dr_, :].T
        gp[dr_ * 32:dr_ * 32 + 6, :, 1] = inp["gdn_dt_bias"][:, dr_, :].T
    m["gdn_gp"] = np.ascontiguousarray(gp.reshape(64, L * 2))
    cw = inp["gdn_conv_w"].reshape(L, 3, 18, 128).transpose(3, 0, 2, 1)
    m["gdn_cw"] = np.ascontiguousarray(cw.reshape(128, L * 54).astype(f))
    rma = np.zeros((64, T), f)
    rma[0:32, tt % 64 == 0] = -1e30
    rma[32:64, tt % 64 == 63] = -1e30
    m["rma"] = rma
    m["ml_normr"] = np.ascontiguousarray(np.tile(inp["mlstm_norm"].reshape(1, L * 128), (64, 1)).astype(f))
    mp = np.zeros((64, L, 2), f)
    for dr_ in range(2):
        mp[dr_ * 32:dr_ * 32 + 6, :, 0] = inp["mlstm_i_bias"][:, dr_, :].T
        mp[dr_ * 32:dr_ * 32 + 6, :, 1] = inp["mlstm_f_bias"][:, dr_, :].T
    m["ml_mp"] = np.ascontiguousarray(mp.reshape(64, L * 2))
    m["tau1"] = np.ascontiguousarray(np.tile(np.arange(1, LC + 1, dtype=f)[None, :], (128, 1)))
    return m


def build(stage=99):
    nc = bass.Bass("TRN2", target_bir_lowering=False)
    es = ExitStack()
    with es:
        def din(name, shape, dt=F32):
            return nc.dram_tensor(name, list(shape), dt, kind="ExternalInput").ap()

        def dout(name, shape, dt=F32):
            return nc.dram_tensor(name, list(shape), dt, kind="ExternalOutput").ap()

        def dscr(name, shape, dt=F32):
            return nc.dram_tensor(name, list(shape), dt, kind="Internal").ap()

        xin = din("xin", [T, D])
        cv_d = din("cv", [128, KC, 2])
        ada_w = din("ada_w", [L, D, 6 * D])
        ada_bT = din("ada_bT", [128, L * 96])
        gT_d = din("gT", [128, 4 * L * KC])
        w_in = din("w_in", [L, D, IN_COLS])
        ident_d = din("ident", [128, 128])
        s5_lre_d = din("s5_lre", [128, L * 32])
        s5_lim_d = din("s5_lim", [128, L * 32])
        s5_ldt_d = din("s5_ldt", [128, L * 32])
        s5_Bb_d = din("s5_Bb", [128, L * 64, 128])
        s5_Cb_d = din("s5_Cb", [128, L * 64, 128])
        s5_dT_d = din("s5_dT", [128, L * 4])
        s5_gbT_d = din("s5_gbT", [128, L * 4])
        s5_gluw_d = din("s5_glu_w", [L, 512, 512])
        tau1_d = din("tau1", [128, LC])
        rm_d = din("rm", [64, T])
        sel_d = din("sel", [64, 12, 128])
        mb_d = din("mb", [64, 4, 64])
        gdn_norm_d = din("gdn_normr", [64, L * 128])
        gdn_gp_d = din("gdn_gp", [64, L * 2])
        gdn_cw_d = din("gdn_cw", [128, L * 54])
        rma_d = din("rma", [64, T])
        ml_norm_d = din("ml_normr", [64, L * 128])
        ml_mp_d = din("ml_mp", [64, L * 2])
        w_out = din("w_out", [L, D, D])
        w_gate = din("ffn_w_gate", [L, D, FFN])
        w_up = din("ffn_w_up", [L, D, FFN])
        w_down = din("ffn_w_down", [L, FFN, D])
        out_d = dout("out", [NLAT, D])
        xres = dscr("xres", [T, D])
        if stage <= 1:
            projT = dout("projT", [47 * 128, T])
            mods_o = dout("mods_o", [128, 96 * 2])
        else:
            projT = dscr("projT", [47 * 128, T])
        if stage == 3:
            dbg_cv = dout("dbg_cv", [128, 3, T])
            dbg_rows = dout("dbg_rows", [64, 6, T])
            dbg_oacc = dout("dbg_oacc", [64, 36, 128])
            dbg_oaccf = dout("dbg_oaccf", [64, 36, 128])
        if stage == 5:
            mixT = din("mixT", [D, T], BF16)
            xres_o = dout("xres_o", [T, D])
        elif 2 <= stage <= 4:
            mixT = dout("mixT", [D, T], BF16)
        else:
            mixT = dscr("mixT", [D, T], BF16)

        kb = KB(nc, es)

        cnt = [0]

        def sb(st, name, shape, dt=F32):
            cnt[0] += 1
            nm = "s%d_%s" % (cnt[0], name)
            return Tl(st.enter_context(nc.sbuf_tensor(nm, list(shape), dt)), nm)

        def ps(st, name, shape, dt=F32):
            cnt[0] += 1
            nm = "p%d_%s" % (cnt[0], name)
            return Tl(st.enter_context(nc.psum_tensor(nm, list(shape), dt)), nm)

        ident = sb(es, "ident", [128, 128])
        gT = sb(es, "gT", [128, 4 * L * KC])
        abT = sb(es, "abT", [128, L * 96])
        cvs = sb(es, "cvs", [128, KC, 2])
        modsT = sb(es, "modsT", [128, 96, 2])
        gsm = sb(es, "gsm", [128, KC, 2])
        gsf = sb(es, "gsf", [128, KC, 2])
        kb.dma("sp", ident[:], ident_d, writes=[ident.b])
        kb.dma("sp", gT[:], gT_d, writes=[gT.b])
        kb.dma("sp", abT[:], ada_bT, writes=[abT.b])
        kb.dma("sp", cvs[:], cv_d, writes=[cvs.b])
        kb.op("act", lambda e: e.activation(out=cvs[:], in_=cvs[:], func=AF.Silu), reads=[cvs.b], writes=[cvs.b])

        banks = [ps(es, "bank%d" % i, [128, 512]) for i in range(8)]
        xres_b = Buf("xres")
        kb.dma("sp", xres, xin, writes=[xres_b])
        kb.barrier()

        for l in range(L):
            with ExitStack() as ph:
                wA = [sb(ph, "wA%d" % i, [128, KC, 512]) for i in range(2)]
                pm = banks[0]
                adv = ada_w[l].rearrange("(k p) c -> p k c", p=128)
                for cb in range(24):
                    w = wA[cb % 2]
                    kb.dma("sp", w[:], adv[:, :, cb * 512:(cb + 1) * 512], writes=[w.b])
                    for jj in range(4):
                        jo = cb * 4 + jj
                        for k in range(KC):
                            kb.op("pe", lambda e, w=w, k=k, jj=jj, jo=jo: e.matmul(
                                out=pm[:, jo * 2:jo * 2 + 2], lhsT=w[:, k, jj * 128:(jj + 1) * 128], rhs=cvs[:, k, :],
                                start=(k == 0), stop=(k == KC - 1)), reads=[w.b, cvs.b], writes=[pm.b], sig=(k == KC - 1))
                for wi in range(2):
                    kb.op("dve", lambda e, wi=wi: e.tensor_tensor(
                        out=modsT[:, :, wi], in0=pm[:, wi:192:2], in1=abT[:, l * 96:(l + 1) * 96], op=ALU.add),
                        reads=[pm.b, abT.b], writes=[modsT.b])
                for (gs, mi, gi) in ((gsm, 1, 0), (gsf, 4, 2)):
                    for wi in range(2):
                        kb.op("dve", lambda e, gs=gs, mi=mi, gi=gi, wi=wi: e.scalar_tensor_tensor(
                            out=gs[:, :, wi], in0=modsT[:, mi * KC:(mi + 1) * KC, wi], scalar=1.0,
                            in1=gT[:, (gi * L + l) * KC:(gi * L + l + 1) * KC], op0=ALU.add, op1=ALU.mult),
                            reads=[modsT.b, gT.b], writes=[gs.b])
                kb.barrier()
            if stage <= 1 and l == 0:
                kb.dma("sp", mods_o, modsT[:].rearrange("p a b -> p (a b)"), reads=[modsT.b])

            with ExitStack() as lay:
                hT = sb(lay, "hT", [128, KC, T], BF16)
                with ExitStack() as ph:
                    xt = [sb(ph, "xt%d" % i, [128, D]) for i in range(2)]
                    junk = sb(ph, "junk", [128, D], BF16)
                    st = [sb(ph, "st%d" % i, [128, 4]) for i in range(2)]
                    for i in range(T // 128):
                        x_ = xt[i % 2]
                        s_ = st[i % 2]
                        wsel = 1 if i < 2 else 0
                        kb.dma("sp", x_[:], xres[i * 128:(i + 1) * 128, :], writes=[x_.b])
                        kb.op("act", lambda e, x_=x_, s_=s_: e.activation(out=junk[:], in_=x_[:], func=AF.Square,
                                                                           accum_out=s_[:, 0:1]),
                              reads=[x_.b], writes=[junk.b, s_.b])
                        kb.op("act", lambda e, s_=s_: e.activation(out=s_[:, 1:2], in_=s_[:, 0:1], func=AF.Sqrt,
                                                                    bias=EPS, scale=1.0 / D), reads=[s_.b], writes=[s_.b])
                        kb.op("dve", lambda e, s_=s_: e.reciprocal(out=s_[:, 2:3], in_=s_[:, 1:2]), reads=[s_.b], writes=[s_.b])
                        kb.op("dve", lambda e, x_=x_, s_=s_: e.tensor_scalar(out=x_[:], in0=x_[:], scalar1=s_[:, 2:3], scalar2=None,
                                                                          op0=ALU.mult), reads=[x_.b, s_.b], writes=[x_.b])
                        for jb in range(4):
                            pt = banks[1 + (i * 4 + jb) % 4]
                            for jj in range(4):
                                j = jb * 4 + jj
                                kb.op("pe", lambda e, pt=pt, x_=x_, jj=jj, j=j: e.transpose(
                                    out=pt[:, jj * 128:(jj + 1) * 128], in_=x_[:, j * 128:(j + 1) * 128], identity=ident[:]),
                                    reads=[x_.b, ident.b], writes=[pt.b])
                            for jj in range(4):
                                j = jb * 4 + jj
                                kb.op("act", lambda e, pt=pt, jj=jj, j=j, i=i, wsel=wsel: e.activation(
                                    out=hT[:, j, i * 128:(i + 1) * 128], in_=pt[:, jj * 128:(jj + 1) * 128], func=AF.Identity,
                                    bias=modsT[:, 0 * KC + j, wsel:wsel + 1], scale=gsm[:, j, wsel:wsel + 1]),
                                    reads=[pt.b, modsT.b, gsm.b], writes=[hT.b])
                    kb.barrier()
                with ExitStack() as ph:
                    wC = [sb(ph, "wC%d" % i, [128, KC, 128], BF16) for i in range(4)]
                    sg = [sb(ph, "sg%d" % i, [128, 512]) for i in range(3)]
                    wv = w_in[l].rearrange("(k p) c -> p k c", p=128)
                    nev = 0
                    for cc in range(47):
                        c0 = cc * 128
                        n = min(128, IN_COLS - c0)
                        w = wC[cc % 4]
                        kb.dma("pool", w[:, :, :n], wv[:, :, c0:c0 + n], writes=[w.b])
                        for tb in range(5):
                            t0 = tb * 512
                            nt = min(512, T - t0)
                            pj = banks[5 + nev % 3]
                            for k in range(KC):
                                kb.op("pe", lambda e, pj=pj, w=w, k=k, n=n, t0=t0, nt=nt: e.matmul(
                                    out=pj[:n, :nt], lhsT=w[:, k, :n], rhs=hT[:, k, t0:t0 + nt],
                                    start=(k == 0), stop=(k == KC - 1)), reads=[w.b, hT.b], writes=[pj.b], sig=(k == KC - 1))
                            s_ = sg[nev % 3]
                            eng = "act" if nev % 2 == 0 else "dve"
                            if eng == "act":
                                kb.op("act", lambda e, s_=s_, pj=pj, n=n, nt=nt: e.activation(out=s_[:n, :nt], in_=pj[:n, :nt],
                                                                                            func=AF.Identity),
                                      reads=[pj.b], writes=[s_.b])
                            else:
                                kb.op("dve", lambda e, s_=s_, pj=pj, n=n, nt=nt: e.tensor_copy(out=s_[:n, :nt], in_=pj[:n, :nt]),
                                      reads=[pj.b], writes=[s_.b])
                            kb.dma("sp", projT[c0:c0 + n, t0:t0 + nt], s_[:n, :nt], reads=[s_.b])
                            nev += 1
                    kb.barrier()
            if stage <= 1:
                break
            wov = w_out[l].rearrange("(k p) c -> p k c", p=128)
            wgv = w_gate[l].rearrange("(k p) c -> p k c", p=128)
            wuv = w_up[l].rearrange("(k p) c -> p k c", p=128)
            wdv = w_down[l].rearrange("(h p) c -> p h c", p=128)
            pre = {}
            for nm_, view_, nch_, nk_ in (("wo", wov, KC, KC), ("wg", wgv, 44, KC), ("wu", wuv, 44, KC), ("wd", wdv, KC, 44)):
                scr_ = dscr("pc_%s%d" % (nm_, l), [nch_, 128, nk_ * 128], BF16)
                lst_ = []
                for j_ in range(nch_):
                    b_ = Buf()
                    dst_ = scr_[j_].rearrange("p (k c) -> p k c", c=128)
                    kb.dma("pool", dst_, view_[:, :, j_ * 128:(j_ + 1) * 128], writes=[b_])
                    lst_.append((dst_, b_))
                pre[nm_] = lst_
            with ExitStack() as ph:
                TWO_PI = 6.283185307179586
                C1 = 6.28125
                C2 = TWO_PI - C1
                PI = 3.141592653589793
                uT = sb(ph, "uT", [128, 4, T])
                yT = sb(ph, "yT", [128, 4, T])
                Bb = sb(ph, "Bb", [128, 32, 128])
                Cb = sb(ph, "Cb", [128, 32, 128])
                lre = sb(ph, "lre", [128, 32]); lim = sb(ph, "lim", [128, 32]); ldt = sb(ph, "ldt", [128, 32])
                tau1 = sb(ph, "tau1", [128, LC])
                dTt = sb(ph, "dTt", [128, L * 4]); gbT = sb(ph, "gbT", [128, L * 4])
                gluw = sb(ph, "gluw", [128, 4, 512])
                kb.dma("sp", uT[:], projT[0:512, :].rearrange("(c p) t -> p c t", p=128), writes=[uT.b])
                kb.dma("sp", lre[:], s5_lre_d[:, l * 32:(l + 1) * 32], writes=[lre.b])
                kb.dma("sp", lim[:], s5_lim_d[:, l * 32:(l + 1) * 32], writes=[lim.b])
                kb.dma("sp", ldt[:], s5_ldt_d[:, l * 32:(l + 1) * 32], writes=[ldt.b])
                kb.dma("sp", tau1[:], tau1_d, writes=[tau1.b])
                kb.dma("sp", dTt[:], s5_dT_d, writes=[dTt.b])
                kb.dma("sp", gbT[:], s5_gbT_d, writes=[gbT.b])
                kb.dma("sp", gluw[:], s5_gluw_d[l].rearrange("(c p) n -> p c n", p=128), writes=[gluw.b])

                def sincos(n, ang, o_sin, o_cos, tf, ti, tm, tb_):
                    B = [tb_]
                    kb.op("dve", lambda e: e.tensor_scalar(out=tf, in0=ang, scalar1=1.0 / TWO_PI, scalar2=None, op0=ALU.mult), reads=B, writes=B)
                    kb.op("dve", lambda e: e.tensor_copy(out=ti, in_=tf), reads=B, writes=B)
                    kb.op("dve", lambda e: e.tensor_copy(out=tf, in_=ti), reads=B, writes=B)
                    kb.op("dve", lambda e: e.scalar_tensor_tensor(out=tm, in0=tf, scalar=-C1, in1=ang, op0=ALU.mult, op1=ALU.add), reads=B, writes=B)
                    kb.op("dve", lambda e: e.scalar_tensor_tensor(out=tm, in0=tf, scalar=-C2, in1=tm, op0=ALU.mult, op1=ALU.add), reads=B, writes=B)

                    def wrap(y):
                        kb.op("dve", lambda e: e.tensor_scalar(out=tf, in0=y, scalar1=PI, scalar2=TWO_PI, op0=ALU.is_gt, op1=ALU.mult), reads=B, writes=B)
                        kb.op("dve", lambda e: e.tensor_tensor(out=y, in0=y, in1=tf, op=ALU.subtract), reads=B, writes=B)
                        kb.op("dve", lambda e: e.tensor_scalar(out=tf, in0=y, scalar1=-PI, scalar2=TWO_PI, op0=ALU.is_lt, op1=ALU.mult), reads=B, writes=B)
                        kb.op("dve", lambda e: e.tensor_tensor(out=y, in0=y, in1=tf, op=ALU.add), reads=B, writes=B)
                    wrap(tm)
                    kb.op("act", lambda e: e.activation(out=o_sin, in_=tm, func=AF.Sin), reads=B, writes=B)
                    kb.op("dve", lambda e: e.tensor_scalar(out=tm, in0=tm, scalar1=PI / 2, scalar2=None, op0=ALU.add), reads=B, writes=B)
                    wrap(tm)
                    kb.op("act", lambda e: e.activation(out=o_cos, in_=tm, func=AF.Sin), reads=B, writes=B)

                with ExitStack() as ph2:
                    tabs = sb(ph2, "tabs", [128, 4, 16 * LC])
                    tw = sb(ph2, "tw", [128, 3, 16 * LC])
                    twi = sb(ph2, "twi", [128, 16 * LC], mybir.dt.int32)
                    sp_ = sb(ph2, "s5par", [128, 16, 16])
                    spi = sb(ph2, "s5pari", [128, 16], mybir.dt.int32)
                    car = sb(ph2, "car", [128, 2, 16])
                    S5U = [sb(ph2, "s5u%d" % i, [128, 6, LC]) for i in range(8)]
                    S5UB = [[Buf("s5u%d_%d" % (i, k_)) for k_ in range(6)] for i in range(8)]
                    S5B = [tabs.b, tw.b, twi.b, sp_.b, spi.b]

                    def P_(k):
                        return sp_[:, k, :]
                    for i in range(2):
                        o = (l * 2 + i) * 16
                        B = [sp_.b]
                        kb.dma("sp", Bb[:], s5_Bb_d[:, (l * 2 + i) * 32:(l * 2 + i + 1) * 32, :], writes=[Bb.b])
                        kb.dma("sp", Cb[:], s5_Cb_d[:, (l * 2 + i) * 32:(l * 2 + i + 1) * 32, :], writes=[Cb.b])
                        kb.op("act", lambda e: e.activation(out=Cb[:, 16:32, :], in_=Cb[:, 16:32, :], func=AF.Identity, scale=-1.0),
                              reads=[Cb.b], writes=[Cb.b])
                        kb.op("act", lambda e: e.activation(out=P_(0), in_=ldt[:, (l * 2 + i) * 16 - l * 32 + 0:(l * 2 + i) * 16 - l * 32 + 16], func=AF.Exp), reads=[ldt.b], writes=B)
                        kb.op("dve", lambda e: e.tensor_tensor(out=P_(1), in0=lre[:, i * 16:(i + 1) * 16], in1=P_(0), op=ALU.mult), reads=[lre.b] + B, writes=B)
                        kb.op("dve", lambda e: e.tensor_tensor(out=P_(2), in0=lim[:, i * 16:(i + 1) * 16], in1=P_(0), op=ALU.mult), reads=[lim.b] + B, writes=B)
                        kb.op("act", lambda e: e.activation(out=P_(3), in_=P_(1), func=AF.Exp), reads=B, writes=B)
                        sincos(16, P_(2), P_(4), P_(5), P_(6), spi[:], P_(7), sp_.b)
                        kb.op("dve", lambda e: e.tensor_tensor(out=P_(6), in0=P_(3), in1=P_(5), op=ALU.mult), reads=B, writes=B)
                        kb.op("dve", lambda e: e.tensor_scalar(out=P_(6), in0=P_(6), scalar1=-1.0, scalar2=None, op0=ALU.add), reads=B, writes=B)
                        kb.op("dve", lambda e: e.tensor_tensor(out=P_(7), in0=P_(3), in1=P_(4), op=ALU.mult), reads=B, writes=B)
                        kb.op("dve", lambda e: e.tensor_tensor(out=P_(8), in0=lre[:, i * 16:(i + 1) * 16], in1=lre[:, i * 16:(i + 1) * 16], op=ALU.mult), reads=[lre.b] + B, writes=B)
                        kb.op("dve", lambda e: e.tensor_tensor(out=P_(9), in0=lim[:, i * 16:(i + 1) * 16], in1=lim[:, i * 16:(i + 1) * 16], op=ALU.mult), reads=[lim.b] + B, writes=B)
                        kb.op("dve", lambda e: e.tensor_tensor(out=P_(8), in0=P_(8), in1=P_(9), op=ALU.add), reads=B, writes=B)
                        kb.op("dve", lambda e: e.reciprocal(out=P_(8), in_=P_(8)), reads=B, writes=B)
                        kb.op("dve", lambda e: e.tensor_tensor(out=P_(10), in0=P_(6), in1=lre[:, i * 16:(i + 1) * 16], op=ALU.mult), reads=[lre.b] + B, writes=B)
                        kb.op("dve", lambda e: e.tensor_tensor(out=P_(9), in0=P_(7), in1=lim[:, i * 16:(i + 1) * 16], op=ALU.mult), reads=[lim.b] + B, writes=B)
                        kb.op("dve", lambda e: e.tensor_tensor(out=P_(10), in0=P_(10), in1=P_(9), op=ALU.add), reads=B, writes=B)
                        kb.op("dve", lambda e: e.tensor_tensor(out=P_(10), in0=P_(10), in1=P_(8), op=ALU.mult), reads=B, writes=B)
                        kb.op("dve", lambda e: e.tensor_tensor(out=P_(11), in0=P_(7), in1=lre[:, i * 16:(i + 1) * 16], op=ALU.mult), reads=[lre.b] + B, writes=B)
                        kb.op("dve", lambda e: e.tensor_tensor(out=P_(9), in0=P_(6), in1=lim[:, i * 16:(i + 1) * 16], op=ALU.mult), reads=[lim.b] + B, writes=B)
                        kb.op("dve", lambda e: e.tensor_tensor(out=P_(11), in0=P_(11), in1=P_(9), op=ALU.subtract), reads=B, writes=B)
                        kb.op("dve", lambda e: e.tensor_tensor(out=P_(11), in0=P_(11), in1=P_(8), op=ALU.mult), reads=B, writes=B)
                        for st in range(16):
                            kb.op("dve", lambda e, st=st: e.tensor_scalar(out=tw[:, 0, st * LC:(st + 1) * LC], in0=tau1[:], scalar1=sp_[:, 2, st:st + 1],
                                                                         scalar2=None, op0=ALU.mult), reads=[tau1.b] + B, writes=[tw.b])
                        sincos(16 * LC, tw[:, 0, :], tabs[:, 3, :], tabs[:, 2, :], tw[:, 1, :], twi[:], tw[:, 2, :], tw.b)
                        kb.op("dve", lambda e: e.tensor_copy(out=tw[:, 0, 0:1], in_=tw[:, 0, 0:1]), reads=[tw.b, tabs.b], writes=[tw.b, tabs.b])
                        for st in range(16):
                            sl = slice(st * LC, (st + 1) * LC)
                            kb.op("dve", lambda e, st=st, sl=sl: e.tensor_scalar(out=tw[:, 1, sl], in0=tabs[:, 3, sl], scalar1=sp_[:, 11, st:st + 1], scalar2=None, op0=ALU.mult), reads=[tabs.b] + B, writes=[tw.b])
                            kb.op("dve", lambda e, st=st, sl=sl: e.scalar_tensor_tensor(out=tabs[:, 0, sl], in0=tabs[:, 2, sl], scalar=sp_[:, 10, st:st + 1], in1=tw[:, 1, sl], op0=ALU.mult, op1=ALU.add), reads=[tw.b] + B, writes=[tabs.b])
                            kb.op("dve", lambda e, st=st, sl=sl: e.tensor_scalar(out=tw[:, 1, sl], in0=tabs[:, 3, sl], scalar1=sp_[:, 10, st:st + 1], scalar2=None, op0=ALU.mult), reads=[tabs.b] + B, writes=[tw.b])
                            kb.op("dve", lambda e, st=st, sl=sl: e.scalar_tensor_tensor(out=tabs[:, 1, sl], in0=tabs[:, 2, sl], scalar=sp_[:, 11, st:st + 1], in1=tw[:, 1, sl], op0=ALU.mult, op1=ALU.subtract), reads=[tw.b] + B, writes=[tabs.b])
                        kb.op("dve", lambda e: e.memset(car[:], 0.0), writes=[car.b])
                        NCH = T // LC
                        NCC = NCTX // LC
                        order = list(range(NCH)) if i == 0 else list(range(NCC - 1, -1, -1)) + list(range(NCH - 1, NCC - 1, -1))

                        def rv(ap):
                            return ap[:, ::-1] if i == 1 else ap
                        lastc = LC - 1 if i == 0 else 0
                        for n in order:
                            t0 = n * LC
                            for hf in range(2):
                                for q in range(8):
                                    st = hf * 8 + q
                                    pb = banks[q // 2]
                                    for ci in range(2):
                                        c0 = (q % 2) * 2 * LC + ci * LC
                                        kb.op("pe", lambda e, ci=ci, pb=pb, st=st, c0=c0: e.matmul(out=pb[:, c0:c0 + LC], lhsT=Bb[:, ci * 16 + st, :], rhs=uT[:, st // 4, t0:t0 + LC],
                                                                                              start=True, stop=True), reads=[Bb.b, uT.b], writes=[pb.b])
                                for q in range(8):
                                    st = hf * 8 + q
                                    pb = banks[q // 2]
                                    sl = slice(st * LC, (st + 1) * LC)
                                    u_ = S5U[q]
                                    ub_ = S5UB[q]
                                    bre = rv(pb[:, (q % 2) * 2 * LC:(q % 2) * 2 * LC + LC]); bim = rv(pb[:, (q % 2) * 2 * LC + LC:(q % 2) * 2 * LC + 2 * LC])
                                    kb.op("dve", lambda e, u_=u_, bre=bre, sl=sl: e.tensor_tensor(out=u_[:, 0, :], in0=bre, in1=tabs[:, 0, sl], op=ALU.mult), reads=[pb.b, tabs.b], writes=[ub_[0]])
                                    kb.op("dve", lambda e, u_=u_, bim=bim, sl=sl: e.tensor_tensor(out=u_[:, 1, :], in0=bim, in1=tabs[:, 1, sl], op=ALU.mult), reads=[pb.b, tabs.b], writes=[ub_[1]])
                                    kb.op("dve", lambda e, u_=u_, bre=bre, sl=sl: e.tensor_tensor(out=u_[:, 2, :], in0=bre, in1=tabs[:, 1, sl], op=ALU.mult), reads=[pb.b, tabs.b], writes=[ub_[2]])
                                    kb.op("dve", lambda e, u_=u_, bim=bim, sl=sl: e.tensor_tensor(out=u_[:, 3, :], in0=bim, in1=tabs[:, 0, sl], op=ALU.mult), reads=[pb.b, tabs.b], writes=[ub_[3]])
                                for q in range(8):
                                    u_ = S5U[q]
                                    ub_ = S5UB[q]
                                    kb.op("dve", lambda e, u_=u_: e.tensor_tensor(out=u_[:, 0, :], in0=u_[:, 0, :], in1=u_[:, 1, :], op=ALU.subtract), reads=[ub_[1]], writes=[ub_[0]])
                                    kb.op("dve", lambda e, u_=u_: e.tensor_tensor(out=u_[:, 2, :], in0=u_[:, 2, :], in1=u_[:, 3, :], op=ALU.add), reads=[ub_[3]], writes=[ub_[2]])
                                for q in range(8):
                                    st = hf * 8 + q
                                    u_ = S5U[q]
                                    ub_ = S5UB[q]
                                    rb = sp_[:, 3, st:st + 1].to_broadcast([128, LC])
                                    kb.op("dve", lambda e, u_=u_, rb=rb, st=st: e.tensor_tensor_scan(out=u_[:, 1, :], data0=rb, data1=u_[:, 0, :], initial=car[:, 0, st:st + 1], op0=ALU.mult, op1=ALU.add),
                                          reads=[ub_[0], sp_.b, car.b], writes=[ub_[1]])
                                    kb.op("dve", lambda e, u_=u_, rb=rb, st=st: e.tensor_tensor_scan(out=u_[:, 3, :], data0=rb, data1=u_[:, 2, :], initial=car[:, 1, st:st + 1], op0=ALU.mult, op1=ALU.add),
                                          reads=[ub_[2], sp_.b, car.b], writes=[ub_[3]])
                                for q in range(8):
                                    st = hf * 8 + q
                                    sl = slice(st * LC, (st + 1) * LC)
                                    u_ = S5U[q]
                                    ub_ = S5UB[q]
                                    kb.op("pool", lambda e, u_=u_, sl=sl: e.tensor_tensor(out=u_[:, 0, :], in0=u_[:, 1, :], in1=tabs[:, 2, sl], op=ALU.mult), reads=[ub_[1], tabs.b], writes=[ub_[0]])
                                    kb.op("pool", lambda e, u_=u_, sl=sl: e.tensor_tensor(out=u_[:, 2, :], in0=u_[:, 3, :], in1=tabs[:, 3, sl], op=ALU.mult), reads=[ub_[3], tabs.b], writes=[ub_[2]])
                                    kb.op("pool", lambda e, u_=u_: e.tensor_tensor(out=rv(u_[:, 4, :]), in0=u_[:, 0, :], in1=u_[:, 2, :], op=ALU.subtract), reads=[ub_[0], ub_[2]], writes=[ub_[4]])
                                    kb.op("pool", lambda e, u_=u_, sl=sl: e.tensor_tensor(out=u_[:, 0, :], in0=u_[:, 1, :], in1=tabs[:, 3, sl], op=ALU.mult), reads=[ub_[1], tabs.b], writes=[ub_[0]])
                                    kb.op("pool", lambda e, u_=u_, sl=sl: e.tensor_tensor(out=u_[:, 2, :], in0=u_[:, 3, :], in1=tabs[:, 2, sl], op=ALU.mult), reads=[ub_[3], tabs.b], writes=[ub_[2]])
                                    kb.op("pool", lambda e, u_=u_: e.tensor_tensor(out=rv(u_[:, 5, :]), in0=u_[:, 0, :], in1=u_[:, 2, :], op=ALU.add), reads=[ub_[0], ub_[2]], writes=[ub_[5]])
                                for q in range(8):
                                    st = hf * 8 + q
                                    u_ = S5U[q]
                                    ub_ = S5UB[q]
                                    kb.op("act", lambda e, u_=u_, st=st: e.activation(out=car[:, 0, st:st + 1], in_=u_[:, 4, lastc:lastc + 1], func=AF.Identity), reads=[ub_[4]], writes=[car.b])
                                    kb.op("act", lambda e, u_=u_, st=st: e.activation(out=car[:, 1, st:st + 1], in_=u_[:, 5, lastc:lastc + 1], func=AF.Identity), reads=[ub_[5]], writes=[car.b])
                                for f2 in range(2):
                                    fc = hf * 2 + f2
                                    py = banks[4 + fc]
                                    for q4 in range(4):
                                        q = f2 * 4 + q4
                                        st = hf * 8 + q
                                        u_ = S5U[q]
                                        ub_ = S5UB[q]
                                        kb.op("pe", lambda e, py=py, st=st, u_=u_, q4=q4: e.matmul(out=py[:, 0:LC], lhsT=Cb[:, st, :], rhs=u_[:, 4, :], start=(q4 == 0), stop=False),
                                              reads=[Cb.b, ub_[4]], writes=[py.b], sig=False)
                                        kb.op("pe", lambda e, py=py, st=st, u_=u_, q4=q4: e.matmul(out=py[:, 0:LC], lhsT=Cb[:, 16 + st, :], rhs=u_[:, 5, :], start=False, stop=(q4 == 3)),
                                              reads=[Cb.b, ub_[5]], writes=[py.b])
                                    if i == 0:
                                        kb.op("act", lambda e, py=py, fc=fc: e.activation(out=yT[:, fc, t0:t0 + LC], in_=py[:, 0:LC], func=AF.Identity), writes=[yT.b, py.b])
                                    else:
                                        kb.op("dve", lambda e, py=py, fc=fc: e.tensor_tensor(out=yT[:, fc, t0:t0 + LC], in0=py[:, 0:LC], in1=yT[:, fc, t0:t0 + LC], op=ALU.add),
                                              writes=[yT.b, py.b])
                    kb.barrier()
                with ExitStack() as ph2:
                    g1 = sb(ph2, "g1", [128, T]); g2 = sb(ph2, "g2", [128, T])
                    og = [sb(ph2, "og%d" % i, [128, 512], BF16) for i in range(2)]
                    sgl = [sb(ph2, "sgl%d" % i, [128, 512]) for i in range(2)]
                    for fc in range(4):
                        kb.op("dve", lambda e: e.scalar_tensor_tensor(out=yT[:, fc, :], in0=uT[:, fc, :], scalar=dTt[:, l * 4 + fc:l * 4 + fc + 1], in1=yT[:, fc, :],
                                                                      op0=ALU.mult, op1=ALU.add), reads=[uT.b, dTt.b, yT.b], writes=[yT.b])
                        kb.op("act", lambda e: e.activation(out=g1[:], in_=yT[:, fc, :], func=AF.Square), reads=[yT.b], writes=[g1.b])
                        kb.op("dve", lambda e: e.tensor_scalar(out=g1[:], in0=g1[:], scalar1=0.044715, scalar2=1.0, op0=ALU.mult, op1=ALU.add), reads=[g1.b], writes=[g1.b])
                        kb.op("dve", lambda e: e.tensor_tensor(out=g1[:], in0=g1[:], in1=yT[:, fc, :], op=ALU.mult), reads=[g1.b, yT.b], writes=[g1.b])
                        kb.op("act", lambda e: e.activation(out=g2[:], in_=g1[:], func=AF.Sigmoid, scale=1.5957691216057308), reads=[g1.b], writes=[g2.b])
                        kb.op("dve", lambda e: e.tensor_tensor(out=yT[:, fc, :], in0=yT[:, fc, :], in1=g2[:], op=ALU.mult), reads=[g2.b, yT.b], writes=[yT.b])
                    ne = 0
                    for fo in range(4):
                        for tb in range(5):
                            t0 = tb * 512
                            nt = min(512, T - t0)
                            pg = banks[4 + ne % 2]
                            for fi in range(4):
                                kb.op("pe", lambda e, fi=fi, pg=pg: e.matmul(out=pg[:, :nt], lhsT=gluw[:, fi, fo * 128:(fo + 1) * 128], rhs=yT[:, fi, t0:t0 + nt],
                                                                             start=(fi == 0), stop=(fi == 3)), reads=[gluw.b, yT.b], writes=[pg.b], sig=(fi == 3))
                            s_ = sgl[ne % 2]; o_ = og[ne % 2]
                            kb.op("act", lambda e, pg=pg, s_=s_: e.activation(out=s_[:, :nt], in_=pg[:, :nt], func=AF.Sigmoid, bias=gbT[:, l * 4 + fo:l * 4 + fo + 1]),
                                  reads=[pg.b, gbT.b], writes=[s_.b])
                            kb.op("dve", lambda e, s_=s_, o_=o_: e.tensor_tensor(out=o_[:, :nt], in0=s_[:, :nt], in1=yT[:, fo, t0:t0 + nt], op=ALU.mult),
                                  reads=[s_.b, yT.b], writes=[o_.b])
                            kb.dma("sp", mixT[fo * 128:(fo + 1) * 128, t0:t0 + nt], o_[:, :nt], reads=[o_.b])
                            ne += 1
                kb.barrier()
            if stage <= 2:
                break
            with ExitStack() as ph:
                QOFF, KOFF, VOFF, ZOFF, AOFF, BOFF = 512, 1280, 2048, 2816, 3584, 3596
                NCK = T // 64
                RM = sb(ph, "RM", [64, T])
                SEL = sb(ph, "SEL", [64, 12, 128]); NSEL = sb(ph, "NSEL", [64, 12, 128])
                MB = sb(ph, "MB", [64, 4, 64])
                ONES = sb(ph, "ONES", [128, 128])
                NORM = sb(ph, "NORM", [64, 128])
                GP = sb(ph, "GP", [64, 2])
                CW = sb(ph, "CW", [128, 18 * 3])
                kb.dma("sp", RM[:], rm_d, writes=[RM.b])
                kb.dma("sp", SEL[:], sel_d, writes=[SEL.b])
                kb.dma("sp", MB[:], mb_d, writes=[MB.b])
                kb.dma("sp", NORM[:], gdn_norm_d[:, l * 128:(l + 1) * 128], writes=[NORM.b])
                kb.dma("sp", GP[:], gdn_gp_d[:, l * 2:(l + 1) * 2], writes=[GP.b])
                kb.dma("sp", CW[:], gdn_cw_d[:, l * 54:(l + 1) * 54], writes=[CW.b])
                kb.op("act", lambda e: e.activation(out=NSEL[:], in_=SEL[:], func=AF.Identity, scale=-1.0), reads=[SEL.b], writes=[NSEL.b])
                kb.op("dve", lambda e: e.memset(ONES[:], 1.0), writes=[ONES.b])
                RA = sb(ph, "RA", [64, T]); RB = sb(ph, "RB", [64, T]); GC = sb(ph, "GC", [64, T])
                R1 = sb(ph, "R1", [64, T]); EG = sb(ph, "EG", [64, T]); BG = sb(ph, "BG", [64, T])
                EGL = sb(ph, "EGL", [64, NCK])
                COL = sb(ph, "COL", [64, NCK, 3, 12])
                EGLB = sb(ph, "EGLB", [128, 12, NCK])
                kb.op("dve", lambda e: e.memset(RA[:], 0.0), writes=[RA.b])
                kb.op("dve", lambda e: e.memset(RB[:], 0.0), writes=[RB.b])
                for dr in range(2):
                    kb.dma("sp", RA[dr * 32:dr * 32 + 6, :], projT[AOFF + dr * 6:AOFF + dr * 6 + 6, :], writes=[RA.b])
                    kb.dma("sp", RB[dr * 32:dr * 32 + 6, :], projT[BOFF + dr * 6:BOFF + dr * 6 + 6, :], writes=[RB.b])
                kb.op("act", lambda e: e.activation(out=RA[:], in_=RA[:], func=AF.Exp, bias=GP[:, 1:2]), reads=[RA.b, GP.b], writes=[RA.b])
                kb.op("act", lambda e: e.activation(out=RA[:], in_=RA[:], func=AF.Ln, bias=1.0), reads=[RA.b], writes=[RA.b])
                kb.op("act", lambda e: e.activation(out=GP[:, 0:1], in_=GP[:, 0:1], func=AF.Exp), reads=[GP.b], writes=[GP.b])
                kb.op("dve", lambda e: e.tensor_scalar(out=RA[:], in0=RA[:], scalar1=GP[:, 0:1], scalar2=-1.0, op0=ALU.mult, op1=ALU.mult), reads=[RA.b, GP.b], writes=[RA.b])
                kb.op("act", lambda e: e.activation(out=RB[:], in_=RB[:], func=AF.Sigmoid), reads=[RB.b], writes=[RB.b])
                kb.op("act", lambda e: e.activation(out=R1[:], in_=RB[:], func=AF.Ln), reads=[RB.b], writes=[R1.b])
                kb.op("dve", lambda e: e.tensor_tensor_scan(out=GC[0:32, :], data0=RM[0:32, :], data1=RA[0:32, :], initial=0.0, op0=ALU.mult, op1=ALU.add),
                      reads=[RM.b, RA.b], writes=[GC.b])
                kb.op("dve", lambda e: e.tensor_tensor_scan(out=GC[32:64, ::-1], data0=RM[32:64, ::-1], data1=RA[32:64, ::-1], initial=0.0, op0=ALU.mult, op1=ALU.add),
                      reads=[RM.b, RA.b], writes=[GC.b])
                kb.op("dve", lambda e: e.tensor_tensor(out=R1[:], in0=R1[:], in1=GC[:], op=ALU.add), reads=[R1.b, GC.b], writes=[R1.b])
                kb.op("act", lambda e: e.activation(out=EG[:], in_=GC[:], func=AF.Exp), reads=[GC.b], writes=[EG.b])
                kb.op("dve", lambda e: e.tensor_tensor(out=BG[:], in0=EG[:], in1=RB[:], op=ALU.mult), reads=[EG.b, RB.b], writes=[BG.b])
                GCL = sb(ph, "GCL", [64, NCK])
                for dr in range(2):
                    pr = slice(dr * 32, dr * 32 + 32)
                    lastpos = 63 if dr == 0 else 0
                    gv = GC[pr, :].rearrange("p (c s) -> p c s", s=64)
                    kb.op("dve", lambda e, pr=pr, gv=gv, lastpos=lastpos: e.tensor_copy(out=GCL[pr, :], in_=gv[:, :, lastpos]), reads=[GC.b], writes=[GCL.b])
                kb.op("act", lambda e: e.activation(out=EGL[:], in_=GCL[:], func=AF.Exp), reads=[GCL.b], writes=[EGL.b])
                for c in range(NCK):
                    kb.op("dve", lambda e, c=c: e.tensor_scalar(out=RA[:, c * 64:(c + 1) * 64], in0=GC[:, c * 64:(c + 1) * 64], scalar1=-1.0, scalar2=GCL[:, c:c + 1],
                                                                 op0=ALU.mult, op1=ALU.add), reads=[GC.b, GCL.b], writes=[RA.b])
                kb.op("act", lambda e: e.activation(out=RA[:], in_=RA[:], func=AF.Exp), reads=[RA.b], writes=[RA.b])
                for c in range(NCK):
                    pcol = banks[c % 4]
                    for qi, row in enumerate((BG, RA, RB)):
                        kb.op("pe", lambda e, row=row, qi=qi, pcol=pcol, c=c: e.matmul(out=pcol[0:64, qi * 12:(qi + 1) * 12], lhsT=row[:, c * 64:(c + 1) * 64],
                                                                                   rhs=SEL[:, :, 0], start=True, stop=True), reads=[row.b, SEL.b], writes=[pcol.b])
                    kb.op("act", lambda e, pcol=pcol, c=c: e.activation(out=COL[:, c, :, :].rearrange("p a b -> p (a b)"), in_=pcol[0:64, 0:36], func=AF.Identity),
                          reads=[pcol.b], writes=[COL.b])
                for r in range(12):
                    pcol = banks[4 + r % 2]
                    kb.op("pe", lambda e, r=r, pcol=pcol: e.matmul(out=pcol[:, 0:NCK], lhsT=SEL[:, r, :], rhs=EGL[:], start=True, stop=True), reads=[SEL.b, EGL.b], writes=[pcol.b])
                    kb.op("act", lambda e, r=r, pcol=pcol: e.activation(out=EGLB[:, r, :], in_=pcol[:, 0:NCK], func=AF.Identity), reads=[pcol.b], writes=[EGLB.b])
                kb.barrier()
                DBG = 9
                if stage == 3:
                    for qi_, row_ in enumerate((GC, R1, EG, RA, RB, BG)):
                        kb.dma("sp", dbg_rows[:, qi_, :], row_[:], reads=[row_.b])
                psn = [0]

                def pget():
                    bk = banks[psn[0] % 8]
                    psn[0] += 1
                    return bk
                RAW = sb(ph, "RAW", [128, 3, T]); CV = sb(ph, "CV", [128, 3, T])
                OACC = sb(ph, "OACC", [64, NCK, 128]); OUT = sb(ph, "OUT", [128, T], BF16)
                Sd = [sb(ph, "Sst%d" % i, [128, 128]) for i in range(2)]
                G = 4
                U = []
                for u in range(2 * G):
                    d_ = {}
                    for nm, shp in (("AB0", [64, 2, 64]), ("AB1", [64, 2, 64]), ("X0", [64, 64]), ("X1", [64, 64]),
                                    ("E", [64, 3, 64]), ("kbg", [64, 128]), ("kdec", [64, 128]), ("vb", [64, 128]),
                                    ("nWT", [128, 64]), ("VN", [64, 128])):
                        d_[nm] = sb(ph, "u%d%s" % (u, nm), shp, BF16 if nm in ("AB0", "AB1", "X0", "X1", "kbg", "vb") else F32)
                    U.append(d_)
                fin = [sb(ph, "fin%d" % i, [64, 128]) for i in range(2)]
                fst = [sb(ph, "fst%d" % i, [64, 4]) for i in range(2)]
                I64 = ident[0:64, 0:64]
                I64b_t = sb(ph, "I64b", [64, 64], BF16)
                kb.op("dve", lambda e: e.tensor_copy(out=I64b_t[:], in_=ident[0:64, 0:64]), reads=[ident.b], writes=[I64b_t.b])
                I64b = I64b_t[:]
                for hd in range(6 if DBG >= 1 else 0):
                    for ci, off in enumerate((QOFF, KOFF, VOFF)):
                        kb.dma("sp", RAW[:, ci, :], projT[off + hd * 128:off + (hd + 1) * 128, :], writes=[RAW.b])
                    for ci in range(3):
                        cch = ci * 6 + hd
                        for (s0, s1) in ((0, NCTX), (NCTX, T)):
                            kb.op("dve", lambda e, ci=ci, cch=cch, s0=s0, s1=s1: e.tensor_scalar(out=CV[:, ci, s0:s1], in0=RAW[:, ci, s0:s1], scalar1=CW[:, cch * 3 + 1:cch * 3 + 2],
                                                                                              scalar2=None, op0=ALU.mult), reads=[RAW.b, CW.b], writes=[CV.b])
                            kb.op("dve", lambda e, ci=ci, cch=cch, s0=s0, s1=s1: e.scalar_tensor_tensor(out=CV[:, ci, s0 + 1:s1], in0=RAW[:, ci, s0:s1 - 1], scalar=CW[:, cch * 3:cch * 3 + 1],
                                                                                                     in1=CV[:, ci, s0 + 1:s1], op0=ALU.mult, op1=ALU.add), reads=[RAW.b, CW.b, CV.b], writes=[CV.b])
                            kb.op("dve", lambda e, ci=ci, cch=cch, s0=s0, s1=s1: e.scalar_tensor_tensor(out=CV[:, ci, s0:s1 - 1], in0=RAW[:, ci, s0 + 1:s1], scalar=CW[:, cch * 3 + 2:cch * 3 + 3],
                                                                                                     in1=CV[:, ci, s0:s1 - 1], op0=ALU.mult, op1=ALU.add), reads=[RAW.b, CW.b, CV.b], writes=[CV.b])
                    kb.op("act", lambda e: e.activation(out=CV[:], in_=CV[:], func=AF.Silu), reads=[CV.b], writes=[CV.b])
                    for ci in range(2):
                        kb.op("act", lambda e, ci=ci: e.activation(out=RAW[:, 0, :], in_=CV[:, ci, :], func=AF.Square), reads=[CV.b], writes=[RAW.b])
                        for tb in range(5):
                            t0 = tb * 512
                            nt = min(512, T - t0)
                            pb = banks[6 + tb % 2]
                            kb.op("pe", lambda e, pb=pb, t0=t0, nt=nt: e.matmul(out=pb[:, :nt], lhsT=ONES[:], rhs=RAW[:, 0, t0:t0 + nt], start=True, stop=True),
                                  reads=[ONES.b, RAW.b], writes=[pb.b])
                            kb.op("act", lambda e, pb=pb, t0=t0, nt=nt: e.activation(out=RAW[:, 1, t0:t0 + nt], in_=pb[:, :nt], func=AF.Sqrt, bias=EPS), reads=[RAW.b], writes=[RAW.b, pb.b])
                        kb.op("dve", lambda e: e.reciprocal(out=RAW[:, 1, :], in_=RAW[:, 1, :]), reads=[RAW.b], writes=[RAW.b])
                        sc_ = (128.0 ** -0.5) if ci == 0 else 1.0
                        kb.op("dve", lambda e, ci=ci, sc_=sc_: e.scalar_tensor_tensor(out=CV[:, ci, :], in0=RAW[:, 1, :], scalar=sc_, in1=CV[:, ci, :], op0=ALU.mult, op1=ALU.mult),
                              reads=[RAW.b, CV.b], writes=[CV.b])
                    QT = lambda a, b_: CV[:, 0, a:b_]
                    KT = lambda a, b_: CV[:, 1, a:b_]
                    VT = lambda a, b_: CV[:, 2, a:b_]
                    kb.op("dve", lambda e: e.memset(OACC[:], 0.0), writes=[OACC.b])
                    for dr in range(2):
                        r = dr * 6 + hd
                        for tb in range(5):
                            t0 = tb * 512
                            nt = min(512, T - t0)
                            pb = banks[6 + tb % 2]
                            kb.op("pe", lambda e, pb=pb, t0=t0, nt=nt, r=r: e.matmul(out=pb[:, :nt], lhsT=SEL[:, r, :], rhs=EG[:, t0:t0 + nt], start=True, stop=True),
                                  reads=[SEL.b, EG.b], writes=[pb.b])
                            kb.op("dve", lambda e, pb=pb, t0=t0, nt=nt, dr=dr: e.tensor_tensor(out=RAW[:, dr, t0:t0 + nt], in0=pb[:, :nt], in1=CV[:, 0, t0:t0 + nt], op=ALU.mult),
                                  reads=[CV.b], writes=[RAW.b, pb.b])
                        kb.op("dve", lambda e, dr=dr: e.memset(Sd[dr][:], 0.0), writes=[Sd[dr].b])
                    orders = [list(range(NCK)), list(range(3, -1, -1)) + list(range(NCK - 1, 3, -1))]
                    masks = [(0, 1, 3), (1, 0, 2)]
                    for g0 in range(0, NCK, G):
                        units = []
                        for dr in range(2):
                            for u, c in enumerate(orders[dr][g0:g0 + G]):
                                units.append((dr, c, U[dr * G + u]))
                        for (dr, c, d_) in units:
                            r = dr * 6 + hd
                            t0 = c * 64
                            bk = pget()
                            kb.op("pe", lambda e, bk=bk, t0=t0: e.transpose(out=bk[0:64, 0:128], in_=KT(t0, t0 + 64), identity=ident[:]), reads=[CV.b, ident.b], writes=[bk.b])
                            kb.op("pe", lambda e, bk=bk, t0=t0: e.transpose(out=bk[0:64, 128:256], in_=VT(t0, t0 + 64), identity=ident[:]), reads=[CV.b, ident.b], writes=[bk.b])
                            kb.op("act", lambda e, bk=bk, d_=d_, c=c, r=r: e.activation(out=d_["kbg"][:], in_=bk[0:64, 0:128], func=AF.Identity, scale=COL[:, c, 0, r:r + 1]), reads=[COL.b], writes=[d_["kbg"].b, bk.b])
                            kb.op("act", lambda e, bk=bk, d_=d_, c=c, r=r: e.activation(out=d_["kdec"][:], in_=bk[0:64, 0:128], func=AF.Identity, scale=COL[:, c, 1, r:r + 1]), reads=[COL.b], writes=[d_["kdec"].b, bk.b])
                            kb.op("act", lambda e, bk=bk, d_=d_, c=c, r=r: e.activation(out=d_["vb"][:], in_=bk[0:64, 128:256], func=AF.Identity, scale=COL[:, c, 2, r:r + 1]), reads=[COL.b], writes=[d_["vb"].b, bk.b])
                        for (dr, c, d_) in units:
                            r = dr * 6 + hd
                            m_s, m_st, m_it = masks[dr]
                            t0 = c * 64
                            ch = slice(t0, t0 + 64)
                            bp = pget()
                            kb.op("pe", lambda e, bp=bp, t0=t0: e.matmul(out=bp[0:64, 0:64], lhsT=KT(t0, t0 + 64), rhs=KT(t0, t0 + 64), start=True, stop=True), reads=[CV.b], writes=[bp.b])
                            kb.op("pe", lambda e, bp=bp, t0=t0: e.matmul(out=bp[0:64, 64:128], lhsT=KT(t0, t0 + 64), rhs=QT(t0, t0 + 64), start=True, stop=True), reads=[CV.b], writes=[bp.b])
                            be = pget()
                            kb.op("pe", lambda e, be=be, ch=ch, r=r: e.matmul(out=be[0:64, 0:64], lhsT=R1[:, ch], rhs=SEL[:, r, 0:64], start=True, stop=False), reads=[R1.b, SEL.b], writes=[be.b], sig=False)
                            kb.op("pe", lambda e, be=be, ch=ch, r=r: e.matmul(out=be[0:64, 0:64], lhsT=NSEL[:, r, 0:64], rhs=GC[:, ch], start=False, stop=False), reads=[GC.b, NSEL.b], writes=[be.b], sig=False)
                            kb.op("pe", lambda e, be=be, m_s=m_s: e.matmul(out=be[0:64, 0:64], lhsT=I64, rhs=MB[:, m_s, :], start=False, stop=True), reads=[MB.b, ident.b], writes=[be.b])
                            kb.op("pe", lambda e, be=be, ch=ch, r=r: e.matmul(out=be[0:64, 64:128], lhsT=GC[:, ch], rhs=NSEL[:, r, 0:64], start=True, stop=False), reads=[GC.b, NSEL.b], writes=[be.b], sig=False)
                            kb.op("pe", lambda e, be=be, ch=ch, r=r: e.matmul(out=be[0:64, 64:128], lhsT=SEL[:, r, 0:64], rhs=R1[:, ch], start=False, stop=False), reads=[R1.b, SEL.b], writes=[be.b], sig=False)
                            kb.op("pe", lambda e, be=be, m_st=m_st: e.matmul(out=be[0:64, 64:128], lhsT=I64, rhs=MB[:, m_st, :], start=False, stop=True), reads=[MB.b, ident.b], writes=[be.b])
                            kb.op("pe", lambda e, be=be, ch=ch, r=r: e.matmul(out=be[0:64, 128:192], lhsT=GC[:, ch], rhs=NSEL[:, r, 0:64], start=True, stop=False), reads=[GC.b, NSEL.b], writes=[be.b], sig=False)
                            kb.op("pe", lambda e, be=be, ch=ch, r=r: e.matmul(out=be[0:64, 128:192], lhsT=SEL[:, r, 0:64], rhs=GC[:, ch], start=False, stop=False), reads=[GC.b, SEL.b], writes=[be.b], sig=False)
                            kb.op("pe", lambda e, be=be, m_it=m_it: e.matmul(out=be[0:64, 128:192], lhsT=I64, rhs=MB[:, m_it, :], start=False, stop=True), reads=[MB.b, ident.b], writes=[be.b])
                            kb.op("act", lambda e, be=be, d_=d_: e.activation(out=d_["E"][:].rearrange("p a b -> p (a b)"), in_=be[0:64, 0:192], func=AF.Exp), writes=[d_["E"].b, be.b])
                            kb.op("dve", lambda e, bp=bp, d_=d_: e.tensor_tensor(out=d_["AB0"][:, 1, :], in0=bp[0:64, 0:64], in1=d_["E"][:, 0, :], op=ALU.mult), reads=[d_["E"].b], writes=[d_["AB0"].b, bp.b])
                            kb.op("dve", lambda e, bp=bp, d_=d_: e.tensor_tensor(out=d_["AB0"][:, 0, :], in0=bp[0:64, 0:64], in1=d_["E"][:, 1, :], op=ALU.mult), reads=[d_["E"].b], writes=[d_["AB0"].b, bp.b])
                            kb.op("dve", lambda e, bp=bp, d_=d_: e.tensor_tensor(out=d_["E"][:, 2, :], in0=bp[0:64, 64:128], in1=d_["E"][:, 2, :], op=ALU.mult), reads=[d_["E"].b], writes=[d_["E"].b, bp.b])
                            kb.op("dve", lambda e, d_=d_: e.tensor_tensor(out=d_["X0"][:], in0=I64, in1=d_["AB0"][:, 0, :], op=ALU.subtract), reads=[ident.b, d_["AB0"].b], writes=[d_["X0"].b])
                        for k in range(1, 6):
                            pa, pn = (k - 1) % 2, k % 2
                            for ui, (dr, c, d_) in enumerate(units):
                                ABp, ABn = d_["AB%d" % pa], d_["AB%d" % pn]
                                bk = pget()
                                if k < 5:
                                    kb.op("pe", lambda e, bk=bk, ABp=ABp: e.matmul(out=bk[0:64, 0:64], lhsT=ABp[:, 1, :], rhs=ABp[:, 0, :], start=True, stop=True), reads=[ABp.b], writes=[bk.b])
                                kb.op("pe", lambda e, bk=bk, ABp=ABp: e.matmul(out=bk[0:64, 64:128], lhsT=ABp[:, 0, :], rhs=ABp[:, 1, :], start=True, stop=True), reads=[ABp.b], writes=[bk.b])
                                lo = 0 if k < 5 else 64
                                if (ui + k) % 2 == 0:
                                    kb.op("act", lambda e, bk=bk, ABn=ABn, lo=lo: e.activation(out=ABn[:].rearrange("p a b -> p (a b)")[:, lo:128], in_=bk[0:64, lo:128], func=AF.Identity), writes=[ABn.b, bk.b])
                                else:
                                    kb.op("dve", lambda e, bk=bk, ABn=ABn, lo=lo: e.tensor_copy(out=ABn[:].rearrange("p a b -> p (a b)")[:, lo:128], in_=bk[0:64, lo:128]), writes=[ABn.b, bk.b])
                            for ui, (dr, c, d_) in enumerate(units):
                                ABn, Xp, Xn = d_["AB%d" % pn], d_["X%d" % pa], d_["X%d" % pn]
                                bk = pget()
                                kb.op("pe", lambda e, bk=bk, Xp=Xp: e.matmul(out=bk[0:64, 0:64], lhsT=I64b, rhs=Xp[:], start=True, stop=False), reads=[Xp.b, I64b_t.b], writes=[bk.b], sig=False)
                                kb.op("pe", lambda e, bk=bk, Xp=Xp, ABn=ABn: e.matmul(out=bk[0:64, 0:64], lhsT=ABn[:, 1, :], rhs=Xp[:], start=False, stop=True), reads=[Xp.b, ABn.b], writes=[bk.b])
                                if (ui + k) % 2 == 1:
                                    kb.op("act", lambda e, bk=bk, Xn=Xn: e.activation(out=Xn[:], in_=bk[0:64, 0:64], func=AF.Identity), writes=[Xn.b, bk.b])
                                else:
                                    kb.op("dve", lambda e, bk=bk, Xn=Xn: e.tensor_copy(out=Xn[:], in_=bk[0:64, 0:64]), writes=[Xn.b, bk.b])
                        for (dr, c, d_) in units:
                            TT = d_["X1"]
                            bk = pget()
                            kb.op("pe", lambda e, bk=bk, d_=d_, TT=TT: e.matmul(out=bk[:, 0:64], lhsT=d_["kbg"][:], rhs=TT[:], start=True, stop=True), reads=[d_["kbg"].b, TT.b], writes=[bk.b])
                            kb.op("act", lambda e, bk=bk, d_=d_: e.activation(out=d_["nWT"][:], in_=bk[:, 0:64], func=AF.Identity, scale=-1.0), writes=[d_["nWT"].b, bk.b])
                        for u in range(G):
                            for dr in range(2):
                                if dr * G + u >= len(units):
                                    continue
                                dr_, c, d_ = units[dr * G + u]
                                r = dr * 6 + hd
                                S = Sd[dr]
                                t0 = c * 64
                                TT = d_["X1"]
                                bk = pget()
                                kb.op("pe", lambda e, bk=bk, d_=d_, TT=TT: e.matmul(out=bk[0:64, 0:128], lhsT=TT[:], rhs=d_["vb"][:], start=True, stop=False), reads=[d_["vb"].b, TT.b], writes=[bk.b], sig=False)
                                kb.op("pe", lambda e, bk=bk, d_=d_, S=S: e.matmul(out=bk[0:64, 0:128], lhsT=d_["nWT"][:], rhs=S[:], start=False, stop=True), reads=[d_["nWT"].b, S.b], writes=[bk.b])
                                kb.op("act", lambda e, bk=bk, d_=d_: e.activation(out=d_["VN"][:], in_=bk[0:64, 0:128], func=AF.Identity), writes=[d_["VN"].b, bk.b])
                                bk = pget()
                                kb.op("pe", lambda e, bk=bk, t0=t0, dr=dr, S=S: e.matmul(out=bk[0:64, 0:128], lhsT=RAW[:, dr, t0:t0 + 64], rhs=S[:], start=True, stop=False), reads=[RAW.b, S.b], writes=[bk.b], sig=False)
                                kb.op("pe", lambda e, bk=bk, d_=d_: e.matmul(out=bk[0:64, 0:128], lhsT=d_["E"][:, 2, :], rhs=d_["VN"][:], start=False, stop=True), reads=[d_["E"].b, d_["VN"].b], writes=[bk.b])
                                kb.op("dve", lambda e, bk=bk, c=c: e.tensor_tensor(out=OACC[:, c, :], in0=bk[0:64, 0:128], in1=OACC[:, c, :], op=ALU.add), writes=[OACC.b, bk.b])
                                bk = pget()
                                kb.op("pe", lambda e, bk=bk, d_=d_: e.matmul(out=bk[:, 0:128], lhsT=d_["kdec"][:], rhs=d_["VN"][:], start=True, stop=True), reads=[d_["kdec"].b, d_["VN"].b], writes=[bk.b])
                                kb.op("dve", lambda e, bk=bk, c=c, r=r, S=S: e.scalar_tensor_tensor(out=S[:], in0=S[:], scalar=EGLB[:, r, c:c + 1], in1=bk[:, 0:128], op0=ALU.mult, op1=ALU.add),
                                      reads=[EGLB.b], writes=[S.b, bk.b])
                    kb.dma("sp", RAW[:, 2, :], projT[ZOFF + hd * 128:ZOFF + (hd + 1) * 128, :], reads=[CV.b], writes=[RAW.b])
                    kb.op("act", lambda e: e.activation(out=RAW[:, 2, :], in_=RAW[:, 2, :], func=AF.Silu), reads=[RAW.b], writes=[RAW.b])
                    for c in range(NCK):
                        f_ = fin[c % 2]; s_ = fst[c % 2]
                        kb.op("act", lambda e, f_=f_, s_=s_, c=c: e.activation(out=f_[:], in_=OACC[:, c, :], func=AF.Square, accum_out=s_[:, 0:1]), reads=[OACC.b], writes=[f_.b, s_.b])
                        kb.op("act", lambda e, s_=s_: e.activation(out=s_[:, 1:2], in_=s_[:, 0:1], func=AF.Sqrt, bias=EPS, scale=1.0 / 128), reads=[s_.b], writes=[s_.b])
                        kb.op("dve", lambda e, s_=s_: e.reciprocal(out=s_[:, 2:3], in_=s_[:, 1:2]), reads=[s_.b], writes=[s_.b])
                        kb.op("dve", lambda e, f_=f_, s_=s_, c=c: e.scalar_tensor_tensor(out=f_[:], in0=OACC[:, c, :], scalar=s_[:, 2:3], in1=NORM[:], op0=ALU.mult, op1=ALU.mult),
                              reads=[OACC.b, s_.b, NORM.b], writes=[f_.b])
                        bk = pget()
                        kb.op("pe", lambda e, bk=bk, f_=f_: e.transpose(out=bk[:, 0:64], in_=f_[:], identity=I64), reads=[f_.b, ident.b], writes=[bk.b])
                        kb.op("dve", lambda e, bk=bk, c=c: e.tensor_tensor(out=OUT[:, c * 64:(c + 1) * 64], in0=bk[:, 0:64], in1=RAW[:, 2, c * 64:(c + 1) * 64], op=ALU.mult),
                              reads=[RAW.b], writes=[OUT.b, bk.b])
                    kb.dma("sp", mixT[512 + hd * 128:512 + (hd + 1) * 128, :], OUT[:], reads=[OUT.b])
                kb.barrier()
            if stage <= 3:
                break
            with ExitStack() as ph:
                MQ, MK, MV, MO, MI, MF = 3608, 3992, 4376, 5144, 5912, 5924
                NCK = T // 64
                SEL = sb(ph, "mSEL", [64, 12, 128]); NSEL = sb(ph, "mNSEL", [64, 12, 128])
                MB = sb(ph, "mMB", [64, 4, 64])
                NORM = sb(ph, "mNORM", [64, 128])
                MP = sb(ph, "mMP", [64, 2])
                kb.dma("sp", SEL[:], sel_d, writes=[SEL.b])
                kb.dma("sp", MB[:], mb_d, writes=[MB.b])
                kb.dma("sp", NORM[:], ml_norm_d[:, l * 128:(l + 1) * 128], writes=[NORM.b])
                kb.dma("sp", MP[:], ml_mp_d[:, l * 2:(l + 1) * 2], writes=[MP.b])
                kb.op("act", lambda e: e.activation(out=NSEL[:], in_=SEL[:], func=AF.Identity, scale=-1.0), reads=[SEL.b], writes=[NSEL.b])
                RI = sb(ph, "mRI", [64, T]); CM = sb(ph, "mCM", [64, T])
                COL = sb(ph, "mCOL", [64, NCK, 4, 12])
                AOB = sb(ph, "mAOB", [128, 2, 12, NCK])
                I64 = ident[0:64, 0:64]

                def seqperm(eng, dst, dstb, src, srcb, npart, p0=0):
                    kb.op(eng, lambda e: (e.tensor_copy(out=dst[p0:p0 + npart, 0:NCTX], in_=src[p0:p0 + npart, 0:NCTX]) if eng != "act" else
                                          e.activation(out=dst[p0:p0 + npart, 0:NCTX], in_=src[p0:p0 + npart, 0:NCTX], func=AF.Identity)), reads=[srcb], writes=[dstb])
                    ov = dst[p0:p0 + npart, NCTX:T].rearrange("p (c r) -> p c r", r=32)
                    iv = src[p0:p0 + npart, NCTX:T].rearrange("p (r c) -> p c r", c=64)
                    kb.op(eng, lambda e: (e.tensor_copy(out=ov, in_=iv) if eng != "act" else e.activation(out=ov, in_=iv, func=AF.Identity)), reads=[srcb], writes=[dstb])

                with ExitStack() as ph2:
                    RM = sb(ph2, "mRM", [64, T]); RMA = sb(ph2, "mRMA", [64, T])
                    RF = sb(ph2, "mRF", [64, T]); BC = sb(ph2, "mBC", [64, T]); X1 = sb(ph2, "mX1", [64, T]); X2 = sb(ph2, "mX2", [64, T])
                    CH = sb(ph2, "mCH", [64, 8, NCK])
                    kb.dma("sp", RM[:], rm_d, writes=[RM.b])
                    kb.dma("sp", RMA[:], rma_d, writes=[RMA.b])
                    kb.op("dve", lambda e: e.memset(X1[:], 0.0), writes=[X1.b])
                    kb.op("dve", lambda e: e.memset(X2[:], 0.0), writes=[X2.b])
                    for dr in range(2):
                        kb.dma("sp", X1[dr * 32:dr * 32 + 6, :], projT[MI + dr * 6:MI + dr * 6 + 6, :], writes=[X1.b])
                        kb.dma("sp", X2[dr * 32:dr * 32 + 6, :], projT[MF + dr * 6:MF + dr * 6 + 6, :], writes=[X2.b])
                    seqperm("dve", RI, RI.b, X1, X1.b, 64)
                    seqperm("dve", RF, RF.b, X2, X2.b, 64)
                    kb.op("act", lambda e: e.activation(out=RI[:], in_=RI[:], func=AF.Identity, bias=MP[:, 0:1]), reads=[RI.b, MP.b], writes=[RI.b])
                    kb.op("dve", lambda e: e.tensor_scalar(out=MP[:, 1:2], in0=MP[:, 1:2], scalar1=-1.0, scalar2=None, op0=ALU.mult), reads=[MP.b], writes=[MP.b])
                    kb.op("act", lambda e: e.activation(out=RF[:], in_=RF[:], func=AF.Exp, bias=MP[:, 1:2], scale=-1.0), reads=[RF.b, MP.b], writes=[RF.b])
                    kb.op("act", lambda e: e.activation(out=RF[:], in_=RF[:], func=AF.Ln, bias=1.0), reads=[RF.b], writes=[RF.b])
                    kb.op("dve", lambda e: e.tensor_scalar(out=RF[:], in0=RF[:], scalar1=-1.0, scalar2=None, op0=ALU.mult), reads=[RF.b], writes=[RF.b])
                    kb.op("dve", lambda e: e.tensor_tensor_scan(out=BC[0:32, :], data0=RM[0:32, :], data1=RF[0:32, :], initial=0.0, op0=ALU.mult, op1=ALU.add), reads=[RM.b, RF.b], writes=[BC.b])
                    kb.op("dve", lambda e: e.tensor_tensor_scan(out=BC[32:64, ::-1], data0=RM[32:64, ::-1], data1=RF[32:64, ::-1], initial=0.0, op0=ALU.mult, op1=ALU.add), reads=[RM.b, RF.b], writes=[BC.b])
                    kb.op("dve", lambda e: e.tensor_tensor(out=RI[:], in0=RI[:], in1=BC[:], op=ALU.subtract), reads=[RI.b, BC.b], writes=[RI.b])
                    kb.op("dve", lambda e: e.tensor_tensor_scan(out=CM[0:32, :], data0=RMA[0:32, :], data1=RI[0:32, :], initial=-1e30, op0=ALU.add, op1=ALU.max), reads=[RMA.b, RI.b], writes=[CM.b])
                    kb.op("dve", lambda e: e.tensor_tensor_scan(out=CM[32:64, ::-1], data0=RMA[32:64, ::-1], data1=RI[32:64, ::-1], initial=-1e30, op0=ALU.add, op1=ALU.max), reads=[RMA.b, RI.b], writes=[CM.b])
                    for dr in range(2):
                        pr = slice(dr * 32, dr * 32 + 32)
                        lastpos = 63 if dr == 0 else 0
                        kb.op("dve", lambda e, pr=pr, lastpos=lastpos: e.tensor_copy(out=CH[pr, 0, :], in_=BC[pr, :].rearrange("p (c s) -> p c s", s=64)[:, :, lastpos]), reads=[BC.b], writes=[CH.b])
                        kb.op("dve", lambda e, pr=pr, lastpos=lastpos: e.tensor_copy(out=CH[pr, 1, :], in_=CM[pr, :].rearrange("p (c s) -> p c s", s=64)[:, :, lastpos]), reads=[CM.b], writes=[CH.b])
                    B_ = [CH.b]
                    kb.op("dve", lambda e: e.tensor_tensor(out=CH[:, 2, :], in0=CH[:, 0, :], in1=CH[:, 1, :], op=ALU.add), reads=B_, writes=B_)
                    kb.op("dve", lambda e: e.tensor_tensor_scan(out=CH[0:32, 3, :], data0=CH[0:32, 0, :], data1=CH[0:32, 2, :], initial=0.0, op0=ALU.add, op1=ALU.max), reads=B_, writes=B_)
                    kb.op("dve", lambda e: e.tensor_tensor_scan(out=CH[32:64, 3, 0:4][:, ::-1], data0=CH[32:64, 0, 0:4][:, ::-1], data1=CH[32:64, 2, 0:4][:, ::-1], initial=0.0, op0=ALU.add, op1=ALU.max), reads=B_, writes=B_)
                    kb.op("dve", lambda e: e.tensor_tensor_scan(out=CH[32:64, 3, 4:NCK][:, ::-1], data0=CH[32:64, 0, 4:NCK][:, ::-1], data1=CH[32:64, 2, 4:NCK][:, ::-1], initial=CH[32:64, 3, 0:1], op0=ALU.add, op1=ALU.max), reads=B_, writes=B_)
                    kb.op("dve", lambda e: e.memset(CH[:, 4, :], 0.0), reads=B_, writes=B_)
                    kb.op("dve", lambda e: e.tensor_copy(out=CH[0:32, 4, 1:NCK], in_=CH[0:32, 3, 0:NCK - 1]), reads=B_, writes=B_)
                    kb.op("dve", lambda e: e.tensor_copy(out=CH[32:64, 4, 0:3], in_=CH[32:64, 3, 1:4]), reads=B_, writes=B_)
                    kb.op("dve", lambda e: e.tensor_copy(out=CH[32:64, 4, 4:NCK - 1], in_=CH[32:64, 3, 5:NCK]), reads=B_, writes=B_)
                    kb.op("dve", lambda e: e.tensor_copy(out=CH[32:64, 4, NCK - 1:NCK], in_=CH[32:64, 3, 0:1]), reads=B_, writes=B_)
                    kb.op("dve", lambda e: e.tensor_tensor(out=CH[:, 5, :], in0=CH[:, 0, :], in1=CH[:, 4, :], op=ALU.add), reads=B_, writes=B_)
                    kb.op("dve", lambda e: e.tensor_tensor(out=CH[:, 5, :], in0=CH[:, 5, :], in1=CH[:, 3, :], op=ALU.subtract), reads=B_, writes=B_)
                    kb.op("dve", lambda e: e.tensor_tensor(out=CH[:, 6, :], in0=CH[:, 2, :], in1=CH[:, 3, :], op=ALU.subtract), reads=B_, writes=B_)
                    kb.op("act", lambda e: e.activation(out=CH[:, 5:7, :], in_=CH[:, 5:7, :], func=AF.Exp), reads=B_, writes=B_)
                    for c in range(NCK):
                        kb.op("dve", lambda e, c=c: e.tensor_scalar(out=RF[:, c * 64:(c + 1) * 64], in0=CM[:, c * 64:(c + 1) * 64], scalar1=-1.0, scalar2=CH[:, 4, c:c + 1],
                                                                     op0=ALU.mult, op1=ALU.add), reads=[CM.b, CH.b], writes=[RF.b])
                        kb.op("dve", lambda e, c=c: e.tensor_scalar(out=X2[:, c * 64:(c + 1) * 64], in0=RI[:, c * 64:(c + 1) * 64], scalar1=CH[:, 1, c:c + 1], scalar2=None,
                                                                     op0=ALU.subtract), reads=[RI.b, CH.b], writes=[X2.b])
                    kb.op("act", lambda e: e.activation(out=X2[:], in_=X2[:], func=AF.Exp), reads=[X2.b], writes=[X2.b])
                    kb.op("dve", lambda e: e.tensor_tensor(out=BC[:], in0=BC[:], in1=CM[:], op=ALU.add), reads=[BC.b, CM.b], writes=[BC.b])
                    kb.op("dve", lambda e: e.scalar_tensor_tensor(out=BC[:], in0=RF[:], scalar=0.0, in1=BC[:], op0=ALU.max, op1=ALU.add), reads=[RF.b, BC.b], writes=[BC.b])
                    kb.op("act", lambda e: e.activation(out=BC[:], in_=BC[:], func=AF.Exp, scale=-1.0), reads=[BC.b], writes=[BC.b])
                    kb.op("dve", lambda e: e.tensor_scalar(out=X1[:], in0=RF[:], scalar1=0.0, scalar2=None, op0=ALU.min), reads=[RF.b], writes=[X1.b])
                    kb.op("act", lambda e: e.activation(out=X1[:], in_=X1[:], func=AF.Exp), reads=[X1.b], writes=[X1.b])
                    kb.op("dve", lambda e: e.tensor_scalar(out=RF[:], in0=RF[:], scalar1=-1.0, scalar2=0.0, op0=ALU.mult, op1=ALU.min), reads=[RF.b], writes=[RF.b])
                    kb.op("act", lambda e: e.activation(out=RF[:], in_=RF[:], func=AF.Exp), reads=[RF.b], writes=[RF.b])
                    for c in range(NCK):
                        pcol = banks[c % 4]
                        for qi, row in enumerate((X1, RF, BC, X2)):
                            kb.op("pe", lambda e, row=row, qi=qi, pcol=pcol, c=c: e.matmul(out=pcol[0:64, qi * 12:(qi + 1) * 12], lhsT=row[:, c * 64:(c + 1) * 64],
                                                                                       rhs=SEL[:, :, 0], start=True, stop=True), reads=[row.b, SEL.b], writes=[pcol.b])
                        kb.op("act", lambda e, pcol=pcol, c=c: e.activation(out=COL[:, c, :, :].rearrange("p a b -> p (a b)"), in_=pcol[0:64, 0:48], func=AF.Identity),
                              writes=[COL.b, pcol.b])
                    for qi in range(2):
                        for r in range(12):
                            pcol = banks[4 + (qi * 12 + r) % 2]
                            kb.op("pe", lambda e, r=r, pcol=pcol, qi=qi: e.matmul(out=pcol[:, 0:NCK], lhsT=SEL[:, r, :], rhs=CH[:, 5 + qi, :], start=True, stop=True), reads=[SEL.b, CH.b], writes=[pcol.b])
                            kb.op("act", lambda e, r=r, pcol=pcol, qi=qi: e.activation(out=AOB[:, qi, r, :], in_=pcol[:, 0:NCK], func=AF.Identity), writes=[AOB.b, pcol.b])
                    kb.barrier()
                psn = [0]

                def pget():
                    bk = banks[psn[0] % 8]
                    psn[0] += 1
                    return bk
                RAW = sb(ph, "mRAW", [128, T])
                QT = sb(ph, "mQT", [64, T]); KT = sb(ph, "mKT", [64, T]); VT = sb(ph, "mVT", [128, T])
                VA = sb(ph, "mVA", [64, NCK, 132])
                HACC = sb(ph, "mHACC", [64, NCK, 128]); OUT = sb(ph, "mOUT", [128, T], BF16)
                CAd = [sb(ph, "mCA%d" % i, [64, 132]) for i in range(2)]
                G = 4
                U = []
                for u in range(2 * G):
                    d_ = {}
                    for nm, shp in (("E", [64, 64]), ("wk", [64, 64]), ("tmp", [64, 132]), ("comb", [64, 132]), ("sc", [64, 4])):
                        d_[nm] = sb(ph, "mu%d%s" % (u, nm), shp)
                    U.append(d_)
                fin = [sb(ph, "mfin%d" % i, [64, 128]) for i in range(2)]
                fst = [sb(ph, "mfst%d" % i, [64, 4]) for i in range(2)]
                kb.op("dve", lambda e: e.memset(VA[:], 1.0), writes=[VA.b])
                for hd in range(6):
                    kb.dma("sp", RAW[0:64, :], projT[MQ + hd * 64:MQ + (hd + 1) * 64, :], writes=[RAW.b])
                    seqperm("act", QT, QT.b, RAW, RAW.b, 64)
                    kb.op("act", lambda e: e.activation(out=QT[:], in_=QT[:], func=AF.Identity, scale=0.125), reads=[QT.b], writes=[QT.b])
                    kb.dma("sp", RAW[0:64, :], projT[MK + hd * 64:MK + (hd + 1) * 64, :], reads=[QT.b], writes=[RAW.b])
                    seqperm("dve", KT, KT.b, RAW, RAW.b, 64)
                    kb.dma("sp", RAW[:], projT[MV + hd * 128:MV + (hd + 1) * 128, :], reads=[KT.b], writes=[RAW.b])
                    seqperm("act", VT, VT.b, RAW, RAW.b, 128)
                    for c in range(NCK):
                        bk = pget()
                        kb.op("pe", lambda e, bk=bk, c=c: e.transpose(out=bk[0:64, 0:128], in_=VT[:, c * 64:(c + 1) * 64], identity=ident[:]), reads=[VT.b, ident.b], writes=[bk.b])
                        if c % 2 == 0:
                            kb.op("act", lambda e, bk=bk, c=c: e.activation(out=VA[:, c, 0:128], in_=bk[0:64, 0:128], func=AF.Identity), writes=[VA.b, bk.b])
                        else:
                            kb.op("dve", lambda e, bk=bk, c=c: e.tensor_copy(out=VA[:, c, 0:128], in_=bk[0:64, 0:128]), writes=[VA.b, bk.b])
                    kb.op("dve", lambda e: e.memset(HACC[:], 0.0), writes=[HACC.b])
                    for dr in range(2):
                        kb.op("dve", lambda e, dr=dr: e.memset(CAd[dr][:], 0.0), writes=[CAd[dr].b])
                    orders = [list(range(NCK)), list(range(3, -1, -1)) + list(range(NCK - 1, 3, -1))]
                    for g0 in range(0, NCK, G):
                        units = []
                        for dr in range(2):
                            for u, c in enumerate(orders[dr][g0:g0 + G]):
                                units.append((dr, c, U[dr * G + u]))
                        for (dr, c, d_) in units:
                            r = dr * 6 + hd
                            m_it = 3 if dr == 0 else 2
                            ch = slice(c * 64, (c + 1) * 64)
                            be = pget()
                            kb.op("pe", lambda e, be=be, ch=ch, r=r: e.matmul(out=be[0:64, 0:64], lhsT=RI[:, ch], rhs=SEL[:, r, 0:64], start=True, stop=False), reads=[RI.b, SEL.b], writes=[be.b], sig=False)
                            kb.op("pe", lambda e, be=be, ch=ch, r=r: e.matmul(out=be[0:64, 0:64], lhsT=NSEL[:, r, 0:64], rhs=CM[:, ch], start=False, stop=False), reads=[CM.b, NSEL.b], writes=[be.b], sig=False)
                            kb.op("pe", lambda e, be=be, m_it=m_it: e.matmul(out=be[0:64, 0:64], lhsT=I64, rhs=MB[:, m_it, :], start=False, stop=True), reads=[MB.b, ident.b], writes=[be.b])
                            kb.op("act", lambda e, be=be, d_=d_: e.activation(out=d_["E"][:], in_=be[0:64, 0:64], func=AF.Exp), writes=[d_["E"].b, be.b])
                            bp = pget()
                            kb.op("pe", lambda e, bp=bp, ch=ch: e.matmul(out=bp[0:64, 0:64], lhsT=KT[:, ch], rhs=QT[:, ch], start=True, stop=True), reads=[KT.b, QT.b], writes=[bp.b])
                            kb.op("pe", lambda e, bp=bp, ch=ch: e.transpose(out=bp[0:64, 64:128], in_=KT[:, ch], identity=I64), reads=[KT.b, ident.b], writes=[bp.b])
                            kb.op("dve", lambda e, bp=bp, d_=d_: e.tensor_tensor(out=d_["E"][:], in0=bp[0:64, 0:64], in1=d_["E"][:], op=ALU.mult), reads=[d_["E"].b], writes=[d_["E"].b, bp.b])
                            kb.op("dve", lambda e, bp=bp, d_=d_, c=c, r=r: e.tensor_scalar(out=d_["wk"][:], in0=bp[0:64, 64:128], scalar1=COL[:, c, 3, r:r + 1], scalar2=None, op0=ALU.mult),
                                  reads=[COL.b], writes=[d_["wk"].b, bp.b])
                        for u in range(G):
                            for dr in range(2):
                                if dr * G + u >= len(units):
                                    continue
                                dr_, c, d_ = units[dr * G + u]
                                r = dr * 6 + hd
                                CA = CAd[dr]
                                ch = slice(c * 64, (c + 1) * 64)
                                b1 = pget()
                                kb.op("pe", lambda e, b1=b1, ch=ch, CA=CA: e.matmul(out=b1[0:64, 0:129], lhsT=QT[:, ch], rhs=CA[:, 0:129], start=True, stop=True), reads=[QT.b, CA.b], writes=[b1.b])
                                kb.op("act", lambda e, b1=b1, d_=d_, c=c, r=r: e.activation(out=d_["tmp"][:, 0:129], in_=b1[0:64, 0:129], func=AF.Identity, scale=COL[:, c, 0, r:r + 1]),
                                      reads=[COL.b], writes=[d_["tmp"].b, b1.b])
                                b2 = pget()
                                kb.op("pe", lambda e, b2=b2, d_=d_, c=c: e.matmul(out=b2[0:64, 0:129], lhsT=d_["E"][:], rhs=VA[:, c, 0:129], start=True, stop=True), reads=[d_["E"].b, VA.b], writes=[b2.b])
                                kb.op("dve", lambda e, b2=b2, d_=d_, c=c, r=r: e.scalar_tensor_tensor(out=d_["comb"][:, 0:129], in0=b2[0:64, 0:129], scalar=COL[:, c, 1, r:r + 1], in1=d_["tmp"][:, 0:129],
                                                                                              op0=ALU.mult, op1=ALU.add), reads=[COL.b, d_["tmp"].b], writes=[d_["comb"].b, b2.b])
                                kb.op("act", lambda e, d_=d_: e.activation(out=d_["sc"][:, 2:3], in_=d_["comb"][:, 128:129], func=AF.Abs), reads=[d_["comb"].b], writes=[d_["sc"].b])
                                kb.op("dve", lambda e, d_=d_, c=c, r=r: e.tensor_scalar(out=d_["sc"][:, 0:1], in0=d_["sc"][:, 2:3], scalar1=COL[:, c, 2, r:r + 1], scalar2=None, op0=ALU.max),
                                      reads=[COL.b, d_["sc"].b], writes=[d_["sc"].b])
                                kb.op("dve", lambda e, d_=d_: e.reciprocal(out=d_["sc"][:, 1:2], in_=d_["sc"][:, 0:1]), reads=[d_["sc"].b], writes=[d_["sc"].b])
                                kb.op("dve", lambda e, d_=d_, c=c: e.scalar_tensor_tensor(out=HACC[:, c, :], in0=d_["comb"][:, 0:128], scalar=d_["sc"][:, 1:2], in1=HACC[:, c, :], op0=ALU.mult, op1=ALU.add),
                                      reads=[d_["comb"].b, d_["sc"].b, HACC.b], writes=[HACC.b])
                                b3 = pget()
                                kb.op("pe", lambda e, b3=b3, d_=d_, c=c: e.matmul(out=b3[0:64, 0:129], lhsT=d_["wk"][:], rhs=VA[:, c, 0:129], start=True, stop=True), reads=[d_["wk"].b, VA.b], writes=[b3.b])
                                kb.op("act", lambda e, c=c, r=r, CA=CA: e.activation(out=CA[:, 0:129], in_=CA[:, 0:129], func=AF.Identity, scale=AOB[0:64, 0, r, c:c + 1]), reads=[AOB.b, CA.b], writes=[CA.b])
                                kb.op("dve", lambda e, b3=b3, c=c, r=r, CA=CA: e.scalar_tensor_tensor(out=CA[:, 0:129], in0=b3[0:64, 0:129], scalar=AOB[0:64, 1, r, c:c + 1], in1=CA[:, 0:129], op0=ALU.mult, op1=ALU.add),
                                      reads=[AOB.b, CA.b], writes=[CA.b, b3.b])
                    kb.dma("sp", RAW[:], projT[MO + hd * 128:MO + (hd + 1) * 128, :], reads=[VT.b], writes=[RAW.b])
                    seqperm("act", VT, VT.b, RAW, RAW.b, 128)
                    kb.op("act", lambda e: e.activation(out=VT[:], in_=VT[:], func=AF.Sigmoid), reads=[VT.b], writes=[VT.b])
                    for c in range(NCK):
                        f_ = fin[c % 2]; s_ = fst[c % 2]
                        kb.op("act", lambda e, f_=f_, s_=s_, c=c: e.activation(out=f_[:], in_=HACC[:, c, :], func=AF.Square, accum_out=s_[:, 0:1]), reads=[HACC.b], writes=[f_.b, s_.b])
                        kb.op("act", lambda e, s_=s_: e.activation(out=s_[:, 1:2], in_=s_[:, 0:1], func=AF.Sqrt, bias=EPS, scale=1.0 / 128), reads=[s_.b], writes=[s_.b])
                        kb.op("dve", lambda e, s_=s_: e.reciprocal(out=s_[:, 2:3], in_=s_[:, 1:2]), reads=[s_.b], writes=[s_.b])
                        kb.op("dve", lambda e, f_=f_, s_=s_, c=c: e.scalar_tensor_tensor(out=f_[:], in0=HACC[:, c, :], scalar=s_[:, 2:3], in1=NORM[:], op0=ALU.mult, op1=ALU.mult),
                              reads=[HACC.b, s_.b, NORM.b], writes=[f_.b])
                        bk = pget()
                        kb.op("pe", lambda e, bk=bk, f_=f_: e.transpose(out=bk[:, 0:64], in_=f_[:], identity=I64), reads=[f_.b, ident.b], writes=[bk.b])
                        kb.op("dve", lambda e, bk=bk, c=c: e.tensor_tensor(out=RAW[:, c * 64:(c + 1) * 64], in0=bk[:, 0:64], in1=VT[:, c * 64:(c + 1) * 64], op=ALU.mult),
                              reads=[VT.b], writes=[RAW.b, bk.b])
                    kb.op("act", lambda e: e.activation(out=OUT[:, 0:NCTX], in_=RAW[:, 0:NCTX], func=AF.Identity), reads=[RAW.b], writes=[OUT.b])
                    kb.op("act", lambda e: e.activation(out=OUT[:, NCTX:T].rearrange("p (r c) -> p c r", c=64), in_=RAW[:, NCTX:T].rearrange("p (c r) -> p c r", r=32), func=AF.Identity),
                          reads=[RAW.b], writes=[OUT.b])
                    kb.dma("sp", mixT[1280 + hd * 128:1280 + (hd + 1) * 128, :], OUT[:], reads=[OUT.b])
                kb.barrier()
            if stage <= 4:
                break
            with ExitStack() as ph:
                last = (l == L - 1)
                mixb = sb(ph, "mixb", [128, KC, 512], BF16)
                yTb = sb(ph, "yTb", [128, KC, 512])
                h2T = sb(ph, "h2T", [128, KC, 512], BF16)
                hidT = sb(ph, "hidT", [128, 44, 512], BF16)
                xt = [sb(ph, "xt%d" % i, [128, D]) for i in range(2)]
                GG = [[sb(ph, "GG%d%d" % (a, w_), [128, D]) for w_ in range(2)] for a in range(2)]
                wk = [sb(ph, "wk%d" % i, [128, KC, 128], BF16) for i in range(4)]
                wd = [sb(ph, "wd%d" % i, [128, 44, 128], BF16) for i in range(2)]
                sgt = [sb(ph, "sgt%d" % i, [128, 512]) for i in range(2)]
                junk = sb(ph, "junk2", [128, 512], BF16)
                stt_ = [sb(ph, "st2%d" % i, [128, 8]) for i in range(2)]
                bc = sb(ph, "bc", [128, 128])
                for a, (mi, gi) in enumerate(((2, 1), (5, 3))):
                    for w_ in range(2):
                        for j in range(KC):
                            kb.op("dve", lambda e, mi=mi, gi=gi, w_=w_, j=j: e.tensor_tensor(
                                out=bc[:, 0:1], in0=modsT[:, mi * KC + j, w_:w_ + 1], in1=gT[:, (gi * L + l) * KC + j:(gi * L + l) * KC + j + 1],
                                op=ALU.mult), reads=[modsT.b, gT.b], writes=[bc.b])
                            kb.op("dve", lambda e: e.tensor_copy(out=bc[:, 1:128], in_=bc[:, 0:1].to_broadcast([128, 127])), reads=[bc.b], writes=[bc.b])
                            pb = banks[j % 4]
                            kb.op("pe", lambda e, pb=pb: e.transpose(out=pb[:, 0:128], in_=bc[:], identity=ident[:]), reads=[bc.b, ident.b], writes=[pb.b])
                            kb.op("act", lambda e, pb=pb, a=a, w_=w_, j=j: e.activation(out=GG[a][w_][:, j * 128:(j + 1) * 128], in_=pb[:, 0:128], func=AF.Identity),
                                  reads=[pb.b], writes=[GG[a][w_].b])
                mixv = mixT.rearrange("(k p) t -> p k t", p=128)
                tstart = NCTX if last else 0
                nwk = [0]
                nx = [0]

                def post_res(tglob, tloc, a, dst):
                    w_ = 1 if tglob < 2 else 0
                    x_ = xt[nx[0] % 2]
                    s_ = stt_[nx[0] % 2]
                    nx[0] += 1
                    kb.dma("sp", x_[:], xres[tglob * 128:(tglob + 1) * 128, :], writes=[x_.b])
                    for jb in range(4):
                        pb = banks[jb]
                        for jj in range(4):
                            j = jb * 4 + jj
                            kb.op("pe", lambda e, pb=pb, jj=jj, j=j: e.transpose(out=pb[:, jj * 128:(jj + 1) * 128], in_=yTb[:, j, tloc * 128:(tloc + 1) * 128],
                                                                              identity=ident[:]), reads=[yTb.b, ident.b], writes=[pb.b])
                        kb.op("act", lambda e, pb=pb, jb=jb, s_=s_: e.activation(out=junk[:], in_=pb[:], func=AF.Square, accum_out=s_[:, jb:jb + 1]),
                              reads=[pb.b], writes=[junk.b, s_.b])
                    kb.op("dve", lambda e, s_=s_: e.tensor_reduce(out=s_[:, 4:5], in_=s_[:, 0:4], axis=AX.X, op=ALU.add), reads=[s_.b], writes=[s_.b])
                    kb.op("act", lambda e, s_=s_: e.activation(out=s_[:, 5:6], in_=s_[:, 4:5], func=AF.Sqrt, bias=EPS, scale=1.0 / D), reads=[s_.b], writes=[s_.b])
                    kb.op("dve", lambda e, s_=s_: e.reciprocal(out=s_[:, 6:7], in_=s_[:, 5:6]), reads=[s_.b], writes=[s_.b])
                    for jb in range(4):
                        pb = banks[jb]
                        sg_ = sgt[jb % 2]
                        kb.op("dve", lambda e, pb=pb, sg_=sg_, jb=jb, s_=s_: e.scalar_tensor_tensor(
                            out=sg_[:], in0=pb[:], scalar=s_[:, 6:7], in1=GG[a][w_][:, jb * 512:(jb + 1) * 512], op0=ALU.mult, op1=ALU.mult),
                            reads=[pb.b, s_.b, GG[a][w_].b], writes=[sg_.b])
                        kb.op("dve", lambda e, sg_=sg_, jb=jb, x_=x_: e.tensor_tensor(out=x_[:, jb * 512:(jb + 1) * 512], in0=x_[:, jb * 512:(jb + 1) * 512], in1=sg_[:],
                                                                                op=ALU.add), reads=[sg_.b, x_.b], writes=[x_.b])
                    kb.dma("sp", dst, x_[:], reads=[x_.b])
                    return x_, s_, w_

                def dense(src, nk, wview, j0, nj, wbufs, t_nt, consume):
                    for j in range(j0, j0 + nj):
                        w = wbufs[nwk[0] % len(wbufs)]
                        nwk[0] += 1
                        src_ap, src_b = wview[j]
                        kb.dma("sp" if nwk[0] % 2 == 0 else "act", w[:, :nk, :], src_ap, reads=[src_b], writes=[w.b])
                        pj = banks[4 + nwk[0] % 2]
                        for k in range(nk):
                            kb.op("pe", lambda e, pj=pj, w=w, k=k: e.matmul(out=pj[:, :t_nt], lhsT=w[:, k, :], rhs=src[:, k, :t_nt], start=(k == 0), stop=(k == nk - 1)),
                                  reads=[w.b, src.b], writes=[pj.b], sig=(k == nk - 1))
                        consume(j, pj)

                t0 = tstart
                while t0 < T:
                    nt = min(512, T - t0)
                    ntl = nt // 128
                    kb.dma("sp", mixb[:, :, :nt], mixv[:, :, t0:t0 + nt], writes=[mixb.b])

                    def cons_y(j, pj):
                        if j % 2 == 0:
                            kb.op("act", lambda e: e.activation(out=yTb[:, j, :nt], in_=pj[:, :nt], func=AF.Identity), reads=[pj.b], writes=[yTb.b])
                        else:
                            kb.op("dve", lambda e: e.tensor_copy(out=yTb[:, j, :nt], in_=pj[:, :nt]), reads=[pj.b], writes=[yTb.b])
                    dense(mixb, KC, pre["wo"], 0, KC, wk, nt, cons_y)
                    for tl in range(ntl):
                        tg = t0 // 128 + tl
                        x_, s_, w_ = post_res(tg, tl, 0, xres[tg * 128:(tg + 1) * 128, :])
                        kb.op("act", lambda e, x_=x_, s_=s_: e.activation(out=junk[:], in_=x_[:, 0:512], func=AF.Square, accum_out=s_[:, 0:1]), reads=[x_.b], writes=[junk.b, s_.b])
                        for q in range(1, 4):
                            kb.op("act", lambda e, x_=x_, s_=s_, q=q: e.activation(out=junk[:], in_=x_[:, q * 512:(q + 1) * 512], func=AF.Square, accum_out=s_[:, q:q + 1]),
                                  reads=[x_.b], writes=[junk.b, s_.b])
                        kb.op("dve", lambda e, s_=s_: e.tensor_reduce(out=s_[:, 4:5], in_=s_[:, 0:4], axis=AX.X, op=ALU.add), reads=[s_.b], writes=[s_.b])
                        kb.op("act", lambda e, s_=s_: e.activation(out=s_[:, 5:6], in_=s_[:, 4:5], func=AF.Sqrt, bias=EPS, scale=1.0 / D), reads=[s_.b], writes=[s_.b])
                        kb.op("dve", lambda e, s_=s_: e.reciprocal(out=s_[:, 6:7], in_=s_[:, 5:6]), reads=[s_.b], writes=[s_.b])
                        kb.op("dve", lambda e, x_=x_, s_=s_: e.tensor_scalar(out=x_[:], in0=x_[:], scalar1=s_[:, 6:7], scalar2=None, op0=ALU.mult), reads=[x_.b, s_.b], writes=[x_.b])
                        for jb in range(4):
                            pt = banks[jb]
                            for jj in range(4):
                                j = jb * 4 + jj
                                kb.op("pe", lambda e, pt=pt, x_=x_, jj=jj, j=j: e.transpose(out=pt[:, jj * 128:(jj + 1) * 128], in_=x_[:, j * 128:(j + 1) * 128], identity=ident[:]),
                                      reads=[x_.b, ident.b], writes=[pt.b])
                            for jj in range(4):
                                j = jb * 4 + jj
                                kb.op("act", lambda e, pt=pt, jj=jj, j=j, tl=tl, w_=w_: e.activation(
                                    out=h2T[:, j, tl * 128:(tl + 1) * 128], in_=pt[:, jj * 128:(jj + 1) * 128], func=AF.Identity,
                                    bias=modsT[:, 3 * KC + j, w_:w_ + 1], scale=gsf[:, j, w_:w_ + 1]), reads=[pt.b, modsT.b, gsf.b], writes=[h2T.b])
                    for hc in range(44):
                        got = {}

                        def cons_g(j, pj):
                            got["g"] = pj
                        dense(h2T, KC, pre["wg"], hc, 1, wk, nt, cons_g)
                        pg = got["g"]
                        sg_ = sgt[hc % 2]
                        kb.op("act", lambda e, pg=pg, sg_=sg_: e.activation(out=sg_[:, :nt], in_=pg[:, :nt], func=AF.Silu), reads=[pg.b], writes=[sg_.b])

                        def cons_u(j, pj):
                            kb.op("dve", lambda e: e.tensor_tensor(out=hidT[:, hc, :nt], in0=pj[:, :nt], in1=sg_[:, :nt], op=ALU.mult), reads=[pj.b, sg_.b], writes=[hidT.b])
                        dense(h2T, KC, pre["wu"], hc, 1, wk, nt, cons_u)
                    dense(hidT, 44, pre["wd"], 0, KC, wd, nt, cons_y)
                    for tl in range(ntl):
                        tg = t0 // 128 + tl
                        dst = out_d[(tg - 2) * 128:(tg - 1) * 128, :] if last else xres[tg * 128:(tg + 1) * 128, :]
                        post_res(tg, tl, 1, dst)
                    t0 += nt
                kb.barrier()
            if stage == 5:
                kb.dma("sp", xres_o, xres)
                break
        kb.barrier()
    return nc


def kernel(**inp):
    inp = {k: np.asarray(v) for k, v in inp.items()}
    nc = build(99)
    base = host_prep(inp, 0)
    in_maps = []
    for core in range(8):
        b = core % 4
        m = dict(base)
        if b != 0:
            pb = host_prep_batch(inp, b)
            m.update(pb)
        in_maps.append(m)
    res = run_bass_kernel_spmd(nc, in_maps, core_ids=list(range(8)))
    out = np.stack([np.asarray(res.results[b]["out"]) for b in range(4)], 0).astype(np.float32)
    return out
```
